# Optimizing a Trainium2 kernel written in Bass

```python
import math
import jax, jax.numpy as jnp
from jax import lax
import numpy as np


D_MODEL = 1024
BATCH = 2
SEQ = 8192
DEPTH = 1

CHUNK = 64
QBLK = 128
EPS = 1e-6
NEG = -1e30

SSM_WIDTH = D_MODEL
SSM_GROUP = 16
SSM_GROUPS = SSM_WIDTH // SSM_GROUP
SSM_STATE = 64
DT_MIN = 1e-3
DT_MAX = 1e-1

N_HEADS = 8
HEAD_DIM = 128
N_KV_HEADS = 2
Q_PER_KV = N_HEADS // N_KV_HEADS
ATT_WIDTH = N_HEADS * HEAD_DIM
KV_WIDTH = N_KV_HEADS * HEAD_DIM
IDX_HEADS = 16
IDX_DIM = 64
TOPK_MAX = 256
ROPE_THETA = 500000.0
ATT_ROT = HEAD_DIM // 4
IDX_ROT = IDX_DIM // 4

FFN_HIDDEN = -(-8 * D_MODEL // (3 * 256)) * 256

SPLIT_SIZES = (SSM_WIDTH, ATT_WIDTH, KV_WIDTH, KV_WIDTH, IDX_HEADS * IDX_DIM, IDX_DIM, IDX_HEADS, D_MODEL, D_MODEL)
IN_WIDTH = SSM_WIDTH + ATT_WIDTH + 2 * KV_WIDTH + IDX_HEADS * IDX_DIM + IDX_DIM + IDX_HEADS + 2 * D_MODEL

kernel_name = 'hybrid_s5_dsa_gated_block'


def _split_points():
    pts, acc = [], 0
    for s in SPLIT_SIZES[:-1]:
        acc += s
        pts.append(acc)
    return pts


def _rmsnorm(x, g):
    xf = x.astype(jnp.float32)
    y = xf * lax.rsqrt(jnp.mean(xf * xf, axis=-1, keepdims=True) + EPS)
    return (y * g.astype(jnp.float32)).astype(x.dtype)


def _rope_partial(x, pos, rot_dim):
    half = rot_dim // 2
    inv_freq = ROPE_THETA ** (-jnp.arange(half, dtype=jnp.float32) / half)
    ang = pos.astype(jnp.float32)[:, None] * inv_freq[None, :]
    cos = jnp.cos(ang)[:, None, :]
    sin = jnp.sin(ang)[:, None, :]
    xr = x[..., :rot_dim].astype(jnp.float32)
    x1, x2 = xr[..., :half], xr[..., half:]
    rot = jnp.concatenate([x1 * cos - x2 * sin, x1 * sin + x2 * cos], axis=-1).astype(x.dtype)
    return jnp.concatenate([rot, x[..., rot_dim:]], axis=-1)


def _s5_branch(u, a_re, a_im, log_dt, b_re, b_im, c_re, c_im, d_skip, w_glu):
    bsz, L, _ = u.shape
    f32 = jnp.float32
    uf = u.astype(f32).reshape(bsz, L, SSM_GROUPS, SSM_GROUP)
    dt = jnp.exp(log_dt.astype(f32))[:, None]
    ar = a_re.astype(f32)
    ai = a_im.astype(f32)
    mag = jnp.exp(ar * dt)
    lb_re = mag * jnp.cos(ai * dt)
    lb_im = mag * jnp.sin(ai * dt)
    num_re = lb_re - 1.0
    num_im = lb_im
    den = ar * ar + ai * ai
    f_re = (num_re * ar + num_im * ai) / den
    f_im = (num_im * ar - num_re * ai) / den
    bu_re = jnp.einsum('blgc,gpc->lbgp', uf, b_re.astype(f32))
    bu_im = jnp.einsum('blgc,gpc->lbgp', uf, b_im.astype(f32))
    x_re = f_re * bu_re - f_im * bu_im
    x_im = f_re * bu_im + f_im * bu_re
    shp = (L, 1, SSM_GROUPS, SSM_STATE)
    a_s_re = jnp.broadcast_to(lb_re, shp)
    a_s_im = jnp.broadcast_to(lb_im, shp)

    def combine(e1, e2):
        a1r, a1i, b1r, b1i = e1
        a2r, a2i, b2r, b2i = e2
        return (a2r * a1r - a2i * a1i,
                a2r * a1i + a2i * a1r,
                a2r * b1r - a2i * b1i + b2r,
                a2r * b1i + a2i * b1r + b2i)

    _, _, h_re, h_im = lax.associative_scan(combine, (a_s_re, a_s_im, x_re, x_im), axis=0)
    y = (jnp.einsum('lbgp,gcp->blgc', h_re, c_re.astype(f32))
         - jnp.einsum('lbgp,gcp->blgc', h_im, c_im.astype(f32))
         + d_skip.astype(f32) * uf)
    y = jax.nn.gelu(y.reshape(bsz, L, SSM_WIDTH))
    y = y * jax.nn.sigmoid(y @ w_glu.astype(f32))
    return y.astype(u.dtype)


def _dsa_branch(q, k, v, qi, ki, wi, pos):
    bsz, L, _ = q.shape
    f32 = jnp.float32
    k_sel = min(TOPK_MAX, L // 4)
    nblk = L // QBLK
    q = _rope_partial(q.reshape(bsz, L, N_HEADS, HEAD_DIM), pos, ATT_ROT)
    k = _rope_partial(k.reshape(bsz, L, N_KV_HEADS, HEAD_DIM), pos, ATT_ROT)
    v = v.reshape(bsz, L, N_KV_HEADS, HEAD_DIM)
    qi = _rope_partial(qi.reshape(bsz, L, IDX_HEADS, IDX_DIM), pos, IDX_ROT)
    ki = _rope_partial(ki.reshape(bsz, L, 1, IDX_DIM), pos, IDX_ROT)[:, :, 0].astype(f32)
    key_chunk = pos // CHUNK

    def to_blocks(a):
        return jnp.moveaxis(a.reshape((bsz, nblk, QBLK) + a.shape[2:]), 1, 0)

    def block_fn(args):
        qb, qib, wb, qpos = args
        rel = jnp.einsum('bqhd,bsd->bqhs', qib.astype(f32), ki) * (IDX_DIM ** -0.5)
        score = jnp.einsum('bqhs,bqh->bqs', jax.nn.relu(rel), wb.astype(f32) * (IDX_HEADS ** -0.5))
        allowed = key_chunk[None, :] <= (qpos // CHUNK)[:, None]
        score = jnp.where(allowed[None], score, NEG)
        top_val, top_idx = lax.top_k(score, k_sel)
        valid = top_val > 0.5 * NEG
        kg = jax.vmap(lambda kk, ii: kk[ii])(k, top_idx)
        vg = jax.vmap(lambda vv, ii: vv[ii])(v, top_idx)
        qg = qb.reshape(bsz, QBLK, N_KV_HEADS, Q_PER_KV, HEAD_DIM)
        s = jnp.einsum('bqgrd,bqkgd->bqgrk', qg, kg).astype(f32) * (HEAD_DIM ** -0.5)
        s = jnp.where(valid[:, :, None, None, :], s, NEG)
        p = jax.nn.softmax(s, axis=-1).astype(v.dtype)
        o = jnp.einsum('bqgrk,bqkgd->bqgrd', p, vg)
        return o.reshape(bsz, QBLK, ATT_WIDTH)

    out = lax.map(block_fn, (to_blocks(q), to_blocks(qi), to_blocks(wi), pos.reshape(nblk, QBLK)))
    return jnp.moveaxis(out, 0, 1).reshape(bsz, L, ATT_WIDTH)


def setup_inputs(seed: int = 0) -> dict:
    key = jax.random.key(seed)
    ks = jax.random.split(key, 19)
    f32 = jnp.float32
    G, P, C = SSM_GROUPS, SSM_STATE, SSM_GROUP

    def nrm(k, shape, scale):
        return jax.random.normal(k, shape, f32) * scale

    x = nrm(ks[0], (BATCH, SEQ, D_MODEL), 1.0)
    norm1_g = 1.0 + nrm(ks[1], (DEPTH, D_MODEL), 0.02)
    w_in = nrm(ks[2], (DEPTH, D_MODEL, IN_WIDTH), D_MODEL ** -0.5)
    a_re = -0.5 * jnp.exp(nrm(ks[3], (DEPTH, G, P), 0.02))
    a_im = math.pi * jnp.arange(P, dtype=f32) + nrm(ks[4], (DEPTH, G, P), 0.02)
    log_dt = jax.random.uniform(ks[5], (DEPTH, G), f32, math.log(DT_MIN), math.log(DT_MAX))
    b_re = nrm(ks[6], (DEPTH, G, P, C), (2 * C) ** -0.5)
    b_im = nrm(ks[7], (DEPTH, G, P, C), (2 * C) ** -0.5)
    c_re = nrm(ks[8], (DEPTH, G, C, P), P ** -0.5)
    c_im = nrm(ks[9], (DEPTH, G, C, P), P ** -0.5)
    d_skip = nrm(ks[10], (DEPTH, G, C), 1.0)
    w_glu = nrm(ks[11], (DEPTH, SSM_WIDTH, SSM_WIDTH), SSM_WIDTH ** -0.5)
    w_branch_a = nrm(ks[12], (DEPTH, SSM_WIDTH, D_MODEL), SSM_WIDTH ** -0.5)
    w_branch_b = nrm(ks[13], (DEPTH, ATT_WIDTH, D_MODEL), ATT_WIDTH ** -0.5)
    w_out = nrm(ks[14], (DEPTH, D_MODEL, D_MODEL), D_MODEL ** -0.5)
    norm2_g = 1.0 + nrm(ks[15], (DEPTH, D_MODEL), 0.02)
    w_ffn_in = nrm(ks[16], (DEPTH, D_MODEL, 2 * FFN_HIDDEN), D_MODEL ** -0.5)
    w_ffn_out = nrm(ks[17], (DEPTH, FFN_HIDDEN, D_MODEL), FFN_HIDDEN ** -0.5)
    norm_f_g = 1.0 + nrm(ks[18], (D_MODEL,), 0.02)
    return {'x': x, 'norm1_g': norm1_g, 'w_in': w_in, 'a_re': a_re, 'a_im': a_im, 'log_dt': log_dt,
            'b_re': b_re, 'b_im': b_im, 'c_re': c_re, 'c_im': c_im, 'd_skip': d_skip, 'w_glu': w_glu,
            'w_branch_a': w_branch_a, 'w_branch_b': w_branch_b, 'w_out': w_out, 'norm2_g': norm2_g,
            'w_ffn_in': w_ffn_in, 'w_ffn_out': w_ffn_out, 'norm_f_g': norm_f_g}


def reference(x, norm1_g, w_in, a_re, a_im, log_dt, b_re, b_im, c_re, c_im, d_skip, w_glu,
              w_branch_a, w_branch_b, w_out, norm2_g, w_ffn_in, w_ffn_out, norm_f_g):
    L = x.shape[1]
    pos = jnp.arange(L, dtype=jnp.int32)
    split_pts = _split_points()
    for i in range(DEPTH):
        h = _rmsnorm(x, norm1_g[i])
        proj = h @ w_in[i]
        u, q, k, v, qi, ki, wi, ga, gb = jnp.split(proj, split_pts, axis=-1)
        ya = _s5_branch(u, a_re[i], a_im[i], log_dt[i], b_re[i], b_im[i], c_re[i], c_im[i], d_skip[i], w_glu[i])
        yb = _dsa_branch(q, k, v, qi, ki, wi, pos)
        merged = jax.nn.sigmoid(ga) * (ya @ w_branch_a[i]) + jax.nn.sigmoid(gb) * (yb @ w_branch_b[i])
        x = x + merged @ w_out[i]
        h2 = _rmsnorm(x, norm2_g[i])
        g, up = jnp.split(h2 @ w_ffn_in[i], [FFN_HIDDEN], axis=-1)
        x = x + (jax.nn.silu(g) * up) @ w_ffn_out[i]
    return _rmsnorm(x, norm_f_g)
```

```python
import math
from contextlib import ExitStack
import numpy as np
import concourse.bass as bass
import concourse.mybir as mybir
from concourse.bass_utils import run_bass_kernel_spmd

F32 = mybir.dt.float32
BF16 = mybir.dt.bfloat16
AF = mybir.ActivationFunctionType
ALU = mybir.AluOpType

D = 1024
L = 8192
NT = 16
FH = 2816
EPS = 1e-6
NEG = -1e30
NITER = 18
BIS_B = 64.0
NDS = 24


class Prog:
    def __init__(self, nc, es):
        self.nc = nc
        self.E = {'pe': nc.tensor, 'act': nc.scalar, 'dve': nc.vector, 'pool': nc.gpsimd, 'sp': nc.sync}
        self.sem = {k: es.enter_context(nc.semaphore('s_' + k)) for k in ['pe', 'act', 'dve', 'pool']}
        self.cnt = {k: 0 for k in self.sem}
        self.dsem = [es.enter_context(nc.semaphore('d%d' % i)) for i in range(NDS)]
        self.dcnt = [0] * NDS
        self.dnext = 0
        self.seen = {e: {} for e in self.E}
        self.lw = {}
        self.rd = {}

    def _semobj(self, key):
        return self.dsem[key[1]] if isinstance(key, tuple) else self.sem[key]

    def _wait(self, eng, ev):
        key, val = ev
        if self.seen[eng].get(key, 0) >= val:
            return
        self.seen[eng][key] = val
        self.E[eng].wait_ge(self._semobj(key), val)

    def _deps(self, eng, r, w):
        evs = {}
        for x in r:
            if x in self.lw:
                k, v = self.lw[x]
                evs[k] = max(evs.get(k, 0), v)
        for x in w:
            if x in self.lw:
                k, v = self.lw[x]
                evs[k] = max(evs.get(k, 0), v)
            for k, v in self.rd.get(x, {}).items():
                evs[k] = max(evs.get(k, 0), v)
        for k, v in evs.items():
            if eng == 'pe' and k == 'pe':
                continue
            self._wait(eng, (k, v))

    def _record(self, me, r, w):
        k, v = me
        for x in r:
            d = self.rd.setdefault(x, {})
            d[k] = max(d.get(k, 0), v)
        for x in w:
            self.lw[x] = me
            self.rd[x] = {}

    def op(self, eng, fn, r=(), w=()):
        self._deps(eng, r, w)
        ins = fn()
        self.cnt[eng] += 1
        ins.then_inc(self.sem[eng], 1)
        self._record((eng, self.cnt[eng]), r, w)

    def dma(self, out, in_, r=(), w=(), q='sp'):
        self._deps(q, r, w)
        i = self.dnext
        self.dnext = (i + 1) % NDS
        if self.dcnt[i] > 0:
            self._wait(q, (('d', i), self.dcnt[i]))
        self.E[q].dma_start(out=out, in_=in_).then_inc(self.dsem[i], 16)
        self.dcnt[i] += 16
        self._record((('d', i), self.dcnt[i]), r, w)

    def barrier(self):
        for e in ['pe', 'act', 'dve', 'pool', 'sp']:
            for k in self.sem:
                if self.cnt[k] > 0:
                    self._wait(e, (k, self.cnt[k]))
            for i in range(NDS):
                if self.dcnt[i] > 0:
                    self._wait(e, (('d', i), self.dcnt[i]))
        self.lw = {}
        self.rd = {}


def build_program(dbg=None):
    nc = bass.Bass("TRN2", target_bir_lowering=False)

    def din(name, shape, dt=F32):
        return nc.dram_tensor(name, list(shape), dt, kind="ExternalInput").ap()

    def dscr(name, shape, dt):
        return nc.dram_tensor(name, list(shape), dt, kind="Internal").ap()

    xp = din("xp", [L, D])
    wu = din("wu", [D, 1024])
    wkv = din("wkv", [D, 640])
    wq = din("wq", [D, 2048])
    wwi = din("wwi", [D, 16])
    wg = din("wg", [D, 2048])
    wglu = din("wglu", [D, D])
    wba = din("wba", [D, D])
    wbb = din("wbb", [D, D])
    wout = din("wout", [D, D])
    wfi = din("wfi", [D, 2 * FH])
    wfo = din("wfo", [FH, D])
    g1T = din("g1T", [128, 8])
    g2T = din("g2T", [128, 8])
    gfb = din("gfb", [128, D])
    AR1 = din("AR1", [128, 1024]); AI1 = din("AI1", [128, 1024]); DT1 = din("DT1", [128, 1024])
    BR1 = din("BR1", [128, 1024]); BI1 = din("BI1", [128, 1024])
    ARE = din("ARE", [128, 32]); AIE = din("AIE", [128, 32]); DTE = din("DTE", [128, 32])
    CTR = din("CTR", [128, 512]); CTI = din("CTI", [128, 512])
    BER = din("BER", [128, 1024]); BEI = din("BEI", [128, 1024])
    DFM = din("DFM", [128, 8])
    identf_d = din("identf", [128, 128])
    permA_d = din("permA", [128, 128]); permI_d = din("permI", [128, 128])
    cAk = din("cAk", [128, L]); sAk = din("sAk", [128, L])
    cIk = din("cIk", [128, L]); sIk = din("sIk", [128, L])
    cAq = din("cAq", [128, 2048]); sAq = din("sAq", [128, 2048])
    cIq = din("cIq", [128, 2048]); sIq = din("sIq", [128, 2048])
    mfirst = din("mfirst", [128, 512]); mlast = din("mlast", [128, 512]); mboth = din("mboth", [128, 512])
    out_d = nc.dram_tensor("out", [2048, D], F32, kind="ExternalOutput").ap()
    dbg_d = None
    if dbg:
        dbg_d = nc.dram_tensor("dbg", list(dbg[1]), F32, kind="ExternalOutput").ap()

    HT_d = dscr("HT_d", [NT, 128, 1024], BF16)
    U_d = dscr("U_d", [NT, 128, 1024], BF16)
    HS_d = dscr("HS_d", [NT, 128, 1024], BF16)
    Y_d = dscr("Y_d", [NT, 128, 1024], BF16)
    YB_d = dscr("YB_d", [NT, 128, 1024], BF16)
    X1_d = dscr("X1_d", [NT, 128, 1024], F32)

    es = ExitStack()
    P = Prog(nc, es)
    V, A, G, T = nc.vector, nc.scalar, nc.gpsimd, nc.tensor

    def sb(stack, name, shape, dt):
        return stack.enter_context(nc.sbuf_tensor(name, list(shape), dt))

    def fin():
        P.barrier()
        return nc

    def stop(name):
        return bool(dbg) and dbg[0] == name

    identf = sb(es, "identf_s", [128, 128], F32)
    identb = sb(es, "identb", [128, 128], BF16)
    permA = sb(es, "permA_s", [128, 128], BF16)
    permI = sb(es, "permI_s", [128, 128], BF16)
    onesb = sb(es, "onesb", [128, 128], BF16)
    g1s = sb(es, "g1s", [128, 8], F32)
    g2s = sb(es, "g2s", [128, 8], F32)
    ptmp = sb(es, "ptmp", [128, 128], F32)
    P.dma(identf[:], identf_d[:, :], w=['identf'])
    P.op('dve', lambda: V.tensor_copy(out=identb[:], in_=identf[:]), r=['identf'], w=['identb'])
    P.dma(ptmp[:], permA_d[:, :], w=['ptmp'])
    P.op('dve', lambda: V.tensor_copy(out=permA[:], in_=ptmp[:]), r=['ptmp'], w=['permA'])
    P.dma(ptmp[:], permI_d[:, :], w=['ptmp'])
    P.op('dve', lambda: V.tensor_copy(out=permI[:], in_=ptmp[:]), r=['ptmp'], w=['permI'])
    P.op('pool', lambda: G.memset(onesb[:], 1.0), w=['onesb'])
    ident32 = sb(es, "ident32", [128, 128], F32)
    P.op('dve', lambda: V.tensor_scalar(out=ident32[:], in0=identf[:], scalar1=1.0 / 32.0, scalar2=None, op0=ALU.mult), r=['identf'], w=['ident32'])
    P.dma(g1s[:], g1T[:, :], w=['g1s'])
    P.dma(g2s[:], g2T[:, :], w=['g2s'])

    pball = es.enter_context(nc.psum_tensor("pball", [128, 4096], F32))
    pb = [pball[:, i * 512:(i + 1) * 512] for i in range(8)]
    pbn = ['pb%d' % i for i in range(8)]

    def pb16(i):
        return pb[i][:, :].bitcast(BF16)

    def load_weight_bf16(stack_tiles, wdram, c0, ncols, gscale, tag, stag=None):
        stg, dst = stack_tiles
        stag = stag or tag
        for kc in range(8):
            P.dma(stg[:, kc, 0:ncols], wdram[kc * 128:(kc + 1) * 128, c0:c0 + ncols], w=[stag + 's%d' % kc])
            if gscale is None:
                if kc % 2 == 0:
                    P.op('act', lambda kc=kc: A.copy(out=dst[:, kc, 0:ncols], in_=stg[:, kc, 0:ncols]), r=[stag + 's%d' % kc], w=[tag])
                else:
                    P.op('dve', lambda kc=kc: V.tensor_copy(out=dst[:, kc, 0:ncols], in_=stg[:, kc, 0:ncols]), r=[stag + 's%d' % kc], w=[tag])
            else:
                P.op('dve', lambda kc=kc: V.tensor_scalar(out=dst[:, kc, 0:ncols], in0=stg[:, kc, 0:ncols], scalar1=gscale[:, kc:kc + 1], scalar2=None, op0=ALU.mult),
                     r=[stag + 's%d' % kc, 'g1s', 'g2s'], w=[tag])

    def norm_transpose(stack_t, xsrc_rows, hT, col0, tagx, gate_bank, act_scale=False):
        xt, junk, ss, hb = stack_t
        P.dma(xt[:], xsrc_rows, w=[tagx + 'xt'])
        P.op('act', lambda: A.activation(out=junk[:], in_=xt[:], func=AF.Square, accum_out=ss[:, 0:1]),
             r=[tagx + 'xt'], w=[tagx + 'junk', tagx + 'ss'])
        P.op('dve', lambda: V.tensor_scalar(out=ss[:, 1:2], in0=ss[:, 0:1], scalar1=1.0 / D, scalar2=EPS, op0=ALU.mult, op1=ALU.add),
             r=[tagx + 'ss'], w=[tagx + 'ss'])
        P.op('act', lambda: A.activation(out=ss[:, 2:3], in_=ss[:, 1:2], func=AF.Sqrt), r=[tagx + 'ss'], w=[tagx + 'ss'])
        P.op('dve', lambda: V.reciprocal(out=ss[:, 3:4], in_=ss[:, 2:3]), r=[tagx + 'ss'], w=[tagx + 'ss'])
        if act_scale:
            P.op('act', lambda: A.activation(out=hb[:], in_=xt[:], func=AF.Copy, scale=ss[:, 3:4]),
                 r=[tagx + 'xt', tagx + 'ss'], w=[tagx + 'hb'])
        else:
            P.op('dve', lambda: V.tensor_scalar(out=hb[:], in0=xt[:], scalar1=ss[:, 3:4], scalar2=None, op0=ALU.mult),
                 r=[tagx + 'xt', tagx + 'ss'], w=[tagx + 'hb'])
        b = gate_bank
        for kc in range(8):
            P.op('pe', lambda kc=kc: T.transpose(out=pb16(b)[:, kc * 128:(kc + 1) * 128], in_=hb[:, kc * 128:(kc + 1) * 128], identity=identb[:]),
                 r=[tagx + 'hb', 'identb'], w=[pbn[b]])
        return b

    def rope(dst, nrows, perm, ctab, stab, tmp1, tmp2, bank, tags_r, tag_w, ncols):
        P.op('pe', lambda: T.matmul(pb[bank][0:nrows, 0:ncols], lhsT=perm[0:nrows, 0:nrows], rhs=dst, start=True, stop=True, skip_group_check=True),
             r=[tag_w, 'permA', 'permI'], w=[pbn[bank]])
        P.op('dve', lambda: V.tensor_tensor(out=tmp1[0:nrows, 0:ncols], in0=dst, in1=ctab, op=ALU.mult), r=[tag_w] + tags_r, w=['ropet1'])
        P.op('dve', lambda: V.tensor_tensor(out=tmp2[0:nrows, 0:ncols], in0=pb[bank][0:nrows, 0:ncols], in1=stab, op=ALU.mult),
             r=[pbn[bank]] + tags_r, w=['ropet2'])
        P.op('dve', lambda: V.tensor_tensor(out=dst, in0=tmp1[0:nrows, 0:ncols], in1=tmp2[0:nrows, 0:ncols], op=ALU.add),
             r=['ropet1', 'ropet2'], w=[tag_w])

    sq = ExitStack()
    QP = sb(sq, "QP", [128, 9, 2, 32], F32)
    FP = sb(sq, "FP", [128, 8, 2, 32], F32)
    s5 = ExitStack()
    W1 = sb(s5, "W1", [128, 8, 2, 8, 128], BF16)
    cosT = sb(s5, "cosT", [128, 32, 64], F32)
    sinT = sb(s5, "sinT", [128, 32, 64], F32)
    R8z = sb(s5, "R8z", [128, 32, 64], F32)
    A8r = sb(s5, "A8r", [128, 32], F32)
    A8i = sb(s5, "A8i", [128, 32], F32)

    def sincos_lb(stack, n, ar, ai, dtl, pref):
        t = {}
        for nm in ['dt', 'ang', 'ex', 'mag', 'nn', 'red', 'sn', 'cs', 'lbr', 'lbi', 'fr', 'fi', 'u1', 'u2', 'u3']:
            t[nm] = sb(stack, pref + nm, [128, n], F32)
        R = [pref]
        def vv(fn):
            P.op('dve', fn, r=R, w=R)
        def aa(fn):
            P.op('act', fn, r=R, w=R)
        aa(lambda: A.activation(out=t['dt'][:], in_=dtl, func=AF.Exp))
        vv(lambda: V.tensor_tensor(out=t['ang'][:], in0=ai, in1=t['dt'][:], op=ALU.mult))
        vv(lambda: V.tensor_tensor(out=t['ex'][:], in0=ar, in1=t['dt'][:], op=ALU.mult))
        aa(lambda: A.activation(out=t['mag'][:], in_=t['ex'][:], func=AF.Exp))
        C1 = float(np.float32(2 * math.pi))
        C2 = float(2 * math.pi - np.float64(np.float32(2 * math.pi)))
        for which, off in (('sn', 0.0), ('cs', math.pi / 2)):
            vv(lambda off=off: V.tensor_scalar(out=t['u1'][:], in0=t['ang'][:], scalar1=off, scalar2=None, op0=ALU.add))
            vv(lambda: V.tensor_scalar(out=t['nn'][:], in0=t['u1'][:], scalar1=math.pi, scalar2=None, op0=ALU.is_gt))
            for kk in (3, 5, 7):
                vv(lambda kk=kk: V.tensor_scalar(out=t['u2'][:], in0=t['u1'][:], scalar1=kk * math.pi, scalar2=None, op0=ALU.is_gt))
                vv(lambda: V.tensor_tensor(out=t['nn'][:], in0=t['nn'][:], in1=t['u2'][:], op=ALU.add))
            vv(lambda: V.scalar_tensor_tensor(out=t['red'][:], in0=t['nn'][:], scalar=-C1, in1=t['u1'][:], op0=ALU.mult, op1=ALU.add))
            vv(lambda: V.scalar_tensor_tensor(out=t['red'][:], in0=t['nn'][:], scalar=-C2, in1=t['red'][:], op0=ALU.mult, op1=ALU.add))
            vv(lambda: V.tensor_scalar(out=t['red'][:], in0=t['red'][:], scalar1=math.pi, scalar2=-math.pi, op0=ALU.min, op1=ALU.max))
            aa(lambda which=which: A.activation(out=t[which][:], in_=t['red'][:], func=AF.Sin))
        vv(lambda: V.tensor_tensor(out=t['lbr'][:], in0=t['mag'][:], in1=t['cs'][:], op=ALU.mult))
        vv(lambda: V.tensor_tensor(out=t['lbi'][:], in0=t['mag'][:], in1=t['sn'][:], op=ALU.mult))
        vv(lambda: V.tensor_tensor(out=t['u1'][:], in0=ar, in1=ar, op=ALU.mult))
        vv(lambda: V.tensor_tensor(out=t['u2'][:], in0=ai, in1=ai, op=ALU.mult))
        vv(lambda: V.tensor_tensor(out=t['u1'][:], in0=t['u1'][:], in1=t['u2'][:], op=ALU.add))
        vv(lambda: V.reciprocal(out=t['u3'][:], in_=t['u1'][:]))
        vv(lambda: V.tensor_scalar(out=t['u1'][:], in0=t['lbr'][:], scalar1=-1.0, scalar2=None, op0=ALU.add))
        vv(lambda: V.tensor_tensor(out=t['fr'][:], in0=t['u1'][:], in1=ar, op=ALU.mult))
        vv(lambda: V.tensor_tensor(out=t['u2'][:], in0=t['lbi'][:], in1=ai, op=ALU.mult))
        vv(lambda: V.tensor_tensor(out=t['fr'][:], in0=t['fr'][:], in1=t['u2'][:], op=ALU.add))
        vv(lambda: V.tensor_tensor(out=t['fr'][:], in0=t['fr'][:], in1=t['u3'][:], op=ALU.mult))
        vv(lambda: V.tensor_tensor(out=t['fi'][:], in0=t['lbi'][:], in1=ar, op=ALU.mult))
        vv(lambda: V.tensor_tensor(out=t['u2'][:], in0=t['u1'][:], in1=ai, op=ALU.mult))
        vv(lambda: V.tensor_tensor(out=t['fi'][:], in0=t['fi'][:], in1=t['u2'][:], op=ALU.subtract))
        vv(lambda: V.tensor_tensor(out=t['fi'][:], in0=t['fi'][:], in1=t['u3'][:], op=ALU.mult))
        return t

    def cmul(outr, outi, ar_, ai_, br_, bi_, t1, t2, R):
        P.op('dve', lambda: V.tensor_tensor(out=t1, in0=ar_, in1=br_, op=ALU.mult), r=R, w=R)
        P.op('dve', lambda: V.tensor_tensor(out=t2, in0=ai_, in1=bi_, op=ALU.mult), r=R, w=R)
        P.op('dve', lambda: V.tensor_tensor(out=outr, in0=t1, in1=t2, op=ALU.subtract), r=R, w=R)
        P.op('dve', lambda: V.tensor_tensor(out=t1, in0=ar_, in1=bi_, op=ALU.mult), r=R, w=R)
        P.op('dve', lambda: V.tensor_tensor(out=t2, in0=ai_, in1=br_, op=ALU.mult), r=R, w=R)
        P.op('dve', lambda: V.tensor_tensor(out=outi, in0=t1, in1=t2, op=ALU.add), r=R, w=R)

    with ExitStack() as st:
        arl = sb(st, "arl", [128, 1024], F32); ail = sb(st, "ail", [128, 1024], F32); dtl = sb(st, "dtl", [128, 1024], F32)
        brl = sb(st, "brl", [128, 1024], F32); bil = sb(st, "bil", [128, 1024], F32)
        for tl, src in ((arl, AR1), (ail, AI1), (dtl, DT1), (brl, BR1), (bil, BI1)):
            P.dma(tl[:], src[:, :], w=['L1'])
        tt = sincos_lb(st, 1024, arl[:], ail[:], dtl[:], 'L1')
        pr = [sb(st, "pr%d" % i, [128, 1024], F32) for i in range(2)]
        pi = [sb(st, "pi%d" % i, [128, 1024], F32) for i in range(2)]
        c1 = sb(st, "c1", [128, 1024], F32); c2 = sb(st, "c2", [128, 1024], F32)
        R = ['L1']
        cur_r, cur_i = tt['fr'], tt['fi']
        for k in range(8):
            s_ = 7 - k
            v3 = lambda ap: ap.rearrange("p (kc n) -> p kc n", kc=8)
            P.op('dve', lambda: V.tensor_tensor(out=c1[:], in0=cur_r[:], in1=brl[:], op=ALU.mult), r=R, w=R)
            P.op('dve', lambda: V.tensor_tensor(out=c2[:], in0=cur_i[:], in1=bil[:], op=ALU.mult), r=R, w=R)
            P.op('dve', lambda s_=s_: V.tensor_tensor(out=W1[:, :, 0, s_, :], in0=v3(c1[:]), in1=v3(c2[:]), op=ALU.subtract), r=R, w=R + ['W1'])
            P.op('dve', lambda: V.tensor_tensor(out=c1[:], in0=cur_r[:], in1=bil[:], op=ALU.mult), r=R, w=R)
            P.op('dve', lambda: V.tensor_tensor(out=c2[:], in0=cur_i[:], in1=brl[:], op=ALU.mult), r=R, w=R)
            P.op('dve', lambda s_=s_: V.tensor_tensor(out=W1[:, :, 1, s_, :], in0=v3(c1[:]), in1=v3(c2[:]), op=ALU.add), r=R, w=R + ['W1'])
            if k < 7:
                nr, ni = pr[k % 2], pi[k % 2]
                cmul(nr[:], ni[:], cur_r[:], cur_i[:], tt['lbr'][:], tt['lbi'][:], c1[:], c2[:], R)
                cur_r, cur_i = nr, ni
    P.barrier()
    if stop('c1'):
        return fin()
    with ExitStack() as st:
        are = sb(st, "are", [128, 32], F32); aie = sb(st, "aie", [128, 32], F32); dte = sb(st, "dte", [128, 32], F32)
        for tl, src in ((are, ARE), (aie, AIE), (dte, DTE)):
            P.dma(tl[:], src[:, :], w=['E'])
        te = sincos_lb(st, 32, are[:], aie[:], dte[:], 'E')
        e1 = sb(st, "e1", [128, 32], F32); e2 = sb(st, "e2", [128, 32], F32)
        R = ['E']
        P.op('dve', lambda: V.memset(QP[:, 0, 0, :], 1.0), r=R, w=R)
        P.op('dve', lambda: V.memset(QP[:, 0, 1, :], 0.0), r=R, w=R)
        for k in range(1, 9):
            cmul(QP[:, k, 0, :], QP[:, k, 1, :], QP[:, k - 1, 0, :], QP[:, k - 1, 1, :], te['lbr'][:], te['lbi'][:], e1[:], e2[:], R)
        P.op('dve', lambda: V.tensor_copy(out=FP[:, 0, 0, :], in_=te['fr'][:]), r=R, w=R)
        P.op('dve', lambda: V.tensor_copy(out=FP[:, 0, 1, :], in_=te['fi'][:]), r=R, w=R)
        for k in range(1, 8):
            cmul(FP[:, k, 0, :], FP[:, k, 1, :], FP[:, k - 1, 0, :], FP[:, k - 1, 1, :], te['lbr'][:], te['lbi'][:], e1[:], e2[:], R)
        P.op('dve', lambda: V.tensor_copy(out=A8r[:], in_=QP[:, 8, 0, :]), r=R, w=R)
        P.op('dve', lambda: V.tensor_copy(out=A8i[:], in_=QP[:, 8, 1, :]), r=R, w=R)
        m2 = sb(st, "m2", [128, 32], F32); m4 = sb(st, "m4", [128, 32], F32); m8 = sb(st, "m8", [128, 32], F32)
        P.op('dve', lambda: V.tensor_tensor(out=m2[:], in0=te['mag'][:], in1=te['mag'][:], op=ALU.mult), r=R, w=R)
        P.op('dve', lambda: V.tensor_tensor(out=m4[:], in0=m2[:], in1=m2[:], op=ALU.mult), r=R, w=R)
        P.op('dve', lambda: V.tensor_tensor(out=m8[:], in0=m4[:], in1=m4[:], op=ALU.mult), r=R, w=R)
        wr = [sb(st, "wr%d" % i, [128, 32], F32) for i in range(4)]
        wi_ = [sb(st, "wi%d" % i, [128, 32], F32) for i in range(4)]
        P.op('dve', lambda: V.tensor_copy(out=wr[0][:], in_=te['cs'][:]), r=R, w=R)
        P.op('dve', lambda: V.tensor_copy(out=wi_[0][:], in_=te['sn'][:]), r=R, w=R)
        for k in range(1, 4):
            cmul(wr[k][:], wi_[k][:], wr[k - 1][:], wi_[k - 1][:], wr[k - 1][:], wi_[k - 1][:], e1[:], e2[:], R)
        P.op('dve', lambda: V.memset(cosT[:, :, 0:1], 1.0), r=R, w=R)
        P.op('dve', lambda: V.memset(sinT[:, :, 0:1], 0.0), r=R, w=R)
        sr = [sb(st, "sr%d" % i, [128, 32], F32) for i in range(2)]
        si = [sb(st, "si%d" % i, [128, 32], F32) for i in range(2)]
        big1 = sb(st, "big1", [128, 32, 32], F32); big2 = sb(st, "big2", [128, 32, 32], F32)
        stepr, stepi = wr[3], wi_[3]
        for lv in range(6):
            n = 1 << lv
            bc = lambda ap, n=n: ap.unsqueeze(2).to_broadcast([128, 32, n])
            P.op('dve', lambda: V.tensor_tensor(out=big1[:, :, 0:n], in0=cosT[:, :, 0:n], in1=bc(stepr[:]), op=ALU.mult), r=R, w=R)
            P.op('dve', lambda: V.tensor_tensor(out=big2[:, :, 0:n], in0=sinT[:, :, 0:n], in1=bc(stepi[:]), op=ALU.mult), r=R, w=R)
            P.op('dve', lambda: V.tensor_tensor(out=cosT[:, :, n:2 * n], in0=big1[:, :, 0:n], in1=big2[:, :, 0:n], op=ALU.subtract), r=R, w=R)
            P.op('dve', lambda: V.tensor_tensor(out=big1[:, :, 0:n], in0=cosT[:, :, 0:n], in1=bc(stepi[:]), op=ALU.mult), r=R, w=R)
            P.op('dve', lambda: V.tensor_tensor(out=big2[:, :, 0:n], in0=sinT[:, :, 0:n], in1=bc(stepr[:]), op=ALU.mult), r=R, w=R)
            P.op('dve', lambda: V.tensor_tensor(out=sinT[:, :, n:2 * n], in0=big1[:, :, 0:n], in1=big2[:, :, 0:n], op=ALU.add), r=R, w=R)
            if lv < 5:
                nr, ni = sr[lv % 2], si[lv % 2]
                cmul(nr[:], ni[:], stepr[:], stepi[:], stepr[:], stepi[:], e1[:], e2[:], R)
                stepr, stepi = nr, ni
        P.op('dve', lambda: V.tensor_copy(out=R8z[:], in_=m8[:].unsqueeze(2).to_broadcast([128, 32, 64])), r=R, w=R)
        P.op('dve', lambda: V.memset(R8z[:, :, 0:1], 0.0), r=R, w=R)
    P.barrier()
    if stop('c2'):
        return fin()

    with ExitStack() as st:
        WuB = sb(st, "WuB", [128, 8, 1024], BF16)
        with ExitStack() as s2:
            wstg = sb(s2, "wstgA", [128, 8, 1024], F32)
            load_weight_bf16((wstg, WuB), wu, 0, 1024, g1s, 'WuB')
            P.barrier()
            if stop('c30'):
                return fin()
        xt = [sb(st, "xtA%d" % i, [128, 1024], F32) for i in range(2)]
        junk = [sb(st, "junkA%d" % i, [128, 1024], F32) for i in range(2)]
        ssA = [sb(st, "ssA%d" % i, [128, 4], F32) for i in range(2)]
        hb = [sb(st, "hbA%d" % i, [128, 1024], BF16) for i in range(2)]
        hT = [sb(st, "hTA%d" % i, [128, 8, 512], BF16) for i in range(2)]
        uT = [sb(st, "uTA%d" % i, [128, 8, 512], BF16) for i in range(2)]
        S0b = [sb(st, "S0_%d" % i, [128, 32, 2, 64], F32) for i in range(2)]
        Z = sb(st, "Z", [128, 32, 2, 64], F32)
        ta = sb(st, "ta", [128, 32, 64], F32)
        tb = sb(st, "tb", [128, 32, 64], F32)
        Hin = sb(st, "Hin", [128, 2, 32], F32)
        cr_ = sb(st, "cr_", [128, 2, 32], F32)
        cq1 = sb(st, "cq1", [128, 32], F32); cq2 = sb(st, "cq2", [128, 32], F32)
        Hb16 = [sb(st, "Hb16_%d" % i, [128, 32, 2, 16], BF16) for i in range(2)]
        P.op('dve', lambda: V.memset(Hin[:], 0.0), w=['Hin'])
        bank_rot = [0]

        def nb():
            b = bank_rot[0]
            bank_rot[0] = (b + 1) % 8
            return b

        def stageA(t):
            hTt = hT[t % 2]; uTt = uT[t % 2]
            hn = 'hT%d' % (t % 2); un = 'uT%d' % (t % 2)
            S0 = S0b[t % 2]; s0n = 'S0_%d' % (t % 2)
            for blk in range(4):
                i2 = blk % 2
                b = nb()
                norm_transpose((xt[i2], junk[i2], ssA[i2], hb[i2]), xp[(t * 4 + blk) * 128:(t * 4 + blk + 1) * 128, :], hTt, blk * 128, 'A%d' % i2, b, act_scale=True)
                P.op('act', lambda b=b, blk=blk: A.copy(out=hTt[:, :, blk * 128:(blk + 1) * 128], in_=pb16(b).rearrange("p (kc n) -> p kc n", kc=8)),
                     r=[pbn[b]], w=[hn])
            for oc in range(8):
                b = nb()
                for kc in range(8):
                    P.op('pe', lambda b=b, oc=oc, kc=kc: T.matmul(pb[b][:, :], lhsT=WuB[:, kc, oc * 128:(oc + 1) * 128], rhs=hTt[:, kc, :],
                                                                   start=(kc == 0), stop=(kc == 7), skip_group_check=True),
                         r=['WuB', hn], w=[pbn[b]])
                P.op('act', lambda b=b, oc=oc: A.copy(out=uTt[:, oc, :], in_=pb[b][:, :]), r=[pbn[b]], w=[un])
            P.dma(HT_d[t].rearrange("p (kc n) -> p kc n", kc=8), hTt[:, :, 384:512], r=[hn], w=['HT_d'])
            P.dma(U_d[t].rearrange("p (kc n) -> p kc n", kc=8), uTt[:, :, 384:512], r=[un], w=['U_d'])
            for kc in range(8):
                for comp in range(2):
                    c0 = comp * 64
                    for s_ in range(8):
                        for pp in range(4):
                            b = 4 * (kc % 2) + pp
                            P.op('pe', lambda b=b, kc=kc, pp=pp, comp=comp, s_=s_, c0=c0: T.matmul(
                                pb[b][:, c0:c0 + 64], lhsT=W1[32 * pp:32 * pp + 32, kc, comp, s_, :], rhs=uTt[32 * pp:32 * pp + 32, kc, s_::8],
                                start=(s_ == 0), stop=(s_ == 7), skip_group_check=True, tile_position=(32 * pp, 0)),
                                r=['W1', un], w=[pbn[b]])
                for pp in range(4):
                    b = 4 * (kc % 2) + pp
                    S0v = S0[:, 4 * kc + pp, :, :].rearrange("p c m -> p (c m)")
                    P.op('act', lambda b=b, S0v=S0v: A.copy(out=S0v, in_=pb[b][:, 0:128]), r=[pbn[b]], w=[s0n])

        def stageB(t):
            S0 = S0b[t % 2]; s0n = 'S0_%d' % (t % 2)
            Hh = S0
            Sr, Si = S0[:, :, 0, :], S0[:, :, 1, :]
            Zr, Zi = Z[:, :, 0, :], Z[:, :, 1, :]
            Rr = [s0n, 'Z', 'ta', 'tb']
            P.op('dve', lambda: V.tensor_tensor(out=ta[:], in0=cosT[:], in1=Sr, op=ALU.mult), r=Rr, w=['ta'])
            P.op('pool', lambda: G.tensor_tensor(out=tb[:], in0=sinT[:], in1=Si, op=ALU.mult), r=Rr, w=['tb'])
            P.op('dve', lambda: V.tensor_tensor(out=Zr, in0=ta[:], in1=tb[:], op=ALU.add), r=Rr, w=['Z'])
            P.op('dve', lambda: V.tensor_tensor(out=ta[:], in0=cosT[:], in1=Si, op=ALU.mult), r=Rr, w=['ta'])
            P.op('pool', lambda: G.tensor_tensor(out=tb[:], in0=sinT[:], in1=Sr, op=ALU.mult), r=Rr, w=['tb'])
            P.op('dve', lambda: V.tensor_tensor(out=Zi, in0=ta[:], in1=tb[:], op=ALU.subtract), r=Rr, w=['Z'])
            Rc = ['Hin', 'cr_', 'cq']
            P.op('dve', lambda: V.tensor_tensor(out=cq1[:], in0=A8r[:], in1=Hin[:, 0, :], op=ALU.mult), r=Rc, w=Rc)
            P.op('dve', lambda: V.tensor_tensor(out=cq2[:], in0=A8i[:], in1=Hin[:, 1, :], op=ALU.mult), r=Rc, w=Rc)
            P.op('dve', lambda: V.tensor_tensor(out=cr_[:, 0, :], in0=cq1[:], in1=cq2[:], op=ALU.subtract), r=Rc, w=Rc)
            P.op('dve', lambda: V.tensor_tensor(out=cq1[:], in0=A8r[:], in1=Hin[:, 1, :], op=ALU.mult), r=Rc, w=Rc)
            P.op('dve', lambda: V.tensor_tensor(out=cq2[:], in0=A8i[:], in1=Hin[:, 0, :], op=ALU.mult), r=Rc, w=Rc)
            P.op('dve', lambda: V.tensor_tensor(out=cr_[:, 1, :], in0=cq1[:], in1=cq2[:], op=ALU.add), r=Rc, w=Rc)
            P.op('dve', lambda: V.tensor_tensor(out=Z[:, :, 0, 0], in0=Z[:, :, 0, 0], in1=cr_[:, 0, :], op=ALU.add), r=Rc + ['Z'], w=['Z'])
            P.op('dve', lambda: V.tensor_tensor(out=Z[:, :, 1, 0], in0=Z[:, :, 1, 0], in1=cr_[:, 1, :], op=ALU.add), r=Rc + ['Z'], w=['Z'])
            fl = lambda ap: ap.rearrange("p a m -> p (a m)")
            P.op('dve', lambda: V.tensor_copy(out=ta[:], in_=Zr), r=['Z'], w=['ta'])
            P.op('pool', lambda: G.tensor_copy(out=tb[:], in_=Zi), r=['Z'], w=['tb'])
            P.op('dve', lambda: V.tensor_tensor_scan(out=fl(ta[:]), data0=fl(R8z[:]), data1=fl(ta[:]), initial=0.0,
                                                    op0=ALU.mult, op1=ALU.add), r=['ta'], w=['ta'])
            P.op('dve', lambda: V.tensor_tensor_scan(out=fl(tb[:]), data0=fl(R8z[:]), data1=fl(tb[:]), initial=0.0,
                                                    op0=ALU.mult, op1=ALU.add), r=['tb'], w=['tb'])
            Rh = ['ta', 'tb', 'Z', s0n]
            P.op('dve', lambda: V.tensor_tensor(out=Zr, in0=cosT[:], in1=ta[:], op=ALU.mult), r=Rh, w=['Z'])
            P.op('pool', lambda: G.tensor_tensor(out=Zi, in0=sinT[:], in1=tb[:], op=ALU.mult), r=Rh, w=['Z'])
            P.op('dve', lambda: V.tensor_tensor(out=Hh[:, :, 0, :], in0=Zr, in1=Zi, op=ALU.subtract), r=Rh, w=[s0n])
            P.op('dve', lambda: V.tensor_tensor(out=Zr, in0=cosT[:], in1=tb[:], op=ALU.mult), r=Rh, w=['Z'])
            P.op('pool', lambda: G.tensor_tensor(out=Zi, in0=sinT[:], in1=ta[:], op=ALU.mult), r=Rh, w=['Z'])
            P.op('dve', lambda: V.tensor_tensor(out=Hh[:, :, 1, :], in0=Zr, in1=Zi, op=ALU.add), r=Rh, w=[s0n])
            P.op('dve', lambda: V.tensor_copy(out=Hin[:, 0, :], in_=Hh[:, :, 0, 63]), r=[s0n], w=['Hin'])
            P.op('dve', lambda: V.tensor_copy(out=Hin[:, 1, :], in_=Hh[:, :, 1, 63]), r=[s0n], w=['Hin'])
            hbt = Hb16[t % 2]
            P.op('dve', lambda hbt=hbt: V.tensor_copy(out=hbt[:], in_=Hh[:, :, :, 47:63]), r=[s0n], w=['Hb16_%d' % (t % 2)])
            P.dma(HS_d[t].rearrange("p (a c m) -> p a c m", a=32, c=2), hbt[:], r=['Hb16_%d' % (t % 2)], w=['HS_d'])

        stageA(0)
        for t in range(NT):
            if t + 1 < NT:
                stageA(t + 1)
            stageB(t)
    P.barrier()
    if stop('c4'):
        return fin()
    s5.close()

    with ExitStack() as st:
        CC = sb(st, "CC", [128, 32, 2, 256], BF16)
        KN = sb(st, "KN", [128, 8, 2, 8, 128], BF16)
        dfm = sb(st, "dfm", [128, 8], F32)
        P.dma(dfm[:], DFM[:, :], w=['dfm'])
        with ExitStack() as s2:
            ctr = sb(s2, "ctr", [128, 32, 16], F32); cti = sb(s2, "cti", [128, 32, 16], F32)
            ber = sb(s2, "ber", [128, 32, 32], F32); bei = sb(s2, "bei", [128, 32, 32], F32)
            P.dma(ctr[:].rearrange("p a c -> p (a c)"), CTR[:, :], w=['ctr'])
            P.dma(cti[:].rearrange("p a c -> p (a c)"), CTI[:, :], w=['cti'])
            P.dma(ber[:].rearrange("p a c -> p (a c)"), BER[:, :], w=['ber'])
            P.dma(bei[:].rearrange("p a c -> p (a c)"), BEI[:, :], w=['bei'])
            ctrb = sb(s2, "ctrb", [128, 32, 16], BF16); nctib = sb(s2, "nctib", [128, 32, 16], BF16)
            P.op('dve', lambda: V.tensor_copy(out=ctrb[:], in_=ctr[:]), r=['ctr'], w=['ctrb'])
            P.op('dve', lambda: V.tensor_scalar(out=nctib[:], in0=cti[:], scalar1=-1.0, scalar2=None, op0=ALU.mult), r=['cti'], w=['nctib'])
            k1 = sb(s2, "k1", [128, 32, 32], F32); k2 = sb(s2, "k2", [128, 32, 32], F32)
            xre = sb(s2, "xre", [128, 32, 32], BF16); xim = sb(s2, "xim", [128, 32, 32], BF16)
            R = ['cc']
            b16 = lambda ap: ap.unsqueeze(2).to_broadcast([128, 32, 16])
            b32 = lambda ap: ap.unsqueeze(2).to_broadcast([128, 32, 32])
            P.op('pool', lambda: G.memset(CC[:].rearrange("p a b c -> p (a b c)"), 0.0), w=['CC'])
            for s_ in range(8):
                qr, qi = QP[:, s_ + 1, 0, :], QP[:, s_ + 1, 1, :]
                P.op('dve', lambda: V.tensor_tensor(out=k1[:, :, 0:16], in0=ctr[:], in1=b16(qr), op=ALU.mult), r=R + ['ctr'], w=R)
                P.op('dve', lambda: V.tensor_tensor(out=k2[:, :, 0:16], in0=cti[:], in1=b16(qi), op=ALU.mult), r=R + ['cti'], w=R)
                for gg in range(2):
                    rs = slice(64 * gg, 64 * gg + 64)
                    P.op('dve', lambda s_=s_, gg=gg, rs=rs: V.tensor_tensor(out=CC[rs, :, 0, gg * 128 + s_ * 16:gg * 128 + (s_ + 1) * 16],
                                                                         in0=k1[rs, :, 0:16], in1=k2[rs, :, 0:16], op=ALU.subtract), r=R, w=R + ['CC'])
                P.op('dve', lambda: V.tensor_tensor(out=k1[:, :, 0:16], in0=ctr[:], in1=b16(qi), op=ALU.mult), r=R, w=R)
                P.op('dve', lambda: V.tensor_tensor(out=k2[:, :, 0:16], in0=cti[:], in1=b16(qr), op=ALU.mult), r=R, w=R)
                P.op('dve', lambda: V.tensor_tensor(out=k1[:, :, 0:16], in0=k1[:, :, 0:16], in1=k2[:, :, 0:16], op=ALU.add), r=R, w=R)
                for gg in range(2):
                    rs = slice(64 * gg, 64 * gg + 64)
                    P.op('dve', lambda s_=s_, gg=gg, rs=rs: V.tensor_scalar(out=CC[rs, :, 1, gg * 128 + s_ * 16:gg * 128 + (s_ + 1) * 16],
                                                                         in0=k1[rs, :, 0:16], scalar1=-1.0, scalar2=None, op0=ALU.mult), r=R, w=R + ['CC'])
            for tau in range(8):
                fr_, fi_ = FP[:, tau, 0, :], FP[:, tau, 1, :]
                P.op('dve', lambda: V.tensor_tensor(out=k1[:], in0=ber[:], in1=b32(fr_), op=ALU.mult), r=R + ['ber', 'xre', 'xim'], w=R)
                P.op('dve', lambda: V.tensor_tensor(out=k2[:], in0=bei[:], in1=b32(fi_), op=ALU.mult), r=R + ['bei'], w=R)
                P.op('dve', lambda: V.tensor_tensor(out=xre[:], in0=k1[:], in1=k2[:], op=ALU.subtract), r=R, w=R + ['xre'])
                P.op('dve', lambda: V.tensor_tensor(out=k1[:], in0=bei[:], in1=b32(fr_), op=ALU.mult), r=R, w=R)
                P.op('dve', lambda: V.tensor_tensor(out=k2[:], in0=ber[:], in1=b32(fi_), op=ALU.mult), r=R, w=R)
                P.op('dve', lambda: V.tensor_tensor(out=xim[:], in0=k1[:], in1=k2[:], op=ALU.add), r=R, w=R + ['xim'])
                for gp in range(32):
                    kc, pp = gp // 4, gp % 4
                    for gg in range(2):
                        bnk = 2 * gg + tau // 4
                        c0 = (tau % 4) * 128 + kc * 16
                        rs = slice(64 * gg, 64 * gg + 64)
                        P.op('pe', lambda bnk=bnk, gp=gp, gg=gg, pp=pp, c0=c0, rs=rs: T.matmul(
                            pb[bnk][32 * pp:32 * pp + 32, c0:c0 + 16], lhsT=xre[rs, gp, :], rhs=ctrb[rs, gp, :], start=True, stop=False,
                            skip_group_check=True, tile_position=(64 * gg, 32 * pp)), r=['xre', 'ctrb'], w=[pbn[bnk]])
                        P.op('pe', lambda bnk=bnk, gp=gp, gg=gg, pp=pp, c0=c0, rs=rs: T.matmul(
                            pb[bnk][32 * pp:32 * pp + 32, c0:c0 + 16], lhsT=xim[rs, gp, :], rhs=nctib[rs, gp, :], start=False, stop=True,
                            skip_group_check=True, tile_position=(64 * gg, 32 * pp)), r=['xim', 'nctib'], w=[pbn[bnk]])
            P.op('pool', lambda: G.memset(KN[:].rearrange("p a b c d -> p (a b c d)"), 0.0), w=['KN'])
            for tau in range(8):
                for gg in range(2):
                    bnk = 2 * gg + tau // 4
                    src = pb[bnk][:, (tau % 4) * 128:(tau % 4) * 128 + 128].rearrange("p (a c) -> p a c", a=8)
                    for sp_ in range(8 - tau):
                        s_ = sp_ + tau
                        P.op('dve', lambda src=src, sp_=sp_, s_=s_, gg=gg: V.tensor_copy(out=KN[:, :, gg, sp_, s_ * 16:(s_ + 1) * 16], in_=src),
                             r=[pbn[bnk]], w=['KN'])
        P.barrier()
        if stop('c5'):
            return fin()
        Hb_all = sb(st, "Hb_all", [128, 32, 2, 256], BF16)
        uo_all = sb(st, "uo_all", [128, 8, 2048], BF16)
        hstg = [sb(st, "hstg%d" % i, [128, 32, 2, 16], BF16) for i in range(2)]
        for t in range(NT):
            hs = hstg[t % 2]; hsn = 'hstg%d' % (t % 2)
            P.dma(hs[:], HS_d[t].rearrange("p (a c m) -> p a c m", a=32, c=2), r=['HS_d'], w=[hsn])
            if t % 2 == 0:
                P.op('act', lambda hs=hs, t=t: A.copy(out=Hb_all[:, :, :, t * 16:(t + 1) * 16], in_=hs[:]), r=[hsn], w=['Hb_all'])
            else:
                P.op('dve', lambda hs=hs, t=t: V.tensor_copy(out=Hb_all[:, :, :, t * 16:(t + 1) * 16], in_=hs[:]), r=[hsn], w=['Hb_all'])
            P.dma(uo_all[:, :, t * 128:(t + 1) * 128], U_d[t].rearrange("p (kc n) -> p kc n", kc=8), r=['U_d'], w=['uo_all'])
        nsb = [sb(st, "nsb%d" % i, [128, 256], F32) for i in range(2)]
        Yg = [sb(st, "Yg%d" % i, [128, 256], BF16) for i in range(2)]
        ytm = [sb(st, "ytm%d" % i, [128, 2, 8, 8, 16], BF16) for i in range(2)]
        ypre = [sb(st, "ypre0", [128, 2048], F32)] * 2
        g1_ = sb(st, "g1_", [128, 2048], F32); g2_ = g1_
        yg = [sb(st, "yg%d" % i, [128, 2048], BF16) for i in range(2)]
        Yd_v = Y_d.rearrange("t p (kc n) -> p t kc n", kc=8)
        for kc in range(8):
            yt = ytm[kc % 2]; ytn = 'ytm%d' % (kc % 2)
            for gl in range(8):
                gp, gg, pp = 4 * kc + gl // 2, gl % 2, gl // 2
                bF = gl % 2
                for comp in range(2):
                    P.op('pe', lambda bF=bF, gp=gp, comp=comp, gg=gg: T.matmul(
                        pb[bF][:, 0:256], lhsT=CC[:, gp, comp, gg * 128:(gg + 1) * 128], rhs=Hb_all[:, gp, comp, :], start=(comp == 0), stop=(comp == 1),
                        skip_group_check=True), r=['Hb_all', 'CC'], w=[pbn[bF]])
                bN = 2 + pp
                for sp_ in range(8):
                    P.op('pe', lambda bN=bN, pp=pp, kc=kc, gg=gg, sp_=sp_: T.matmul(
                        pb[bN][:, 0:256], lhsT=KN[32 * pp:32 * pp + 32, kc, gg, sp_, :], rhs=uo_all[32 * pp:32 * pp + 32, kc, sp_::8],
                        start=(sp_ == 0), stop=(sp_ == 7), skip_group_check=True, tile_position=(32 * pp, 0)), r=['uo_all', 'KN'], w=[pbn[bN]])
                ns = nsb[gl % 2]; nsn = 'nsb%d' % (gl % 2)
                ygt = Yg[gl % 2]; ygn = 'Yg%d' % (gl % 2)
                P.op('act', lambda bN=bN, ns=ns: A.copy(out=ns[:], in_=pb[bN][:, 0:256]), r=[pbn[bN]], w=[nsn])
                P.op('dve', lambda bF=bF, ns=ns, ygt=ygt: V.tensor_tensor(out=ygt[:], in0=pb[bF][:, 0:256], in1=ns[:], op=ALU.add), r=[pbn[bF], nsn], w=[ygn])
                for mh in range(2):
                    P.op('pe', lambda ygt=ygt, mh=mh: T.transpose(out=pb16(6)[:, mh * 128:(mh + 1) * 128], in_=ygt[:, mh * 128:(mh + 1) * 128], identity=identb[:]),
                         r=[ygn, 'identb'], w=[pbn[6]])
                P.op('act', lambda yt=yt, gl=gl: A.copy(out=yt[:, :, :, gl, :], in_=pb16(6)[:, 0:256].rearrange("p (h s c) -> p h s c", h=2, s=8)),
                     r=[pbn[6]], w=[ytn])
            yp = ypre[0]; ypn = 'ypre0'
            for mh in range(2):
                for s_ in range(8):
                    P.op('pe', lambda yt=yt, mh=mh, s_=s_: T.transpose(out=pb16(7)[:, s_ * 128:(s_ + 1) * 128], in_=yt[:, mh, s_, :, :].rearrange("p g c -> p (g c)"),
                                                                       identity=identb[:]), r=[ytn, 'identb'], w=[pbn[7]])
                ov = yp[:, mh * 1024:(mh + 1) * 1024].rearrange("p (m s) -> p s m", s=8)
                uv = uo_all[:, kc, mh * 1024:(mh + 1) * 1024].rearrange("p (m s) -> p s m", s=8)
                P.op('dve', lambda kc=kc, ov=ov, uv=uv: V.scalar_tensor_tensor(
                    out=ov, in0=uv, scalar=dfm[:, kc:kc + 1], in1=pb16(7)[:, 0:1024].rearrange("p (s m) -> p s m", s=8), op0=ALU.mult, op1=ALU.add),
                    r=['uo_all', 'dfm', pbn[7]], w=[ypn])
            yf = yp[:]
            ygo = yg[kc % 2]; ygon = 'yg%d' % (kc % 2)
            P.op('dve', lambda yf=yf: V.tensor_tensor(out=g1_[:], in0=yf, in1=yf, op=ALU.mult), r=[ypn], w=['g1_'])
            P.op('dve', lambda: V.tensor_scalar(out=g1_[:], in0=g1_[:], scalar1=0.044715, scalar2=1.0, op0=ALU.mult, op1=ALU.add), r=['g1_'], w=['g1_'])
            P.op('dve', lambda yf=yf: V.tensor_tensor(out=g1_[:], in0=g1_[:], in1=yf, op=ALU.mult), r=['g1_', ypn], w=['g1_'])
            P.op('act', lambda: A.activation(out=g1_[:], in_=g1_[:], func=AF.Sigmoid, scale=2.0 * 0.7978845608028654), r=['g1_'], w=['g1_'])
            P.op('dve', lambda ygo=ygo, yf=yf: V.tensor_tensor(out=ygo[:], in0=g1_[:], in1=yf, op=ALU.mult), r=['g1_', ypn], w=[ygon])
            P.dma(Yd_v[:, :, kc, :], ygo[:].rearrange("p (t n) -> p t n", t=NT), r=[ygon], w=['Y_d'])
    P.barrier()

    sq.close()
    if dbg and dbg[0] == 'Y':
        with ExitStack() as st:
            tmpb = sb(st, "tmpb", [128, 1024], BF16); tmpf = sb(st, "tmpf", [128, 1024], F32)
            for t in range(NT):
                P.dma(tmpb[:], Y_d[t], r=['Y_d'], w=['tmpb'])
                P.op('dve', lambda: V.tensor_copy(out=tmpf[:], in_=tmpb[:]), r=['tmpb'], w=['tmpf'])
                P.dma(dbg_d[t], tmpf[:], r=['tmpf'], w=['dbg'])
        P.barrier()
        for e in ['sp']:
            pass
        es.close()
        return nc

    if stop('Y2'):
        return fin()
    ISQ = 1.0 / math.sqrt(128.0)

    with ExitStack() as st:
        kT_all = sb(st, "kT_all", [128, 2, L], BF16)
        V_all = sb(st, "V_all", [128, 64, 256], BF16)
        kiT_all = sb(st, "kiT_all", [128, L], BF16)
        with ExitStack() as s2:
            WkvB = sb(s2, "WkvB", [128, 8, 640], BF16)
            with ExitStack() as s3:
                wstg = sb(s3, "wstgK", [128, 8, 640], F32)
                load_weight_bf16((wstg, WkvB), wkv, 0, 640, g1s, 'WkvB')
                P.barrier()
            xt = [sb(s2, "xtK%d" % i, [128, 1024], F32) for i in range(4)]
            junk = [sb(s2, "junkK0", [128, 1024], F32)] * 4
            ssA = [sb(s2, "ssK%d" % i, [128, 4], F32) for i in range(4)]
            hb = [sb(s2, "hbK%d" % i, [128, 1024], BF16) for i in range(4)]
            hT = [sb(s2, "hTK%d" % i, [128, 8, 512], BF16) for i in range(2)]
            ctab = sb(s2, "ctabK", [128, 512], F32); stab = sb(s2, "stabK", [128, 512], F32)
            ctabI = sb(s2, "ctabI", [128, 512], F32); stabI = sb(s2, "stabI", [128, 512], F32)
            rt1 = sb(s2, "rt1", [128, 512], F32); rt2 = sb(s2, "rt2", [128, 512], F32)
            brot = [0]

            def nb2():
                b = brot[0]
                brot[0] = (b + 1) % 8
                return b
            for t in range(NT):
                hTt = hT[t % 2]; hn = 'hTK%d' % (t % 2)
                for blk in range(4):
                    i2 = blk % 4
                    b = nb2()
                    norm_transpose((xt[i2], junk[i2], ssA[i2], hb[i2]), xp[(t * 4 + blk) * 128:(t * 4 + blk + 1) * 128, :], hTt, blk * 128, 'K%d' % i2, b)
                    if blk % 2 == 0:
                        P.op('act', lambda b=b, blk=blk: A.copy(out=hTt[:, :, blk * 128:(blk + 1) * 128], in_=pb16(b).rearrange("p (kc n) -> p kc n", kc=8)),
                             r=[pbn[b]], w=[hn])
                    else:
                        P.op('dve', lambda b=b, blk=blk: V.tensor_copy(out=hTt[:, :, blk * 128:(blk + 1) * 128], in_=pb16(b).rearrange("p (kc n) -> p kc n", kc=8)),
                             r=[pbn[b]], w=[hn])
                cs = slice(t * 512, (t + 1) * 512)
                P.dma(ctab[:], cAk[:, cs], w=['ctabK']); P.dma(stab[:], sAk[:, cs], w=['stabK'])
                P.dma(ctabI[:], cIk[:, cs], w=['ctabI']); P.dma(stabI[:], sIk[:, cs], w=['stabI'])
                for oc in range(3):
                    b = nb2()
                    for kc in range(8):
                        P.op('pe', lambda b=b, oc=oc, kc=kc: T.matmul(pb[b][:, :], lhsT=WkvB[:, kc, oc * 128:(oc + 1) * 128], rhs=hTt[:, kc, :],
                                                                       start=(kc == 0), stop=(kc == 7), skip_group_check=True),
                             r=['WkvB', hn], w=[pbn[b]])
                    if oc < 2:
                        dst = kT_all[:, oc, cs]
                        P.op('act', lambda b=b, dst=dst: A.copy(out=dst, in_=pb[b][:, :]), r=[pbn[b]], w=['kT_all'])
                        rope(dst, 128, permA, ctab[:], stab[:], rt1, rt2, nb2(), ['ctabK', 'stabK'], 'kT_all', 512)
                    else:
                        dst = kiT_all[:, cs]
                        P.op('act', lambda b=b, dst=dst: A.copy(out=dst, in_=pb[b][:, :]), r=[pbn[b]], w=['kiT_all'])
                        rope(dst, 128, permI, ctabI[:], stabI[:], rt1, rt2, nb2(), ['ctabI', 'stabI'], 'kiT_all', 512)
                for blk in range(4):
                    b = nb2()
                    for kc in range(8):
                        P.op('pe', lambda b=b, blk=blk, kc=kc: T.matmul(pb[b][:, 0:256], lhsT=hTt[:, kc, blk * 128:(blk + 1) * 128], rhs=WkvB[:, kc, 384:640],
                                                                         start=(kc == 0), stop=(kc == 7), skip_group_check=True),
                             r=['WkvB', hn], w=[pbn[b]])
                    P.op('dve', lambda b=b, blk=blk, t=t: V.tensor_copy(out=V_all[:, 4 * t + blk, :], in_=pb[b][:, 0:256]), r=[pbn[b]], w=['V_all'])
        P.barrier()
        if stop('KV'):
            return fin()
        hTo = sb(st, "hTo", [128, 8, 512], BF16)
        qT = sb(st, "qT", [128, 4, 8, 128], BF16)
        qiT = sb(st, "qiT", [128, 8, 512], BF16)
        wis = sb(st, "wis", [128, 4, 16], F32)
        wst = [sb(st, "wstQ0", [128, 8, 128], F32)] * 2
        wbf = [sb(st, "wbfQ%d" % i, [128, 8, 128], BF16) for i in range(2)]
        qtmp = sb(st, "qtmp", [128, 512], BF16)
        ctq = sb(st, "ctq", [128, 512], F32); stq = sb(st, "stq", [128, 512], F32)
        rt1 = sb(st, "rt1q", [128, 512], F32); rt2 = sb(st, "rt2q", [128, 512], F32)
        Dg = sb(st, "Dg", [128, 16, 128], BF16)
        Rb = [sb(st, "Rb%d" % i, [128, 1024], BF16) for i in range(3)]
        score = sb(st, "score", [128, L], F32)
        cjunk = sb(st, "cjunk", [128, L], mybir.dt.uint8)
        mtile = [sb(st, "mtile%d" % i, [128, 512], F32) for i in range(3)]
        P.dma(mtile[0][:], mfirst[:, :], w=['mt0']); P.dma(mtile[1][:], mlast[:, :], w=['mt1']); P.dma(mtile[2][:], mboth[:, :], w=['mt2'])
        bs = sb(st, "bs", [128, 8], F32)
        mq = [sb(st, "mq0", [128, 512], BF16)] * 2
        maskT = sb(st, "maskT", [128, 64, 128], BF16)
        Eb = [sb(st, "Eb%d" % i, [128, 1024], BF16) for i in range(2)]
        PT = [sb(st, "PT%d" % i, [128, 1024], BF16) for i in range(2)]
        rden = rt1
        ybt = [sb(st, "ybt0", [128, 1024], BF16)] * 2
        for half in range(4):
            for i in range(4):
                P.dma(hTo[:, :, i * 128:(i + 1) * 128], HT_d[4 * half + i].rearrange("p (kc n) -> p kc n", kc=8), r=['HT_d'], w=['hTo'])
            wcnt = 0
            for hh in range(16):
                wi2 = wcnt % 2; wcnt += 1
                load_weight_bf16((wst[wi2], wbf[wi2]), wq, hh * 128, 128, g1s, 'wbfQ%d' % wi2, stag='wstQ')
                for j in range(1):
                    b = j
                    for kc in range(8):
                        P.op('pe', lambda b=b, kc=kc, wi2=wi2, j=j: T.matmul(pb[b][:, :], lhsT=wbf[wi2][:, kc, :], rhs=hTo[:, kc, j * 512:(j + 1) * 512],
                                                                             start=(kc == 0), stop=(kc == 7), skip_group_check=True),
                             r=['wbfQ%d' % wi2, 'hTo'], w=[pbn[b]])
                    tcs = slice((half * 4) * 128, (half * 4) * 128 + 512)
                    if hh < 8:
                        P.dma(ctq[:], cAq[:, tcs], w=['ctq']); P.dma(stq[:], sAq[:, tcs], w=['stq'])
                        P.op('act', lambda b=b: A.copy(out=qtmp[:], in_=pb[b][:, :]), r=[pbn[b]], w=['qtmp'])
                        rope(qtmp[:], 128, permA, ctq[:], stq[:], rt1, rt2, 2 + j, ['ctq', 'stq'], 'qtmp', 512)
                        P.op('act', lambda hh=hh, j=j: A.copy(out=qT[:, 4 * j:4 * j + 4, hh, :], in_=qtmp[:].rearrange("p (a n) -> p a n", a=4)),
                             r=['qtmp'], w=['qT'])
                    else:
                        P.dma(ctq[:], cIq[:, tcs], w=['ctq']); P.dma(stq[:], sIq[:, tcs], w=['stq'])
                        dst = qiT[:, hh - 8, j * 512:(j + 1) * 512]
                        P.op('act', lambda b=b, dst=dst: A.copy(out=dst, in_=pb[b][:, :]), r=[pbn[b]], w=['qiT'])
                        rope(dst, 128, permI, ctq[:], stq[:], rt1, rt2, 2 + j, ['ctq', 'stq'], 'qiT', 512)
            load_weight_bf16((wst[0], wbf[0]), wwi, 0, 16, g1s, 'wbfQ0', stag='wstQ')
            for blk in range(4):
                b = 4 + blk % 2
                for kc in range(8):
                    P.op('pe', lambda b=b, kc=kc, blk=blk: T.matmul(pb[b][:, 0:16], lhsT=hTo[:, kc, blk * 128:(blk + 1) * 128], rhs=wbf[0][:, kc, 0:16],
                                                                     start=(kc == 0), stop=(kc == 7), skip_group_check=True),
                         r=['wbfQ0', 'hTo'], w=[pbn[b]])
                P.op('dve', lambda b=b, blk=blk: V.tensor_copy(out=wis[:, blk, :], in_=pb[b][:, 0:16]), r=[pbn[b]], w=['wis'])
            for blk in range(4):
                gi = 4 * half + blk
                nkt = gi + 1
                n = nkt * 512
                for h in range(16):
                    if h % 2 == 0:
                        P.op('act', lambda h=h, blk=blk: A.activation(out=Dg[:, h, :], in_=ident32[:], func=AF.Copy, scale=wis[:, blk, h:h + 1]),
                             r=['wis', 'ident32'], w=['Dg'])
                    else:
                        P.op('dve', lambda h=h, blk=blk: V.tensor_scalar(out=Dg[:, h, :], in0=ident32[:], scalar1=wis[:, blk, h:h + 1], scalar2=None, op0=ALU.mult),
                             r=['wis', 'ident32'], w=['Dg'])
                for kt in range(nkt):
                    bsc = 6 + kt % 2

                    def rel_pair(p, kt=kt, blk=blk):
                        X = 2 * (p % 3)
                        for hh in range(2):
                            rs = slice(64 * hh, 64 * hh + 64)
                            P.op('pe', lambda hh=hh, rs=rs: T.matmul(
                                pb[X + hh][:, :], lhsT=qiT[rs, p, blk * 128:(blk + 1) * 128], rhs=kiT_all[rs, kt * 512:(kt + 1) * 512],
                                start=True, stop=True, skip_group_check=True, tile_position=(64 * hh, 0)), r=['qiT', 'kiT_all'], w=[pbn[X + hh]])
                        rbuf = Rb[p % 3]; rn = 'Rb%d' % (p % 3)
                        src2 = pball[:, X * 512:(X + 2) * 512]
                        if p % 2 == 0:
                            P.op('act', lambda: A.activation(out=rbuf[:], in_=src2, func=AF.Relu), r=[pbn[X], pbn[X + 1]], w=[rn])
                        else:
                            P.op('dve', lambda: V.tensor_scalar(out=rbuf[:], in0=src2, scalar1=0.0, scalar2=None, op0=ALU.max), r=[pbn[X], pbn[X + 1]], w=[rn])

                    def score_pair(p, bsc=bsc):
                        rbuf = Rb[p % 3]; rn = 'Rb%d' % (p % 3)
                        for hh in range(2):
                            h = 2 * p + hh
                            P.op('pe', lambda h=h, hh=hh: T.matmul(pb[bsc][:, :], lhsT=Dg[:, h, :], rhs=rbuf[:, hh * 512:(hh + 1) * 512], start=(h == 0), stop=(h == 15),
                                                                   skip_group_check=True), r=['Dg', rn], w=[pbn[bsc]])
                    for step in range(8 + 2):
                        if step < 8:
                            rel_pair(step)
                        if step >= 2:
                            score_pair(step - 2)
                    mt = None
                    if kt == 0 and kt == nkt - 1:
                        mt = 2
                    elif kt == 0:
                        mt = 0
                    elif kt == nkt - 1:
                        mt = 1
                    dsts = score[:, kt * 512:(kt + 1) * 512]
                    if mt is None:
                        P.op('act', lambda bsc=bsc, dsts=dsts: A.copy(out=dsts, in_=pb[bsc][:, :]), r=[pbn[bsc]], w=['score'])
                    else:
                        P.op('dve', lambda bsc=bsc, dsts=dsts, mt=mt: V.tensor_tensor(out=dsts, in0=pb[bsc][:, :], in1=mtile[mt][:], op=ALU.add),
                             r=[pbn[bsc], 'mt%d' % mt], w=['score'])
                n_act = 512 * ((gi + 1) // 2)
                thr_c = 255.5 - 0.5 * n_act
                stps = [BIS_B * 2.0 / (2.0 ** (it + 1)) for it in range(NITER)]
                P.op('dve', lambda: V.memset(bs[:, 1:2], -BIS_B + stps[0]), r=['bs'], w=['bs'])
                for it in range(NITER):
                    if n_act > 0:
                        P.op('act', lambda n_act=n_act: A.activation(out=cjunk[:, 0:n_act], in_=score[:, 0:n_act], func=AF.Sign, bias=bs[:, 1:2], scale=-1.0,
                                                                    accum_out=bs[:, 5:6]), r=['bs', 'score'], w=['bs5', 'cjunkA'])
                    P.op('dve', lambda n=n, n_act=n_act: V.tensor_scalar(out=cjunk[:, n_act:n], in0=score[:, n_act:n], scalar1=bs[:, 1:2], scalar2=0.0, op0=ALU.is_ge, op1=ALU.add,
                                                                        accum_out=bs[:, 2:3]), r=['bs', 'score'], w=['bs', 'cjunk'])
                    if n_act > 0:
                        P.op('dve', lambda: V.scalar_tensor_tensor(out=bs[:, 6:7], in0=bs[:, 5:6], scalar=-0.5, in1=bs[:, 2:3], op0=ALU.mult, op1=ALU.add), r=['bs', 'bs5'], w=['bs'])
                        tcol = 6
                    else:
                        tcol = 2
                    P.op('dve', lambda it=it, tcol=tcol, thr_c=thr_c: V.tensor_scalar(out=bs[:, 3:4], in0=bs[:, tcol:tcol + 1], scalar1=thr_c, scalar2=stps[it], op0=ALU.is_ge, op1=ALU.mult),
                         r=['bs'], w=['bs'])
                    if it < NITER - 1:
                        P.op('dve', lambda it=it: V.scalar_tensor_tensor(out=bs[:, 1:2], in0=bs[:, 3:4], scalar=-stps[it + 1], in1=bs[:, 1:2], op0=ALU.add, op1=ALU.add), r=['bs'], w=['bs'])
                    else:
                        P.op('dve', lambda it=it: V.scalar_tensor_tensor(out=bs[:, 0:1], in0=bs[:, 3:4], scalar=-stps[it], in1=bs[:, 1:2], op0=ALU.add, op1=ALU.add), r=['bs'], w=['bs'])
                for kt in range(nkt):
                    mqt = mq[0]; mn = 'mq0'
                    P.op('dve', lambda mqt=mqt, kt=kt: V.tensor_scalar(out=mqt[:], in0=score[:, kt * 512:(kt + 1) * 512], scalar1=bs[:, 0:1], scalar2=None, op0=ALU.is_ge),
                         r=['bs', 'score'], w=[mn])
                    bt_ = 4 + kt % 2
                    for a in range(4):
                        P.op('pe', lambda mqt=mqt, a=a, bt_=bt_: T.transpose(out=pb16(bt_)[:, a * 128:(a + 1) * 128], in_=mqt[:, a * 128:(a + 1) * 128], identity=identb[:]),
                             r=[mn, 'identb'], w=[pbn[bt_]])
                    P.op('act', lambda kt=kt, bt_=bt_: A.copy(out=maskT[:, 4 * kt:4 * kt + 4, :], in_=pb16(bt_)[:, 0:512].rearrange("p (a n) -> p a n", a=4)),
                         r=[pbn[bt_]], w=['maskT'])
                ybtt = ybt[0]; ybn = 'ybt0'
                nkb = 4 * nkt
                for g in range(2):
                    rhsq = qT[:, blk, 4 * g:4 * g + 4, :].rearrange("p a n -> p (a n)")
                    npair = nkb // 2

                    def qk_pair(pq, g=g, rhsq=rhsq):
                        X = 2 * (pq % 2)
                        for j in range(2):
                            kb = 2 * pq + j
                            P.op('pe', lambda kb=kb, j=j: T.matmul(pb[X + j][:, :], lhsT=kT_all[:, g, kb * 128:(kb + 1) * 128], rhs=rhsq,
                                                                   start=True, stop=True, skip_group_check=True), r=['kT_all', 'qT'], w=[pbn[X + j]])
                        e = Eb[pq % 2]; en = 'Eb%d' % (pq % 2)
                        P.op('act', lambda: A.activation(out=e[:], in_=pball[:, X * 512:(X + 2) * 512], func=AF.Exp, scale=ISQ), r=[pbn[X], pbn[X + 1]], w=[en])
                        pt = PT[pq % 2]; pn = 'PT%d' % (pq % 2)
                        P.op('dve', lambda: V.tensor_tensor(out=pt[:].rearrange("p (k a n) -> p k a n", k=2, a=4), in0=e[:].rearrange("p (k a n) -> p k a n", k=2, a=4),
                                                            in1=maskT[:, 2 * pq:2 * pq + 2, :].unsqueeze(2).to_broadcast([128, 2, 4, 128]), op=ALU.mult),
                             r=[en, 'maskT'], w=[pn])

                    def pv_pair(pq, g=g, nkb=nkb):
                        pt = PT[pq % 2]; pn = 'PT%d' % (pq % 2)
                        for j in range(2):
                            kb = 2 * pq + j
                            P.op('pe', lambda kb=kb, j=j: T.matmul(pb[6][:, :], lhsT=V_all[:, kb, g * 128:(g + 1) * 128], rhs=pt[:, j * 512:(j + 1) * 512],
                                                                   start=(kb == 0), stop=(kb == nkb - 1), skip_group_check=True), r=['V_all', pn], w=[pbn[6]])
                            P.op('pe', lambda kb=kb, j=j: T.matmul(pb[7][:, :], lhsT=onesb[:], rhs=pt[:, j * 512:(j + 1) * 512],
                                                                   start=(kb == 0), stop=(kb == nkb - 1), skip_group_check=True), r=['onesb', pn], w=[pbn[7]])
                    for step in range(npair + 1):
                        if step < npair:
                            qk_pair(step)
                        if step >= 1:
                            pv_pair(step - 1)
                    P.op('dve', lambda: V.reciprocal(out=rden[:], in_=pb[7][:, :]), r=[pbn[7]], w=['ropet1'])
                    P.op('dve', lambda g=g, ybtt=ybtt: V.tensor_tensor(out=ybtt[:, g * 512:(g + 1) * 512], in0=pb[6][:, :], in1=rden[:], op=ALU.mult),
                         r=[pbn[6], 'ropet1'], w=[ybn])
                P.dma(YB_d[gi], ybtt[:], r=[ybn], w=['YB_d'])
    P.barrier()
    if stop('ATT'):
        return fin()

    with ExitStack() as st:
        h2T = sb(st, "h2T", [128, 8, 2048], BF16)
        sA = ExitStack()
        mT = sb(sA, "mT", [128, 8, 2048], BF16)
        with ExitStack() as s2:
            yaT = sb(s2, "yaT", [128, 8, 2048], BF16)
            wst = [sb(s2, "wstD%d" % i, [128, 8, 128], F32) for i in range(4)]
            wbf = [sb(s2, "wbfD%d" % i, [128, 8, 128], BF16) for i in range(4)]
            sg = [sb(s2, "sgD%d" % i, [128, 512], F32) for i in range(4)]
            with ExitStack() as s3:
                yT = sb(s3, "yT", [128, 8, 2048], BF16)
                for t in range(NT):
                    P.dma(yT[:, :, t * 128:(t + 1) * 128], Y_d[t].rearrange("p (kc n) -> p kc n", kc=8), r=['Y_d'], w=['yT'])
                for oc in range(8):
                    w2 = oc % 2
                    load_weight_bf16((wst[w2], wbf[w2]), wglu, oc * 128, 128, None, 'wbfD%d' % w2)
                    for j in range(4):
                        b = (oc * 4 + j) % 8
                        for kc in range(8):
                            P.op('pe', lambda b=b, kc=kc, w2=w2, j=j: T.matmul(pb[b][:, :], lhsT=wbf[w2][:, kc, :], rhs=yT[:, kc, j * 512:(j + 1) * 512],
                                                                               start=(kc == 0), stop=(kc == 7), skip_group_check=True), r=['wbfD%d' % w2, 'yT'], w=[pbn[b]])
                        sgt = sg[j % 2]; sn = 'sgD%d' % (j % 2)
                        P.op('act', lambda b=b, sgt=sgt: A.activation(out=sgt[:], in_=pb[b][:, :], func=AF.Sigmoid), r=[pbn[b]], w=[sn])
                        P.op('dve', lambda oc=oc, j=j, sgt=sgt: V.tensor_tensor(out=yaT[:, oc, j * 512:(j + 1) * 512], in0=sgt[:], in1=yT[:, oc, j * 512:(j + 1) * 512], op=ALU.mult),
                             r=[sn, 'yT'], w=['yaT'])
                P.barrier()
            ybT = sb(s2, "ybT", [128, 8, 2048], BF16)
            hTa = sb(s2, "hTa", [128, 8, 2048], BF16)
            for t in range(NT):
                P.dma(ybT[:, :, t * 128:(t + 1) * 128], YB_d[t].rearrange("p (h n) -> p h n", h=8), r=['YB_d'], w=['ybT'])
                P.dma(hTa[:, :, t * 128:(t + 1) * 128], HT_d[t].rearrange("p (kc n) -> p kc n", kc=8), r=['HT_d'], w=['hTa'])
            m1 = sb(s2, "mm1", [128, 512], F32); m2 = sb(s2, "mm2", [128, 512], F32)
            for oc in range(8):
                load_weight_bf16((wst[0], wbf[0]), wba, oc * 128, 128, None, 'wbfD0')
                load_weight_bf16((wst[1], wbf[1]), wbb, oc * 128, 128, None, 'wbfD1')
                load_weight_bf16((wst[2], wbf[2]), wg, oc * 128, 128, g1s, 'wbfD2')
                load_weight_bf16((wst[3], wbf[3]), wg, 1024 + oc * 128, 128, g1s, 'wbfD3')
                for j in range(4):
                    js = slice(j * 512, (j + 1) * 512)
                    acts = [yaT, ybT, hTa, hTa]; anm = ['yaT', 'ybT', 'hTa', 'hTa']
                    for q4 in range(4):
                        b = 4 * (j % 2) + q4
                        for kc in range(8):
                            P.op('pe', lambda b=b, kc=kc, q4=q4, js=js, acts=acts: T.matmul(pb[b][:, :], lhsT=wbf[q4][:, kc, :], rhs=acts[q4][:, kc, js],
                                                                                          start=(kc == 0), stop=(kc == 7), skip_group_check=True),
                                 r=['wbfD%d' % q4, anm[q4]], w=[pbn[b]])
                    b0 = 4 * (j % 2)
                    P.op('act', lambda b0=b0: A.activation(out=sg[0][:], in_=pb[b0 + 2][:, :], func=AF.Sigmoid), r=[pbn[b0 + 2]], w=['sgD0'])
                    P.op('act', lambda b0=b0: A.activation(out=sg[1][:], in_=pb[b0 + 3][:, :], func=AF.Sigmoid), r=[pbn[b0 + 3]], w=['sgD1'])
                    P.op('dve', lambda b0=b0: V.tensor_tensor(out=m1[:], in0=pb[b0][:, :], in1=sg[0][:], op=ALU.mult), r=[pbn[b0], 'sgD0'], w=['m1'])
                    P.op('dve', lambda b0=b0: V.tensor_tensor(out=m2[:], in0=pb[b0 + 1][:, :], in1=sg[1][:], op=ALU.mult), r=[pbn[b0 + 1], 'sgD1'], w=['m2'])
                    P.op('dve', lambda oc=oc, js=js: V.tensor_tensor(out=mT[:, oc, js], in0=m1[:], in1=m2[:], op=ALU.add), r=['m1', 'm2'], w=['mT'])
            P.barrier()
        if stop('MRG'):
            return fin()
        with ExitStack() as s2:
            WoB = sb(s2, "WoB", [128, 8, 1024], BF16)
            with ExitStack() as s3:
                wstg = sb(s3, "wstgO", [128, 8, 1024], F32)
                load_weight_bf16((wstg, WoB), wout, 0, 1024, None, 'WoB')
                P.barrier()
            xt = [sb(s2, "xtF%d" % i, [128, 1024], F32) for i in range(2)]
            x1t = [sb(s2, "x1F%d" % i, [128, 1024], F32) for i in range(2)]
            junk = sb(s2, "junkF", [128, 1024], F32)
            ssF = [sb(s2, "ssF%d" % i, [128, 4], F32) for i in range(2)]
            hbF = [sb(s2, "hbF%d" % i, [128, 1024], BF16) for i in range(2)]
            for gi in range(NT):
                i2 = gi % 2
                rows = slice((4 * gi + 3) * 128, (4 * gi + 4) * 128)
                P.dma(xt[i2][:], xp[rows, :], w=['xtF%d' % i2])
                for hf in range(2):
                    b = 2 * i2 + hf
                    for kc in range(8):
                        P.op('pe', lambda b=b, kc=kc, gi=gi, hf=hf: T.matmul(pb[b][:, :], lhsT=mT[:, kc, gi * 128:(gi + 1) * 128], rhs=WoB[:, kc, hf * 512:(hf + 1) * 512],
                                                                             start=(kc == 0), stop=(kc == 7), skip_group_check=True), r=['mT', 'WoB'], w=[pbn[b]])
                    P.op('dve', lambda b=b, hf=hf, i2=i2: V.tensor_tensor(out=x1t[i2][:, hf * 512:(hf + 1) * 512], in0=pb[b][:, :], in1=xt[i2][:, hf * 512:(hf + 1) * 512], op=ALU.add),
                         r=[pbn[b], 'xtF%d' % i2], w=['x1F%d' % i2])
                P.dma(X1_d[gi], x1t[i2][:], r=['x1F%d' % i2], w=['X1_d'])
                tg = 'F%d' % i2
                P.op('act', lambda i2=i2: A.activation(out=junk[:], in_=x1t[i2][:], func=AF.Square, accum_out=ssF[i2][:, 0:1]), r=['x1F%d' % i2], w=['junkF', tg + 'ss'])
                P.op('dve', lambda i2=i2: V.tensor_scalar(out=ssF[i2][:, 1:2], in0=ssF[i2][:, 0:1], scalar1=1.0 / D, scalar2=EPS, op0=ALU.mult, op1=ALU.add), r=[tg + 'ss'], w=[tg + 'ss'])
                P.op('act', lambda i2=i2: A.activation(out=ssF[i2][:, 2:3], in_=ssF[i2][:, 1:2], func=AF.Sqrt), r=[tg + 'ss'], w=[tg + 'ss'])
                P.op('dve', lambda i2=i2: V.reciprocal(out=ssF[i2][:, 3:4], in_=ssF[i2][:, 2:3]), r=[tg + 'ss'], w=[tg + 'ss'])
                P.op('dve', lambda i2=i2: V.tensor_scalar(out=hbF[i2][:], in0=x1t[i2][:], scalar1=ssF[i2][:, 3:4], scalar2=None, op0=ALU.mult), r=['x1F%d' % i2, tg + 'ss'], w=[tg + 'hb'])
                bt_ = 4 + i2
                for kc in range(8):
                    P.op('pe', lambda kc=kc, i2=i2, bt_=bt_: T.transpose(out=pb16(bt_)[:, kc * 128:(kc + 1) * 128], in_=hbF[i2][:, kc * 128:(kc + 1) * 128], identity=identb[:]),
                         r=[tg + 'hb', 'identb'], w=[pbn[bt_]])
                P.op('act', lambda gi=gi, bt_=bt_: A.copy(out=h2T[:, :, gi * 128:(gi + 1) * 128], in_=pb16(bt_).rearrange("p (kc n) -> p kc n", kc=8)), r=[pbn[bt_]], w=['h2T'])
            P.barrier()
        sA.close()
        if stop('F'):
            return fin()
        actT = sb(st, "actT", [128, 22, 2048], BF16)
        with ExitStack() as s2:
            wst = [sb(s2, "wstG%d" % i, [128, 8, 128], F32) for i in range(2)]
            wbf = [sb(s2, "wbfG%d" % i, [128, 8, 128], BF16) for i in range(2)]
            sgl = [sb(s2, "sgG%d" % i, [128, 512], F32) for i in range(2)]
            for jh in range(22):
                load_weight_bf16((wst[0], wbf[0]), wfi, jh * 128, 128, g2s, 'wbfG0')
                load_weight_bf16((wst[1], wbf[1]), wfi, FH + jh * 128, 128, g2s, 'wbfG1')
                for tg_ in range(4):
                    ts_ = slice(tg_ * 512, (tg_ + 1) * 512)
                    b0 = 2 * (tg_ % 4)
                    for q2 in range(2):
                        for kc in range(8):
                            P.op('pe', lambda b0=b0, q2=q2, kc=kc, ts_=ts_: T.matmul(pb[b0 + q2][:, :], lhsT=wbf[q2][:, kc, :], rhs=h2T[:, kc, ts_],
                                                                                   start=(kc == 0), stop=(kc == 7), skip_group_check=True), r=['wbfG%d' % q2, 'h2T'], w=[pbn[b0 + q2]])
                    sgt = sgl[tg_ % 2]; sn = 'sgG%d' % (tg_ % 2)
                    P.op('act', lambda b0=b0, sgt=sgt: A.activation(out=sgt[:], in_=pb[b0][:, :], func=AF.Silu), r=[pbn[b0]], w=[sn])
                    P.op('dve', lambda b0=b0, sgt=sgt, jh=jh, ts_=ts_: V.tensor_tensor(out=actT[:, jh, ts_], in0=pb[b0 + 1][:, :], in1=sgt[:], op=ALU.mult),
                         r=[pbn[b0 + 1], sn], w=['actT'])
            P.barrier()
        with ExitStack() as s2:
            WfoB = sb(s2, "WfoB", [128, 22, 1024], BF16)
            wstg2 = [sb(s2, "wstgFo%d" % i, [128, 1024], F32) for i in range(2)]
            for jh in range(22):
                i2 = jh % 2
                P.dma(wstg2[i2][:], wfo[jh * 128:(jh + 1) * 128, :], w=['wstgFo%d' % i2])
                if i2 == 0:
                    P.op('act', lambda jh=jh, i2=i2: A.copy(out=WfoB[:, jh, :], in_=wstg2[i2][:]), r=['wstgFo%d' % i2], w=['WfoB'])
                else:
                    P.op('dve', lambda jh=jh, i2=i2: V.tensor_copy(out=WfoB[:, jh, :], in_=wstg2[i2][:]), r=['wstgFo%d' % i2], w=['WfoB'])
            gft = sb(s2, "gft", [128, 1024], F32)
            P.dma(gft[:], gfb[:, :], w=['gft'])
            x1r = [sb(s2, "x1r%d" % i, [128, 1024], F32) for i in range(2)]
            x2 = [sb(s2, "x2_%d" % i, [128, 1024], F32) for i in range(2)]
            junkG = sb(s2, "junkG", [128, 1024], F32)
            ssG = [sb(s2, "ssG%d" % i, [128, 4], F32) for i in range(2)]
            ot = [sb(s2, "ot%d" % i, [128, 1024], F32) for i in range(2)]
            for gi in range(NT):
                i2 = gi % 2
                P.dma(x1r[i2][:], X1_d[gi], r=['X1_d'], w=['x1r%d' % i2])
                for hf in range(2):
                    b = 2 * i2 + hf
                    for jh in range(22):
                        P.op('pe', lambda b=b, jh=jh, gi=gi, hf=hf: T.matmul(pb[b][:, :], lhsT=actT[:, jh, gi * 128:(gi + 1) * 128], rhs=WfoB[:, jh, hf * 512:(hf + 1) * 512],
                                                                             start=(jh == 0), stop=(jh == 21), skip_group_check=True), r=['actT', 'WfoB'], w=[pbn[b]])
                    P.op('dve', lambda b=b, hf=hf, i2=i2: V.tensor_tensor(out=x2[i2][:, hf * 512:(hf + 1) * 512], in0=pb[b][:, :], in1=x1r[i2][:, hf * 512:(hf + 1) * 512], op=ALU.add),
                         r=[pbn[b], 'x1r%d' % i2], w=['x2_%d' % i2])
                tg = 'G%d' % i2
                P.op('act', lambda i2=i2: A.activation(out=junkG[:], in_=x2[i2][:], func=AF.Square, accum_out=ssG[i2][:, 0:1]), r=['x2_%d' % i2], w=['junkG', tg + 'ss'])
                P.op('dve', lambda i2=i2: V.tensor_scalar(out=ssG[i2][:, 1:2], in0=ssG[i2][:, 0:1], scalar1=1.0 / D, scalar2=EPS, op0=ALU.mult, op1=ALU.add), r=[tg + 'ss'], w=[tg + 'ss'])
                P.op('act', lambda i2=i2: A.activation(out=ssG[i2][:, 2:3], in_=ssG[i2][:, 1:2], func=AF.Sqrt), r=[tg + 'ss'], w=[tg + 'ss'])
                P.op('dve', lambda i2=i2: V.reciprocal(out=ssG[i2][:, 3:4], in_=ssG[i2][:, 2:3]), r=[tg + 'ss'], w=[tg + 'ss'])
                P.op('dve', lambda i2=i2: V.tensor_scalar(out=ot[i2][:], in0=x2[i2][:], scalar1=ssG[i2][:, 3:4], scalar2=None, op0=ALU.mult), r=['x2_%d' % i2, tg + 'ss'], w=['ot%d' % i2])
                P.op('dve', lambda i2=i2: V.tensor_tensor(out=ot[i2][:], in0=ot[i2][:], in1=gft[:], op=ALU.mult), r=['ot%d' % i2, 'gft'], w=['ot%d' % i2])
                P.dma(out_d[gi * 128:(gi + 1) * 128, :], ot[i2][:], r=['ot%d' % i2], w=['out_d'])
            P.barrier()
    P.barrier()
    return nc


def _host_prep(inputs):
    x = np.asarray(inputs['x'], np.float32)
    w_in = np.asarray(inputs['w_in'], np.float32)[0]
    pts = np.cumsum([1024, 1024, 256, 256, 1024, 64, 16, 1024, 1024])
    wu, wq_, wk, wv, wqi, wki, wwi, wga, wgb = np.split(w_in, pts[:-1], axis=1)
    a_re = np.asarray(inputs['a_re'], np.float32)[0]; a_im = np.asarray(inputs['a_im'], np.float32)[0]
    log_dt = np.asarray(inputs['log_dt'], np.float32)[0]
    b_re = np.asarray(inputs['b_re'], np.float32)[0]; b_im = np.asarray(inputs['b_im'], np.float32)[0]
    c_re = np.asarray(inputs['c_re'], np.float32)[0]; c_im = np.asarray(inputs['c_im'], np.float32)[0]
    d_skip = np.asarray(inputs['d_skip'], np.float32)[0]

    def gT(g):
        return np.ascontiguousarray(np.asarray(g, np.float32).reshape(8, 128).T)

    r = np.arange(128); kc = np.arange(8)
    pp = r // 32; ggr = (r // 16) % 2; cr = r % 16
    AR1 = np.zeros((128, 8, 2, 64), np.float32); AI1 = np.zeros_like(AR1); DT1 = np.zeros_like(AR1)
    BR1 = np.zeros_like(AR1); BI1 = np.zeros_like(AR1)
    for k in range(8):
        for g2 in range(2):
            g = 2 * (4 * k + pp) + g2
            AR1[:, k, g2, :] = a_re[g, :]
            AI1[:, k, g2, :] = a_im[g, :]
            DT1[:, k, g2, :] = log_dt[g][:, None]
            sel = (ggr == g2)
            BR1[sel, k, g2, :] = b_re[g[sel], :, cr[sel]]
            BI1[sel, k, g2, :] = b_im[g[sel], :, cr[sel]]
    gg = np.arange(128) // 64; p_ = np.arange(128) % 64
    gidx = 2 * np.arange(32)[None, :] + gg[:, None]
    ARE = a_re[gidx, p_[:, None]]; AIE = a_im[gidx, p_[:, None]]; DTE = log_dt[gidx]
    CTR = c_re[gidx, :, p_[:, None]]
    CTI = c_im[gidx, :, p_[:, None]]
    BER = np.zeros((128, 32, 2, 16), np.float32); BEI = np.zeros_like(BER)
    for g2 in range(2):
        sel = gg == g2
        BER[sel, :, g2, :] = b_re[gidx[sel], p_[sel][:, None], :]
        BEI[sel, :, g2, :] = b_im[gidx[sel], p_[sel][:, None], :]
    DFM = np.ascontiguousarray(d_skip.reshape(8, 128).T)
    common = dict(
        wu=np.ascontiguousarray(wu),
        wkv=np.ascontiguousarray(np.concatenate([wk, wki, wki, wv], axis=1)),
        wq=np.ascontiguousarray(np.concatenate([wq_, wqi], axis=1)),
        wwi=np.ascontiguousarray(wwi),
        wg=np.ascontiguousarray(np.concatenate([wga, wgb], axis=1)),
        wglu=np.asarray(inputs['w_glu'], np.float32)[0], wba=np.asarray(inputs['w_branch_a'], np.float32)[0],
        wbb=np.asarray(inputs['w_branch_b'], np.float32)[0], wout=np.asarray(inputs['w_out'], np.float32)[0],
        wfi=np.asarray(inputs['w_ffn_in'], np.float32)[0], wfo=np.asarray(inputs['w_ffn_out'], np.float32)[0],
        g1T=gT(inputs['norm1_g'][0]), g2T=gT(inputs['norm2_g'][0]),
        gfb=np.ascontiguousarray(np.broadcast_to(np.asarray(inputs['norm_f_g'], np.float32)[None, :], (128, D))),
        AR1=AR1.reshape(128, 1024), AI1=AI1.reshape(128, 1024), DT1=DT1.reshape(128, 1024),
        BR1=BR1.reshape(128, 1024), BI1=BI1.reshape(128, 1024),
        ARE=np.ascontiguousarray(ARE), AIE=np.ascontiguousarray(AIE), DTE=np.ascontiguousarray(DTE),
        CTR=np.ascontiguousarray(CTR).reshape(128, 512), CTI=np.ascontiguousarray(CTI).reshape(128, 512),
        BER=BER.reshape(128, 1024), BEI=BEI.reshape(128, 1024), DFM=DFM,
        identf=np.eye(128, dtype=np.float32),
    )
    permA = np.zeros((128, 128), np.float32)
    for m in range(32):
        permA[m + 16 if m < 16 else m - 16, m] = 1
    permI = np.zeros((128, 128), np.float32)
    for hb in (0, 64):
        for m in range(16):
            permI[hb + (m + 8 if m < 8 else m - 8), hb + m] = 1
    common['permA'] = permA; common['permI'] = permI

    def tables(pos, kind):
        pos = pos.astype(np.float32)
        cos = np.ones((128, pos.shape[0]), np.float32); sin = np.zeros_like(cos)
        if kind == 'A':
            half = 16
            inv = (np.float32(500000.0) ** (-np.arange(half, dtype=np.float32) / half)).astype(np.float32)
            ang = pos[None, :] * inv[:, None]
            cos[0:16] = np.cos(ang); cos[16:32] = np.cos(ang)
            sin[0:16] = -np.sin(ang); sin[16:32] = np.sin(ang)
        else:
            half = 8
            inv = (np.float32(500000.0) ** (-np.arange(half, dtype=np.float32) / half)).astype(np.float32)
            ang = pos[None, :] * inv[:, None]
            for hb in (0, 64):
                cos[hb:hb + 8] = np.cos(ang); cos[hb + 8:hb + 16] = np.cos(ang)
                sin[hb:hb + 8] = -np.sin(ang); sin[hb + 8:hb + 16] = np.sin(ang)
        return cos, sin

    in_maps = []
    for c in range(8):
        b, r_ = c // 4, c % 4
        pad = (3 - r_) * 128
        xpad = np.zeros((L, D), np.float32)
        xpad[pad:] = x[b, :L - pad]
        pos_all = np.maximum(np.arange(L) - pad, 0)
        own = np.concatenate([np.arange(128) + (4 * i + 3) * 128 for i in range(NT)])
        pos_own = own - pad
        m = dict(common)
        m['xp'] = xpad
        m['cAk'], m['sAk'] = tables(pos_all, 'A')
        m['cIk'], m['sIk'] = tables(pos_all, 'I')
        m['cAq'], m['sAq'] = tables(pos_own, 'A')
        m['cIq'], m['sIq'] = tables(pos_own, 'I')
        q = np.arange(128)[:, None]; kk = np.arange(512)[None, :]
        caus = np.where(((kk < 384) | ((kk - 384) // 64 <= q // 64)), 0.0, NEG).astype(np.float32)
        padm = np.where(kk >= pad, 0.0, NEG).astype(np.float32) * np.ones((128, 1), np.float32)
        m['mfirst'] = np.ascontiguousarray(padm)
        m['mlast'] = np.ascontiguousarray(caus)
        m['mboth'] = np.minimum(padm, caus).astype(np.float32)
        in_maps.append(m)
    return in_maps


def kernel(**inputs):
    in_maps = _host_prep(inputs)
    nc = build_program()
    res = run_bass_kernel_spmd(nc, in_maps, core_ids=list(range(8)))
    out = np.zeros((2, L, D), np.float32)
    for c in range(8):
        b, r_ = c // 4, c % 4
        o = res.results[c]["out"].reshape(NT, 128, D)
        for i in range(NT):
            j = 4 * i + r_
            out[b, j * 128:(j + 1) * 128] = o[i]
    return out
```

```python
import math
from contextlib import ExitStack
import numpy as np
import concourse.bass as bass
import concourse.mybir as mybir
from concourse.bass_utils import run_bass_kernel_spmd

F32 = mybir.dt.float32
BF16 = mybir.dt.bfloat16
AF = mybir.ActivationFunctionType
ALU = mybir.AluOpType

D = 1024
L = 8192
NT = 16
FH = 2816
EPS = 1e-6
NEG = -1e30
NITER = 18
BIS_B = 64.0
NDS = 24


class Prog:
    def __init__(self, nc, es):
        self.nc = nc
        self.E = {'pe': nc.tensor, 'act': nc.scalar, 'dve': nc.vector, 'pool': nc.gpsimd, 'sp': nc.sync}
        self.sem = {k: es.enter_context(nc.semaphore('s_' + k)) for k in ['pe', 'act', 'dve', 'pool']}
        self.cnt = {k: 0 for k in self.sem}
        self.dsem = [es.enter_context(nc.semaphore('d%d' % i)) for i in range(NDS)]
        self.dcnt = [0] * NDS
        self.dnext = 0
        self.seen = {e: {} for e in self.E}
        self.lw = {}
        self.rd = {}

    def _semobj(self, key):
        return self.dsem[key[1]] if isinstance(key, tuple) else self.sem[key]

    def _wait(self, eng, ev):
        key, val = ev
        if self.seen[eng].get(key, 0) >= val:
            return
        self.seen[eng][key] = val
        self.E[eng].wait_ge(self._semobj(key), val)

    def _deps(self, eng, r, w):
        evs = {}
        for x in r:
            if x in self.lw:
                k, v = self.lw[x]
                evs[k] = max(evs.get(k, 0), v)
        for x in w:
            if x in self.lw:
                k, v = self.lw[x]
                evs[k] = max(evs.get(k, 0), v)
            for k, v in self.rd.get(x, {}).items():
                evs[k] = max(evs.get(k, 0), v)
        for k, v in evs.items():
            if eng == 'pe' and k == 'pe':
                continue
            self._wait(eng, (k, v))

    def _record(self, me, r, w):
        k, v = me
        for x in r:
            d = self.rd.setdefault(x, {})
            d[k] = max(d.get(k, 0), v)
        for x in w:
            self.lw[x] = me
            self.rd[x] = {}

    def op(self, eng, fn, r=(), w=()):
        self._deps(eng, r, w)
        ins = fn()
        self.cnt[eng] += 1
        ins.then_inc(self.sem[eng], 1)
        self._record((eng, self.cnt[eng]), r, w)

    def dma(self, out, in_, r=(), w=(), q='sp'):
        self._deps(q, r, w)
        i = self.dnext
        self.dnext = (i + 1) % NDS
        if self.dcnt[i] > 0:
            self._wait(q, (('d', i), self.dcnt[i]))
        self.E[q].dma_start(out=out, in_=in_).then_inc(self.dsem[i], 16)
        self.dcnt[i] += 16
        self._record((('d', i), self.dcnt[i]), r, w)

    def barrier(self):
        for e in ['pe', 'act', 'dve', 'pool', 'sp']:
            for k in self.sem:
                if self.cnt[k] > 0:
                    self._wait(e, (k, self.cnt[k]))
            for i in range(NDS):
                if self.dcnt[i] > 0:
                    self._wait(e, (('d', i), self.dcnt[i]))
        self.lw = {}
        self.rd = {}


def build_program(dbg=None):
    nc = bass.Bass("TRN2", target_bir_lowering=False)

    def din(name, shape, dt=F32):
        return nc.dram_tensor(name, list(shape), dt, kind="ExternalInput").ap()

    def dscr(name, shape, dt):
        return nc.dram_tensor(name, list(shape), dt, kind="Internal").ap()

    xp = din("xp", [L, D])
    wu = din("wu", [D, 1024])
    wkv = din("wkv", [D, 640])
    wq = din("wq", [D, 2048])
    wwi = din("wwi", [D, 16])
    wg = din("wg", [D, 2048])
    wglu = din("wglu", [D, D])
    wba = din("wba", [D, D])
    wbb = din("wbb", [D, D])
    wout = din("wout", [D, D])
    wfi = din("wfi", [D, 2 * FH])
    wfo = din("wfo", [FH, D])
    g1T = din("g1T", [128, 8])
    g2T = din("g2T", [128, 8])
    gfb = din("gfb", [128, D])
    AR1 = din("AR1", [128, 1024]); AI1 = din("AI1", [128, 1024]); DT1 = din("DT1", [128, 1024])
    BR1 = din("BR1", [128, 1024]); BI1 = din("BI1", [128, 1024])
    ARE = din("ARE", [128, 32]); AIE = din("AIE", [128, 32]); DTE = din("DTE", [128, 32])
    CTR = din("CTR", [128, 512]); CTI = din("CTI", [128, 512])
    BER = din("BER", [128, 1024]); BEI = din("BEI", [128, 1024])
    DFM = din("DFM", [128, 8])
    identf_d = din("identf", [128, 128])
    permA_d = din("permA", [128, 128]); permI_d = din("permI", [128, 128])
    cAk = din("cAk", [128, L]); sAk = din("sAk", [128, L])
    cIk = din("cIk", [128, L]); sIk = din("sIk", [128, L])
    cAq = din("cAq", [128, 2048]); sAq = din("sAq", [128, 2048])
    cIq = din("cIq", [128, 2048]); sIq = din("sIq", [128, 2048])
    mfirst = din("mfirst", [128, 512]); mlast = din("mlast", [128, 512]); mboth = din("mboth", [128, 512])
    out_d = nc.dram_tensor("out", [2048, D], F32, kind="ExternalOutput").ap()
    dbg_d = None
    if dbg:
        dbg_d = nc.dram_tensor("dbg", list(dbg[1]), F32, kind="ExternalOutput").ap()

    HT_d = dscr("HT_d", [NT, 128, 1024], BF16)
    U_d = dscr("U_d", [NT, 128, 1024], BF16)
    HS_d = dscr("HS_d", [NT, 128, 1024], BF16)
    Y_d = dscr("Y_d", [NT, 128, 1024], BF16)
    YB_d = dscr("YB_d", [NT, 128, 1024], BF16)
    X1_d = dscr("X1_d", [NT, 128, 1024], F32)

    es = ExitStack()
    P = Prog(nc, es)
    V, A, G, T = nc.vector, nc.scalar, nc.gpsimd, nc.tensor

    def sb(stack, name, shape, dt):
        return stack.enter_context(nc.sbuf_tensor(name, list(shape), dt))

    def fin():
        P.barrier()
        return nc

    def stop(name):
        return bool(dbg) and dbg[0] == name

    identf = sb(es, "identf_s", [128, 128], F32)
    identb = sb(es, "identb", [128, 128], BF16)
    permA = sb(es, "permA_s", [128, 128], BF16)
    permI = sb(es, "permI_s", [128, 128], BF16)
    onesb = sb(es, "onesb", [128, 128], BF16)
    g1s = sb(es, "g1s", [128, 8], F32)
    g2s = sb(es, "g2s", [128, 8], F32)
    ptmp = sb(es, "ptmp", [128, 128], F32)
    P.dma(identf[:], identf_d[:, :], w=['identf'])
    P.op('dve', lambda: V.tensor_copy(out=identb[:], in_=identf[:]), r=['identf'], w=['identb'])
    P.dma(ptmp[:], permA_d[:, :], w=['ptmp'])
    P.op('dve', lambda: V.tensor_copy(out=permA[:], in_=ptmp[:]), r=['ptmp'], w=['permA'])
    P.dma(ptmp[:], permI_d[:, :], w=['ptmp'])
    P.op('dve', lambda: V.tensor_copy(out=permI[:], in_=ptmp[:]), r=['ptmp'], w=['permI'])
    P.op('pool', lambda: G.memset(onesb[:], 1.0), w=['onesb'])
    ident32 = sb(es, "ident32", [128, 128], F32)
    P.op('dve', lambda: V.tensor_scalar(out=ident32[:], in0=identf[:], scalar1=1.0 / 32.0, scalar2=None, op0=ALU.mult), r=['identf'], w=['ident32'])
    P.dma(g1s[:], g1T[:, :], w=['g1s'])
    P.dma(g2s[:], g2T[:, :], w=['g2s'])

    pball = es.enter_context(nc.psum_tensor("pball", [128, 4096], F32))
    pb = [pball[:, i * 512:(i + 1) * 512] for i in range(8)]
    pbn = ['pb%d' % i for i in range(8)]

    def pb16(i):
        return pb[i][:, :].bitcast(BF16)

    def load_weight_bf16(stack_tiles, wdram, c0, ncols, gscale, tag, stag=None):
        stg, dst = stack_tiles
        stag = stag or tag
        for kc in range(8):
            P.dma(stg[:, kc, 0:ncols], wdram[kc * 128:(kc + 1) * 128, c0:c0 + ncols], w=[stag + 's%d' % kc])
            if gscale is None:
                if kc % 2 == 0:
                    P.op('act', lambda kc=kc: A.copy(out=dst[:, kc, 0:ncols], in_=stg[:, kc, 0:ncols]), r=[stag + 's%d' % kc], w=[tag])
                else:
                    P.op('dve', lambda kc=kc: V.tensor_copy(out=dst[:, kc, 0:ncols], in_=stg[:, kc, 0:ncols]), r=[stag + 's%d' % kc], w=[tag])
            else:
                P.op('dve', lambda kc=kc: V.tensor_scalar(out=dst[:, kc, 0:ncols], in0=stg[:, kc, 0:ncols], scalar1=gscale[:, kc:kc + 1], scalar2=None, op0=ALU.mult),
                     r=[stag + 's%d' % kc, 'g1s', 'g2s'], w=[tag])

    def norm_transpose(stack_t, xsrc_rows, hT, col0, tagx, gate_bank, act_scale=False):
        xt, junk, ss, hb = stack_t
        P.dma(xt[:], xsrc_rows, w=[tagx + 'xt'])
        P.op('act', lambda: A.activation(out=junk[:], in_=xt[:], func=AF.Square, accum_out=ss[:, 0:1]),
             r=[tagx + 'xt'], w=[tagx + 'junk', tagx + 'ss'])
        P.op('dve', lambda: V.tensor_scalar(out=ss[:, 1:2], in0=ss[:, 0:1], scalar1=1.0 / D, scalar2=EPS, op0=ALU.mult, op1=ALU.add),
             r=[tagx + 'ss'], w=[tagx + 'ss'])
        P.op('act', lambda: A.activation(out=ss[:, 2:3], in_=ss[:, 1:2], func=AF.Sqrt), r=[tagx + 'ss'], w=[tagx + 'ss'])
        P.op('dve', lambda: V.reciprocal(out=ss[:, 3:4], in_=ss[:, 2:3]), r=[tagx + 'ss'], w=[tagx + 'ss'])
        if act_scale:
            P.op('act', lambda: A.activation(out=hb[:], in_=xt[:], func=AF.Copy, scale=ss[:, 3:4]),
                 r=[tagx + 'xt', tagx + 'ss'], w=[tagx + 'hb'])
        else:
            P.op('dve', lambda: V.tensor_scalar(out=hb[:], in0=xt[:], scalar1=ss[:, 3:4], scalar2=None, op0=ALU.mult),
                 r=[tagx + 'xt', tagx + 'ss'], w=[tagx + 'hb'])
        b = gate_bank
        for kc in range(8):
            P.op('pe', lambda kc=kc: T.transpose(out=pb16(b)[:, kc * 128:(kc + 1) * 128], in_=hb[:, kc * 128:(kc + 1) * 128], identity=identb[:]),
                 r=[tagx + 'hb', 'identb'], w=[pbn[b]])
        return b

    def rope(dst, nrows, perm, ctab, stab, tmp1, tmp2, bank, tags_r, tag_w, ncols):
        P.op('pe', lambda: T.matmul(pb[bank][0:nrows, 0:ncols], lhsT=perm[0:nrows, 0:nrows], rhs=dst, start=True, stop=True, skip_group_check=True),
             r=[tag_w, 'permA', 'permI'], w=[pbn[bank]])
        P.op('dve', lambda: V.tensor_tensor(out=tmp1[0:nrows, 0:ncols], in0=dst, in1=ctab, op=ALU.mult), r=[tag_w] + tags_r, w=['ropet1'])
        P.op('dve', lambda: V.tensor_tensor(out=tmp2[0:nrows, 0:ncols], in0=pb[bank][0:nrows, 0:ncols], in1=stab, op=ALU.mult),
             r=[pbn[bank]] + tags_r, w=['ropet2'])
        P.op('dve', lambda: V.tensor_tensor(out=dst, in0=tmp1[0:nrows, 0:ncols], in1=tmp2[0:nrows, 0:ncols], op=ALU.add),
             r=['ropet1', 'ropet2'], w=[tag_w])

    sq = ExitStack()
    QP = sb(sq, "QP", [128, 9, 2, 32], F32)
    FP = sb(sq, "FP", [128, 8, 2, 32], F32)
    s5 = ExitStack()
    W1 = sb(s5, "W1", [128, 8, 2, 8, 128], BF16)
    cosT = sb(s5, "cosT", [128, 32, 64], F32)
    sinT = sb(s5, "sinT", [128, 32, 64], F32)
    R8z = sb(s5, "R8z", [128, 32, 64], F32)
    A8r = sb(s5, "A8r", [128, 32], F32)
    A8i = sb(s5, "A8i", [128, 32], F32)

    def sincos_lb(stack, n, ar, ai, dtl, pref):
        t = {}
        for nm in ['dt', 'ang', 'ex', 'mag', 'nn', 'red', 'sn', 'cs', 'lbr', 'lbi', 'fr', 'fi', 'u1', 'u2', 'u3']:
            t[nm] = sb(stack, pref + nm, [128, n], F32)
        R = [pref]
        def vv(fn):
            P.op('dve', fn, r=R, w=R)
        def aa(fn):
            P.op('act', fn, r=R, w=R)
        aa(lambda: A.activation(out=t['dt'][:], in_=dtl, func=AF.Exp))
        vv(lambda: V.tensor_tensor(out=t['ang'][:], in0=ai, in1=t['dt'][:], op=ALU.mult))
        vv(lambda: V.tensor_tensor(out=t['ex'][:], in0=ar, in1=t['dt'][:], op=ALU.mult))
        aa(lambda: A.activation(out=t['mag'][:], in_=t['ex'][:], func=AF.Exp))
        C1 = float(np.float32(2 * math.pi))
        C2 = float(2 * math.pi - np.float64(np.float32(2 * math.pi)))
        for which, off in (('sn', 0.0), ('cs', math.pi / 2)):
            vv(lambda off=off: V.tensor_scalar(out=t['u1'][:], in0=t['ang'][:], scalar1=off, scalar2=None, op0=ALU.add))
            vv(lambda: V.tensor_scalar(out=t['nn'][:], in0=t['u1'][:], scalar1=math.pi, scalar2=None, op0=ALU.is_gt))
            for kk in (3, 5, 7):
                vv(lambda kk=kk: V.tensor_scalar(out=t['u2'][:], in0=t['u1'][:], scalar1=kk * math.pi, scalar2=None, op0=ALU.is_gt))
                vv(lambda: V.tensor_tensor(out=t['nn'][:], in0=t['nn'][:], in1=t['u2'][:], op=ALU.add))
            vv(lambda: V.scalar_tensor_tensor(out=t['red'][:], in0=t['nn'][:], scalar=-C1, in1=t['u1'][:], op0=ALU.mult, op1=ALU.add))
            vv(lambda: V.scalar_tensor_tensor(out=t['red'][:], in0=t['nn'][:], scalar=-C2, in1=t['red'][:], op0=ALU.mult, op1=ALU.add))
            vv(lambda: V.tensor_scalar(out=t['red'][:], in0=t['red'][:], scalar1=math.pi, scalar2=-math.pi, op0=ALU.min, op1=ALU.max))
            aa(lambda which=which: A.activation(out=t[which][:], in_=t['red'][:], func=AF.Sin))
        vv(lambda: V.tensor_tensor(out=t['lbr'][:], in0=t['mag'][:], in1=t['cs'][:], op=ALU.mult))
        vv(lambda: V.tensor_tensor(out=t['lbi'][:], in0=t['mag'][:], in1=t['sn'][:], op=ALU.mult))
        vv(lambda: V.tensor_tensor(out=t['u1'][:], in0=ar, in1=ar, op=ALU.mult))
        vv(lambda: V.tensor_tensor(out=t['u2'][:], in0=ai, in1=ai, op=ALU.mult))
        vv(lambda: V.tensor_tensor(out=t['u1'][:], in0=t['u1'][:], in1=t['u2'][:], op=ALU.add))
        vv(lambda: V.reciprocal(out=t['u3'][:], in_=t['u1'][:]))
        vv(lambda: V.tensor_scalar(out=t['u1'][:], in0=t['lbr'][:], scalar1=-1.0, scalar2=None, op0=ALU.add))
        vv(lambda: V.tensor_tensor(out=t['fr'][:], in0=t['u1'][:], in1=ar, op=ALU.mult))
        vv(lambda: V.tensor_tensor(out=t['u2'][:], in0=t['lbi'][:], in1=ai, op=ALU.mult))
        vv(lambda: V.tensor_tensor(out=t['fr'][:], in0=t['fr'][:], in1=t['u2'][:], op=ALU.add))
        vv(lambda: V.tensor_tensor(out=t['fr'][:], in0=t['fr'][:], in1=t['u3'][:], op=ALU.mult))
        vv(lambda: V.tensor_tensor(out=t['fi'][:], in0=t['lbi'][:], in1=ar, op=ALU.mult))
        vv(lambda: V.tensor_tensor(out=t['u2'][:], in0=t['u1'][:], in1=ai, op=ALU.mult))
        vv(lambda: V.tensor_tensor(out=t['fi'][:], in0=t['fi'][:], in1=t['u2'][:], op=ALU.subtract))
        vv(lambda: V.tensor_tensor(out=t['fi'][:], in0=t['fi'][:], in1=t['u3'][:], op=ALU.mult))
        return t

    def cmul(outr, outi, ar_, ai_, br_, bi_, t1, t2, R):
        P.op('dve', lambda: V.tensor_tensor(out=t1, in0=ar_, in1=br_, op=ALU.mult), r=R, w=R)
        P.op('dve', lambda: V.tensor_tensor(out=t2, in0=ai_, in1=bi_, op=ALU.mult), r=R, w=R)
        P.op('dve', lambda: V.tensor_tensor(out=outr, in0=t1, in1=t2, op=ALU.subtract), r=R, w=R)
        P.op('dve', lambda: V.tensor_tensor(out=t1, in0=ar_, in1=bi_, op=ALU.mult), r=R, w=R)
        P.op('dve', lambda: V.tensor_tensor(out=t2, in0=ai_, in1=br_, op=ALU.mult), r=R, w=R)
        P.op('dve', lambda: V.tensor_tensor(out=outi, in0=t1, in1=t2, op=ALU.add), r=R, w=R)

    with ExitStack() as st:
        arl = sb(st, "arl", [128, 1024], F32); ail = sb(st, "ail", [128, 1024], F32); dtl = sb(st, "dtl", [128, 1024], F32)
        brl = sb(st, "brl", [128, 1024], F32); bil = sb(st, "bil", [128, 1024], F32)
        for tl, src in ((arl, AR1), (ail, AI1), (dtl, DT1), (brl, BR1), (bil, BI1)):
            P.dma(tl[:], src[:, :], w=['L1'])
        tt = sincos_lb(st, 1024, arl[:], ail[:], dtl[:], 'L1')
        pr = [sb(st, "pr%d" % i, [128, 1024], F32) for i in range(2)]
        pi = [sb(st, "pi%d" % i, [128, 1024], F32) for i in range(2)]
        c1 = sb(st, "c1", [128, 1024], F32); c2 = sb(st, "c2", [128, 1024], F32)
        R = ['L1']
        cur_r, cur_i = tt['fr'], tt['fi']
        for k in range(8):
            s_ = 7 - k
            v3 = lambda ap: ap.rearrange("p (kc n) -> p kc n", kc=8)
            P.op('dve', lambda: V.tensor_tensor(out=c1[:], in0=cur_r[:], in1=brl[:], op=ALU.mult), r=R, w=R)
            P.op('dve', lambda: V.tensor_tensor(out=c2[:], in0=cur_i[:], in1=bil[:], op=ALU.mult), r=R, w=R)
            P.op('dve', lambda s_=s_: V.tensor_tensor(out=W1[:, :, 0, s_, :], in0=v3(c1[:]), in1=v3(c2[:]), op=ALU.subtract), r=R, w=R + ['W1'])
            P.op('dve', lambda: V.tensor_tensor(out=c1[:], in0=cur_r[:], in1=bil[:], op=ALU.mult), r=R, w=R)
            P.op('dve', lambda: V.tensor_tensor(out=c2[:], in0=cur_i[:], in1=brl[:], op=ALU.mult), r=R, w=R)
            P.op('dve', lambda s_=s_: V.tensor_tensor(out=W1[:, :, 1, s_, :], in0=v3(c1[:]), in1=v3(c2[:]), op=ALU.add), r=R, w=R + ['W1'])
            if k < 7:
                nr, ni = pr[k % 2], pi[k % 2]
                cmul(nr[:], ni[:], cur_r[:], cur_i[:], tt['lbr'][:], tt['lbi'][:], c1[:], c2[:], R)
                cur_r, cur_i = nr, ni
    P.barrier()
    if stop('c1'):
        return fin()
    with ExitStack() as st:
        are = sb(st, "are", [128, 32], F32); aie = sb(st, "aie", [128, 32], F32); dte = sb(st, "dte", [128, 32], F32)
        for tl, src in ((are, ARE), (aie, AIE), (dte, DTE)):
            P.dma(tl[:], src[:, :], w=['E'])
        te = sincos_lb(st, 32, are[:], aie[:], dte[:], 'E')
        e1 = sb(st, "e1", [128, 32], F32); e2 = sb(st, "e2", [128, 32], F32)
        R = ['E']
        P.op('dve', lambda: V.memset(QP[:, 0, 0, :], 1.0), r=R, w=R)
        P.op('dve', lambda: V.memset(QP[:, 0, 1, :], 0.0), r=R, w=R)
        for k in range(1, 9):
            cmul(QP[:, k, 0, :], QP[:, k, 1, :], QP[:, k - 1, 0, :], QP[:, k - 1, 1, :], te['lbr'][:], te['lbi'][:], e1[:], e2[:], R)
        P.op('dve', lambda: V.tensor_copy(out=FP[:, 0, 0, :], in_=te['fr'][:]), r=R, w=R)
        P.op('dve', lambda: V.tensor_copy(out=FP[:, 0, 1, :], in_=te['fi'][:]), r=R, w=R)
        for k in range(1, 8):
            cmul(FP[:, k, 0, :], FP[:, k, 1, :], FP[:, k - 1, 0, :], FP[:, k - 1, 1, :], te['lbr'][:], te['lbi'][:], e1[:], e2[:], R)
        P.op('dve', lambda: V.tensor_copy(out=A8r[:], in_=QP[:, 8, 0, :]), r=R, w=R)
        P.op('dve', lambda: V.tensor_copy(out=A8i[:], in_=QP[:, 8, 1, :]), r=R, w=R)
        m2 = sb(st, "m2", [128, 32], F32); m4 = sb(st, "m4", [128, 32], F32); m8 = sb(st, "m8", [128, 32], F32)
        P.op('dve', lambda: V.tensor_tensor(out=m2[:], in0=te['mag'][:], in1=te['mag'][:], op=ALU.mult), r=R, w=R)
        P.op('dve', lambda: V.tensor_tensor(out=m4[:], in0=m2[:], in1=m2[:], op=ALU.mult), r=R, w=R)
        P.op('dve', lambda: V.tensor_tensor(out=m8[:], in0=m4[:], in1=m4[:], op=ALU.mult), r=R, w=R)
        wr = [sb(st, "wr%d" % i, [128, 32], F32) for i in range(4)]
        wi_ = [sb(st, "wi%d" % i, [128, 32], F32) for i in range(4)]
        P.op('dve', lambda: V.tensor_copy(out=wr[0][:], in_=te['cs'][:]), r=R, w=R)
        P.op('dve', lambda: V.tensor_copy(out=wi_[0][:], in_=te['sn'][:]), r=R, w=R)
        for k in range(1, 4):
            cmul(wr[k][:], wi_[k][:], wr[k - 1][:], wi_[k - 1][:], wr[k - 1][:], wi_[k - 1][:], e1[:], e2[:], R)
        P.op('dve', lambda: V.memset(cosT[:, :, 0:1], 1.0), r=R, w=R)
        P.op('dve', lambda: V.memset(sinT[:, :, 0:1], 0.0), r=R, w=R)
        sr = [sb(st, "sr%d" % i, [128, 32], F32) for i in range(2)]
        si = [sb(st, "si%d" % i, [128, 32], F32) for i in range(2)]
        big1 = sb(st, "big1", [128, 32, 32], F32); big2 = sb(st, "big2", [128, 32, 32], F32)
        stepr, stepi = wr[3], wi_[3]
        for lv in range(6):
            n = 1 << lv
            bc = lambda ap, n=n: ap.unsqueeze(2).to_broadcast([128, 32, n])
            P.op('dve', lambda: V.tensor_tensor(out=big1[:, :, 0:n], in0=cosT[:, :, 0:n], in1=bc(stepr[:]), op=ALU.mult), r=R, w=R)
            P.op('dve', lambda: V.tensor_tensor(out=big2[:, :, 0:n], in0=sinT[:, :, 0:n], in1=bc(stepi[:]), op=ALU.mult), r=R, w=R)
            P.op('dve', lambda: V.tensor_tensor(out=cosT[:, :, n:2 * n], in0=big1[:, :, 0:n], in1=big2[:, :, 0:n], op=ALU.subtract), r=R, w=R)
            P.op('dve', lambda: V.tensor_tensor(out=big1[:, :, 0:n], in0=cosT[:, :, 0:n], in1=bc(stepi[:]), op=ALU.mult), r=R, w=R)
            P.op('dve', lambda: V.tensor_tensor(out=big2[:, :, 0:n], in0=sinT[:, :, 0:n], in1=bc(stepr[:]), op=ALU.mult), r=R, w=R)
            P.op('dve', lambda: V.tensor_tensor(out=sinT[:, :, n:2 * n], in0=big1[:, :, 0:n], in1=big2[:, :, 0:n], op=ALU.add), r=R, w=R)
            if lv < 5:
                nr, ni = sr[lv % 2], si[lv % 2]
                cmul(nr[:], ni[:], stepr[:], stepi[:], stepr[:], stepi[:], e1[:], e2[:], R)
                stepr, stepi = nr, ni
        P.op('dve', lambda: V.tensor_copy(out=R8z[:], in_=m8[:].unsqueeze(2).to_broadcast([128, 32, 64])), r=R, w=R)
        P.op('dve', lambda: V.memset(R8z[:, :, 0:1], 0.0), r=R, w=R)
    P.barrier()
    if stop('c2'):
        return fin()

    with ExitStack() as st:
        WuB = sb(st, "WuB", [128, 8, 1024], BF16)
        with ExitStack() as s2:
            wstg = sb(s2, "wstgA", [128, 8, 1024], F32)
            load_weight_bf16((wstg, WuB), wu, 0, 1024, g1s, 'WuB')
            P.barrier()
            if stop('c30'):
                return fin()
        xt = [sb(st, "xtA%d" % i, [128, 1024], F32) for i in range(2)]
        junk = [sb(st, "junkA%d" % i, [128, 1024], F32) for i in range(2)]
        ssA = [sb(st, "ssA%d" % i, [128, 4], F32) for i in range(2)]
        hb = [sb(st, "hbA%d" % i, [128, 1024], BF16) for i in range(2)]
        hT = [sb(st, "hTA%d" % i, [128, 8, 512], BF16) for i in range(2)]
        uT = [sb(st, "uTA%d" % i, [128, 8, 512], BF16) for i in range(2)]
        S0b = [sb(st, "S0_%d" % i, [128, 32, 2, 64], F32) for i in range(2)]
        Z = sb(st, "Z", [128, 32, 2, 64], F32)
        ta = sb(st, "ta", [128, 32, 64], F32)
        tb = sb(st, "tb", [128, 32, 64], F32)
        Hin = sb(st, "Hin", [128, 2, 32], F32)
        cr_ = sb(st, "cr_", [128, 2, 32], F32)
        cq1 = sb(st, "cq1", [128, 32], F32); cq2 = sb(st, "cq2", [128, 32], F32)
        Hb16 = [sb(st, "Hb16_%d" % i, [128, 32, 2, 16], BF16) for i in range(2)]
        P.op('dve', lambda: V.memset(Hin[:], 0.0), w=['Hin'])
        bank_rot = [0]

        def nb():
            b = bank_rot[0]
            bank_rot[0] = (b + 1) % 8
            return b

        def stageA(t):
            hTt = hT[t % 2]; uTt = uT[t % 2]
            hn = 'hT%d' % (t % 2); un = 'uT%d' % (t % 2)
            S0 = S0b[t % 2]; s0n = 'S0_%d' % (t % 2)
            for blk in range(4):
                i2 = blk % 2
                b = nb()
                norm_transpose((xt[i2], junk[i2], ssA[i2], hb[i2]), xp[(t * 4 + blk) * 128:(t * 4 + blk + 1) * 128, :], hTt, blk * 128, 'A%d' % i2, b, act_scale=True)
                P.op('act', lambda b=b, blk=blk: A.copy(out=hTt[:, :, blk * 128:(blk + 1) * 128], in_=pb16(b).rearrange("p (kc n) -> p kc n", kc=8)),
                     r=[pbn[b]], w=[hn])
            for oc in range(8):
                b = nb()
                for kc in range(8):
                    P.op('pe', lambda b=b, oc=oc, kc=kc: T.matmul(pb[b][:, :], lhsT=WuB[:, kc, oc * 128:(oc + 1) * 128], rhs=hTt[:, kc, :],
                                                                   start=(kc == 0), stop=(kc == 7), skip_group_check=True),
                         r=['WuB', hn], w=[pbn[b]])
                P.op('act', lambda b=b, oc=oc: A.copy(out=uTt[:, oc, :], in_=pb[b][:, :]), r=[pbn[b]], w=[un])
            P.dma(HT_d[t].rearrange("p (kc n) -> p kc n", kc=8), hTt[:, :, 384:512], r=[hn], w=['HT_d'])
            P.dma(U_d[t].rearrange("p (kc n) -> p kc n", kc=8), uTt[:, :, 384:512], r=[un], w=['U_d'])
            for kc in range(8):
                for comp in range(2):
                    c0 = comp * 64
                    for s_ in range(8):
                        for pp in range(4):
                            b = 4 * (kc % 2) + pp
                            P.op('pe', lambda b=b, kc=kc, pp=pp, comp=comp, s_=s_, c0=c0: T.matmul(
                                pb[b][:, c0:c0 + 64], lhsT=W1[32 * pp:32 * pp + 32, kc, comp, s_, :], rhs=uTt[32 * pp:32 * pp + 32, kc, s_::8],
                                start=(s_ == 0), stop=(s_ == 7), skip_group_check=True, tile_position=(32 * pp, 0)),
                                r=['W1', un], w=[pbn[b]])
                for pp in range(4):
                    b = 4 * (kc % 2) + pp
                    S0v = S0[:, 4 * kc + pp, :, :].rearrange("p c m -> p (c m)")
                    P.op('act', lambda b=b, S0v=S0v: A.copy(out=S0v, in_=pb[b][:, 0:128]), r=[pbn[b]], w=[s0n])

        def stageB(t):
            S0 = S0b[t % 2]; s0n = 'S0_%d' % (t % 2)
            Hh = S0
            Sr, Si = S0[:, :, 0, :], S0[:, :, 1, :]
            Zr, Zi = Z[:, :, 0, :], Z[:, :, 1, :]
            Rr = [s0n, 'Z', 'ta', 'tb']
            P.op('dve', lambda: V.tensor_tensor(out=ta[:], in0=cosT[:], in1=Sr, op=ALU.mult), r=Rr, w=['ta'])
            P.op('pool', lambda: G.tensor_tensor(out=tb[:], in0=sinT[:], in1=Si, op=ALU.mult), r=Rr, w=['tb'])
            P.op('dve', lambda: V.tensor_tensor(out=Zr, in0=ta[:], in1=tb[:], op=ALU.add), r=Rr, w=['Z'])
            P.op('dve', lambda: V.tensor_tensor(out=ta[:], in0=cosT[:], in1=Si, op=ALU.mult), r=Rr, w=['ta'])
            P.op('pool', lambda: G.tensor_tensor(out=tb[:], in0=sinT[:], in1=Sr, op=ALU.mult), r=Rr, w=['tb'])
            P.op('dve', lambda: V.tensor_tensor(out=Zi, in0=ta[:], in1=tb[:], op=ALU.subtract), r=Rr, w=['Z'])
            Rc = ['Hin', 'cr_', 'cq']
            P.op('dve', lambda: V.tensor_tensor(out=cq1[:], in0=A8r[:], in1=Hin[:, 0, :], op=ALU.mult), r=Rc, w=Rc)
            P.op('dve', lambda: V.tensor_tensor(out=cq2[:], in0=A8i[:], in1=Hin[:, 1, :], op=ALU.mult), r=Rc, w=Rc)
            P.op('dve', lambda: V.tensor_tensor(out=cr_[:, 0, :], in0=cq1[:], in1=cq2[:], op=ALU.subtract), r=Rc, w=Rc)
            P.op('dve', lambda: V.tensor_tensor(out=cq1[:], in0=A8r[:], in1=Hin[:, 1, :], op=ALU.mult), r=Rc, w=Rc)
            P.op('dve', lambda: V.tensor_tensor(out=cq2[:], in0=A8i[:], in1=Hin[:, 0, :], op=ALU.mult), r=Rc, w=Rc)
            P.op('dve', lambda: V.tensor_tensor(out=cr_[:, 1, :], in0=cq1[:], in1=cq2[:], op=ALU.add), r=Rc, w=Rc)
            P.op('dve', lambda: V.tensor_tensor(out=Z[:, :, 0, 0], in0=Z[:, :, 0, 0], in1=cr_[:, 0, :], op=ALU.add), r=Rc + ['Z'], w=['Z'])
            P.op('dve', lambda: V.tensor_tensor(out=Z[:, :, 1, 0], in0=Z[:, :, 1, 0], in1=cr_[:, 1, :], op=ALU.add), r=Rc + ['Z'], w=['Z'])
            fl = lambda ap: ap.rearrange("p a m -> p (a m)")
            P.op('dve', lambda: V.tensor_copy(out=ta[:], in_=Zr), r=['Z'], w=['ta'])
            P.op('pool', lambda: G.tensor_copy(out=tb[:], in_=Zi), r=['Z'], w=['tb'])
            P.op('dve', lambda: V.tensor_tensor_scan(out=fl(ta[:]), data0=fl(R8z[:]), data1=fl(ta[:]), initial=0.0,
                                                    op0=ALU.mult, op1=ALU.add), r=['ta'], w=['ta'])
            P.op('dve', lambda: V.tensor_tensor_scan(out=fl(tb[:]), data0=fl(R8z[:]), data1=fl(tb[:]), initial=0.0,
                                                    op0=ALU.mult, op1=ALU.add), r=['tb'], w=['tb'])
            Rh = ['ta', 'tb', 'Z', s0n]
            P.op('dve', lambda: V.tensor_tensor(out=Zr, in0=cosT[:], in1=ta[:], op=ALU.mult), r=Rh, w=['Z'])
            P.op('pool', lambda: G.tensor_tensor(out=Zi, in0=sinT[:], in1=tb[:], op=ALU.mult), r=Rh, w=['Z'])
            P.op('dve', lambda: V.tensor_tensor(out=Hh[:, :, 0, :], in0=Zr, in1=Zi, op=ALU.subtract), r=Rh, w=[s0n])
            P.op('dve', lambda: V.tensor_tensor(out=Zr, in0=cosT[:], in1=tb[:], op=ALU.mult), r=Rh, w=['Z'])
            P.op('pool', lambda: G.tensor_tensor(out=Zi, in0=sinT[:], in1=ta[:], op=ALU.mult), r=Rh, w=['Z'])
            P.op('dve', lambda: V.tensor_tensor(out=Hh[:, :, 1, :], in0=Zr, in1=Zi, op=ALU.add), r=Rh, w=[s0n])
            P.op('dve', lambda: V.tensor_copy(out=Hin[:, 0, :], in_=Hh[:, :, 0, 63]), r=[s0n], w=['Hin'])
            P.op('dve', lambda: V.tensor_copy(out=Hin[:, 1, :], in_=Hh[:, :, 1, 63]), r=[s0n], w=['Hin'])
            hbt = Hb16[t % 2]
            P.op('dve', lambda hbt=hbt: V.tensor_copy(out=hbt[:], in_=Hh[:, :, :, 47:63]), r=[s0n], w=['Hb16_%d' % (t % 2)])
            P.dma(HS_d[t].rearrange("p (a c m) -> p a c m", a=32, c=2), hbt[:], r=['Hb16_%d' % (t % 2)], w=['HS_d'])

        stageA(0)
        for t in range(NT):
            if t + 1 < NT:
                stageA(t + 1)
            stageB(t)
    P.barrier()
    if stop('c4'):
        return fin()
    s5.close()

    with ExitStack() as st:
        CC = sb(st, "CC", [128, 32, 2, 256], BF16)
        KN = sb(st, "KN", [128, 8, 2, 8, 128], BF16)
        dfm = sb(st, "dfm", [128, 8], F32)
        P.dma(dfm[:], DFM[:, :], w=['dfm'])
        with ExitStack() as s2:
            ctr = sb(s2, "ctr", [128, 32, 16], F32); cti = sb(s2, "cti", [128, 32, 16], F32)
            ber = sb(s2, "ber", [128, 32, 32], F32); bei = sb(s2, "bei", [128, 32, 32], F32)
            P.dma(ctr[:].rearrange("p a c -> p (a c)"), CTR[:, :], w=['ctr'])
            P.dma(cti[:].rearrange("p a c -> p (a c)"), CTI[:, :], w=['cti'])
            P.dma(ber[:].rearrange("p a c -> p (a c)"), BER[:, :], w=['ber'])
            P.dma(bei[:].rearrange("p a c -> p (a c)"), BEI[:, :], w=['bei'])
            ctrb = sb(s2, "ctrb", [128, 32, 16], BF16); nctib = sb(s2, "nctib", [128, 32, 16], BF16)
            P.op('dve', lambda: V.tensor_copy(out=ctrb[:], in_=ctr[:]), r=['ctr'], w=['ctrb'])
            P.op('dve', lambda: V.tensor_scalar(out=nctib[:], in0=cti[:], scalar1=-1.0, scalar2=None, op0=ALU.mult), r=['cti'], w=['nctib'])
            k1 = sb(s2, "k1", [128, 32, 32], F32); k2 = sb(s2, "k2", [128, 32, 32], F32)
            xre = sb(s2, "xre", [128, 32, 32], BF16); xim = sb(s2, "xim", [128, 32, 32], BF16)
            R = ['cc']
            b16 = lambda ap: ap.unsqueeze(2).to_broadcast([128, 32, 16])
            b32 = lambda ap: ap.unsqueeze(2).to_broadcast([128, 32, 32])
            P.op('pool', lambda: G.memset(CC[:].rearrange("p a b c -> p (a b c)"), 0.0), w=['CC'])
            for s_ in range(8):
                qr, qi = QP[:, s_ + 1, 0, :], QP[:, s_ + 1, 1, :]
                P.op('dve', lambda: V.tensor_tensor(out=k1[:, :, 0:16], in0=ctr[:], in1=b16(qr), op=ALU.mult), r=R + ['ctr'], w=R)
                P.op('dve', lambda: V.tensor_tensor(out=k2[:, :, 0:16], in0=cti[:], in1=b16(qi), op=ALU.mult), r=R + ['cti'], w=R)
                for gg in range(2):
                    rs = slice(64 * gg, 64 * gg + 64)
                    P.op('dve', lambda s_=s_, gg=gg, rs=rs: V.tensor_tensor(out=CC[rs, :, 0, gg * 128 + s_ * 16:gg * 128 + (s_ + 1) * 16],
                                                                         in0=k1[rs, :, 0:16], in1=k2[rs, :, 0:16], op=ALU.subtract), r=R, w=R + ['CC'])
                P.op('dve', lambda: V.tensor_tensor(out=k1[:, :, 0:16], in0=ctr[:], in1=b16(qi), op=ALU.mult), r=R, w=R)
                P.op('dve', lambda: V.tensor_tensor(out=k2[:, :, 0:16], in0=cti[:], in1=b16(qr), op=ALU.mult), r=R, w=R)
                P.op('dve', lambda: V.tensor_tensor(out=k1[:, :, 0:16], in0=k1[:, :, 0:16], in1=k2[:, :, 0:16], op=ALU.add), r=R, w=R)
                for gg in range(2):
                    rs = slice(64 * gg, 64 * gg + 64)
                    P.op('dve', lambda s_=s_, gg=gg, rs=rs: V.tensor_scalar(out=CC[rs, :, 1, gg * 128 + s_ * 16:gg * 128 + (s_ + 1) * 16],
                                                                         in0=k1[rs, :, 0:16], scalar1=-1.0, scalar2=None, op0=ALU.mult), r=R, w=R + ['CC'])
            for tau in range(8):
                fr_, fi_ = FP[:, tau, 0, :], FP[:, tau, 1, :]
                P.op('dve', lambda: V.tensor_tensor(out=k1[:], in0=ber[:], in1=b32(fr_), op=ALU.mult), r=R + ['ber', 'xre', 'xim'], w=R)
                P.op('dve', lambda: V.tensor_tensor(out=k2[:], in0=bei[:], in1=b32(fi_), op=ALU.mult), r=R + ['bei'], w=R)
                P.op('dve', lambda: V.tensor_tensor(out=xre[:], in0=k1[:], in1=k2[:], op=ALU.subtract), r=R, w=R + ['xre'])
                P.op('dve', lambda: V.tensor_tensor(out=k1[:], in0=bei[:], in1=b32(fr_), op=ALU.mult), r=R, w=R)
                P.op('dve', lambda: V.tensor_tensor(out=k2[:], in0=ber[:], in1=b32(fi_), op=ALU.mult), r=R, w=R)
                P.op('dve', lambda: V.tensor_tensor(out=xim[:], in0=k1[:], in1=k2[:], op=ALU.add), r=R, w=R + ['xim'])
                for gp in range(32):
                    kc, pp = gp // 4, gp % 4
                    for gg in range(2):
                        bnk = 2 * gg + tau // 4
                        c0 = (tau % 4) * 128 + kc * 16
                        rs = slice(64 * gg, 64 * gg + 64)
                        P.op('pe', lambda bnk=bnk, gp=gp, gg=gg, pp=pp, c0=c0, rs=rs: T.matmul(
                            pb[bnk][32 * pp:32 * pp + 32, c0:c0 + 16], lhsT=xre[rs, gp, :], rhs=ctrb[rs, gp, :], start=True, stop=False,
                            skip_group_check=True, tile_position=(64 * gg, 32 * pp)), r=['xre', 'ctrb'], w=[pbn[bnk]])
                        P.op('pe', lambda bnk=bnk, gp=gp, gg=gg, pp=pp, c0=c0, rs=rs: T.matmul(
                            pb[bnk][32 * pp:32 * pp + 32, c0:c0 + 16], lhsT=xim[rs, gp, :], rhs=nctib[rs, gp, :], start=False, stop=True,
                            skip_group_check=True, tile_position=(64 * gg, 32 * pp)), r=['xim', 'nctib'], w=[pbn[bnk]])
            P.op('pool', lambda: G.memset(KN[:].rearrange("p a b c d -> p (a b c d)"), 0.0), w=['KN'])
            for tau in range(8):
                for gg in range(2):
                    bnk = 2 * gg + tau // 4
                    src = pb[bnk][:, (tau % 4) * 128:(tau % 4) * 128 + 128].rearrange("p (a c) -> p a c", a=8)
                    for sp_ in range(8 - tau):
                        s_ = sp_ + tau
                        P.op('dve', lambda src=src, sp_=sp_, s_=s_, gg=gg: V.tensor_copy(out=KN[:, :, gg, sp_, s_ * 16:(s_ + 1) * 16], in_=src),
                             r=[pbn[bnk]], w=['KN'])
        P.barrier()
        if stop('c5'):
            return fin()
        Hb_all = sb(st, "Hb_all", [128, 32, 2, 256], BF16)
        uo_all = sb(st, "uo_all", [128, 8, 2048], BF16)
        hstg = [sb(st, "hstg%d" % i, [128, 32, 2, 16], BF16) for i in range(2)]
        for t in range(NT):
            hs = hstg[t % 2]; hsn = 'hstg%d' % (t % 2)
            P.dma(hs[:], HS_d[t].rearrange("p (a c m) -> p a c m", a=32, c=2), r=['HS_d'], w=[hsn])
            if t % 2 == 0:
                P.op('act', lambda hs=hs, t=t: A.copy(out=Hb_all[:, :, :, t * 16:(t + 1) * 16], in_=hs[:]), r=[hsn], w=['Hb_all'])
            else:
                P.op('dve', lambda hs=hs, t=t: V.tensor_copy(out=Hb_all[:, :, :, t * 16:(t + 1) * 16], in_=hs[:]), r=[hsn], w=['Hb_all'])
            P.dma(uo_all[:, :, t * 128:(t + 1) * 128], U_d[t].rearrange("p (kc n) -> p kc n", kc=8), r=['U_d'], w=['uo_all'])
        nsb = [sb(st, "nsb%d" % i, [128, 256], F32) for i in range(2)]
        Yg = [sb(st, "Yg%d" % i, [128, 256], BF16) for i in range(2)]
        ytm = [sb(st, "ytm%d" % i, [128, 2, 8, 8, 16], BF16) for i in range(2)]
        ypre = [sb(st, "ypre0", [128, 2048], F32)] * 2
        g1_ = sb(st, "g1_", [128, 2048], F32); g2_ = g1_
        yg = [sb(st, "yg%d" % i, [128, 2048], BF16) for i in range(2)]
        Yd_v = Y_d.rearrange("t p (kc n) -> p t kc n", kc=8)
        for kc in range(8):
            yt = ytm[kc % 2]; ytn = 'ytm%d' % (kc % 2)
            for gl in range(8):
                gp, gg, pp = 4 * kc + gl // 2, gl % 2, gl // 2
                bF = gl % 2
                for comp in range(2):
                    P.op('pe', lambda bF=bF, gp=gp, comp=comp, gg=gg: T.matmul(
                        pb[bF][:, 0:256], lhsT=CC[:, gp, comp, gg * 128:(gg + 1) * 128], rhs=Hb_all[:, gp, comp, :], start=(comp == 0), stop=(comp == 1),
                        skip_group_check=True), r=['Hb_all', 'CC'], w=[pbn[bF]])
                bN = 2 + pp
                for sp_ in range(8):
                    P.op('pe', lambda bN=bN, pp=pp, kc=kc, gg=gg, sp_=sp_: T.matmul(
                        pb[bN][:, 0:256], lhsT=KN[32 * pp:32 * pp + 32, kc, gg, sp_, :], rhs=uo_all[32 * pp:32 * pp + 32, kc, sp_::8],
                        start=(sp_ == 0), stop=(sp_ == 7), skip_group_check=True, tile_position=(32 * pp, 0)), r=['uo_all', 'KN'], w=[pbn[bN]])
                ns = nsb[gl % 2]; nsn = 'nsb%d' % (gl % 2)
                ygt = Yg[gl % 2]; ygn = 'Yg%d' % (gl % 2)
                P.op('act', lambda bN=bN, ns=ns: A.copy(out=ns[:], in_=pb[bN][:, 0:256]), r=[pbn[bN]], w=[nsn])
                P.op('dve', lambda bF=bF, ns=ns, ygt=ygt: V.tensor_tensor(out=ygt[:], in0=pb[bF][:, 0:256], in1=ns[:], op=ALU.add), r=[pbn[bF], nsn], w=[ygn])
                for mh in range(2):
                    P.op('pe', lambda ygt=ygt, mh=mh: T.transpose(out=pb16(6)[:, mh * 128:(mh + 1) * 128], in_=ygt[:, mh * 128:(mh + 1) * 128], identity=identb[:]),
                         r=[ygn, 'identb'], w=[pbn[6]])
                P.op('act', lambda yt=yt, gl=gl: A.copy(out=yt[:, :, :, gl, :], in_=pb16(6)[:, 0:256].rearrange("p (h s c) -> p h s c", h=2, s=8)),
                     r=[pbn[6]], w=[ytn])
            yp = ypre[0]; ypn = 'ypre0'
            for mh in range(2):
                for s_ in range(8):
                    P.op('pe', lambda yt=yt, mh=mh, s_=s_: T.transpose(out=pb16(7)[:, s_ * 128:(s_ + 1) * 128], in_=yt[:, mh, s_, :, :].rearrange("p g c -> p (g c)"),
                                                                       identity=identb[:]), r=[ytn, 'identb'], w=[pbn[7]])
                ov = yp[:, mh * 1024:(mh + 1) * 1024].rearrange("p (m s) -> p s m", s=8)
                uv = uo_all[:, kc, mh * 1024:(mh + 1) * 1024].rearrange("p (m s) -> p s m", s=8)
                P.op('dve', lambda kc=kc, ov=ov, uv=uv: V.scalar_tensor_tensor(
                    out=ov, in0=uv, scalar=dfm[:, kc:kc + 1], in1=pb16(7)[:, 0:1024].rearrange("p (s m) -> p s m", s=8), op0=ALU.mult, op1=ALU.add),
                    r=['uo_all', 'dfm', pbn[7]], w=[ypn])
            yf = yp[:]
            ygo = yg[kc % 2]; ygon = 'yg%d' % (kc % 2)
            P.op('dve', lambda yf=yf: V.tensor_tensor(out=g1_[:], in0=yf, in1=yf, op=ALU.mult), r=[ypn], w=['g1_'])
            P.op('dve', lambda: V.tensor_scalar(out=g1_[:], in0=g1_[:], scalar1=0.044715, scalar2=1.0, op0=ALU.mult, op1=ALU.add), r=['g1_'], w=['g1_'])
            P.op('dve', lambda yf=yf: V.tensor_tensor(out=g1_[:], in0=g1_[:], in1=yf, op=ALU.mult), r=['g1_', ypn], w=['g1_'])
            P.op('act', lambda: A.activation(out=g1_[:], in_=g1_[:], func=AF.Sigmoid, scale=2.0 * 0.7978845608028654), r=['g1_'], w=['g1_'])
            P.op('dve', lambda ygo=ygo, yf=yf: V.tensor_tensor(out=ygo[:], in0=g1_[:], in1=yf, op=ALU.mult), r=['g1_', ypn], w=[ygon])
            P.dma(Yd_v[:, :, kc, :], ygo[:].rearrange("p (t n) -> p t n", t=NT), r=[ygon], w=['Y_d'])
    P.barrier()

    sq.close()
    if dbg and dbg[0] == 'Y':
        with ExitStack() as st:
            tmpb = sb(st, "tmpb", [128, 1024], BF16); tmpf = sb(st, "tmpf", [128, 1024], F32)
            for t in range(NT):
                P.dma(tmpb[:], Y_d[t], r=['Y_d'], w=['tmpb'])
                P.op('dve', lambda: V.tensor_copy(out=tmpf[:], in_=tmpb[:]), r=['tmpb'], w=['tmpf'])
                P.dma(dbg_d[t], tmpf[:], r=['tmpf'], w=['dbg'])
        P.barrier()
        for e in ['sp']:
            pass
        es.close()
        return nc

    if stop('Y2'):
        return fin()
    ISQ = 1.0 / math.sqrt(128.0)

    with ExitStack() as st:
        kT_all = sb(st, "kT_all", [128, 2, L], BF16)
        V_all = sb(st, "V_all", [128, 64, 256], BF16)
        kiT_all = sb(st, "kiT_all", [128, L], BF16)
        with ExitStack() as s2:
            WkvB = sb(s2, "WkvB", [128, 8, 640], BF16)
            with ExitStack() as s3:
                wstg = sb(s3, "wstgK", [128, 8, 640], F32)
                load_weight_bf16((wstg, WkvB), wkv, 0, 640, g1s, 'WkvB')
                P.barrier()
            xt = [sb(s2, "xtK%d" % i, [128, 1024], F32) for i in range(4)]
            junk = [sb(s2, "junkK0", [128, 1024], F32)] * 4
            ssA = [sb(s2, "ssK%d" % i, [128, 4], F32) for i in range(4)]
            hb = [sb(s2, "hbK%d" % i, [128, 1024], BF16) for i in range(4)]
            hT = [sb(s2, "hTK%d" % i, [128, 8, 512], BF16) for i in range(2)]
            ctab = sb(s2, "ctabK", [128, 512], F32); stab = sb(s2, "stabK", [128, 512], F32)
            ctabI = sb(s2, "ctabI", [128, 512], F32); stabI = sb(s2, "stabI", [128, 512], F32)
            rt1 = sb(s2, "rt1", [128, 512], F32); rt2 = sb(s2, "rt2", [128, 512], F32)
            brot = [0]

            def nb2():
                b = brot[0]
                brot[0] = (b + 1) % 8
                return b
            for t in range(NT):
                hTt = hT[t % 2]; hn = 'hTK%d' % (t % 2)
                for blk in range(4):
                    i2 = blk % 4
                    b = nb2()
                    norm_transpose((xt[i2], junk[i2], ssA[i2], hb[i2]), xp[(t * 4 + blk) * 128:(t * 4 + blk + 1) * 128, :], hTt, blk * 128, 'K%d' % i2, b)
                    if blk % 2 == 0:
                        P.op('act', lambda b=b, blk=blk: A.copy(out=hTt[:, :, blk * 128:(blk + 1) * 128], in_=pb16(b).rearrange("p (kc n) -> p kc n", kc=8)),
                             r=[pbn[b]], w=[hn])
                    else:
                        P.op('dve', lambda b=b, blk=blk: V.tensor_copy(out=hTt[:, :, blk * 128:(blk + 1) * 128], in_=pb16(b).rearrange("p (kc n) -> p kc n", kc=8)),
                             r=[pbn[b]], w=[hn])
                cs = slice(t * 512, (t + 1) * 512)
                P.dma(ctab[:], cAk[:, cs], w=['ctabK']); P.dma(stab[:], sAk[:, cs], w=['stabK'])
                P.dma(ctabI[:], cIk[:, cs], w=['ctabI']); P.dma(stabI[:], sIk[:, cs], w=['stabI'])
                for oc in range(3):
                    b = nb2()
                    for kc in range(8):
                        P.op('pe', lambda b=b, oc=oc, kc=kc: T.matmul(pb[b][:, :], lhsT=WkvB[:, kc, oc * 128:(oc + 1) * 128], rhs=hTt[:, kc, :],
                                                                       start=(kc == 0), stop=(kc == 7), skip_group_check=True),
                             r=['WkvB', hn], w=[pbn[b]])
                    if oc < 2:
                        dst = kT_all[:, oc, cs]
                        P.op('act', lambda b=b, dst=dst: A.copy(out=dst, in_=pb[b][:, :]), r=[pbn[b]], w=['kT_all'])
                        rope(dst, 128, permA, ctab[:], stab[:], rt1, rt2, nb2(), ['ctabK', 'stabK'], 'kT_all', 512)
                    else:
                        dst = kiT_all[:, cs]
                        P.op('act', lambda b=b, dst=dst: A.copy(out=dst, in_=pb[b][:, :]), r=[pbn[b]], w=['kiT_all'])
                        rope(dst, 128, permI, ctabI[:], stabI[:], rt1, rt2, nb2(), ['ctabI', 'stabI'], 'kiT_all', 512)
                for blk in range(4):
                    b = nb2()
                    for kc in range(8):
                        P.op('pe', lambda b=b, blk=blk, kc=kc: T.matmul(pb[b][:, 0:256], lhsT=hTt[:, kc, blk * 128:(blk + 1) * 128], rhs=WkvB[:, kc, 384:640],
                                                                         start=(kc == 0), stop=(kc == 7), skip_group_check=True),
                             r=['WkvB', hn], w=[pbn[b]])
                    P.op('dve', lambda b=b, blk=blk, t=t: V.tensor_copy(out=V_all[:, 4 * t + blk, :], in_=pb[b][:, 0:256]), r=[pbn[b]], w=['V_all'])
        P.barrier()
        if stop('KV'):
            return fin()
        hTo = sb(st, "hTo", [128, 8, 512], BF16)
        qT = sb(st, "qT", [128, 4, 8, 128], BF16)
        qiT = sb(st, "qiT", [128, 8, 512], BF16)
        wis = sb(st, "wis", [128, 4, 16], F32)
        wst = [sb(st, "wstQ0", [128, 8, 128], F32)] * 2
        wbf = [sb(st, "wbfQ%d" % i, [128, 8, 128], BF16) for i in range(2)]
        qtmp = sb(st, "qtmp", [128, 512], BF16)
        ctq = sb(st, "ctq", [128, 512], F32); stq = sb(st, "stq", [128, 512], F32)
        rt1 = sb(st, "rt1q", [128, 512], F32); rt2 = sb(st, "rt2q", [128, 512], F32)
        Dg = sb(st, "Dg", [128, 16, 128], BF16)
        Rb = [sb(st, "Rb%d" % i, [128, 1024], BF16) for i in range(3)]
        score = sb(st, "score", [128, L], F32)
        cjunk = sb(st, "cjunk", [128, L], mybir.dt.uint8)
        mtile = [sb(st, "mtile%d" % i, [128, 512], F32) for i in range(3)]
        P.dma(mtile[0][:], mfirst[:, :], w=['mt0']); P.dma(mtile[1][:], mlast[:, :], w=['mt1']); P.dma(mtile[2][:], mboth[:, :], w=['mt2'])
        bs = sb(st, "bs", [128, 8], F32)
        mq = [sb(st, "mq0", [128, 512], BF16)] * 2
        maskT = sb(st, "maskT", [128, 64, 128], BF16)
        Eb = [sb(st, "Eb%d" % i, [128, 1024], BF16) for i in range(2)]
        PT = [sb(st, "PT%d" % i, [128, 1024], BF16) for i in range(2)]
        rden = rt1
        ybt = [sb(st, "ybt0", [128, 1024], BF16)] * 2
        for half in range(4):
            for i in range(4):
                P.dma(hTo[:, :, i * 128:(i + 1) * 128], HT_d[4 * half + i].rearrange("p (kc n) -> p kc n", kc=8), r=['HT_d'], w=['hTo'])
            wcnt = 0
            for hh in range(16):
                wi2 = wcnt % 2; wcnt += 1
                load_weight_bf16((wst[wi2], wbf[wi2]), wq, hh * 128, 128, g1s, 'wbfQ%d' % wi2, stag='wstQ')
                for j in range(1):
                    b = j
                    for kc in range(8):
                        P.op('pe', lambda b=b, kc=kc, wi2=wi2, j=j: T.matmul(pb[b][:, :], lhsT=wbf[wi2][:, kc, :], rhs=hTo[:, kc, j * 512:(j + 1) * 512],
                                                                             start=(kc == 0), stop=(kc == 7), skip_group_check=True),
                             r=['wbfQ%d' % wi2, 'hTo'], w=[pbn[b]])
                    tcs = slice((half * 4) * 128, (half * 4) * 128 + 512)
                    if hh < 8:
                        P.dma(ctq[:], cAq[:, tcs], w=['ctq']); P.dma(stq[:], sAq[:, tcs], w=['stq'])
                        P.op('act', lambda b=b: A.copy(out=qtmp[:], in_=pb[b][:, :]), r=[pbn[b]], w=['qtmp'])
                        rope(qtmp[:], 128, permA, ctq[:], stq[:], rt1, rt2, 2 + j, ['ctq', 'stq'], 'qtmp', 512)
                        P.op('act', lambda hh=hh, j=j: A.copy(out=qT[:, 4 * j:4 * j + 4, hh, :], in_=qtmp[:].rearrange("p (a n) -> p a n", a=4)),
                             r=['qtmp'], w=['qT'])
                    else:
                        P.dma(ctq[:], cIq[:, tcs], w=['ctq']); P.dma(stq[:], sIq[:, tcs], w=['stq'])
                        dst = qiT[:, hh - 8, j * 512:(j + 1) * 512]
                        P.op('act', lambda b=b, dst=dst: A.copy(out=dst, in_=pb[b][:, :]), r=[pbn[b]], w=['qiT'])
                        rope(dst, 128, permI, ctq[:], stq[:], rt1, rt2, 2 + j, ['ctq', 'stq'], 'qiT', 512)
            load_weight_bf16((wst[0], wbf[0]), wwi, 0, 16, g1s, 'wbfQ0', stag='wstQ')
            for blk in range(4):
                b = 4 + blk % 2
                for kc in range(8):
                    P.op('pe', lambda b=b, kc=kc, blk=blk: T.matmul(pb[b][:, 0:16], lhsT=hTo[:, kc, blk * 128:(blk + 1) * 128], rhs=wbf[0][:, kc, 0:16],
                                                                     start=(kc == 0), stop=(kc == 7), skip_group_check=True),
                         r=['wbfQ0', 'hTo'], w=[pbn[b]])
                P.op('dve', lambda b=b, blk=blk: V.tensor_copy(out=wis[:, blk, :], in_=pb[b][:, 0:16]), r=[pbn[b]], w=['wis'])
            for blk in range(4):
                gi = 4 * half + blk
                nkt = gi + 1
                n = nkt * 512
                for h in range(16):
                    if h % 2 == 0:
                        P.op('act', lambda h=h, blk=blk: A.activation(out=Dg[:, h, :], in_=ident32[:], func=AF.Copy, scale=wis[:, blk, h:h + 1]),
                             r=['wis', 'ident32'], w=['Dg'])
                    else:
                        P.op('dve', lambda h=h, blk=blk: V.tensor_scalar(out=Dg[:, h, :], in0=ident32[:], scalar1=wis[:, blk, h:h + 1], scalar2=None, op0=ALU.mult),
                             r=['wis', 'ident32'], w=['Dg'])
                for kt in range(nkt):
                    bsc = 6 + kt % 2

                    def rel_pair(p, kt=kt, blk=blk):
                        X = 2 * (p % 3)
                        for hh in range(2):
                            rs = slice(64 * hh, 64 * hh + 64)
                            P.op('pe', lambda hh=hh, rs=rs: T.matmul(
                                pb[X + hh][:, :], lhsT=qiT[rs, p, blk * 128:(blk + 1) * 128], rhs=kiT_all[rs, kt * 512:(kt + 1) * 512],
                                start=True, stop=True, skip_group_check=True, tile_position=(64 * hh, 0)), r=['qiT', 'kiT_all'], w=[pbn[X + hh]])
                        rbuf = Rb[p % 3]; rn = 'Rb%d' % (p % 3)
                        src2 = pball[:, X * 512:(X + 2) * 512]
                        if p % 2 == 0:
                            P.op('act', lambda: A.activation(out=rbuf[:], in_=src2, func=AF.Relu), r=[pbn[X], pbn[X + 1]], w=[rn])
                        else:
                            P.op('dve', lambda: V.tensor_scalar(out=rbuf[:], in0=src2, scalar1=0.0, scalar2=None, op0=ALU.max), r=[pbn[X], pbn[X + 1]], w=[rn])

                    def score_pair(p, bsc=bsc):
                        rbuf = Rb[p % 3]; rn = 'Rb%d' % (p % 3)
                        for hh in range(2):
                            h = 2 * p + hh
                            P.op('pe', lambda h=h, hh=hh: T.matmul(pb[bsc][:, :], lhsT=Dg[:, h, :], rhs=rbuf[:, hh * 512:(hh + 1) * 512], start=(h == 0), stop=(h == 15),
                                                                   skip_group_check=True), r=['Dg', rn], w=[pbn[bsc]])
                    for step in range(8 + 2):
                        if step < 8:
                            rel_pair(step)
                        if step >= 2:
                            score_pair(step - 2)
                    mt = None
                    if kt == 0 and kt == nkt - 1:
                        mt = 2
                    elif kt == 0:
                        mt = 0
                    elif kt == nkt - 1:
                        mt = 1
                    dsts = score[:, kt * 512:(kt + 1) * 512]
                    if mt is None:
                        P.op('act', lambda bsc=bsc, dsts=dsts: A.copy(out=dsts, in_=pb[bsc][:, :]), r=[pbn[bsc]], w=['score'])
                    else:
                        P.op('dve', lambda bsc=bsc, dsts=dsts, mt=mt: V.tensor_tensor(out=dsts, in0=pb[bsc][:, :], in1=mtile[mt][:], op=ALU.add),
                             r=[pbn[bsc], 'mt%d' % mt], w=['score'])
                n_act = 512 * ((gi + 1) // 2)
                thr_c = 255.5 - 0.5 * n_act
                stps = [BIS_B * 2.0 / (2.0 ** (it + 1)) for it in range(NITER)]
                P.op('dve', lambda: V.memset(bs[:, 1:2], -BIS_B + stps[0]), r=['bs1'], w=['bs1'])
                for it in range(NITER):
                    if n_act > 0:
                        P.op('act', lambda n_act=n_act: A.activation(out=cjunk[:, 0:n_act], in_=score[:, 0:n_act], func=AF.Sign, bias=bs[:, 1:2], scale=-1.0,
                                                                    accum_out=bs[:, 5:6]), r=['bs1', 'score'], w=['bs5', 'cjunkA'])
                    P.op('dve', lambda n=n, n_act=n_act: V.tensor_scalar(out=cjunk[:, n_act:n], in0=score[:, n_act:n], scalar1=bs[:, 1:2], scalar2=0.0, op0=ALU.is_ge, op1=ALU.add,
                                                                        accum_out=bs[:, 2:3]), r=['bs1', 'score'], w=['bs2', 'cjunk'])
                    if n_act > 0:
                        P.op('dve', lambda: V.scalar_tensor_tensor(out=bs[:, 6:7], in0=bs[:, 5:6], scalar=-0.5, in1=bs[:, 2:3], op0=ALU.mult, op1=ALU.add), r=['bs2', 'bs5'], w=['bs6'])
                        tcol = 6
                    else:
                        tcol = 2
                    P.op('dve', lambda it=it, tcol=tcol, thr_c=thr_c: V.tensor_scalar(out=bs[:, 3:4], in0=bs[:, tcol:tcol + 1], scalar1=thr_c, scalar2=stps[it], op0=ALU.is_ge, op1=ALU.mult),
                         r=['bs%d' % tcol], w=['bs3'])
                    if it < NITER - 1:
                        P.op('dve', lambda it=it: V.scalar_tensor_tensor(out=bs[:, 1:2], in0=bs[:, 3:4], scalar=-stps[it + 1], in1=bs[:, 1:2], op0=ALU.add, op1=ALU.add), r=['bs3', 'bs1'], w=['bs1'])
                    else:
                        P.op('dve', lambda it=it: V.scalar_tensor_tensor(out=bs[:, 0:1], in0=bs[:, 3:4], scalar=-stps[it], in1=bs[:, 1:2], op0=ALU.add, op1=ALU.add), r=['bs3', 'bs1'], w=['bs'])
                for kt in range(nkt):
                    mqt = mq[0]; mn = 'mq0'
                    P.op('dve', lambda mqt=mqt, kt=kt: V.tensor_scalar(out=mqt[:], in0=score[:, kt * 512:(kt + 1) * 512], scalar1=bs[:, 0:1], scalar2=None, op0=ALU.is_ge),
                         r=['bs', 'score'], w=[mn])
                    bt_ = 4 + kt % 2
                    for a in range(4):
                        P.op('pe', lambda mqt=mqt, a=a, bt_=bt_: T.transpose(out=pb16(bt_)[:, a * 128:(a + 1) * 128], in_=mqt[:, a * 128:(a + 1) * 128], identity=identb[:]),
                             r=[mn, 'identb'], w=[pbn[bt_]])
                    P.op('act', lambda kt=kt, bt_=bt_: A.copy(out=maskT[:, 4 * kt:4 * kt + 4, :], in_=pb16(bt_)[:, 0:512].rearrange("p (a n) -> p a n", a=4)),
                         r=[pbn[bt_]], w=['maskT'])
                ybtt = ybt[0]; ybn = 'ybt0'
                nkb = 4 * nkt
                for g in range(2):
                    rhsq = qT[:, blk, 4 * g:4 * g + 4, :].rearrange("p a n -> p (a n)")
                    npair = nkb // 2

                    def qk_pair(pq, g=g, rhsq=rhsq):
                        X = 2 * (pq % 2)
                        for j in range(2):
                            kb = 2 * pq + j
                            P.op('pe', lambda kb=kb, j=j: T.matmul(pb[X + j][:, :], lhsT=kT_all[:, g, kb * 128:(kb + 1) * 128], rhs=rhsq,
                                                                   start=True, stop=True, skip_group_check=True), r=['kT_all', 'qT'], w=[pbn[X + j]])
                        e = Eb[pq % 2]; en = 'Eb%d' % (pq % 2)
                        P.op('act', lambda: A.activation(out=e[:], in_=pball[:, X * 512:(X + 2) * 512], func=AF.Exp, scale=ISQ), r=[pbn[X], pbn[X + 1]], w=[en])
                        pt = PT[pq % 2]; pn = 'PT%d' % (pq % 2)
                        P.op('dve', lambda: V.tensor_tensor(out=pt[:].rearrange("p (k a n) -> p k a n", k=2, a=4), in0=e[:].rearrange("p (k a n) -> p k a n", k=2, a=4),
                                                            in1=maskT[:, 2 * pq:2 * pq + 2, :].unsqueeze(2).to_broadcast([128, 2, 4, 128]), op=ALU.mult),
                             r=[en, 'maskT'], w=[pn])

                    def pv_pair(pq, g=g, nkb=nkb):
                        pt = PT[pq % 2]; pn = 'PT%d' % (pq % 2)
                        for j in range(2):
                            kb = 2 * pq + j
                            P.op('pe', lambda kb=kb, j=j: T.matmul(pb[6][:, :], lhsT=V_all[:, kb, g * 128:(g + 1) * 128], rhs=pt[:, j * 512:(j + 1) * 512],
                                                                   start=(kb == 0), stop=(kb == nkb - 1), skip_group_check=True), r=['V_all', pn], w=[pbn[6]])
                            P.op('pe', lambda kb=kb, j=j: T.matmul(pb[7][:, :], lhsT=onesb[:], rhs=pt[:, j * 512:(j + 1) * 512],
                                                                   start=(kb == 0), stop=(kb == nkb - 1), skip_group_check=True), r=['onesb', pn], w=[pbn[7]])
                    for step in range(npair + 1):
                        if step < npair:
                            qk_pair(step)
                        if step >= 1:
                            pv_pair(step - 1)
                    P.op('dve', lambda: V.reciprocal(out=rden[:], in_=pb[7][:, :]), r=[pbn[7]], w=['ropet1'])
                    P.op('dve', lambda g=g, ybtt=ybtt: V.tensor_tensor(out=ybtt[:, g * 512:(g + 1) * 512], in0=pb[6][:, :], in1=rden[:], op=ALU.mult),
                         r=[pbn[6], 'ropet1'], w=[ybn])
                P.dma(YB_d[gi], ybtt[:], r=[ybn], w=['YB_d'])
    P.barrier()
    if stop('ATT'):
        return fin()

    with ExitStack() as st:
        h2T = sb(st, "h2T", [128, 8, 2048], BF16)
        sA = ExitStack()
        mT = sb(sA, "mT", [128, 8, 2048], BF16)
        with ExitStack() as s2:
            yaT = sb(s2, "yaT", [128, 8, 2048], BF16)
            wst = [sb(s2, "wstD%d" % i, [128, 8, 128], F32) for i in range(4)]
            wbf = [sb(s2, "wbfD%d" % i, [128, 8, 128], BF16) for i in range(4)]
            sg = [sb(s2, "sgD%d" % i, [128, 512], F32) for i in range(4)]
            with ExitStack() as s3:
                yT = sb(s3, "yT", [128, 8, 2048], BF16)
                for t in range(NT):
                    P.dma(yT[:, :, t * 128:(t + 1) * 128], Y_d[t].rearrange("p (kc n) -> p kc n", kc=8), r=['Y_d'], w=['yT'])
                for oc in range(8):
                    w2 = oc % 2
                    load_weight_bf16((wst[w2], wbf[w2]), wglu, oc * 128, 128, None, 'wbfD%d' % w2)
                    for j in range(4):
                        b = (oc * 4 + j) % 8
                        for kc in range(8):
                            P.op('pe', lambda b=b, kc=kc, w2=w2, j=j: T.matmul(pb[b][:, :], lhsT=wbf[w2][:, kc, :], rhs=yT[:, kc, j * 512:(j + 1) * 512],
                                                                               start=(kc == 0), stop=(kc == 7), skip_group_check=True), r=['wbfD%d' % w2, 'yT'], w=[pbn[b]])
                        sgt = sg[j % 2]; sn = 'sgD%d' % (j % 2)
                        P.op('act', lambda b=b, sgt=sgt: A.activation(out=sgt[:], in_=pb[b][:, :], func=AF.Sigmoid), r=[pbn[b]], w=[sn])
                        P.op('dve', lambda oc=oc, j=j, sgt=sgt: V.tensor_tensor(out=yaT[:, oc, j * 512:(j + 1) * 512], in0=sgt[:], in1=yT[:, oc, j * 512:(j + 1) * 512], op=ALU.mult),
                             r=[sn, 'yT'], w=['yaT'])
                P.barrier()
            ybT = sb(s2, "ybT", [128, 8, 2048], BF16)
            hTa = sb(s2, "hTa", [128, 8, 2048], BF16)
            for t in range(NT):
                P.dma(ybT[:, :, t * 128:(t + 1) * 128], YB_d[t].rearrange("p (h n) -> p h n", h=8), r=['YB_d'], w=['ybT'])
                P.dma(hTa[:, :, t * 128:(t + 1) * 128], HT_d[t].rearrange("p (kc n) -> p kc n", kc=8), r=['HT_d'], w=['hTa'])
            m1 = sb(s2, "mm1", [128, 512], F32); m2 = sb(s2, "mm2", [128, 512], F32)
            for oc in range(8):
                load_weight_bf16((wst[0], wbf[0]), wba, oc * 128, 128, None, 'wbfD0')
                load_weight_bf16((wst[1], wbf[1]), wbb, oc * 128, 128, None, 'wbfD1')
                load_weight_bf16((wst[2], wbf[2]), wg, oc * 128, 128, g1s, 'wbfD2')
                load_weight_bf16((wst[3], wbf[3]), wg, 1024 + oc * 128, 128, g1s, 'wbfD3')
                for j in range(4):
                    js = slice(j * 512, (j + 1) * 512)
                    acts = [yaT, ybT, hTa, hTa]; anm = ['yaT', 'ybT', 'hTa', 'hTa']
                    for q4 in range(4):
                        b = 4 * (j % 2) + q4
                        for kc in range(8):
                            P.op('pe', lambda b=b, kc=kc, q4=q4, js=js, acts=acts: T.matmul(pb[b][:, :], lhsT=wbf[q4][:, kc, :], rhs=acts[q4][:, kc, js],
                                                                                          start=(kc == 0), stop=(kc == 7), skip_group_check=True),
                                 r=['wbfD%d' % q4, anm[q4]], w=[pbn[b]])
                    b0 = 4 * (j % 2)
                    P.op('act', lambda b0=b0: A.activation(out=sg[0][:], in_=pb[b0 + 2][:, :], func=AF.Sigmoid), r=[pbn[b0 + 2]], w=['sgD0'])
                    P.op('act', lambda b0=b0: A.activation(out=sg[1][:], in_=pb[b0 + 3][:, :], func=AF.Sigmoid), r=[pbn[b0 + 3]], w=['sgD1'])
                    P.op('dve', lambda b0=b0: V.tensor_tensor(out=m1[:], in0=pb[b0][:, :], in1=sg[0][:], op=ALU.mult), r=[pbn[b0], 'sgD0'], w=['m1'])
                    P.op('dve', lambda b0=b0: V.tensor_tensor(out=m2[:], in0=pb[b0 + 1][:, :], in1=sg[1][:], op=ALU.mult), r=[pbn[b0 + 1], 'sgD1'], w=['m2'])
                    P.op('dve', lambda oc=oc, js=js: V.tensor_tensor(out=mT[:, oc, js], in0=m1[:], in1=m2[:], op=ALU.add), r=['m1', 'm2'], w=['mT'])
            P.barrier()
        if stop('MRG'):
            return fin()
        with ExitStack() as s2:
            WoB = sb(s2, "WoB", [128, 8, 1024], BF16)
            with ExitStack() as s3:
                wstg = sb(s3, "wstgO", [128, 8, 1024], F32)
                load_weight_bf16((wstg, WoB), wout, 0, 1024, None, 'WoB')
                P.barrier()
            xt = [sb(s2, "xtF%d" % i, [128, 1024], F32) for i in range(2)]
            x1t = [sb(s2, "x1F%d" % i, [128, 1024], F32) for i in range(2)]
            junk = sb(s2, "junkF", [128, 1024], F32)
            ssF = [sb(s2, "ssF%d" % i, [128, 4], F32) for i in range(2)]
            hbF = [sb(s2, "hbF%d" % i, [128, 1024], BF16) for i in range(2)]
            for gi in range(NT):
                i2 = gi % 2
                rows = slice((4 * gi + 3) * 128, (4 * gi + 4) * 128)
                P.dma(xt[i2][:], xp[rows, :], w=['xtF%d' % i2])
                for hf in range(2):
                    b = 2 * i2 + hf
                    for kc in range(8):
                        P.op('pe', lambda b=b, kc=kc, gi=gi, hf=hf: T.matmul(pb[b][:, :], lhsT=mT[:, kc, gi * 128:(gi + 1) * 128], rhs=WoB[:, kc, hf * 512:(hf + 1) * 512],
                                                                             start=(kc == 0), stop=(kc == 7), skip_group_check=True), r=['mT', 'WoB'], w=[pbn[b]])
                    P.op('dve', lambda b=b, hf=hf, i2=i2: V.tensor_tensor(out=x1t[i2][:, hf * 512:(hf + 1) * 512], in0=pb[b][:, :], in1=xt[i2][:, hf * 512:(hf + 1) * 512], op=ALU.add),
                         r=[pbn[b], 'xtF%d' % i2], w=['x1F%d' % i2])
                P.dma(X1_d[gi], x1t[i2][:], r=['x1F%d' % i2], w=['X1_d'])
                tg = 'F%d' % i2
                P.op('act', lambda i2=i2: A.activation(out=junk[:], in_=x1t[i2][:], func=AF.Square, accum_out=ssF[i2][:, 0:1]), r=['x1F%d' % i2], w=['junkF', tg + 'ss'])
                P.op('dve', lambda i2=i2: V.tensor_scalar(out=ssF[i2][:, 1:2], in0=ssF[i2][:, 0:1], scalar1=1.0 / D, scalar2=EPS, op0=ALU.mult, op1=ALU.add), r=[tg + 'ss'], w=[tg + 'ss'])
                P.op('act', lambda i2=i2: A.activation(out=ssF[i2][:, 2:3], in_=ssF[i2][:, 1:2], func=AF.Sqrt), r=[tg + 'ss'], w=[tg + 'ss'])
                P.op('dve', lambda i2=i2: V.reciprocal(out=ssF[i2][:, 3:4], in_=ssF[i2][:, 2:3]), r=[tg + 'ss'], w=[tg + 'ss'])
                P.op('dve', lambda i2=i2: V.tensor_scalar(out=hbF[i2][:], in0=x1t[i2][:], scalar1=ssF[i2][:, 3:4], scalar2=None, op0=ALU.mult), r=['x1F%d' % i2, tg + 'ss'], w=[tg + 'hb'])
                bt_ = 4 + i2
                for kc in range(8):
                    P.op('pe', lambda kc=kc, i2=i2, bt_=bt_: T.transpose(out=pb16(bt_)[:, kc * 128:(kc + 1) * 128], in_=hbF[i2][:, kc * 128:(kc + 1) * 128], identity=identb[:]),
                         r=[tg + 'hb', 'identb'], w=[pbn[bt_]])
                P.op('act', lambda gi=gi, bt_=bt_: A.copy(out=h2T[:, :, gi * 128:(gi + 1) * 128], in_=pb16(bt_).rearrange("p (kc n) -> p kc n", kc=8)), r=[pbn[bt_]], w=['h2T'])
            P.barrier()
        sA.close()
        if stop('F'):
            return fin()
        actT = sb(st, "actT", [128, 22, 2048], BF16)
        with ExitStack() as s2:
            wst = [sb(s2, "wstG%d" % i, [128, 8, 128], F32) for i in range(2)]
            wbf = [sb(s2, "wbfG%d" % i, [128, 8, 128], BF16) for i in range(2)]
            sgl = [sb(s2, "sgG%d" % i, [128, 512], F32) for i in range(2)]
            for jh in range(22):
                load_weight_bf16((wst[0], wbf[0]), wfi, jh * 128, 128, g2s, 'wbfG0')
                load_weight_bf16((wst[1], wbf[1]), wfi, FH + jh * 128, 128, g2s, 'wbfG1')
                for tg_ in range(4):
                    ts_ = slice(tg_ * 512, (tg_ + 1) * 512)
                    b0 = 2 * (tg_ % 4)
                    for q2 in range(2):
                        for kc in range(8):
                            P.op('pe', lambda b0=b0, q2=q2, kc=kc, ts_=ts_: T.matmul(pb[b0 + q2][:, :], lhsT=wbf[q2][:, kc, :], rhs=h2T[:, kc, ts_],
                                                                                   start=(kc == 0), stop=(kc == 7), skip_group_check=True), r=['wbfG%d' % q2, 'h2T'], w=[pbn[b0 + q2]])
                    sgt = sgl[tg_ % 2]; sn = 'sgG%d' % (tg_ % 2)
                    P.op('act', lambda b0=b0, sgt=sgt: A.activation(out=sgt[:], in_=pb[b0][:, :], func=AF.Silu), r=[pbn[b0]], w=[sn])
                    P.op('dve', lambda b0=b0, sgt=sgt, jh=jh, ts_=ts_: V.tensor_tensor(out=actT[:, jh, ts_], in0=pb[b0 + 1][:, :], in1=sgt[:], op=ALU.mult),
                         r=[pbn[b0 + 1], sn], w=['actT'])
            P.barrier()
        with ExitStack() as s2:
            WfoB = sb(s2, "WfoB", [128, 22, 1024], BF16)
            wstg2 = [sb(s2, "wstgFo%d" % i, [128, 1024], F32) for i in range(2)]
            for jh in range(22):
                i2 = jh % 2
                P.dma(wstg2[i2][:], wfo[jh * 128:(jh + 1) * 128, :], w=['wstgFo%d' % i2])
                if i2 == 0:
                    P.op('act', lambda jh=jh, i2=i2: A.copy(out=WfoB[:, jh, :], in_=wstg2[i2][:]), r=['wstgFo%d' % i2], w=['WfoB'])
                else:
                    P.op('dve', lambda jh=jh, i2=i2: V.tensor_copy(out=WfoB[:, jh, :], in_=wstg2[i2][:]), r=['wstgFo%d' % i2], w=['WfoB'])
            gft = sb(s2, "gft", [128, 1024], F32)
            P.dma(gft[:], gfb[:, :], w=['gft'])
            x1r = [sb(s2, "x1r%d" % i, [128, 1024], F32) for i in range(2)]
            x2 = [sb(s2, "x2_%d" % i, [128, 1024], F32) for i in range(2)]
            junkG = sb(s2, "junkG", [128, 1024], F32)
            ssG = [sb(s2, "ssG%d" % i, [128, 4], F32) for i in range(2)]
            ot = [sb(s2, "ot%d" % i, [128, 1024], F32) for i in range(2)]
            for gi in range(NT):
                i2 = gi % 2
                P.dma(x1r[i2][:], X1_d[gi], r=['X1_d'], w=['x1r%d' % i2])
                for hf in range(2):
                    b = 2 * i2 + hf
                    for jh in range(22):
                        P.op('pe', lambda b=b, jh=jh, gi=gi, hf=hf: T.matmul(pb[b][:, :], lhsT=actT[:, jh, gi * 128:(gi + 1) * 128], rhs=WfoB[:, jh, hf * 512:(hf + 1) * 512],
                                                                             start=(jh == 0), stop=(jh == 21), skip_group_check=True), r=['actT', 'WfoB'], w=[pbn[b]])
                    P.op('dve', lambda b=b, hf=hf, i2=i2: V.tensor_tensor(out=x2[i2][:, hf * 512:(hf + 1) * 512], in0=pb[b][:, :], in1=x1r[i2][:, hf * 512:(hf + 1) * 512], op=ALU.add),
                         r=[pbn[b], 'x1r%d' % i2], w=['x2_%d' % i2])
                tg = 'G%d' % i2
                P.op('act', lambda i2=i2: A.activation(out=junkG[:], in_=x2[i2][:], func=AF.Square, accum_out=ssG[i2][:, 0:1]), r=['x2_%d' % i2], w=['junkG', tg + 'ss'])
                P.op('dve', lambda i2=i2: V.tensor_scalar(out=ssG[i2][:, 1:2], in0=ssG[i2][:, 0:1], scalar1=1.0 / D, scalar2=EPS, op0=ALU.mult, op1=ALU.add), r=[tg + 'ss'], w=[tg + 'ss'])
                P.op('act', lambda i2=i2: A.activation(out=ssG[i2][:, 2:3], in_=ssG[i2][:, 1:2], func=AF.Sqrt), r=[tg + 'ss'], w=[tg + 'ss'])
                P.op('dve', lambda i2=i2: V.reciprocal(out=ssG[i2][:, 3:4], in_=ssG[i2][:, 2:3]), r=[tg + 'ss'], w=[tg + 'ss'])
                P.op('dve', lambda i2=i2: V.tensor_scalar(out=ot[i2][:], in0=x2[i2][:], scalar1=ssG[i2][:, 3:4], scalar2=None, op0=ALU.mult), r=['x2_%d' % i2, tg + 'ss'], w=['ot%d' % i2])
                P.op('dve', lambda i2=i2: V.tensor_tensor(out=ot[i2][:], in0=ot[i2][:], in1=gft[:], op=ALU.mult), r=['ot%d' % i2, 'gft'], w=['ot%d' % i2])
                P.dma(out_d[gi * 128:(gi + 1) * 128, :], ot[i2][:], r=['ot%d' % i2], w=['out_d'])
            P.barrier()
    P.barrier()
    return nc


def _host_prep(inputs):
    x = np.asarray(inputs['x'], np.float32)
    w_in = np.asarray(inputs['w_in'], np.float32)[0]
    pts = np.cumsum([1024, 1024, 256, 256, 1024, 64, 16, 1024, 1024])
    wu, wq_, wk, wv, wqi, wki, wwi, wga, wgb = np.split(w_in, pts[:-1], axis=1)
    a_re = np.asarray(inputs['a_re'], np.float32)[0]; a_im = np.asarray(inputs['a_im'], np.float32)[0]
    log_dt = np.asarray(inputs['log_dt'], np.float32)[0]
    b_re = np.asarray(inputs['b_re'], np.float32)[0]; b_im = np.asarray(inputs['b_im'], np.float32)[0]
    c_re = np.asarray(inputs['c_re'], np.float32)[0]; c_im = np.asarray(inputs['c_im'], np.float32)[0]
    d_skip = np.asarray(inputs['d_skip'], np.float32)[0]

    def gT(g):
        return np.ascontiguousarray(np.asarray(g, np.float32).reshape(8, 128).T)

    r = np.arange(128); kc = np.arange(8)
    pp = r // 32; ggr = (r // 16) % 2; cr = r % 16
    AR1 = np.zeros((128, 8, 2, 64), np.float32); AI1 = np.zeros_like(AR1); DT1 = np.zeros_like(AR1)
    BR1 = np.zeros_like(AR1); BI1 = np.zeros_like(AR1)
    for k in range(8):
        for g2 in range(2):
            g = 2 * (4 * k + pp) + g2
            AR1[:, k, g2, :] = a_re[g, :]
            AI1[:, k, g2, :] = a_im[g, :]
            DT1[:, k, g2, :] = log_dt[g][:, None]
            sel = (ggr == g2)
            BR1[sel, k, g2, :] = b_re[g[sel], :, cr[sel]]
            BI1[sel, k, g2, :] = b_im[g[sel], :, cr[sel]]
    gg = np.arange(128) // 64; p_ = np.arange(128) % 64
    gidx = 2 * np.arange(32)[None, :] + gg[:, None]
    ARE = a_re[gidx, p_[:, None]]; AIE = a_im[gidx, p_[:, None]]; DTE = log_dt[gidx]
    CTR = c_re[gidx, :, p_[:, None]]
    CTI = c_im[gidx, :, p_[:, None]]
    BER = np.zeros((128, 32, 2, 16), np.float32); BEI = np.zeros_like(BER)
    for g2 in range(2):
        sel = gg == g2
        BER[sel, :, g2, :] = b_re[gidx[sel], p_[sel][:, None], :]
        BEI[sel, :, g2, :] = b_im[gidx[sel], p_[sel][:, None], :]
    DFM = np.ascontiguousarray(d_skip.reshape(8, 128).T)
    common = dict(
        wu=np.ascontiguousarray(wu),
        wkv=np.ascontiguousarray(np.concatenate([wk, wki, wki, wv], axis=1)),
        wq=np.ascontiguousarray(np.concatenate([wq_, wqi], axis=1)),
        wwi=np.ascontiguousarray(wwi),
        wg=np.ascontiguousarray(np.concatenate([wga, wgb], axis=1)),
        wglu=np.asarray(inputs['w_glu'], np.float32)[0], wba=np.asarray(inputs['w_branch_a'], np.float32)[0],
        wbb=np.asarray(inputs['w_branch_b'], np.float32)[0], wout=np.asarray(inputs['w_out'], np.float32)[0],
        wfi=np.asarray(inputs['w_ffn_in'], np.float32)[0], wfo=np.asarray(inputs['w_ffn_out'], np.float32)[0],
        g1T=gT(inputs['norm1_g'][0]), g2T=gT(inputs['norm2_g'][0]),
        gfb=np.ascontiguousarray(np.broadcast_to(np.asarray(inputs['norm_f_g'], np.float32)[None, :], (128, D))),
        AR1=AR1.reshape(128, 1024), AI1=AI1.reshape(128, 1024), DT1=DT1.reshape(128, 1024),
        BR1=BR1.reshape(128, 1024), BI1=BI1.reshape(128, 1024),
        ARE=np.ascontiguousarray(ARE), AIE=np.ascontiguousarray(AIE), DTE=np.ascontiguousarray(DTE),
        CTR=np.ascontiguousarray(CTR).reshape(128, 512), CTI=np.ascontiguousarray(CTI).reshape(128, 512),
        BER=BER.reshape(128, 1024), BEI=BEI.reshape(128, 1024), DFM=DFM,
        identf=np.eye(128, dtype=np.float32),
    )
    permA = np.zeros((128, 128), np.float32)
    for m in range(32):
        permA[m + 16 if m < 16 else m - 16, m] = 1
    permI = np.zeros((128, 128), np.float32)
    for hb in (0, 64):
        for m in range(16):
            permI[hb + (m + 8 if m < 8 else m - 8), hb + m] = 1
    common['permA'] = permA; common['permI'] = permI

    def tables(pos, kind):
        pos = pos.astype(np.float32)
        cos = np.ones((128, pos.shape[0]), np.float32); sin = np.zeros_like(cos)
        if kind == 'A':
            half = 16
            inv = (np.float32(500000.0) ** (-np.arange(half, dtype=np.float32) / half)).astype(np.float32)
            ang = pos[None, :] * inv[:, None]
            cos[0:16] = np.cos(ang); cos[16:32] = np.cos(ang)
            sin[0:16] = -np.sin(ang); sin[16:32] = np.sin(ang)
        else:
            half = 8
            inv = (np.float32(500000.0) ** (-np.arange(half, dtype=np.float32) / half)).astype(np.float32)
            ang = pos[None, :] * inv[:, None]
            for hb in (0, 64):
                cos[hb:hb + 8] = np.cos(ang); cos[hb + 8:hb + 16] = np.cos(ang)
                sin[hb:hb + 8] = -np.sin(ang); sin[hb + 8:hb + 16] = np.sin(ang)
        return cos, sin

    in_maps = []
    for c in range(8):
        b, r_ = c // 4, c % 4
        pad = (3 - r_) * 128
        xpad = np.zeros((L, D), np.float32)
        xpad[pad:] = x[b, :L - pad]
        pos_all = np.maximum(np.arange(L) - pad, 0)
        own = np.concatenate([np.arange(128) + (4 * i + 3) * 128 for i in range(NT)])
        pos_own = own - pad
        m = dict(common)
        m['xp'] = xpad
        m['cAk'], m['sAk'] = tables(pos_all, 'A')
        m['cIk'], m['sIk'] = tables(pos_all, 'I')
        m['cAq'], m['sAq'] = tables(pos_own, 'A')
        m['cIq'], m['sIq'] = tables(pos_own, 'I')
        q = np.arange(128)[:, None]; kk = np.arange(512)[None, :]
        caus = np.where(((kk < 384) | ((kk - 384) // 64 <= q // 64)), 0.0, NEG).astype(np.float32)
        padm = np.where(kk >= pad, 0.0, NEG).astype(np.float32) * np.ones((128, 1), np.float32)
        m['mfirst'] = np.ascontiguousarray(padm)
        m['mlast'] = np.ascontiguousarray(caus)
        m['mboth'] = np.minimum(padm, caus).astype(np.float32)
        in_maps.append(m)
    return in_maps


def kernel(**inputs):
    in_maps = _host_prep(inputs)
    nc = build_program()
    res = run_bass_kernel_spmd(nc, in_maps, core_ids=list(range(8)))
    out = np.zeros((2, L, D), np.float32)
    for c in range(8):
        b, r_ = c // 4, c % 4
        o = res.results[c]["out"].reshape(NT, 128, D)
        for i in range(NT):
            j = 4 * i + r_
            out[b, j * 128:(j + 1) * 128] = o[i]
    return out
```

```python
import math
from contextlib import ExitStack
import numpy as np
import concourse.bass as bass
import concourse.mybir as mybir
from concourse.bass_utils import run_bass_kernel_spmd

F32 = mybir.dt.float32
BF16 = mybir.dt.bfloat16
AF = mybir.ActivationFunctionType
ALU = mybir.AluOpType

D = 1024
L = 8192
NT = 16
FH = 2816
EPS = 1e-6
NEG = -1e30
NITER = 18
BIS_B = 64.0
NDS = 24


class Prog:
    def __init__(self, nc, es):
        self.nc = nc
        self.E = {'pe': nc.tensor, 'act': nc.scalar, 'dve': nc.vector, 'pool': nc.gpsimd, 'sp': nc.sync}
        self.sem = {k: es.enter_context(nc.semaphore('s_' + k)) for k in ['pe', 'act', 'dve', 'pool']}
        self.cnt = {k: 0 for k in self.sem}
        self.dsem = [es.enter_context(nc.semaphore('d%d' % i)) for i in range(NDS)]
        self.dcnt = [0] * NDS
        self.dnext = 0
        self.seen = {e: {} for e in self.E}
        self.lw = {}
        self.rd = {}

    def _semobj(self, key):
        return self.dsem[key[1]] if isinstance(key, tuple) else self.sem[key]

    def _wait(self, eng, ev):
        key, val = ev
        if self.seen[eng].get(key, 0) >= val:
            return
        self.seen[eng][key] = val
        self.E[eng].wait_ge(self._semobj(key), val)

    def _deps(self, eng, r, w):
        evs = {}
        for x in r:
            if x in self.lw:
                k, v = self.lw[x]
                evs[k] = max(evs.get(k, 0), v)
        for x in w:
            if x in self.lw:
                k, v = self.lw[x]
                evs[k] = max(evs.get(k, 0), v)
            for k, v in self.rd.get(x, {}).items():
                evs[k] = max(evs.get(k, 0), v)
        for k, v in evs.items():
            if eng == 'pe' and k == 'pe':
                continue
            self._wait(eng, (k, v))

    def _record(self, me, r, w):
        k, v = me
        for x in r:
            d = self.rd.setdefault(x, {})
            d[k] = max(d.get(k, 0), v)
        for x in w:
            self.lw[x] = me
            self.rd[x] = {}

    def op(self, eng, fn, r=(), w=()):
        self._deps(eng, r, w)
        ins = fn()
        self.cnt[eng] += 1
        ins.then_inc(self.sem[eng], 1)
        self._record((eng, self.cnt[eng]), r, w)

    def dma(self, out, in_, r=(), w=(), q='sp'):
        self._deps(q, r, w)
        i = self.dnext
        self.dnext = (i + 1) % NDS
        if self.dcnt[i] > 0:
            self._wait(q, (('d', i), self.dcnt[i]))
        self.E[q].dma_start(out=out, in_=in_).then_inc(self.dsem[i], 16)
        self.dcnt[i] += 16
        self._record((('d', i), self.dcnt[i]), r, w)

    def barrier(self):
        for e in ['pe', 'act', 'dve', 'pool', 'sp']:
            for k in self.sem:
                if self.cnt[k] > 0:
                    self._wait(e, (k, self.cnt[k]))
            for i in range(NDS):
                if self.dcnt[i] > 0:
                    self._wait(e, (('d', i), self.dcnt[i]))
        self.lw = {}
        self.rd = {}


def build_program(dbg=None):
    nc = bass.Bass("TRN2", target_bir_lowering=False)

    def din(name, shape, dt=F32):
        return nc.dram_tensor(name, list(shape), dt, kind="ExternalInput").ap()

    def dscr(name, shape, dt):
        return nc.dram_tensor(name, list(shape), dt, kind="Internal").ap()

    xp = din("xp", [L, D])
    wu = din("wu", [D, 1024])
    wkv = din("wkv", [D, 640])
    wq = din("wq", [D, 2048])
    wwi = din("wwi", [D, 16])
    wg = din("wg", [D, 2048])
    wglu = din("wglu", [D, D])
    wba = din("wba", [D, D])
    wbb = din("wbb", [D, D])
    wout = din("wout", [D, D])
    wfi = din("wfi", [D, 2 * FH])
    wfo = din("wfo", [FH, D])
    g1T = din("g1T", [128, 8])
    g2T = din("g2T", [128, 8])
    gfb = din("gfb", [128, D])
    AR1 = din("AR1", [128, 1024]); AI1 = din("AI1", [128, 1024]); DT1 = din("DT1", [128, 1024])
    BR1 = din("BR1", [128, 1024]); BI1 = din("BI1", [128, 1024])
    ARE = din("ARE", [128, 32]); AIE = din("AIE", [128, 32]); DTE = din("DTE", [128, 32])
    CTR = din("CTR", [128, 512]); CTI = din("CTI", [128, 512])
    BER = din("BER", [128, 1024]); BEI = din("BEI", [128, 1024])
    DFM = din("DFM", [128, 8])
    identf_d = din("identf", [128, 128])
    permA_d = din("permA", [128, 128]); permI_d = din("permI", [128, 128])
    cAk = din("cAk", [128, L]); sAk = din("sAk", [128, L])
    cIk = din("cIk", [128, L]); sIk = din("sIk", [128, L])
    cAq = din("cAq", [128, 2048]); sAq = din("sAq", [128, 2048])
    cIq = din("cIq", [128, 2048]); sIq = din("sIq", [128, 2048])
    mfirst = din("mfirst", [128, 512]); mlast = din("mlast", [128, 512]); mboth = din("mboth", [128, 512])
    out_d = nc.dram_tensor("out", [2048, D], F32, kind="ExternalOutput").ap()
    dbg_d = None
    if dbg:
        dbg_d = nc.dram_tensor("dbg", list(dbg[1]), F32, kind="ExternalOutput").ap()

    HT_d = dscr("HT_d", [NT, 128, 1024], BF16)
    U_d = dscr("U_d", [NT, 128, 1024], BF16)
    HS_d = dscr("HS_d", [NT, 128, 1024], BF16)
    Y_d = dscr("Y_d", [NT, 128, 1024], BF16)
    YB_d = dscr("YB_d", [NT, 128, 1024], BF16)
    X1_d = dscr("X1_d", [NT, 128, 1024], F32)

    es = ExitStack()
    P = Prog(nc, es)
    V, A, G, T = nc.vector, nc.scalar, nc.gpsimd, nc.tensor

    def sb(stack, name, shape, dt):
        return stack.enter_context(nc.sbuf_tensor(name, list(shape), dt))

    def fin():
        P.barrier()
        return nc

    def stop(name):
        return bool(dbg) and dbg[0] == name

    identf = sb(es, "identf_s", [128, 128], F32)
    identb = sb(es, "identb", [128, 128], BF16)
    permA = sb(es, "permA_s", [128, 128], BF16)
    permI = sb(es, "permI_s", [128, 128], BF16)
    onesb = sb(es, "onesb", [128, 128], BF16)
    g1s = sb(es, "g1s", [128, 8], F32)
    g2s = sb(es, "g2s", [128, 8], F32)
    ptmp = sb(es, "ptmp", [128, 128], F32)
    P.dma(identf[:], identf_d[:, :], w=['identf'])
    P.op('dve', lambda: V.tensor_copy(out=identb[:], in_=identf[:]), r=['identf'], w=['identb'])
    P.dma(ptmp[:], permA_d[:, :], w=['ptmp'])
    P.op('dve', lambda: V.tensor_copy(out=permA[:], in_=ptmp[:]), r=['ptmp'], w=['permA'])
    P.dma(ptmp[:], permI_d[:, :], w=['ptmp'])
    P.op('dve', lambda: V.tensor_copy(out=permI[:], in_=ptmp[:]), r=['ptmp'], w=['permI'])
    P.op('pool', lambda: G.memset(onesb[:], 1.0), w=['onesb'])
    ident32 = sb(es, "ident32", [128, 128], F32)
    P.op('dve', lambda: V.tensor_scalar(out=ident32[:], in0=identf[:], scalar1=1.0 / 32.0, scalar2=None, op0=ALU.mult), r=['identf'], w=['ident32'])
    P.dma(g1s[:], g1T[:, :], w=['g1s'])
    P.dma(g2s[:], g2T[:, :], w=['g2s'])

    pball = es.enter_context(nc.psum_tensor("pball", [128, 4096], F32))
    pb = [pball[:, i * 512:(i + 1) * 512] for i in range(8)]
    pbn = ['pb%d' % i for i in range(8)]

    def pb16(i):
        return pb[i][:, :].bitcast(BF16)

    def load_weight_bf16(stack_tiles, wdram, c0, ncols, gscale, tag, stag=None):
        stg, dst = stack_tiles
        stag = stag or tag
        for kc in range(8):
            P.dma(stg[:, kc, 0:ncols], wdram[kc * 128:(kc + 1) * 128, c0:c0 + ncols], w=[stag + 's%d' % kc])
            if gscale is None:
                if kc % 2 == 0:
                    P.op('act', lambda kc=kc: A.copy(out=dst[:, kc, 0:ncols], in_=stg[:, kc, 0:ncols]), r=[stag + 's%d' % kc], w=[tag])
                else:
                    P.op('dve', lambda kc=kc: V.tensor_copy(out=dst[:, kc, 0:ncols], in_=stg[:, kc, 0:ncols]), r=[stag + 's%d' % kc], w=[tag])
            else:
                P.op('dve', lambda kc=kc: V.tensor_scalar(out=dst[:, kc, 0:ncols], in0=stg[:, kc, 0:ncols], scalar1=gscale[:, kc:kc + 1], scalar2=None, op0=ALU.mult),
                     r=[stag + 's%d' % kc, 'g1s', 'g2s'], w=[tag])

    def norm_transpose(stack_t, xsrc_rows, hT, col0, tagx, gate_bank, act_scale=False):
        xt, junk, ss, hb = stack_t
        P.dma(xt[:], xsrc_rows, w=[tagx + 'xt'])
        P.op('act', lambda: A.activation(out=junk[:], in_=xt[:], func=AF.Square, accum_out=ss[:, 0:1]),
             r=[tagx + 'xt'], w=[tagx + 'junk', tagx + 'ss'])
        P.op('dve', lambda: V.tensor_scalar(out=ss[:, 1:2], in0=ss[:, 0:1], scalar1=1.0 / D, scalar2=EPS, op0=ALU.mult, op1=ALU.add),
             r=[tagx + 'ss'], w=[tagx + 'ss'])
        P.op('act', lambda: A.activation(out=ss[:, 2:3], in_=ss[:, 1:2], func=AF.Sqrt), r=[tagx + 'ss'], w=[tagx + 'ss'])
        P.op('dve', lambda: V.reciprocal(out=ss[:, 3:4], in_=ss[:, 2:3]), r=[tagx + 'ss'], w=[tagx + 'ss'])
        if act_scale:
            P.op('act', lambda: A.activation(out=hb[:], in_=xt[:], func=AF.Copy, scale=ss[:, 3:4]),
                 r=[tagx + 'xt', tagx + 'ss'], w=[tagx + 'hb'])
        else:
            P.op('dve', lambda: V.tensor_scalar(out=hb[:], in0=xt[:], scalar1=ss[:, 3:4], scalar2=None, op0=ALU.mult),
                 r=[tagx + 'xt', tagx + 'ss'], w=[tagx + 'hb'])
        b = gate_bank
        for kc in range(8):
            P.op('pe', lambda kc=kc: T.transpose(out=pb16(b)[:, kc * 128:(kc + 1) * 128], in_=hb[:, kc * 128:(kc + 1) * 128], identity=identb[:]),
                 r=[tagx + 'hb', 'identb'], w=[pbn[b]])
        return b

    def rope(dst, nrows, perm, ctab, stab, tmp1, tmp2, bank, tags_r, tag_w, ncols):
        P.op('pe', lambda: T.matmul(pb[bank][0:nrows, 0:ncols], lhsT=perm[0:nrows, 0:nrows], rhs=dst, start=True, stop=True, skip_group_check=True),
             r=[tag_w, 'permA', 'permI'], w=[pbn[bank]])
        P.op('dve', lambda: V.tensor_tensor(out=tmp1[0:nrows, 0:ncols], in0=dst, in1=ctab, op=ALU.mult), r=[tag_w] + tags_r, w=['ropet1'])
        P.op('dve', lambda: V.tensor_tensor(out=tmp2[0:nrows, 0:ncols], in0=pb[bank][0:nrows, 0:ncols], in1=stab, op=ALU.mult),
             r=[pbn[bank]] + tags_r, w=['ropet2'])
        P.op('dve', lambda: V.tensor_tensor(out=dst, in0=tmp1[0:nrows, 0:ncols], in1=tmp2[0:nrows, 0:ncols], op=ALU.add),
             r=['ropet1', 'ropet2'], w=[tag_w])

    sq = ExitStack()
    QP = sb(sq, "QP", [128, 9, 2, 32], F32)
    FP = sb(sq, "FP", [128, 8, 2, 32], F32)
    s5 = ExitStack()
    W1 = sb(s5, "W1", [128, 8, 2, 8, 128], BF16)
    cosT = sb(s5, "cosT", [128, 32, 64], F32)
    sinT = sb(s5, "sinT", [128, 32, 64], F32)
    R8z = sb(s5, "R8z", [128, 32, 64], F32)
    A8r = sb(s5, "A8r", [128, 32], F32)
    A8i = sb(s5, "A8i", [128, 32], F32)

    def sincos_lb(stack, n, ar, ai, dtl, pref):
        t = {}
        for nm in ['dt', 'ang', 'ex', 'mag', 'nn', 'red', 'sn', 'cs', 'lbr', 'lbi', 'fr', 'fi', 'u1', 'u2', 'u3']:
            t[nm] = sb(stack, pref + nm, [128, n], F32)
        R = [pref]
        def vv(fn):
            P.op('dve', fn, r=R, w=R)
        def aa(fn):
            P.op('act', fn, r=R, w=R)
        aa(lambda: A.activation(out=t['dt'][:], in_=dtl, func=AF.Exp))
        vv(lambda: V.tensor_tensor(out=t['ang'][:], in0=ai, in1=t['dt'][:], op=ALU.mult))
        vv(lambda: V.tensor_tensor(out=t['ex'][:], in0=ar, in1=t['dt'][:], op=ALU.mult))
        aa(lambda: A.activation(out=t['mag'][:], in_=t['ex'][:], func=AF.Exp))
        C1 = float(np.float32(2 * math.pi))
        C2 = float(2 * math.pi - np.float64(np.float32(2 * math.pi)))
        for which, off in (('sn', 0.0), ('cs', math.pi / 2)):
            vv(lambda off=off: V.tensor_scalar(out=t['u1'][:], in0=t['ang'][:], scalar1=off, scalar2=None, op0=ALU.add))
            vv(lambda: V.tensor_scalar(out=t['nn'][:], in0=t['u1'][:], scalar1=math.pi, scalar2=None, op0=ALU.is_gt))
            for kk in (3, 5, 7):
                vv(lambda kk=kk: V.tensor_scalar(out=t['u2'][:], in0=t['u1'][:], scalar1=kk * math.pi, scalar2=None, op0=ALU.is_gt))
                vv(lambda: V.tensor_tensor(out=t['nn'][:], in0=t['nn'][:], in1=t['u2'][:], op=ALU.add))
            vv(lambda: V.scalar_tensor_tensor(out=t['red'][:], in0=t['nn'][:], scalar=-C1, in1=t['u1'][:], op0=ALU.mult, op1=ALU.add))
            vv(lambda: V.scalar_tensor_tensor(out=t['red'][:], in0=t['nn'][:], scalar=-C2, in1=t['red'][:], op0=ALU.mult, op1=ALU.add))
            vv(lambda: V.tensor_scalar(out=t['red'][:], in0=t['red'][:], scalar1=math.pi, scalar2=-math.pi, op0=ALU.min, op1=ALU.max))
            aa(lambda which=which: A.activation(out=t[which][:], in_=t['red'][:], func=AF.Sin))
        vv(lambda: V.tensor_tensor(out=t['lbr'][:], in0=t['mag'][:], in1=t['cs'][:], op=ALU.mult))
        vv(lambda: V.tensor_tensor(out=t['lbi'][:], in0=t['mag'][:], in1=t['sn'][:], op=ALU.mult))
        vv(lambda: V.tensor_tensor(out=t['u1'][:], in0=ar, in1=ar, op=ALU.mult))
        vv(lambda: V.tensor_tensor(out=t['u2'][:], in0=ai, in1=ai, op=ALU.mult))
        vv(lambda: V.tensor_tensor(out=t['u1'][:], in0=t['u1'][:], in1=t['u2'][:], op=ALU.add))
        vv(lambda: V.reciprocal(out=t['u3'][:], in_=t['u1'][:]))
        vv(lambda: V.tensor_scalar(out=t['u1'][:], in0=t['lbr'][:], scalar1=-1.0, scalar2=None, op0=ALU.add))
        vv(lambda: V.tensor_tensor(out=t['fr'][:], in0=t['u1'][:], in1=ar, op=ALU.mult))
        vv(lambda: V.tensor_tensor(out=t['u2'][:], in0=t['lbi'][:], in1=ai, op=ALU.mult))
        vv(lambda: V.tensor_tensor(out=t['fr'][:], in0=t['fr'][:], in1=t['u2'][:], op=ALU.add))
        vv(lambda: V.tensor_tensor(out=t['fr'][:], in0=t['fr'][:], in1=t['u3'][:], op=ALU.mult))
        vv(lambda: V.tensor_tensor(out=t['fi'][:], in0=t['lbi'][:], in1=ar, op=ALU.mult))
        vv(lambda: V.tensor_tensor(out=t['u2'][:], in0=t['u1'][:], in1=ai, op=ALU.mult))
        vv(lambda: V.tensor_tensor(out=t['fi'][:], in0=t['fi'][:], in1=t['u2'][:], op=ALU.subtract))
        vv(lambda: V.tensor_tensor(out=t['fi'][:], in0=t['fi'][:], in1=t['u3'][:], op=ALU.mult))
        return t

    def cmul(outr, outi, ar_, ai_, br_, bi_, t1, t2, R):
        P.op('dve', lambda: V.tensor_tensor(out=t1, in0=ar_, in1=br_, op=ALU.mult), r=R, w=R)
        P.op('dve', lambda: V.tensor_tensor(out=t2, in0=ai_, in1=bi_, op=ALU.mult), r=R, w=R)
        P.op('dve', lambda: V.tensor_tensor(out=outr, in0=t1, in1=t2, op=ALU.subtract), r=R, w=R)
        P.op('dve', lambda: V.tensor_tensor(out=t1, in0=ar_, in1=bi_, op=ALU.mult), r=R, w=R)
        P.op('dve', lambda: V.tensor_tensor(out=t2, in0=ai_, in1=br_, op=ALU.mult), r=R, w=R)
        P.op('dve', lambda: V.tensor_tensor(out=outi, in0=t1, in1=t2, op=ALU.add), r=R, w=R)

    with ExitStack() as st:
        arl = sb(st, "arl", [128, 1024], F32); ail = sb(st, "ail", [128, 1024], F32); dtl = sb(st, "dtl", [128, 1024], F32)
        brl = sb(st, "brl", [128, 1024], F32); bil = sb(st, "bil", [128, 1024], F32)
        for tl, src in ((arl, AR1), (ail, AI1), (dtl, DT1), (brl, BR1), (bil, BI1)):
            P.dma(tl[:], src[:, :], w=['L1'])
        tt = sincos_lb(st, 1024, arl[:], ail[:], dtl[:], 'L1')
        pr = [sb(st, "pr%d" % i, [128, 1024], F32) for i in range(2)]
        pi = [sb(st, "pi%d" % i, [128, 1024], F32) for i in range(2)]
        c1 = sb(st, "c1", [128, 1024], F32); c2 = sb(st, "c2", [128, 1024], F32)
        R = ['L1']
        cur_r, cur_i = tt['fr'], tt['fi']
        for k in range(8):
            s_ = 7 - k
            v3 = lambda ap: ap.rearrange("p (kc n) -> p kc n", kc=8)
            P.op('dve', lambda: V.tensor_tensor(out=c1[:], in0=cur_r[:], in1=brl[:], op=ALU.mult), r=R, w=R)
            P.op('dve', lambda: V.tensor_tensor(out=c2[:], in0=cur_i[:], in1=bil[:], op=ALU.mult), r=R, w=R)
            P.op('dve', lambda s_=s_: V.tensor_tensor(out=W1[:, :, 0, s_, :], in0=v3(c1[:]), in1=v3(c2[:]), op=ALU.subtract), r=R, w=R + ['W1'])
            P.op('dve', lambda: V.tensor_tensor(out=c1[:], in0=cur_r[:], in1=bil[:], op=ALU.mult), r=R, w=R)
            P.op('dve', lambda: V.tensor_tensor(out=c2[:], in0=cur_i[:], in1=brl[:], op=ALU.mult), r=R, w=R)
            P.op('dve', lambda s_=s_: V.tensor_tensor(out=W1[:, :, 1, s_, :], in0=v3(c1[:]), in1=v3(c2[:]), op=ALU.add), r=R, w=R + ['W1'])
            if k < 7:
                nr, ni = pr[k % 2], pi[k % 2]
                cmul(nr[:], ni[:], cur_r[:], cur_i[:], tt['lbr'][:], tt['lbi'][:], c1[:], c2[:], R)
                cur_r, cur_i = nr, ni
    P.barrier()
    if stop('c1'):
        return fin()
    with ExitStack() as st:
        are = sb(st, "are", [128, 32], F32); aie = sb(st, "aie", [128, 32], F32); dte = sb(st, "dte", [128, 32], F32)
        for tl, src in ((are, ARE), (aie, AIE), (dte, DTE)):
            P.dma(tl[:], src[:, :], w=['E'])
        te = sincos_lb(st, 32, are[:], aie[:], dte[:], 'E')
        e1 = sb(st, "e1", [128, 32], F32); e2 = sb(st, "e2", [128, 32], F32)
        R = ['E']
        P.op('dve', lambda: V.memset(QP[:, 0, 0, :], 1.0), r=R, w=R)
        P.op('dve', lambda: V.memset(QP[:, 0, 1, :], 0.0), r=R, w=R)
        for k in range(1, 9):
            cmul(QP[:, k, 0, :], QP[:, k, 1, :], QP[:, k - 1, 0, :], QP[:, k - 1, 1, :], te['lbr'][:], te['lbi'][:], e1[:], e2[:], R)
        P.op('dve', lambda: V.tensor_copy(out=FP[:, 0, 0, :], in_=te['fr'][:]), r=R, w=R)
        P.op('dve', lambda: V.tensor_copy(out=FP[:, 0, 1, :], in_=te['fi'][:]), r=R, w=R)
        for k in range(1, 8):
            cmul(FP[:, k, 0, :], FP[:, k, 1, :], FP[:, k - 1, 0, :], FP[:, k - 1, 1, :], te['lbr'][:], te['lbi'][:], e1[:], e2[:], R)
        P.op('dve', lambda: V.tensor_copy(out=A8r[:], in_=QP[:, 8, 0, :]), r=R, w=R)
        P.op('dve', lambda: V.tensor_copy(out=A8i[:], in_=QP[:, 8, 1, :]), r=R, w=R)
        m2 = sb(st, "m2", [128, 32], F32); m4 = sb(st, "m4", [128, 32], F32); m8 = sb(st, "m8", [128, 32], F32)
        P.op('dve', lambda: V.tensor_tensor(out=m2[:], in0=te['mag'][:], in1=te['mag'][:], op=ALU.mult), r=R, w=R)
        P.op('dve', lambda: V.tensor_tensor(out=m4[:], in0=m2[:], in1=m2[:], op=ALU.mult), r=R, w=R)
        P.op('dve', lambda: V.tensor_tensor(out=m8[:], in0=m4[:], in1=m4[:], op=ALU.mult), r=R, w=R)
        wr = [sb(st, "wr%d" % i, [128, 32], F32) for i in range(4)]
        wi_ = [sb(st, "wi%d" % i, [128, 32], F32) for i in range(4)]
        P.op('dve', lambda: V.tensor_copy(out=wr[0][:], in_=te['cs'][:]), r=R, w=R)
        P.op('dve', lambda: V.tensor_copy(out=wi_[0][:], in_=te['sn'][:]), r=R, w=R)
        for k in range(1, 4):
            cmul(wr[k][:], wi_[k][:], wr[k - 1][:], wi_[k - 1][:], wr[k - 1][:], wi_[k - 1][:], e1[:], e2[:], R)
        P.op('dve', lambda: V.memset(cosT[:, :, 0:1], 1.0), r=R, w=R)
        P.op('dve', lambda: V.memset(sinT[:, :, 0:1], 0.0), r=R, w=R)
        sr = [sb(st, "sr%d" % i, [128, 32], F32) for i in range(2)]
        si = [sb(st, "si%d" % i, [128, 32], F32) for i in range(2)]
        big1 = sb(st, "big1", [128, 32, 32], F32); big2 = sb(st, "big2", [128, 32, 32], F32)
        stepr, stepi = wr[3], wi_[3]
        for lv in range(6):
            n = 1 << lv
            bc = lambda ap, n=n: ap.unsqueeze(2).to_broadcast([128, 32, n])
            P.op('dve', lambda: V.tensor_tensor(out=big1[:, :, 0:n], in0=cosT[:, :, 0:n], in1=bc(stepr[:]), op=ALU.mult), r=R, w=R)
            P.op('dve', lambda: V.tensor_tensor(out=big2[:, :, 0:n], in0=sinT[:, :, 0:n], in1=bc(stepi[:]), op=ALU.mult), r=R, w=R)
            P.op('dve', lambda: V.tensor_tensor(out=cosT[:, :, n:2 * n], in0=big1[:, :, 0:n], in1=big2[:, :, 0:n], op=ALU.subtract), r=R, w=R)
            P.op('dve', lambda: V.tensor_tensor(out=big1[:, :, 0:n], in0=cosT[:, :, 0:n], in1=bc(stepi[:]), op=ALU.mult), r=R, w=R)
            P.op('dve', lambda: V.tensor_tensor(out=big2[:, :, 0:n], in0=sinT[:, :, 0:n], in1=bc(stepr[:]), op=ALU.mult), r=R, w=R)
            P.op('dve', lambda: V.tensor_tensor(out=sinT[:, :, n:2 * n], in0=big1[:, :, 0:n], in1=big2[:, :, 0:n], op=ALU.add), r=R, w=R)
            if lv < 5:
                nr, ni = sr[lv % 2], si[lv % 2]
                cmul(nr[:], ni[:], stepr[:], stepi[:], stepr[:], stepi[:], e1[:], e2[:], R)
                stepr, stepi = nr, ni
        P.op('dve', lambda: V.tensor_copy(out=R8z[:], in_=m8[:].unsqueeze(2).to_broadcast([128, 32, 64])), r=R, w=R)
        P.op('dve', lambda: V.memset(R8z[:, :, 0:1], 0.0), r=R, w=R)
    P.barrier()
    if stop('c2'):
        return fin()

    with ExitStack() as st:
        WuB = sb(st, "WuB", [128, 8, 1024], BF16)
        with ExitStack() as s2:
            wstg = sb(s2, "wstgA", [128, 8, 1024], F32)
            load_weight_bf16((wstg, WuB), wu, 0, 1024, g1s, 'WuB')
            P.barrier()
            if stop('c30'):
                return fin()
        xt = [sb(st, "xtA%d" % i, [128, 1024], F32) for i in range(2)]
        junk = [sb(st, "junkA%d" % i, [128, 1024], F32) for i in range(2)]
        ssA = [sb(st, "ssA%d" % i, [128, 4], F32) for i in range(2)]
        hb = [sb(st, "hbA%d" % i, [128, 1024], BF16) for i in range(2)]
        hT = [sb(st, "hTA%d" % i, [128, 8, 512], BF16) for i in range(2)]
        uT = [sb(st, "uTA%d" % i, [128, 8, 512], BF16) for i in range(2)]
        S0b = [sb(st, "S0_%d" % i, [128, 32, 2, 64], F32) for i in range(2)]
        Z = sb(st, "Z", [128, 32, 2, 64], F32)
        ta = sb(st, "ta", [128, 32, 64], F32)
        tb = sb(st, "tb", [128, 32, 64], F32)
        Hin = sb(st, "Hin", [128, 2, 32], F32)
        cr_ = sb(st, "cr_", [128, 2, 32], F32)
        cq1 = sb(st, "cq1", [128, 32], F32); cq2 = sb(st, "cq2", [128, 32], F32)
        Hb16 = [sb(st, "Hb16_%d" % i, [128, 32, 2, 16], BF16) for i in range(2)]
        P.op('dve', lambda: V.memset(Hin[:], 0.0), w=['Hin'])
        bank_rot = [0]

        def nb():
            b = bank_rot[0]
            bank_rot[0] = (b + 1) % 8
            return b

        def stageA(t):
            hTt = hT[t % 2]; uTt = uT[t % 2]
            hn = 'hT%d' % (t % 2); un = 'uT%d' % (t % 2)
            S0 = S0b[t % 2]; s0n = 'S0_%d' % (t % 2)
            for blk in range(4):
                i2 = blk % 2
                b = nb()
                norm_transpose((xt[i2], junk[i2], ssA[i2], hb[i2]), xp[(t * 4 + blk) * 128:(t * 4 + blk + 1) * 128, :], hTt, blk * 128, 'A%d' % i2, b, act_scale=True)
                P.op('act', lambda b=b, blk=blk: A.copy(out=hTt[:, :, blk * 128:(blk + 1) * 128], in_=pb16(b).rearrange("p (kc n) -> p kc n", kc=8)),
                     r=[pbn[b]], w=[hn])
            for oc in range(8):
                b = nb()
                for kc in range(8):
                    P.op('pe', lambda b=b, oc=oc, kc=kc: T.matmul(pb[b][:, :], lhsT=WuB[:, kc, oc * 128:(oc + 1) * 128], rhs=hTt[:, kc, :],
                                                                   start=(kc == 0), stop=(kc == 7), skip_group_check=True),
                         r=['WuB', hn], w=[pbn[b]])
                P.op('act', lambda b=b, oc=oc: A.copy(out=uTt[:, oc, :], in_=pb[b][:, :]), r=[pbn[b]], w=[un])
            P.dma(HT_d[t].rearrange("p (kc n) -> p kc n", kc=8), hTt[:, :, 384:512], r=[hn], w=['HT_d'])
            P.dma(U_d[t].rearrange("p (kc n) -> p kc n", kc=8), uTt[:, :, 384:512], r=[un], w=['U_d'])
            for kc in range(8):
                for comp in range(2):
                    c0 = comp * 64
                    for s_ in range(8):
                        for pp in range(4):
                            b = 4 * (kc % 2) + pp
                            P.op('pe', lambda b=b, kc=kc, pp=pp, comp=comp, s_=s_, c0=c0: T.matmul(
                                pb[b][:, c0:c0 + 64], lhsT=W1[32 * pp:32 * pp + 32, kc, comp, s_, :], rhs=uTt[32 * pp:32 * pp + 32, kc, s_::8],
                                start=(s_ == 0), stop=(s_ == 7), skip_group_check=True, tile_position=(32 * pp, 0)),
                                r=['W1', un], w=[pbn[b]])
                for pp in range(4):
                    b = 4 * (kc % 2) + pp
                    S0v = S0[:, 4 * kc + pp, :, :].rearrange("p c m -> p (c m)")
                    P.op('act', lambda b=b, S0v=S0v: A.copy(out=S0v, in_=pb[b][:, 0:128]), r=[pbn[b]], w=[s0n])

        def stageB(t):
            S0 = S0b[t % 2]; s0n = 'S0_%d' % (t % 2)
            Hh = S0
            Sr, Si = S0[:, :, 0, :], S0[:, :, 1, :]
            Zr, Zi = Z[:, :, 0, :], Z[:, :, 1, :]
            Rr = [s0n, 'Z', 'ta', 'tb']
            P.op('dve', lambda: V.tensor_tensor(out=ta[:], in0=cosT[:], in1=Sr, op=ALU.mult), r=Rr, w=['ta'])
            P.op('pool', lambda: G.tensor_tensor(out=tb[:], in0=sinT[:], in1=Si, op=ALU.mult), r=Rr, w=['tb'])
            P.op('dve', lambda: V.tensor_tensor(out=Zr, in0=ta[:], in1=tb[:], op=ALU.add), r=Rr, w=['Z'])
            P.op('dve', lambda: V.tensor_tensor(out=ta[:], in0=cosT[:], in1=Si, op=ALU.mult), r=Rr, w=['ta'])
            P.op('pool', lambda: G.tensor_tensor(out=tb[:], in0=sinT[:], in1=Sr, op=ALU.mult), r=Rr, w=['tb'])
            P.op('dve', lambda: V.tensor_tensor(out=Zi, in0=ta[:], in1=tb[:], op=ALU.subtract), r=Rr, w=['Z'])
            Rc = ['Hin', 'cr_', 'cq']
            P.op('dve', lambda: V.tensor_tensor(out=cq1[:], in0=A8r[:], in1=Hin[:, 0, :], op=ALU.mult), r=Rc, w=Rc)
            P.op('dve', lambda: V.tensor_tensor(out=cq2[:], in0=A8i[:], in1=Hin[:, 1, :], op=ALU.mult), r=Rc, w=Rc)
            P.op('dve', lambda: V.tensor_tensor(out=cr_[:, 0, :], in0=cq1[:], in1=cq2[:], op=ALU.subtract), r=Rc, w=Rc)
            P.op('dve', lambda: V.tensor_tensor(out=cq1[:], in0=A8r[:], in1=Hin[:, 1, :], op=ALU.mult), r=Rc, w=Rc)
            P.op('dve', lambda: V.tensor_tensor(out=cq2[:], in0=A8i[:], in1=Hin[:, 0, :], op=ALU.mult), r=Rc, w=Rc)
            P.op('dve', lambda: V.tensor_tensor(out=cr_[:, 1, :], in0=cq1[:], in1=cq2[:], op=ALU.add), r=Rc, w=Rc)
            P.op('dve', lambda: V.tensor_tensor(out=Z[:, :, 0, 0], in0=Z[:, :, 0, 0], in1=cr_[:, 0, :], op=ALU.add), r=Rc + ['Z'], w=['Z'])
            P.op('dve', lambda: V.tensor_tensor(out=Z[:, :, 1, 0], in0=Z[:, :, 1, 0], in1=cr_[:, 1, :], op=ALU.add), r=Rc + ['Z'], w=['Z'])
            fl = lambda ap: ap.rearrange("p a m -> p (a m)")
            P.op('dve', lambda: V.tensor_copy(out=ta[:], in_=Zr), r=['Z'], w=['ta'])
            P.op('pool', lambda: G.tensor_copy(out=tb[:], in_=Zi), r=['Z'], w=['tb'])
            P.op('dve', lambda: V.tensor_tensor_scan(out=fl(ta[:]), data0=fl(R8z[:]), data1=fl(ta[:]), initial=0.0,
                                                    op0=ALU.mult, op1=ALU.add), r=['ta'], w=['ta'])
            P.op('dve', lambda: V.tensor_tensor_scan(out=fl(tb[:]), data0=fl(R8z[:]), data1=fl(tb[:]), initial=0.0,
                                                    op0=ALU.mult, op1=ALU.add), r=['tb'], w=['tb'])
            Rh = ['ta', 'tb', 'Z', s0n]
            P.op('dve', lambda: V.tensor_tensor(out=Zr, in0=cosT[:], in1=ta[:], op=ALU.mult), r=Rh, w=['Z'])
            P.op('pool', lambda: G.tensor_tensor(out=Zi, in0=sinT[:], in1=tb[:], op=ALU.mult), r=Rh, w=['Z'])
            P.op('dve', lambda: V.tensor_tensor(out=Hh[:, :, 0, :], in0=Zr, in1=Zi, op=ALU.subtract), r=Rh, w=[s0n])
            P.op('dve', lambda: V.tensor_tensor(out=Zr, in0=cosT[:], in1=tb[:], op=ALU.mult), r=Rh, w=['Z'])
            P.op('pool', lambda: G.tensor_tensor(out=Zi, in0=sinT[:], in1=ta[:], op=ALU.mult), r=Rh, w=['Z'])
            P.op('dve', lambda: V.tensor_tensor(out=Hh[:, :, 1, :], in0=Zr, in1=Zi, op=ALU.add), r=Rh, w=[s0n])
            P.op('dve', lambda: V.tensor_copy(out=Hin[:, 0, :], in_=Hh[:, :, 0, 63]), r=[s0n], w=['Hin'])
            P.op('dve', lambda: V.tensor_copy(out=Hin[:, 1, :], in_=Hh[:, :, 1, 63]), r=[s0n], w=['Hin'])
            hbt = Hb16[t % 2]
            P.op('dve', lambda hbt=hbt: V.tensor_copy(out=hbt[:], in_=Hh[:, :, :, 47:63]), r=[s0n], w=['Hb16_%d' % (t % 2)])
            P.dma(HS_d[t].rearrange("p (a c m) -> p a c m", a=32, c=2), hbt[:], r=['Hb16_%d' % (t % 2)], w=['HS_d'])

        stageA(0)
        for t in range(NT):
            if t + 1 < NT:
                stageA(t + 1)
            stageB(t)
    P.barrier()
    if stop('c4'):
        return fin()
    s5.close()

    with ExitStack() as st:
        CC = sb(st, "CC", [128, 32, 2, 256], BF16)
        KN = sb(st, "KN", [128, 8, 2, 8, 128], BF16)
        dfm = sb(st, "dfm", [128, 8], F32)
        P.dma(dfm[:], DFM[:, :], w=['dfm'])
        with ExitStack() as s2:
            ctr = sb(s2, "ctr", [128, 32, 16], F32); cti = sb(s2, "cti", [128, 32, 16], F32)
            ber = sb(s2, "ber", [128, 32, 32], F32); bei = sb(s2, "bei", [128, 32, 32], F32)
            P.dma(ctr[:].rearrange("p a c -> p (a c)"), CTR[:, :], w=['ctr'])
            P.dma(cti[:].rearrange("p a c -> p (a c)"), CTI[:, :], w=['cti'])
            P.dma(ber[:].rearrange("p a c -> p (a c)"), BER[:, :], w=['ber'])
            P.dma(bei[:].rearrange("p a c -> p (a c)"), BEI[:, :], w=['bei'])
            ctrb = sb(s2, "ctrb", [128, 32, 16], BF16); nctib = sb(s2, "nctib", [128, 32, 16], BF16)
            P.op('dve', lambda: V.tensor_copy(out=ctrb[:], in_=ctr[:]), r=['ctr'], w=['ctrb'])
            P.op('dve', lambda: V.tensor_scalar(out=nctib[:], in0=cti[:], scalar1=-1.0, scalar2=None, op0=ALU.mult), r=['cti'], w=['nctib'])
            k1 = sb(s2, "k1", [128, 32, 32], F32); k2 = sb(s2, "k2", [128, 32, 32], F32)
            xre = sb(s2, "xre", [128, 32, 32], BF16); xim = sb(s2, "xim", [128, 32, 32], BF16)
            R = ['cc']
            b16 = lambda ap: ap.unsqueeze(2).to_broadcast([128, 32, 16])
            b32 = lambda ap: ap.unsqueeze(2).to_broadcast([128, 32, 32])
            P.op('pool', lambda: G.memset(CC[:].rearrange("p a b c -> p (a b c)"), 0.0), w=['CC'])
            for s_ in range(8):
                qr, qi = QP[:, s_ + 1, 0, :], QP[:, s_ + 1, 1, :]
                P.op('dve', lambda: V.tensor_tensor(out=k1[:, :, 0:16], in0=ctr[:], in1=b16(qr), op=ALU.mult), r=R + ['ctr'], w=R)
                P.op('dve', lambda: V.tensor_tensor(out=k2[:, :, 0:16], in0=cti[:], in1=b16(qi), op=ALU.mult), r=R + ['cti'], w=R)
                for gg in range(2):
                    rs = slice(64 * gg, 64 * gg + 64)
                    P.op('dve', lambda s_=s_, gg=gg, rs=rs: V.tensor_tensor(out=CC[rs, :, 0, gg * 128 + s_ * 16:gg * 128 + (s_ + 1) * 16],
                                                                         in0=k1[rs, :, 0:16], in1=k2[rs, :, 0:16], op=ALU.subtract), r=R, w=R + ['CC'])
                P.op('dve', lambda: V.tensor_tensor(out=k1[:, :, 0:16], in0=ctr[:], in1=b16(qi), op=ALU.mult), r=R, w=R)
                P.op('dve', lambda: V.tensor_tensor(out=k2[:, :, 0:16], in0=cti[:], in1=b16(qr), op=ALU.mult), r=R, w=R)
                P.op('dve', lambda: V.tensor_tensor(out=k1[:, :, 0:16], in0=k1[:, :, 0:16], in1=k2[:, :, 0:16], op=ALU.add), r=R, w=R)
                for gg in range(2):
                    rs = slice(64 * gg, 64 * gg + 64)
                    P.op('dve', lambda s_=s_, gg=gg, rs=rs: V.tensor_scalar(out=CC[rs, :, 1, gg * 128 + s_ * 16:gg * 128 + (s_ + 1) * 16],
                                                                         in0=k1[rs, :, 0:16], scalar1=-1.0, scalar2=None, op0=ALU.mult), r=R, w=R + ['CC'])
            for tau in range(8):
                fr_, fi_ = FP[:, tau, 0, :], FP[:, tau, 1, :]
                P.op('dve', lambda: V.tensor_tensor(out=k1[:], in0=ber[:], in1=b32(fr_), op=ALU.mult), r=R + ['ber', 'xre', 'xim'], w=R)
                P.op('dve', lambda: V.tensor_tensor(out=k2[:], in0=bei[:], in1=b32(fi_), op=ALU.mult), r=R + ['bei'], w=R)
                P.op('dve', lambda: V.tensor_tensor(out=xre[:], in0=k1[:], in1=k2[:], op=ALU.subtract), r=R, w=R + ['xre'])
                P.op('dve', lambda: V.tensor_tensor(out=k1[:], in0=bei[:], in1=b32(fr_), op=ALU.mult), r=R, w=R)
                P.op('dve', lambda: V.tensor_tensor(out=k2[:], in0=ber[:], in1=b32(fi_), op=ALU.mult), r=R, w=R)
                P.op('dve', lambda: V.tensor_tensor(out=xim[:], in0=k1[:], in1=k2[:], op=ALU.add), r=R, w=R + ['xim'])
                for gp in range(32):
                    kc, pp = gp // 4, gp % 4
                    for gg in range(2):
                        bnk = 2 * gg + tau // 4
                        c0 = (tau % 4) * 128 + kc * 16
                        rs = slice(64 * gg, 64 * gg + 64)
                        P.op('pe', lambda bnk=bnk, gp=gp, gg=gg, pp=pp, c0=c0, rs=rs: T.matmul(
                            pb[bnk][32 * pp:32 * pp + 32, c0:c0 + 16], lhsT=xre[rs, gp, :], rhs=ctrb[rs, gp, :], start=True, stop=False,
                            skip_group_check=True, tile_position=(64 * gg, 32 * pp)), r=['xre', 'ctrb'], w=[pbn[bnk]])
                        P.op('pe', lambda bnk=bnk, gp=gp, gg=gg, pp=pp, c0=c0, rs=rs: T.matmul(
                            pb[bnk][32 * pp:32 * pp + 32, c0:c0 + 16], lhsT=xim[rs, gp, :], rhs=nctib[rs, gp, :], start=False, stop=True,
                            skip_group_check=True, tile_position=(64 * gg, 32 * pp)), r=['xim', 'nctib'], w=[pbn[bnk]])
            P.op('pool', lambda: G.memset(KN[:].rearrange("p a b c d -> p (a b c d)"), 0.0), w=['KN'])
            for tau in range(8):
                for gg in range(2):
                    bnk = 2 * gg + tau // 4
                    src = pb[bnk][:, (tau % 4) * 128:(tau % 4) * 128 + 128].rearrange("p (a c) -> p a c", a=8)
                    for sp_ in range(8 - tau):
                        s_ = sp_ + tau
                        P.op('dve', lambda src=src, sp_=sp_, s_=s_, gg=gg: V.tensor_copy(out=KN[:, :, gg, sp_, s_ * 16:(s_ + 1) * 16], in_=src),
                             r=[pbn[bnk]], w=['KN'])
        P.barrier()
        if stop('c5'):
            return fin()
        Hb_all = sb(st, "Hb_all", [128, 32, 2, 256], BF16)
        uo_all = sb(st, "uo_all", [128, 8, 2048], BF16)
        hstg = [sb(st, "hstg%d" % i, [128, 32, 2, 16], BF16) for i in range(2)]
        for t in range(NT):
            hs = hstg[t % 2]; hsn = 'hstg%d' % (t % 2)
            P.dma(hs[:], HS_d[t].rearrange("p (a c m) -> p a c m", a=32, c=2), r=['HS_d'], w=[hsn])
            if t % 2 == 0:
                P.op('act', lambda hs=hs, t=t: A.copy(out=Hb_all[:, :, :, t * 16:(t + 1) * 16], in_=hs[:]), r=[hsn], w=['Hb_all'])
            else:
                P.op('dve', lambda hs=hs, t=t: V.tensor_copy(out=Hb_all[:, :, :, t * 16:(t + 1) * 16], in_=hs[:]), r=[hsn], w=['Hb_all'])
            P.dma(uo_all[:, :, t * 128:(t + 1) * 128], U_d[t].rearrange("p (kc n) -> p kc n", kc=8), r=['U_d'], w=['uo_all'])
        nsb = [sb(st, "nsb%d" % i, [128, 256], F32) for i in range(2)]
        Yg = [sb(st, "Yg%d" % i, [128, 256], BF16) for i in range(2)]
        ytm = [sb(st, "ytm%d" % i, [128, 2, 8, 8, 16], BF16) for i in range(2)]
        ypre = [sb(st, "ypre0", [128, 2048], F32)] * 2
        g1_ = sb(st, "g1_", [128, 2048], F32); g2_ = g1_
        yg = [sb(st, "yg%d" % i, [128, 2048], BF16) for i in range(2)]
        Yd_v = Y_d.rearrange("t p (kc n) -> p t kc n", kc=8)
        for kc in range(8):
            yt = ytm[kc % 2]; ytn = 'ytm%d' % (kc % 2)
            for gl in range(8):
                gp, gg, pp = 4 * kc + gl // 2, gl % 2, gl // 2
                bF = gl % 2
                for comp in range(2):
                    P.op('pe', lambda bF=bF, gp=gp, comp=comp, gg=gg: T.matmul(
                        pb[bF][:, 0:256], lhsT=CC[:, gp, comp, gg * 128:(gg + 1) * 128], rhs=Hb_all[:, gp, comp, :], start=(comp == 0), stop=(comp == 1),
                        skip_group_check=True), r=['Hb_all', 'CC'], w=[pbn[bF]])
                bN = 2 + pp
                for sp_ in range(8):
                    P.op('pe', lambda bN=bN, pp=pp, kc=kc, gg=gg, sp_=sp_: T.matmul(
                        pb[bN][:, 0:256], lhsT=KN[32 * pp:32 * pp + 32, kc, gg, sp_, :], rhs=uo_all[32 * pp:32 * pp + 32, kc, sp_::8],
                        start=(sp_ == 0), stop=(sp_ == 7), skip_group_check=True, tile_position=(32 * pp, 0)), r=['uo_all', 'KN'], w=[pbn[bN]])
                ns = nsb[gl % 2]; nsn = 'nsb%d' % (gl % 2)
                ygt = Yg[gl % 2]; ygn = 'Yg%d' % (gl % 2)
                P.op('act', lambda bN=bN, ns=ns: A.copy(out=ns[:], in_=pb[bN][:, 0:256]), r=[pbn[bN]], w=[nsn])
                P.op('dve', lambda bF=bF, ns=ns, ygt=ygt: V.tensor_tensor(out=ygt[:], in0=pb[bF][:, 0:256], in1=ns[:], op=ALU.add), r=[pbn[bF], nsn], w=[ygn])
                for mh in range(2):
                    P.op('pe', lambda ygt=ygt, mh=mh: T.transpose(out=pb16(6)[:, mh * 128:(mh + 1) * 128], in_=ygt[:, mh * 128:(mh + 1) * 128], identity=identb[:]),
                         r=[ygn, 'identb'], w=[pbn[6]])
                P.op('act', lambda yt=yt, gl=gl: A.copy(out=yt[:, :, :, gl, :], in_=pb16(6)[:, 0:256].rearrange("p (h s c) -> p h s c", h=2, s=8)),
                     r=[pbn[6]], w=[ytn])
            yp = ypre[0]; ypn = 'ypre0'
            for mh in range(2):
                for s_ in range(8):
                    P.op('pe', lambda yt=yt, mh=mh, s_=s_: T.transpose(out=pb16(7)[:, s_ * 128:(s_ + 1) * 128], in_=yt[:, mh, s_, :, :].rearrange("p g c -> p (g c)"),
                                                                       identity=identb[:]), r=[ytn, 'identb'], w=[pbn[7]])
                ov = yp[:, mh * 1024:(mh + 1) * 1024].rearrange("p (m s) -> p s m", s=8)
                uv = uo_all[:, kc, mh * 1024:(mh + 1) * 1024].rearrange("p (m s) -> p s m", s=8)
                P.op('dve', lambda kc=kc, ov=ov, uv=uv: V.scalar_tensor_tensor(
                    out=ov, in0=uv, scalar=dfm[:, kc:kc + 1], in1=pb16(7)[:, 0:1024].rearrange("p (s m) -> p s m", s=8), op0=ALU.mult, op1=ALU.add),
                    r=['uo_all', 'dfm', pbn[7]], w=[ypn])
            yf = yp[:]
            ygo = yg[kc % 2]; ygon = 'yg%d' % (kc % 2)
            P.op('dve', lambda yf=yf: V.tensor_tensor(out=g1_[:], in0=yf, in1=yf, op=ALU.mult), r=[ypn], w=['g1_'])
            P.op('dve', lambda: V.tensor_scalar(out=g1_[:], in0=g1_[:], scalar1=0.044715, scalar2=1.0, op0=ALU.mult, op1=ALU.add), r=['g1_'], w=['g1_'])
            P.op('dve', lambda yf=yf: V.tensor_tensor(out=g1_[:], in0=g1_[:], in1=yf, op=ALU.mult), r=['g1_', ypn], w=['g1_'])
            P.op('act', lambda: A.activation(out=g1_[:], in_=g1_[:], func=AF.Sigmoid, scale=2.0 * 0.7978845608028654), r=['g1_'], w=['g1_'])
            P.op('dve', lambda ygo=ygo, yf=yf: V.tensor_tensor(out=ygo[:], in0=g1_[:], in1=yf, op=ALU.mult), r=['g1_', ypn], w=[ygon])
            P.dma(Yd_v[:, :, kc, :], ygo[:].rearrange("p (t n) -> p t n", t=NT), r=[ygon], w=['Y_d'])
    P.barrier()

    sq.close()
    if dbg and dbg[0] == 'Y':
        with ExitStack() as st:
            tmpb = sb(st, "tmpb", [128, 1024], BF16); tmpf = sb(st, "tmpf", [128, 1024], F32)
            for t in range(NT):
                P.dma(tmpb[:], Y_d[t], r=['Y_d'], w=['tmpb'])
                P.op('dve', lambda: V.tensor_copy(out=tmpf[:], in_=tmpb[:]), r=['tmpb'], w=['tmpf'])
                P.dma(dbg_d[t], tmpf[:], r=['tmpf'], w=['dbg'])
        P.barrier()
        for e in ['sp']:
            pass
        es.close()
        return nc

    if stop('Y2'):
        return fin()
    ISQ = 1.0 / math.sqrt(128.0)

    with ExitStack() as st:
        kT_all = sb(st, "kT_all", [128, 2, L], BF16)
        V_all = sb(st, "V_all", [128, 64, 256], BF16)
        kiT_all = sb(st, "kiT_all", [128, L], BF16)
        with ExitStack() as s2:
            WkvB = sb(s2, "WkvB", [128, 8, 640], BF16)
            with ExitStack() as s3:
                wstg = sb(s3, "wstgK", [128, 8, 640], F32)
                load_weight_bf16((wstg, WkvB), wkv, 0, 640, g1s, 'WkvB')
                P.barrier()
            xt = [sb(s2, "xtK%d" % i, [128, 1024], F32) for i in range(4)]
            junk = [sb(s2, "junkK0", [128, 1024], F32)] * 4
            ssA = [sb(s2, "ssK%d" % i, [128, 4], F32) for i in range(4)]
            hb = [sb(s2, "hbK%d" % i, [128, 1024], BF16) for i in range(4)]
            hT = [sb(s2, "hTK%d" % i, [128, 8, 512], BF16) for i in range(2)]
            ctab = sb(s2, "ctabK", [128, 512], F32); stab = sb(s2, "stabK", [128, 512], F32)
            ctabI = sb(s2, "ctabI", [128, 512], F32); stabI = sb(s2, "stabI", [128, 512], F32)
            rt1 = sb(s2, "rt1", [128, 512], F32); rt2 = sb(s2, "rt2", [128, 512], F32)
            brot = [0]

            def nb2():
                b = brot[0]
                brot[0] = (b + 1) % 8
                return b
            for t in range(NT):
                hTt = hT[t % 2]; hn = 'hTK%d' % (t % 2)
                for blk in range(4):
                    i2 = blk % 4
                    b = nb2()
                    norm_transpose((xt[i2], junk[i2], ssA[i2], hb[i2]), xp[(t * 4 + blk) * 128:(t * 4 + blk + 1) * 128, :], hTt, blk * 128, 'K%d' % i2, b)
                    if blk % 2 == 0:
                        P.op('act', lambda b=b, blk=blk: A.copy(out=hTt[:, :, blk * 128:(blk + 1) * 128], in_=pb16(b).rearrange("p (kc n) -> p kc n", kc=8)),
                             r=[pbn[b]], w=[hn])
                    else:
                        P.op('dve', lambda b=b, blk=blk: V.tensor_copy(out=hTt[:, :, blk * 128:(blk + 1) * 128], in_=pb16(b).rearrange("p (kc n) -> p kc n", kc=8)),
                             r=[pbn[b]], w=[hn])
                cs = slice(t * 512, (t + 1) * 512)
                P.dma(ctab[:], cAk[:, cs], w=['ctabK']); P.dma(stab[:], sAk[:, cs], w=['stabK'])
                P.dma(ctabI[:], cIk[:, cs], w=['ctabI']); P.dma(stabI[:], sIk[:, cs], w=['stabI'])
                for oc in range(3):
                    b = nb2()
                    for kc in range(8):
                        P.op('pe', lambda b=b, oc=oc, kc=kc: T.matmul(pb[b][:, :], lhsT=WkvB[:, kc, oc * 128:(oc + 1) * 128], rhs=hTt[:, kc, :],
                                                                       start=(kc == 0), stop=(kc == 7), skip_group_check=True),
                             r=['WkvB', hn], w=[pbn[b]])
                    if oc < 2:
                        dst = kT_all[:, oc, cs]
                        P.op('act', lambda b=b, dst=dst: A.copy(out=dst, in_=pb[b][:, :]), r=[pbn[b]], w=['kT_all'])
                        rope(dst, 128, permA, ctab[:], stab[:], rt1, rt2, nb2(), ['ctabK', 'stabK'], 'kT_all', 512)
                    else:
                        dst = kiT_all[:, cs]
                        P.op('act', lambda b=b, dst=dst: A.copy(out=dst, in_=pb[b][:, :]), r=[pbn[b]], w=['kiT_all'])
                        rope(dst, 128, permI, ctabI[:], stabI[:], rt1, rt2, nb2(), ['ctabI', 'stabI'], 'kiT_all', 512)
                for blk in range(4):
                    b = nb2()
                    for kc in range(8):
                        P.op('pe', lambda b=b, blk=blk, kc=kc: T.matmul(pb[b][:, 0:256], lhsT=hTt[:, kc, blk * 128:(blk + 1) * 128], rhs=WkvB[:, kc, 384:640],
                                                                         start=(kc == 0), stop=(kc == 7), skip_group_check=True),
                             r=['WkvB', hn], w=[pbn[b]])
                    P.op('dve', lambda b=b, blk=blk, t=t: V.tensor_copy(out=V_all[:, 4 * t + blk, :], in_=pb[b][:, 0:256]), r=[pbn[b]], w=['V_all'])
        P.barrier()
        if stop('KV'):
            return fin()
        hTo = sb(st, "hTo", [128, 8, 512], BF16)
        qT = sb(st, "qT", [128, 4, 8, 128], BF16)
        qiT = sb(st, "qiT", [128, 8, 512], BF16)
        wis = sb(st, "wis", [128, 4, 16], F32)
        wst = [sb(st, "wstQ0", [128, 8, 128], F32)] * 2
        wbf = [sb(st, "wbfQ%d" % i, [128, 8, 128], BF16) for i in range(2)]
        qtmp = sb(st, "qtmp", [128, 512], BF16)
        ctq = sb(st, "ctq", [128, 512], F32); stq = sb(st, "stq", [128, 512], F32)
        rt1 = sb(st, "rt1q", [128, 512], F32); rt2 = sb(st, "rt2q", [128, 512], F32)
        Dg = sb(st, "Dg", [128, 16, 128], BF16)
        Rb = [sb(st, "Rb%d" % i, [128, 1024], BF16) for i in range(3)]
        score = sb(st, "score", [128, L], F32)
        cjunk = sb(st, "cjunk", [128, L], mybir.dt.uint8)
        mtile = [sb(st, "mtile%d" % i, [128, 512], F32) for i in range(3)]
        P.dma(mtile[0][:], mfirst[:, :], w=['mt0']); P.dma(mtile[1][:], mlast[:, :], w=['mt1']); P.dma(mtile[2][:], mboth[:, :], w=['mt2'])
        bs = sb(st, "bs", [128, 8], F32)
        mq = [sb(st, "mq0", [128, 512], BF16)] * 2
        maskT = sb(st, "maskT", [128, 64, 128], BF16)
        Eb = [sb(st, "Eb%d" % i, [128, 1024], BF16) for i in range(2)]
        PT = [sb(st, "PT%d" % i, [128, 1024], BF16) for i in range(2)]
        rden = rt1
        ybt = [sb(st, "ybt0", [128, 1024], BF16)] * 2
        for half in range(4):
            for i in range(4):
                P.dma(hTo[:, :, i * 128:(i + 1) * 128], HT_d[4 * half + i].rearrange("p (kc n) -> p kc n", kc=8), r=['HT_d'], w=['hTo'])
            wcnt = 0
            for hh in range(16):
                wi2 = wcnt % 2; wcnt += 1
                load_weight_bf16((wst[wi2], wbf[wi2]), wq, hh * 128, 128, g1s, 'wbfQ%d' % wi2, stag='wstQ')
                for j in range(1):
                    b = j
                    for kc in range(8):
                        P.op('pe', lambda b=b, kc=kc, wi2=wi2, j=j: T.matmul(pb[b][:, :], lhsT=wbf[wi2][:, kc, :], rhs=hTo[:, kc, j * 512:(j + 1) * 512],
                                                                             start=(kc == 0), stop=(kc == 7), skip_group_check=True),
                             r=['wbfQ%d' % wi2, 'hTo'], w=[pbn[b]])
                    tcs = slice((half * 4) * 128, (half * 4) * 128 + 512)
                    if hh < 8:
                        P.dma(ctq[:], cAq[:, tcs], w=['ctq']); P.dma(stq[:], sAq[:, tcs], w=['stq'])
                        P.op('act', lambda b=b: A.copy(out=qtmp[:], in_=pb[b][:, :]), r=[pbn[b]], w=['qtmp'])
                        rope(qtmp[:], 128, permA, ctq[:], stq[:], rt1, rt2, 2 + j, ['ctq', 'stq'], 'qtmp', 512)
                        P.op('act', lambda hh=hh, j=j: A.copy(out=qT[:, 4 * j:4 * j + 4, hh, :], in_=qtmp[:].rearrange("p (a n) -> p a n", a=4)),
                             r=['qtmp'], w=['qT'])
                    else:
                        P.dma(ctq[:], cIq[:, tcs], w=['ctq']); P.dma(stq[:], sIq[:, tcs], w=['stq'])
                        dst = qiT[:, hh - 8, j * 512:(j + 1) * 512]
                        P.op('act', lambda b=b, dst=dst: A.copy(out=dst, in_=pb[b][:, :]), r=[pbn[b]], w=['qiT'])
                        rope(dst, 128, permI, ctq[:], stq[:], rt1, rt2, 2 + j, ['ctq', 'stq'], 'qiT', 512)
            load_weight_bf16((wst[0], wbf[0]), wwi, 0, 16, g1s, 'wbfQ0', stag='wstQ')
            for blk in range(4):
                b = 4 + blk % 2
                for kc in range(8):
                    P.op('pe', lambda b=b, kc=kc, blk=blk: T.matmul(pb[b][:, 0:16], lhsT=hTo[:, kc, blk * 128:(blk + 1) * 128], rhs=wbf[0][:, kc, 0:16],
                                                                     start=(kc == 0), stop=(kc == 7), skip_group_check=True),
                         r=['wbfQ0', 'hTo'], w=[pbn[b]])
                P.op('dve', lambda b=b, blk=blk: V.tensor_copy(out=wis[:, blk, :], in_=pb[b][:, 0:16]), r=[pbn[b]], w=['wis'])
            pending = None
            for blk in range(4):
                gi = 4 * half + blk
                nkt = gi + 1
                n = nkt * 512
                for h in range(16):
                    if h % 2 == 0:
                        P.op('act', lambda h=h, blk=blk: A.activation(out=Dg[:, h, :], in_=ident32[:], func=AF.Copy, scale=wis[:, blk, h:h + 1]),
                             r=['wis', 'ident32'], w=['Dg'])
                    else:
                        P.op('dve', lambda h=h, blk=blk: V.tensor_scalar(out=Dg[:, h, :], in0=ident32[:], scalar1=wis[:, blk, h:h + 1], scalar2=None, op0=ALU.mult),
                             r=['wis', 'ident32'], w=['Dg'])
                for kt in range(nkt):
                    bsc = 6 + kt % 2

                    def rel_pair(p, kt=kt, blk=blk):
                        X = 2 * (p % 3)
                        for hh in range(2):
                            rs = slice(64 * hh, 64 * hh + 64)
                            P.op('pe', lambda hh=hh, rs=rs: T.matmul(
                                pb[X + hh][:, :], lhsT=qiT[rs, p, blk * 128:(blk + 1) * 128], rhs=kiT_all[rs, kt * 512:(kt + 1) * 512],
                                start=True, stop=True, skip_group_check=True, tile_position=(64 * hh, 0)), r=['qiT', 'kiT_all'], w=[pbn[X + hh]])
                        rbuf = Rb[p % 3]; rn = 'Rb%d' % (p % 3)
                        src2 = pball[:, X * 512:(X + 2) * 512]
                        if p % 2 == 0:
                            P.op('act', lambda: A.activation(out=rbuf[:], in_=src2, func=AF.Relu), r=[pbn[X], pbn[X + 1]], w=[rn])
                        else:
                            P.op('dve', lambda: V.tensor_scalar(out=rbuf[:], in0=src2, scalar1=0.0, scalar2=None, op0=ALU.max), r=[pbn[X], pbn[X + 1]], w=[rn])

                    def score_pair(p, bsc=bsc):
                        rbuf = Rb[p % 3]; rn = 'Rb%d' % (p % 3)
                        for hh in range(2):
                            h = 2 * p + hh
                            P.op('pe', lambda h=h, hh=hh: T.matmul(pb[bsc][:, :], lhsT=Dg[:, h, :], rhs=rbuf[:, hh * 512:(hh + 1) * 512], start=(h == 0), stop=(h == 15),
                                                                   skip_group_check=True), r=['Dg', rn], w=[pbn[bsc]])
                    for step in range(8 + 2):
                        if step < 8:
                            rel_pair(step)
                        if step >= 2:
                            score_pair(step - 2)
                    mt = None
                    if kt == 0 and kt == nkt - 1:
                        mt = 2
                    elif kt == 0:
                        mt = 0
                    elif kt == nkt - 1:
                        mt = 1
                    dsts = score[:, kt * 512:(kt + 1) * 512]
                    if mt is None:
                        P.op('act', lambda bsc=bsc, dsts=dsts: A.copy(out=dsts, in_=pb[bsc][:, :]), r=[pbn[bsc]], w=['score'])
                    else:
                        P.op('dve', lambda bsc=bsc, dsts=dsts, mt=mt: V.tensor_tensor(out=dsts, in0=pb[bsc][:, :], in1=mtile[mt][:], op=ALU.add),
                             r=[pbn[bsc], 'mt%d' % mt], w=['score'])
                n_act = 512 * ((gi + 1) // 2)
                thr_c = 255.5 - 0.5 * n_act
                stps = [BIS_B * 2.0 / (2.0 ** (it + 1)) for it in range(NITER)]
                P.op('dve', lambda: V.memset(bs[:, 1:2], -BIS_B + stps[0]), r=['bs1'], w=['bs1'])
                def bis_iter(it, n=n, n_act=n_act, thr_c=thr_c, stps=stps):
                    if n_act > 0:
                        P.op('act', lambda n_act=n_act: A.activation(out=cjunk[:, 0:n_act], in_=score[:, 0:n_act], func=AF.Sign, bias=bs[:, 1:2], scale=-1.0,
                                                                    accum_out=bs[:, 5:6]), r=['bs1', 'score'], w=['bs5', 'cjunkA'])
                    P.op('dve', lambda n=n, n_act=n_act: V.tensor_scalar(out=cjunk[:, n_act:n], in0=score[:, n_act:n], scalar1=bs[:, 1:2], scalar2=0.0, op0=ALU.is_ge, op1=ALU.add,
                                                                        accum_out=bs[:, 2:3]), r=['bs1', 'score'], w=['bs2', 'cjunk'])
                    if n_act > 0:
                        P.op('dve', lambda: V.scalar_tensor_tensor(out=bs[:, 6:7], in0=bs[:, 5:6], scalar=-0.5, in1=bs[:, 2:3], op0=ALU.mult, op1=ALU.add), r=['bs2', 'bs5'], w=['bs6'])
                        tcol = 6
                    else:
                        tcol = 2
                    P.op('dve', lambda it=it, tcol=tcol, thr_c=thr_c: V.tensor_scalar(out=bs[:, 3:4], in0=bs[:, tcol:tcol + 1], scalar1=thr_c, scalar2=stps[it], op0=ALU.is_ge, op1=ALU.mult),
                         r=['bs%d' % tcol], w=['bs3'])
                    if it < NITER - 1:
                        P.op('dve', lambda it=it: V.scalar_tensor_tensor(out=bs[:, 1:2], in0=bs[:, 3:4], scalar=-stps[it + 1], in1=bs[:, 1:2], op0=ALU.add, op1=ALU.add), r=['bs3', 'bs1'], w=['bs1'])
                    else:
                        P.op('dve', lambda it=it: V.scalar_tensor_tensor(out=bs[:, 0:1], in0=bs[:, 3:4], scalar=-stps[it], in1=bs[:, 1:2], op0=ALU.add, op1=ALU.add), r=['bs3', 'bs1'], w=['bs'])
                if pending is not None:
                    na = len(pending)
                    for it in range(NITER):
                        bis_iter(it)
                        for f in pending[it * na // NITER:(it + 1) * na // NITER]:
                            f()
                    pending = None
                else:
                    for it in range(NITER):
                        bis_iter(it)
                for kt in range(nkt):
                    mqt = mq[0]; mn = 'mq0'
                    P.op('dve', lambda mqt=mqt, kt=kt: V.tensor_scalar(out=mqt[:], in0=score[:, kt * 512:(kt + 1) * 512], scalar1=bs[:, 0:1], scalar2=None, op0=ALU.is_ge),
                         r=['bs', 'score'], w=[mn])
                    bt_ = 4 + kt % 2
                    for a in range(4):
                        P.op('pe', lambda mqt=mqt, a=a, bt_=bt_: T.transpose(out=pb16(bt_)[:, a * 128:(a + 1) * 128], in_=mqt[:, a * 128:(a + 1) * 128], identity=identb[:]),
                             r=[mn, 'identb'], w=[pbn[bt_]])
                    P.op('act', lambda kt=kt, bt_=bt_: A.copy(out=maskT[:, 4 * kt:4 * kt + 4, :], in_=pb16(bt_)[:, 0:512].rearrange("p (a n) -> p a n", a=4)),
                         r=[pbn[bt_]], w=['maskT'])
                def make_att(blk=blk, gi=gi, nkt=nkt):
                    steps = []
                    ybtt = ybt[0]; ybn = 'ybt0'
                    nkb = 4 * nkt
                    npair = nkb // 2
                    for g in range(2):
                        rhsq = qT[:, blk, 4 * g:4 * g + 4, :].rearrange("p a n -> p (a n)")

                        def qk_pair(pq, g=g, rhsq=rhsq):
                            X = 2 * (pq % 2)
                            for j in range(2):
                                kb = 2 * pq + j
                                P.op('pe', lambda kb=kb, j=j: T.matmul(pb[X + j][:, :], lhsT=kT_all[:, g, kb * 128:(kb + 1) * 128], rhs=rhsq,
                                                                       start=True, stop=True, skip_group_check=True), r=['kT_all', 'qT'], w=[pbn[X + j]])
                            e = Eb[pq % 2]; en = 'Eb%d' % (pq % 2)
                            P.op('act', lambda: A.activation(out=e[:], in_=pball[:, X * 512:(X + 2) * 512], func=AF.Exp, scale=ISQ), r=[pbn[X], pbn[X + 1]], w=[en])
                            pt = PT[pq % 2]; pn = 'PT%d' % (pq % 2)
                            P.op('dve', lambda: V.tensor_tensor(out=pt[:].rearrange("p (k a n) -> p k a n", k=2, a=4), in0=e[:].rearrange("p (k a n) -> p k a n", k=2, a=4),
                                                                in1=maskT[:, 2 * pq:2 * pq + 2, :].unsqueeze(2).to_broadcast([128, 2, 4, 128]), op=ALU.mult),
                                 r=[en, 'maskT'], w=[pn])

                        def pv_pair(pq, g=g, nkb=nkb):
                            pt = PT[pq % 2]; pn = 'PT%d' % (pq % 2)
                            for j in range(2):
                                kb = 2 * pq + j
                                P.op('pe', lambda kb=kb, j=j: T.matmul(pb[6][:, :], lhsT=V_all[:, kb, g * 128:(g + 1) * 128], rhs=pt[:, j * 512:(j + 1) * 512],
                                                                       start=(kb == 0), stop=(kb == nkb - 1), skip_group_check=True), r=['V_all', pn], w=[pbn[6]])
                                P.op('pe', lambda kb=kb, j=j: T.matmul(pb[7][:, :], lhsT=onesb[:], rhs=pt[:, j * 512:(j + 1) * 512],
                                                                       start=(kb == 0), stop=(kb == nkb - 1), skip_group_check=True), r=['onesb', pn], w=[pbn[7]])

                        def one_step(step, qk_pair=qk_pair, pv_pair=pv_pair, npair=npair):
                            if step < npair:
                                qk_pair(step)
                            if step >= 1:
                                pv_pair(step - 1)
                        for step in range(npair + 1):
                            steps.append(lambda step=step, one_step=one_step: one_step(step))

                        def fin_g(g=g):
                            P.op('dve', lambda: V.reciprocal(out=rden[:], in_=pb[7][:, :]), r=[pbn[7]], w=['ropet1'])
                            P.op('dve', lambda: V.tensor_tensor(out=ybtt[:, g * 512:(g + 1) * 512], in0=pb[6][:, :], in1=rden[:], op=ALU.mult),
                                 r=[pbn[6], 'ropet1'], w=[ybn])
                        steps.append(fin_g)
                    steps.append(lambda: P.dma(YB_d[gi], ybtt[:], r=[ybn], w=['YB_d']))
                    return steps
                att = make_att()
                if blk == 3:
                    for f in att:
                        f()
                    pending = None
                else:
                    pending = att
    P.barrier()
    if stop('ATT'):
        return fin()

    with ExitStack() as st:
        h2T = sb(st, "h2T", [128, 8, 2048], BF16)
        sA = ExitStack()
        mT = sb(sA, "mT", [128, 8, 2048], BF16)
        with ExitStack() as s2:
            yaT = sb(s2, "yaT", [128, 8, 2048], BF16)
            wst = [sb(s2, "wstD%d" % i, [128, 8, 128], F32) for i in range(4)]
            wbf = [sb(s2, "wbfD%d" % i, [128, 8, 128], BF16) for i in range(4)]
            sg = [sb(s2, "sgD%d" % i, [128, 512], F32) for i in range(4)]
            with ExitStack() as s3:
                yT = sb(s3, "yT", [128, 8, 2048], BF16)
                for t in range(NT):
                    P.dma(yT[:, :, t * 128:(t + 1) * 128], Y_d[t].rearrange("p (kc n) -> p kc n", kc=8), r=['Y_d'], w=['yT'])
                for oc in range(8):
                    w2 = oc % 2
                    load_weight_bf16((wst[w2], wbf[w2]), wglu, oc * 128, 128, None, 'wbfD%d' % w2)
                    for j in range(4):
                        b = (oc * 4 + j) % 8
                        for kc in range(8):
                            P.op('pe', lambda b=b, kc=kc, w2=w2, j=j: T.matmul(pb[b][:, :], lhsT=wbf[w2][:, kc, :], rhs=yT[:, kc, j * 512:(j + 1) * 512],
                                                                               start=(kc == 0), stop=(kc == 7), skip_group_check=True), r=['wbfD%d' % w2, 'yT'], w=[pbn[b]])
                        sgt = sg[j % 2]; sn = 'sgD%d' % (j % 2)
                        P.op('act', lambda b=b, sgt=sgt: A.activation(out=sgt[:], in_=pb[b][:, :], func=AF.Sigmoid), r=[pbn[b]], w=[sn])
                        P.op('dve', lambda oc=oc, j=j, sgt=sgt: V.tensor_tensor(out=yaT[:, oc, j * 512:(j + 1) * 512], in0=sgt[:], in1=yT[:, oc, j * 512:(j + 1) * 512], op=ALU.mult),
                             r=[sn, 'yT'], w=['yaT'])
                P.barrier()
            ybT = sb(s2, "ybT", [128, 8, 2048], BF16)
            hTa = sb(s2, "hTa", [128, 8, 2048], BF16)
            for t in range(NT):
                P.dma(ybT[:, :, t * 128:(t + 1) * 128], YB_d[t].rearrange("p (h n) -> p h n", h=8), r=['YB_d'], w=['ybT'])
                P.dma(hTa[:, :, t * 128:(t + 1) * 128], HT_d[t].rearrange("p (kc n) -> p kc n", kc=8), r=['HT_d'], w=['hTa'])
            m1 = sb(s2, "mm1", [128, 512], F32); m2 = sb(s2, "mm2", [128, 512], F32)
            for oc in range(8):
                load_weight_bf16((wst[0], wbf[0]), wba, oc * 128, 128, None, 'wbfD0')
                load_weight_bf16((wst[1], wbf[1]), wbb, oc * 128, 128, None, 'wbfD1')
                load_weight_bf16((wst[2], wbf[2]), wg, oc * 128, 128, g1s, 'wbfD2')
                load_weight_bf16((wst[3], wbf[3]), wg, 1024 + oc * 128, 128, g1s, 'wbfD3')
                for j in range(4):
                    js = slice(j * 512, (j + 1) * 512)
                    acts = [yaT, ybT, hTa, hTa]; anm = ['yaT', 'ybT', 'hTa', 'hTa']
                    for q4 in range(4):
                        b = 4 * (j % 2) + q4
                        for kc in range(8):
                            P.op('pe', lambda b=b, kc=kc, q4=q4, js=js, acts=acts: T.matmul(pb[b][:, :], lhsT=wbf[q4][:, kc, :], rhs=acts[q4][:, kc, js],
                                                                                          start=(kc == 0), stop=(kc == 7), skip_group_check=True),
                                 r=['wbfD%d' % q4, anm[q4]], w=[pbn[b]])
                    b0 = 4 * (j % 2)
                    P.op('act', lambda b0=b0: A.activation(out=sg[0][:], in_=pb[b0 + 2][:, :], func=AF.Sigmoid), r=[pbn[b0 + 2]], w=['sgD0'])
                    P.op('act', lambda b0=b0: A.activation(out=sg[1][:], in_=pb[b0 + 3][:, :], func=AF.Sigmoid), r=[pbn[b0 + 3]], w=['sgD1'])
                    P.op('dve', lambda b0=b0: V.tensor_tensor(out=m1[:], in0=pb[b0][:, :], in1=sg[0][:], op=ALU.mult), r=[pbn[b0], 'sgD0'], w=['m1'])
                    P.op('dve', lambda b0=b0: V.tensor_tensor(out=m2[:], in0=pb[b0 + 1][:, :], in1=sg[1][:], op=ALU.mult), r=[pbn[b0 + 1], 'sgD1'], w=['m2'])
                    P.op('dve', lambda oc=oc, js=js: V.tensor_tensor(out=mT[:, oc, js], in0=m1[:], in1=m2[:], op=ALU.add), r=['m1', 'm2'], w=['mT'])
            P.barrier()
        if stop('MRG'):
            return fin()
        with ExitStack() as s2:
            WoB = sb(s2, "WoB", [128, 8, 1024], BF16)
            with ExitStack() as s3:
                wstg = sb(s3, "wstgO", [128, 8, 1024], F32)
                load_weight_bf16((wstg, WoB), wout, 0, 1024, None, 'WoB')
                P.barrier()
            xt = [sb(s2, "xtF%d" % i, [128, 1024], F32) for i in range(2)]
            x1t = [sb(s2, "x1F%d" % i, [128, 1024], F32) for i in range(2)]
            junk = sb(s2, "junkF", [128, 1024], F32)
            ssF = [sb(s2, "ssF%d" % i, [128, 4], F32) for i in range(2)]
            hbF = [sb(s2, "hbF%d" % i, [128, 1024], BF16) for i in range(2)]
            for gi in range(NT):
                i2 = gi % 2
                rows = slice((4 * gi + 3) * 128, (4 * gi + 4) * 128)
                P.dma(xt[i2][:], xp[rows, :], w=['xtF%d' % i2])
                for hf in range(2):
                    b = 2 * i2 + hf
                    for kc in range(8):
                        P.op('pe', lambda b=b, kc=kc, gi=gi, hf=hf: T.matmul(pb[b][:, :], lhsT=mT[:, kc, gi * 128:(gi + 1) * 128], rhs=WoB[:, kc, hf * 512:(hf + 1) * 512],
                                                                             start=(kc == 0), stop=(kc == 7), skip_group_check=True), r=['mT', 'WoB'], w=[pbn[b]])
                    P.op('dve', lambda b=b, hf=hf, i2=i2: V.tensor_tensor(out=x1t[i2][:, hf * 512:(hf + 1) * 512], in0=pb[b][:, :], in1=xt[i2][:, hf * 512:(hf + 1) * 512], op=ALU.add),
                         r=[pbn[b], 'xtF%d' % i2], w=['x1F%d' % i2])
                P.dma(X1_d[gi], x1t[i2][:], r=['x1F%d' % i2], w=['X1_d'])
                tg = 'F%d' % i2
                P.op('act', lambda i2=i2: A.activation(out=junk[:], in_=x1t[i2][:], func=AF.Square, accum_out=ssF[i2][:, 0:1]), r=['x1F%d' % i2], w=['junkF', tg + 'ss'])
                P.op('dve', lambda i2=i2: V.tensor_scalar(out=ssF[i2][:, 1:2], in0=ssF[i2][:, 0:1], scalar1=1.0 / D, scalar2=EPS, op0=ALU.mult, op1=ALU.add), r=[tg + 'ss'], w=[tg + 'ss'])
                P.op('act', lambda i2=i2: A.activation(out=ssF[i2][:, 2:3], in_=ssF[i2][:, 1:2], func=AF.Sqrt), r=[tg + 'ss'], w=[tg + 'ss'])
                P.op('dve', lambda i2=i2: V.reciprocal(out=ssF[i2][:, 3:4], in_=ssF[i2][:, 2:3]), r=[tg + 'ss'], w=[tg + 'ss'])
                P.op('dve', lambda i2=i2: V.tensor_scalar(out=hbF[i2][:], in0=x1t[i2][:], scalar1=ssF[i2][:, 3:4], scalar2=None, op0=ALU.mult), r=['x1F%d' % i2, tg + 'ss'], w=[tg + 'hb'])
                bt_ = 4 + i2
                for kc in range(8):
                    P.op('pe', lambda kc=kc, i2=i2, bt_=bt_: T.transpose(out=pb16(bt_)[:, kc * 128:(kc + 1) * 128], in_=hbF[i2][:, kc * 128:(kc + 1) * 128], identity=identb[:]),
                         r=[tg + 'hb', 'identb'], w=[pbn[bt_]])
                P.op('act', lambda gi=gi, bt_=bt_: A.copy(out=h2T[:, :, gi * 128:(gi + 1) * 128], in_=pb16(bt_).rearrange("p (kc n) -> p kc n", kc=8)), r=[pbn[bt_]], w=['h2T'])
            P.barrier()
        sA.close()
        if stop('F'):
            return fin()
        actT = sb(st, "actT", [128, 22, 2048], BF16)
        with ExitStack() as s2:
            wst = [sb(s2, "wstG%d" % i, [128, 8, 128], F32) for i in range(2)]
            wbf = [sb(s2, "wbfG%d" % i, [128, 8, 128], BF16) for i in range(2)]
            sgl = [sb(s2, "sgG%d" % i, [128, 512], F32) for i in range(2)]
            for jh in range(22):
                load_weight_bf16((wst[0], wbf[0]), wfi, jh * 128, 128, g2s, 'wbfG0')
                load_weight_bf16((wst[1], wbf[1]), wfi, FH + jh * 128, 128, g2s, 'wbfG1')
                for tg_ in range(4):
                    ts_ = slice(tg_ * 512, (tg_ + 1) * 512)
                    b0 = 2 * (tg_ % 4)
                    for q2 in range(2):
                        for kc in range(8):
                            P.op('pe', lambda b0=b0, q2=q2, kc=kc, ts_=ts_: T.matmul(pb[b0 + q2][:, :], lhsT=wbf[q2][:, kc, :], rhs=h2T[:, kc, ts_],
                                                                                   start=(kc == 0), stop=(kc == 7), skip_group_check=True), r=['wbfG%d' % q2, 'h2T'], w=[pbn[b0 + q2]])
                    sgt = sgl[tg_ % 2]; sn = 'sgG%d' % (tg_ % 2)
                    P.op('act', lambda b0=b0, sgt=sgt: A.activation(out=sgt[:], in_=pb[b0][:, :], func=AF.Silu), r=[pbn[b0]], w=[sn])
                    P.op('dve', lambda b0=b0, sgt=sgt, jh=jh, ts_=ts_: V.tensor_tensor(out=actT[:, jh, ts_], in0=pb[b0 + 1][:, :], in1=sgt[:], op=ALU.mult),
                         r=[pbn[b0 + 1], sn], w=['actT'])
            P.barrier()
        with ExitStack() as s2:
            WfoB = sb(s2, "WfoB", [128, 22, 1024], BF16)
            wstg2 = [sb(s2, "wstgFo%d" % i, [128, 1024], F32) for i in range(2)]
            for jh in range(22):
                i2 = jh % 2
                P.dma(wstg2[i2][:], wfo[jh * 128:(jh + 1) * 128, :], w=['wstgFo%d' % i2])
                if i2 == 0:
                    P.op('act', lambda jh=jh, i2=i2: A.copy(out=WfoB[:, jh, :], in_=wstg2[i2][:]), r=['wstgFo%d' % i2], w=['WfoB'])
                else:
                    P.op('dve', lambda jh=jh, i2=i2: V.tensor_copy(out=WfoB[:, jh, :], in_=wstg2[i2][:]), r=['wstgFo%d' % i2], w=['WfoB'])
            gft = sb(s2, "gft", [128, 1024], F32)
            P.dma(gft[:], gfb[:, :], w=['gft'])
            x1r = [sb(s2, "x1r%d" % i, [128, 1024], F32) for i in range(2)]
            x2 = [sb(s2, "x2_%d" % i, [128, 1024], F32) for i in range(2)]
            junkG = sb(s2, "junkG", [128, 1024], F32)
            ssG = [sb(s2, "ssG%d" % i, [128, 4], F32) for i in range(2)]
            ot = [sb(s2, "ot%d" % i, [128, 1024], F32) for i in range(2)]
            for gi in range(NT):
                i2 = gi % 2
                P.dma(x1r[i2][:], X1_d[gi], r=['X1_d'], w=['x1r%d' % i2])
                for hf in range(2):
                    b = 2 * i2 + hf
                    for jh in range(22):
                        P.op('pe', lambda b=b, jh=jh, gi=gi, hf=hf: T.matmul(pb[b][:, :], lhsT=actT[:, jh, gi * 128:(gi + 1) * 128], rhs=WfoB[:, jh, hf * 512:(hf + 1) * 512],
                                                                             start=(jh == 0), stop=(jh == 21), skip_group_check=True), r=['actT', 'WfoB'], w=[pbn[b]])
                    P.op('dve', lambda b=b, hf=hf, i2=i2: V.tensor_tensor(out=x2[i2][:, hf * 512:(hf + 1) * 512], in0=pb[b][:, :], in1=x1r[i2][:, hf * 512:(hf + 1) * 512], op=ALU.add),
                         r=[pbn[b], 'x1r%d' % i2], w=['x2_%d' % i2])
                tg = 'G%d' % i2
                P.op('act', lambda i2=i2: A.activation(out=junkG[:], in_=x2[i2][:], func=AF.Square, accum_out=ssG[i2][:, 0:1]), r=['x2_%d' % i2], w=['junkG', tg + 'ss'])
                P.op('dve', lambda i2=i2: V.tensor_scalar(out=ssG[i2][:, 1:2], in0=ssG[i2][:, 0:1], scalar1=1.0 / D, scalar2=EPS, op0=ALU.mult, op1=ALU.add), r=[tg + 'ss'], w=[tg + 'ss'])
                P.op('act', lambda i2=i2: A.activation(out=ssG[i2][:, 2:3], in_=ssG[i2][:, 1:2], func=AF.Sqrt), r=[tg + 'ss'], w=[tg + 'ss'])
                P.op('dve', lambda i2=i2: V.reciprocal(out=ssG[i2][:, 3:4], in_=ssG[i2][:, 2:3]), r=[tg + 'ss'], w=[tg + 'ss'])
                P.op('dve', lambda i2=i2: V.tensor_scalar(out=ot[i2][:], in0=x2[i2][:], scalar1=ssG[i2][:, 3:4], scalar2=None, op0=ALU.mult), r=['x2_%d' % i2, tg + 'ss'], w=['ot%d' % i2])
                P.op('dve', lambda i2=i2: V.tensor_tensor(out=ot[i2][:], in0=ot[i2][:], in1=gft[:], op=ALU.mult), r=['ot%d' % i2, 'gft'], w=['ot%d' % i2])
                P.dma(out_d[gi * 128:(gi + 1) * 128, :], ot[i2][:], r=['ot%d' % i2], w=['out_d'])
            P.barrier()
    P.barrier()
    return nc


def _host_prep(inputs):
    x = np.asarray(inputs['x'], np.float32)
    w_in = np.asarray(inputs['w_in'], np.float32)[0]
    pts = np.cumsum([1024, 1024, 256, 256, 1024, 64, 16, 1024, 1024])
    wu, wq_, wk, wv, wqi, wki, wwi, wga, wgb = np.split(w_in, pts[:-1], axis=1)
    a_re = np.asarray(inputs['a_re'], np.float32)[0]; a_im = np.asarray(inputs['a_im'], np.float32)[0]
    log_dt = np.asarray(inputs['log_dt'], np.float32)[0]
    b_re = np.asarray(inputs['b_re'], np.float32)[0]; b_im = np.asarray(inputs['b_im'], np.float32)[0]
    c_re = np.asarray(inputs['c_re'], np.float32)[0]; c_im = np.asarray(inputs['c_im'], np.float32)[0]
    d_skip = np.asarray(inputs['d_skip'], np.float32)[0]

    def gT(g):
        return np.ascontiguousarray(np.asarray(g, np.float32).reshape(8, 128).T)

    r = np.arange(128); kc = np.arange(8)
    pp = r // 32; ggr = (r // 16) % 2; cr = r % 16
    AR1 = np.zeros((128, 8, 2, 64), np.float32); AI1 = np.zeros_like(AR1); DT1 = np.zeros_like(AR1)
    BR1 = np.zeros_like(AR1); BI1 = np.zeros_like(AR1)
    for k in range(8):
        for g2 in range(2):
            g = 2 * (4 * k + pp) + g2
            AR1[:, k, g2, :] = a_re[g, :]
            AI1[:, k, g2, :] = a_im[g, :]
            DT1[:, k, g2, :] = log_dt[g][:, None]
            sel = (ggr == g2)
            BR1[sel, k, g2, :] = b_re[g[sel], :, cr[sel]]
            BI1[sel, k, g2, :] = b_im[g[sel], :, cr[sel]]
    gg = np.arange(128) // 64; p_ = np.arange(128) % 64
    gidx = 2 * np.arange(32)[None, :] + gg[:, None]
    ARE = a_re[gidx, p_[:, None]]; AIE = a_im[gidx, p_[:, None]]; DTE = log_dt[gidx]
    CTR = c_re[gidx, :, p_[:, None]]
    CTI = c_im[gidx, :, p_[:, None]]
    BER = np.zeros((128, 32, 2, 16), np.float32); BEI = np.zeros_like(BER)
    for g2 in range(2):
        sel = gg == g2
        BER[sel, :, g2, :] = b_re[gidx[sel], p_[sel][:, None], :]
        BEI[sel, :, g2, :] = b_im[gidx[sel], p_[sel][:, None], :]
    DFM = np.ascontiguousarray(d_skip.reshape(8, 128).T)
    common = dict(
        wu=np.ascontiguousarray(wu),
        wkv=np.ascontiguousarray(np.concatenate([wk, wki, wki, wv], axis=1)),
        wq=np.ascontiguousarray(np.concatenate([wq_, wqi], axis=1)),
        wwi=np.ascontiguousarray(wwi),
        wg=np.ascontiguousarray(np.concatenate([wga, wgb], axis=1)),
        wglu=np.asarray(inputs['w_glu'], np.float32)[0], wba=np.asarray(inputs['w_branch_a'], np.float32)[0],
        wbb=np.asarray(inputs['w_branch_b'], np.float32)[0], wout=np.asarray(inputs['w_out'], np.float32)[0],
        wfi=np.asarray(inputs['w_ffn_in'], np.float32)[0], wfo=np.asarray(inputs['w_ffn_out'], np.float32)[0],
        g1T=gT(inputs['norm1_g'][0]), g2T=gT(inputs['norm2_g'][0]),
        gfb=np.ascontiguousarray(np.broadcast_to(np.asarray(inputs['norm_f_g'], np.float32)[None, :], (128, D))),
        AR1=AR1.reshape(128, 1024), AI1=AI1.reshape(128, 1024), DT1=DT1.reshape(128, 1024),
        BR1=BR1.reshape(128, 1024), BI1=BI1.reshape(128, 1024),
        ARE=np.ascontiguousarray(ARE), AIE=np.ascontiguousarray(AIE), DTE=np.ascontiguousarray(DTE),
        CTR=np.ascontiguousarray(CTR).reshape(128, 512), CTI=np.ascontiguousarray(CTI).reshape(128, 512),
        BER=BER.reshape(128, 1024), BEI=BEI.reshape(128, 1024), DFM=DFM,
        identf=np.eye(128, dtype=np.float32),
    )
    permA = np.zeros((128, 128), np.float32)
    for m in range(32):
        permA[m + 16 if m < 16 else m - 16, m] = 1
    permI = np.zeros((128, 128), np.float32)
    for hb in (0, 64):
        for m in range(16):
            permI[hb + (m + 8 if m < 8 else m - 8), hb + m] = 1
    common['permA'] = permA; common['permI'] = permI

    def tables(pos, kind):
        pos = pos.astype(np.float32)
        cos = np.ones((128, pos.shape[0]), np.float32); sin = np.zeros_like(cos)
        if kind == 'A':
            half = 16
            inv = (np.float32(500000.0) ** (-np.arange(half, dtype=np.float32) / half)).astype(np.float32)
            ang = pos[None, :] * inv[:, None]
            cos[0:16] = np.cos(ang); cos[16:32] = np.cos(ang)
            sin[0:16] = -np.sin(ang); sin[16:32] = np.sin(ang)
        else:
            half = 8
            inv = (np.float32(500000.0) ** (-np.arange(half, dtype=np.float32) / half)).astype(np.float32)
            ang = pos[None, :] * inv[:, None]
            for hb in (0, 64):
                cos[hb:hb + 8] = np.cos(ang); cos[hb + 8:hb + 16] = np.cos(ang)
                sin[hb:hb + 8] = -np.sin(ang); sin[hb + 8:hb + 16] = np.sin(ang)
        return cos, sin

    in_maps = []
    for c in range(8):
        b, r_ = c // 4, c % 4
        pad = (3 - r_) * 128
        xpad = np.zeros((L, D), np.float32)
        xpad[pad:] = x[b, :L - pad]
        pos_all = np.maximum(np.arange(L) - pad, 0)
        own = np.concatenate([np.arange(128) + (4 * i + 3) * 128 for i in range(NT)])
        pos_own = own - pad
        m = dict(common)
        m['xp'] = xpad
        m['cAk'], m['sAk'] = tables(pos_all, 'A')
        m['cIk'], m['sIk'] = tables(pos_all, 'I')
        m['cAq'], m['sAq'] = tables(pos_own, 'A')
        m['cIq'], m['sIq'] = tables(pos_own, 'I')
        q = np.arange(128)[:, None]; kk = np.arange(512)[None, :]
        caus = np.where(((kk < 384) | ((kk - 384) // 64 <= q // 64)), 0.0, NEG).astype(np.float32)
        padm = np.where(kk >= pad, 0.0, NEG).astype(np.float32) * np.ones((128, 1), np.float32)
        m['mfirst'] = np.ascontiguousarray(padm)
        m['mlast'] = np.ascontiguousarray(caus)
        m['mboth'] = np.minimum(padm, caus).astype(np.float32)
        in_maps.append(m)
    return in_maps


def kernel(**inputs):
    in_maps = _host_prep(inputs)
    nc = build_program()
    res = run_bass_kernel_spmd(nc, in_maps, core_ids=list(range(8)))
    out = np.zeros((2, L, D), np.float32)
    for c in range(8):
        b, r_ = c // 4, c % 4
        o = res.results[c]["out"].reshape(NT, 128, D)
        for i in range(NT):
            j = 4 * i + r_
            out[b, j * 128:(j + 1) * 128] = o[i]
    return out
```

```python
import math
from contextlib import ExitStack
import numpy as np
import concourse.bass as bass
import concourse.mybir as mybir
from concourse.bass_utils import run_bass_kernel_spmd

F32 = mybir.dt.float32
BF16 = mybir.dt.bfloat16
AF = mybir.ActivationFunctionType
ALU = mybir.AluOpType

D = 1024
L = 8192
NT = 16
FH = 2816
EPS = 1e-6
NEG = -1e30
NITER = 18
BIS_B = 64.0
NDS = 24


class Prog:
    def __init__(self, nc, es):
        self.nc = nc
        self.E = {'pe': nc.tensor, 'act': nc.scalar, 'dve': nc.vector, 'pool': nc.gpsimd, 'sp': nc.sync}
        self.sem = {k: es.enter_context(nc.semaphore('s_' + k)) for k in ['pe', 'act', 'dve', 'pool']}
        self.cnt = {k: 0 for k in self.sem}
        self.dsem = [es.enter_context(nc.semaphore('d%d' % i)) for i in range(NDS)]
        self.dcnt = [0] * NDS
        self.dnext = 0
        self.seen = {e: {} for e in self.E}
        self.lw = {}
        self.rd = {}

    def _semobj(self, key):
        return self.dsem[key[1]] if isinstance(key, tuple) else self.sem[key]

    def _wait(self, eng, ev):
        key, val = ev
        if self.seen[eng].get(key, 0) >= val:
            return
        self.seen[eng][key] = val
        self.E[eng].wait_ge(self._semobj(key), val)

    def _deps(self, eng, r, w):
        evs = {}
        for x in r:
            if x in self.lw:
                k, v = self.lw[x]
                evs[k] = max(evs.get(k, 0), v)
        for x in w:
            if x in self.lw:
                k, v = self.lw[x]
                evs[k] = max(evs.get(k, 0), v)
            for k, v in self.rd.get(x, {}).items():
                evs[k] = max(evs.get(k, 0), v)
        for k, v in evs.items():
            if eng == 'pe' and k == 'pe':
                continue
            self._wait(eng, (k, v))

    def _record(self, me, r, w):
        k, v = me
        for x in r:
            d = self.rd.setdefault(x, {})
            d[k] = max(d.get(k, 0), v)
        for x in w:
            self.lw[x] = me
            self.rd[x] = {}

    def op(self, eng, fn, r=(), w=()):
        self._deps(eng, r, w)
        ins = fn()
        self.cnt[eng] += 1
        ins.then_inc(self.sem[eng], 1)
        self._record((eng, self.cnt[eng]), r, w)

    def dma(self, out, in_, r=(), w=(), q='sp'):
        self._deps(q, r, w)
        i = self.dnext
        self.dnext = (i + 1) % NDS
        if self.dcnt[i] > 0:
            self._wait(q, (('d', i), self.dcnt[i]))
        self.E[q].dma_start(out=out, in_=in_).then_inc(self.dsem[i], 16)
        self.dcnt[i] += 16
        self._record((('d', i), self.dcnt[i]), r, w)

    def barrier(self):
        for e in ['pe', 'act', 'dve', 'pool', 'sp']:
            for k in self.sem:
                if self.cnt[k] > 0:
                    self._wait(e, (k, self.cnt[k]))
            for i in range(NDS):
                if self.dcnt[i] > 0:
                    self._wait(e, (('d', i), self.dcnt[i]))
        self.lw = {}
        self.rd = {}


def build_program(dbg=None):
    nc = bass.Bass("TRN2", target_bir_lowering=False)

    def din(name, shape, dt=F32):
        return nc.dram_tensor(name, list(shape), dt, kind="ExternalInput").ap()

    def dscr(name, shape, dt):
        return nc.dram_tensor(name, list(shape), dt, kind="Internal").ap()

    xp = din("xp", [L, D])
    wu = din("wu", [D, 1024])
    wkv = din("wkv", [D, 640])
    wq = din("wq", [D, 2048])
    wwi = din("wwi", [D, 16])
    wg = din("wg", [D, 2048])
    wglu = din("wglu", [D, D])
    wba = din("wba", [D, D])
    wbb = din("wbb", [D, D])
    wout = din("wout", [D, D])
    wfi = din("wfi", [D, 2 * FH])
    wfo = din("wfo", [FH, D])
    g1T = din("g1T", [128, 8])
    g2T = din("g2T", [128, 8])
    gfb = din("gfb", [128, D])
    AR1 = din("AR1", [128, 1024]); AI1 = din("AI1", [128, 1024]); DT1 = din("DT1", [128, 1024])
    BR1 = din("BR1", [128, 1024]); BI1 = din("BI1", [128, 1024])
    ARE = din("ARE", [128, 32]); AIE = din("AIE", [128, 32]); DTE = din("DTE", [128, 32])
    CTR = din("CTR", [128, 512]); CTI = din("CTI", [128, 512])
    BER = din("BER", [128, 1024]); BEI = din("BEI", [128, 1024])
    DFM = din("DFM", [128, 8])
    identf_d = din("identf", [128, 128])
    permA_d = din("permA", [128, 128]); permI_d = din("permI", [128, 128])
    cAk = din("cAk", [128, L]); sAk = din("sAk", [128, L])
    cIk = din("cIk", [128, L]); sIk = din("sIk", [128, L])
    cAq = din("cAq", [128, 2048]); sAq = din("sAq", [128, 2048])
    cIq = din("cIq", [128, 2048]); sIq = din("sIq", [128, 2048])
    mfirst = din("mfirst", [128, 512]); mlast = din("mlast", [128, 512]); mboth = din("mboth", [128, 512])
    out_d = nc.dram_tensor("out", [2048, D], F32, kind="ExternalOutput").ap()
    dbg_d = None
    if dbg:
        dbg_d = nc.dram_tensor("dbg", list(dbg[1]), F32, kind="ExternalOutput").ap()

    HT_d = dscr("HT_d", [NT, 128, 1024], BF16)
    U_d = dscr("U_d", [NT, 128, 1024], BF16)
    HS_d = dscr("HS_d", [NT, 128, 1024], BF16)
    Y_d = dscr("Y_d", [NT, 128, 1024], BF16)
    YB_d = dscr("YB_d", [NT, 128, 1024], BF16)
    X1_d = dscr("X1_d", [NT, 128, 1024], F32)

    es = ExitStack()
    P = Prog(nc, es)
    V, A, G, T = nc.vector, nc.scalar, nc.gpsimd, nc.tensor

    def sb(stack, name, shape, dt):
        return stack.enter_context(nc.sbuf_tensor(name, list(shape), dt))

    def fin():
        P.barrier()
        return nc

    def stop(name):
        return bool(dbg) and dbg[0] == name

    identf = sb(es, "identf_s", [128, 128], F32)
    identb = sb(es, "identb", [128, 128], BF16)
    permA = sb(es, "permA_s", [128, 128], BF16)
    permI = sb(es, "permI_s", [128, 128], BF16)
    onesb = sb(es, "onesb", [128, 128], BF16)
    g1s = sb(es, "g1s", [128, 8], F32)
    g2s = sb(es, "g2s", [128, 8], F32)
    ptmp = sb(es, "ptmp", [128, 128], F32)
    P.dma(identf[:], identf_d[:, :], w=['identf'])
    P.op('dve', lambda: V.tensor_copy(out=identb[:], in_=identf[:]), r=['identf'], w=['identb'])
    P.dma(ptmp[:], permA_d[:, :], w=['ptmp'])
    P.op('dve', lambda: V.tensor_copy(out=permA[:], in_=ptmp[:]), r=['ptmp'], w=['permA'])
    P.dma(ptmp[:], permI_d[:, :], w=['ptmp'])
    P.op('dve', lambda: V.tensor_copy(out=permI[:], in_=ptmp[:]), r=['ptmp'], w=['permI'])
    P.op('pool', lambda: G.memset(onesb[:], 1.0), w=['onesb'])
    ident32 = sb(es, "ident32", [128, 128], F32)
    P.op('dve', lambda: V.tensor_scalar(out=ident32[:], in0=identf[:], scalar1=1.0 / 32.0, scalar2=None, op0=ALU.mult), r=['identf'], w=['ident32'])
    P.dma(g1s[:], g1T[:, :], w=['g1s'])
    P.dma(g2s[:], g2T[:, :], w=['g2s'])

    pball = es.enter_context(nc.psum_tensor("pball", [128, 4096], F32))
    pb = [pball[:, i * 512:(i + 1) * 512] for i in range(8)]
    pbn = ['pb%d' % i for i in range(8)]

    def pb16(i):
        return pb[i][:, :].bitcast(BF16)

    def load_weight_bf16(stack_tiles, wdram, c0, ncols, gscale, tag, stag=None):
        stg, dst = stack_tiles
        stag = stag or tag
        for kc in range(8):
            P.dma(stg[:, kc, 0:ncols], wdram[kc * 128:(kc + 1) * 128, c0:c0 + ncols], w=[stag + 's%d' % kc])
            if gscale is None:
                if kc % 2 == 0:
                    P.op('act', lambda kc=kc: A.copy(out=dst[:, kc, 0:ncols], in_=stg[:, kc, 0:ncols]), r=[stag + 's%d' % kc], w=[tag])
                else:
                    P.op('dve', lambda kc=kc: V.tensor_copy(out=dst[:, kc, 0:ncols], in_=stg[:, kc, 0:ncols]), r=[stag + 's%d' % kc], w=[tag])
            else:
                P.op('dve', lambda kc=kc: V.tensor_scalar(out=dst[:, kc, 0:ncols], in0=stg[:, kc, 0:ncols], scalar1=gscale[:, kc:kc + 1], scalar2=None, op0=ALU.mult),
                     r=[stag + 's%d' % kc, 'g1s', 'g2s'], w=[tag])

    def norm_transpose(stack_t, xsrc_rows, hT, col0, tagx, gate_bank, act_scale=False):
        xt, junk, ss, hb = stack_t
        P.dma(xt[:], xsrc_rows, w=[tagx + 'xt'])
        P.op('act', lambda: A.activation(out=junk[:], in_=xt[:], func=AF.Square, accum_out=ss[:, 0:1]),
             r=[tagx + 'xt'], w=[tagx + 'junk', tagx + 'ss'])
        P.op('dve', lambda: V.tensor_scalar(out=ss[:, 1:2], in0=ss[:, 0:1], scalar1=1.0 / D, scalar2=EPS, op0=ALU.mult, op1=ALU.add),
             r=[tagx + 'ss'], w=[tagx + 'ss'])
        P.op('act', lambda: A.activation(out=ss[:, 2:3], in_=ss[:, 1:2], func=AF.Sqrt), r=[tagx + 'ss'], w=[tagx + 'ss'])
        P.op('dve', lambda: V.reciprocal(out=ss[:, 3:4], in_=ss[:, 2:3]), r=[tagx + 'ss'], w=[tagx + 'ss'])
        if act_scale:
            P.op('act', lambda: A.activation(out=hb[:], in_=xt[:], func=AF.Copy, scale=ss[:, 3:4]),
                 r=[tagx + 'xt', tagx + 'ss'], w=[tagx + 'hb'])
        else:
            P.op('dve', lambda: V.tensor_scalar(out=hb[:], in0=xt[:], scalar1=ss[:, 3:4], scalar2=None, op0=ALU.mult),
                 r=[tagx + 'xt', tagx + 'ss'], w=[tagx + 'hb'])
        b = gate_bank
        for kc in range(8):
            P.op('pe', lambda kc=kc: T.transpose(out=pb16(b)[:, kc * 128:(kc + 1) * 128], in_=hb[:, kc * 128:(kc + 1) * 128], identity=identb[:]),
                 r=[tagx + 'hb', 'identb'], w=[pbn[b]])
        return b

    def rope(dst, nrows, perm, ctab, stab, tmp1, tmp2, bank, tags_r, tag_w, ncols):
        P.op('pe', lambda: T.matmul(pb[bank][0:nrows, 0:ncols], lhsT=perm[0:nrows, 0:nrows], rhs=dst, start=True, stop=True, skip_group_check=True),
             r=[tag_w, 'permA', 'permI'], w=[pbn[bank]])
        P.op('dve', lambda: V.tensor_tensor(out=tmp1[0:nrows, 0:ncols], in0=dst, in1=ctab, op=ALU.mult), r=[tag_w] + tags_r, w=['ropet1'])
        P.op('dve', lambda: V.tensor_tensor(out=tmp2[0:nrows, 0:ncols], in0=pb[bank][0:nrows, 0:ncols], in1=stab, op=ALU.mult),
             r=[pbn[bank]] + tags_r, w=['ropet2'])
        P.op('dve', lambda: V.tensor_tensor(out=dst, in0=tmp1[0:nrows, 0:ncols], in1=tmp2[0:nrows, 0:ncols], op=ALU.add),
             r=['ropet1', 'ropet2'], w=[tag_w])

    sq = ExitStack()
    QP = sb(sq, "QP", [128, 9, 2, 32], F32)
    FP = sb(sq, "FP", [128, 8, 2, 32], F32)
    s5 = ExitStack()
    W1 = sb(s5, "W1", [128, 8, 2, 8, 128], BF16)
    cosT = sb(s5, "cosT", [128, 32, 64], F32)
    sinT = sb(s5, "sinT", [128, 32, 64], F32)
    R8z = sb(s5, "R8z", [128, 32, 64], F32)
    A8r = sb(s5, "A8r", [128, 32], F32)
    A8i = sb(s5, "A8i", [128, 32], F32)

    def sincos_lb(stack, n, ar, ai, dtl, pref):
        t = {}
        for nm in ['dt', 'ang', 'ex', 'mag', 'nn', 'red', 'sn', 'cs', 'lbr', 'lbi', 'fr', 'fi', 'u1', 'u2', 'u3']:
            t[nm] = sb(stack, pref + nm, [128, n], F32)
        R = [pref]
        def vv(fn):
            P.op('dve', fn, r=R, w=R)
        def aa(fn):
            P.op('act', fn, r=R, w=R)
        aa(lambda: A.activation(out=t['dt'][:], in_=dtl, func=AF.Exp))
        vv(lambda: V.tensor_tensor(out=t['ang'][:], in0=ai, in1=t['dt'][:], op=ALU.mult))
        vv(lambda: V.tensor_tensor(out=t['ex'][:], in0=ar, in1=t['dt'][:], op=ALU.mult))
        aa(lambda: A.activation(out=t['mag'][:], in_=t['ex'][:], func=AF.Exp))
        C1 = float(np.float32(2 * math.pi))
        C2 = float(2 * math.pi - np.float64(np.float32(2 * math.pi)))
        for which, off in (('sn', 0.0), ('cs', math.pi / 2)):
            vv(lambda off=off: V.tensor_scalar(out=t['u1'][:], in0=t['ang'][:], scalar1=off, scalar2=None, op0=ALU.add))
            vv(lambda: V.tensor_scalar(out=t['nn'][:], in0=t['u1'][:], scalar1=math.pi, scalar2=None, op0=ALU.is_gt))
            for kk in (3, 5, 7):
                vv(lambda kk=kk: V.tensor_scalar(out=t['u2'][:], in0=t['u1'][:], scalar1=kk * math.pi, scalar2=None, op0=ALU.is_gt))
                vv(lambda: V.tensor_tensor(out=t['nn'][:], in0=t['nn'][:], in1=t['u2'][:], op=ALU.add))
            vv(lambda: V.scalar_tensor_tensor(out=t['red'][:], in0=t['nn'][:], scalar=-C1, in1=t['u1'][:], op0=ALU.mult, op1=ALU.add))
            vv(lambda: V.scalar_tensor_tensor(out=t['red'][:], in0=t['nn'][:], scalar=-C2, in1=t['red'][:], op0=ALU.mult, op1=ALU.add))
            vv(lambda: V.tensor_scalar(out=t['red'][:], in0=t['red'][:], scalar1=math.pi, scalar2=-math.pi, op0=ALU.min, op1=ALU.max))
            aa(lambda which=which: A.activation(out=t[which][:], in_=t['red'][:], func=AF.Sin))
        vv(lambda: V.tensor_tensor(out=t['lbr'][:], in0=t['mag'][:], in1=t['cs'][:], op=ALU.mult))
        vv(lambda: V.tensor_tensor(out=t['lbi'][:], in0=t['mag'][:], in1=t['sn'][:], op=ALU.mult))
        vv(lambda: V.tensor_tensor(out=t['u1'][:], in0=ar, in1=ar, op=ALU.mult))
        vv(lambda: V.tensor_tensor(out=t['u2'][:], in0=ai, in1=ai, op=ALU.mult))
        vv(lambda: V.tensor_tensor(out=t['u1'][:], in0=t['u1'][:], in1=t['u2'][:], op=ALU.add))
        vv(lambda: V.reciprocal(out=t['u3'][:], in_=t['u1'][:]))
        vv(lambda: V.tensor_scalar(out=t['u1'][:], in0=t['lbr'][:], scalar1=-1.0, scalar2=None, op0=ALU.add))
        vv(lambda: V.tensor_tensor(out=t['fr'][:], in0=t['u1'][:], in1=ar, op=ALU.mult))
        vv(lambda: V.tensor_tensor(out=t['u2'][:], in0=t['lbi'][:], in1=ai, op=ALU.mult))
        vv(lambda: V.tensor_tensor(out=t['fr'][:], in0=t['fr'][:], in1=t['u2'][:], op=ALU.add))
        vv(lambda: V.tensor_tensor(out=t['fr'][:], in0=t['fr'][:], in1=t['u3'][:], op=ALU.mult))
        vv(lambda: V.tensor_tensor(out=t['fi'][:], in0=t['lbi'][:], in1=ar, op=ALU.mult))
        vv(lambda: V.tensor_tensor(out=t['u2'][:], in0=t['u1'][:], in1=ai, op=ALU.mult))
        vv(lambda: V.tensor_tensor(out=t['fi'][:], in0=t['fi'][:], in1=t['u2'][:], op=ALU.subtract))
        vv(lambda: V.tensor_tensor(out=t['fi'][:], in0=t['fi'][:], in1=t['u3'][:], op=ALU.mult))
        return t

    def cmul(outr, outi, ar_, ai_, br_, bi_, t1, t2, R):
        P.op('dve', lambda: V.tensor_tensor(out=t1, in0=ar_, in1=br_, op=ALU.mult), r=R, w=R)
        P.op('dve', lambda: V.tensor_tensor(out=t2, in0=ai_, in1=bi_, op=ALU.mult), r=R, w=R)
        P.op('dve', lambda: V.tensor_tensor(out=outr, in0=t1, in1=t2, op=ALU.subtract), r=R, w=R)
        P.op('dve', lambda: V.tensor_tensor(out=t1, in0=ar_, in1=bi_, op=ALU.mult), r=R, w=R)
        P.op('dve', lambda: V.tensor_tensor(out=t2, in0=ai_, in1=br_, op=ALU.mult), r=R, w=R)
        P.op('dve', lambda: V.tensor_tensor(out=outi, in0=t1, in1=t2, op=ALU.add), r=R, w=R)

    with ExitStack() as st:
        arl = sb(st, "arl", [128, 1024], F32); ail = sb(st, "ail", [128, 1024], F32); dtl = sb(st, "dtl", [128, 1024], F32)
        brl = sb(st, "brl", [128, 1024], F32); bil = sb(st, "bil", [128, 1024], F32)
        for tl, src in ((arl, AR1), (ail, AI1), (dtl, DT1), (brl, BR1), (bil, BI1)):
            P.dma(tl[:], src[:, :], w=['L1'])
        tt = sincos_lb(st, 1024, arl[:], ail[:], dtl[:], 'L1')
        pr = [sb(st, "pr%d" % i, [128, 1024], F32) for i in range(2)]
        pi = [sb(st, "pi%d" % i, [128, 1024], F32) for i in range(2)]
        c1 = sb(st, "c1", [128, 1024], F32); c2 = sb(st, "c2", [128, 1024], F32)
        R = ['L1']
        cur_r, cur_i = tt['fr'], tt['fi']
        for k in range(8):
            s_ = 7 - k
            v3 = lambda ap: ap.rearrange("p (kc n) -> p kc n", kc=8)
            P.op('dve', lambda: V.tensor_tensor(out=c1[:], in0=cur_r[:], in1=brl[:], op=ALU.mult), r=R, w=R)
            P.op('dve', lambda: V.tensor_tensor(out=c2[:], in0=cur_i[:], in1=bil[:], op=ALU.mult), r=R, w=R)
            P.op('dve', lambda s_=s_: V.tensor_tensor(out=W1[:, :, 0, s_, :], in0=v3(c1[:]), in1=v3(c2[:]), op=ALU.subtract), r=R, w=R + ['W1'])
            P.op('dve', lambda: V.tensor_tensor(out=c1[:], in0=cur_r[:], in1=bil[:], op=ALU.mult), r=R, w=R)
            P.op('dve', lambda: V.tensor_tensor(out=c2[:], in0=cur_i[:], in1=brl[:], op=ALU.mult), r=R, w=R)
            P.op('dve', lambda s_=s_: V.tensor_tensor(out=W1[:, :, 1, s_, :], in0=v3(c1[:]), in1=v3(c2[:]), op=ALU.add), r=R, w=R + ['W1'])
            if k < 7:
                nr, ni = pr[k % 2], pi[k % 2]
                cmul(nr[:], ni[:], cur_r[:], cur_i[:], tt['lbr'][:], tt['lbi'][:], c1[:], c2[:], R)
                cur_r, cur_i = nr, ni
    P.barrier()
    if stop('c1'):
        return fin()
    with ExitStack() as st:
        are = sb(st, "are", [128, 32], F32); aie = sb(st, "aie", [128, 32], F32); dte = sb(st, "dte", [128, 32], F32)
        for tl, src in ((are, ARE), (aie, AIE), (dte, DTE)):
            P.dma(tl[:], src[:, :], w=['E'])
        te = sincos_lb(st, 32, are[:], aie[:], dte[:], 'E')
        e1 = sb(st, "e1", [128, 32], F32); e2 = sb(st, "e2", [128, 32], F32)
        R = ['E']
        P.op('dve', lambda: V.memset(QP[:, 0, 0, :], 1.0), r=R, w=R)
        P.op('dve', lambda: V.memset(QP[:, 0, 1, :], 0.0), r=R, w=R)
        for k in range(1, 9):
            cmul(QP[:, k, 0, :], QP[:, k, 1, :], QP[:, k - 1, 0, :], QP[:, k - 1, 1, :], te['lbr'][:], te['lbi'][:], e1[:], e2[:], R)
        P.op('dve', lambda: V.tensor_copy(out=FP[:, 0, 0, :], in_=te['fr'][:]), r=R, w=R)
        P.op('dve', lambda: V.tensor_copy(out=FP[:, 0, 1, :], in_=te['fi'][:]), r=R, w=R)
        for k in range(1, 8):
            cmul(FP[:, k, 0, :], FP[:, k, 1, :], FP[:, k - 1, 0, :], FP[:, k - 1, 1, :], te['lbr'][:], te['lbi'][:], e1[:], e2[:], R)
        P.op('dve', lambda: V.tensor_copy(out=A8r[:], in_=QP[:, 8, 0, :]), r=R, w=R)
        P.op('dve', lambda: V.tensor_copy(out=A8i[:], in_=QP[:, 8, 1, :]), r=R, w=R)
        m2 = sb(st, "m2", [128, 32], F32); m4 = sb(st, "m4", [128, 32], F32); m8 = sb(st, "m8", [128, 32], F32)
        P.op('dve', lambda: V.tensor_tensor(out=m2[:], in0=te['mag'][:], in1=te['mag'][:], op=ALU.mult), r=R, w=R)
        P.op('dve', lambda: V.tensor_tensor(out=m4[:], in0=m2[:], in1=m2[:], op=ALU.mult), r=R, w=R)
        P.op('dve', lambda: V.tensor_tensor(out=m8[:], in0=m4[:], in1=m4[:], op=ALU.mult), r=R, w=R)
        wr = [sb(st, "wr%d" % i, [128, 32], F32) for i in range(4)]
        wi_ = [sb(st, "wi%d" % i, [128, 32], F32) for i in range(4)]
        P.op('dve', lambda: V.tensor_copy(out=wr[0][:], in_=te['cs'][:]), r=R, w=R)
        P.op('dve', lambda: V.tensor_copy(out=wi_[0][:], in_=te['sn'][:]), r=R, w=R)
        for k in range(1, 4):
            cmul(wr[k][:], wi_[k][:], wr[k - 1][:], wi_[k - 1][:], wr[k - 1][:], wi_[k - 1][:], e1[:], e2[:], R)
        P.op('dve', lambda: V.memset(cosT[:, :, 0:1], 1.0), r=R, w=R)
        P.op('dve', lambda: V.memset(sinT[:, :, 0:1], 0.0), r=R, w=R)
        sr = [sb(st, "sr%d" % i, [128, 32], F32) for i in range(2)]
        si = [sb(st, "si%d" % i, [128, 32], F32) for i in range(2)]
        big1 = sb(st, "big1", [128, 32, 32], F32); big2 = sb(st, "big2", [128, 32, 32], F32)
        stepr, stepi = wr[3], wi_[3]
        for lv in range(6):
            n = 1 << lv
            bc = lambda ap, n=n: ap.unsqueeze(2).to_broadcast([128, 32, n])
            P.op('dve', lambda: V.tensor_tensor(out=big1[:, :, 0:n], in0=cosT[:, :, 0:n], in1=bc(stepr[:]), op=ALU.mult), r=R, w=R)
            P.op('dve', lambda: V.tensor_tensor(out=big2[:, :, 0:n], in0=sinT[:, :, 0:n], in1=bc(stepi[:]), op=ALU.mult), r=R, w=R)
            P.op('dve', lambda: V.tensor_tensor(out=cosT[:, :, n:2 * n], in0=big1[:, :, 0:n], in1=big2[:, :, 0:n], op=ALU.subtract), r=R, w=R)
            P.op('dve', lambda: V.tensor_tensor(out=big1[:, :, 0:n], in0=cosT[:, :, 0:n], in1=bc(stepi[:]), op=ALU.mult), r=R, w=R)
            P.op('dve', lambda: V.tensor_tensor(out=big2[:, :, 0:n], in0=sinT[:, :, 0:n], in1=bc(stepr[:]), op=ALU.mult), r=R, w=R)
            P.op('dve', lambda: V.tensor_tensor(out=sinT[:, :, n:2 * n], in0=big1[:, :, 0:n], in1=big2[:, :, 0:n], op=ALU.add), r=R, w=R)
            if lv < 5:
                nr, ni = sr[lv % 2], si[lv % 2]
                cmul(nr[:], ni[:], stepr[:], stepi[:], stepr[:], stepi[:], e1[:], e2[:], R)
                stepr, stepi = nr, ni
        P.op('dve', lambda: V.tensor_copy(out=R8z[:], in_=m8[:].unsqueeze(2).to_broadcast([128, 32, 64])), r=R, w=R)
        P.op('dve', lambda: V.memset(R8z[:, :, 0:1], 0.0), r=R, w=R)
    P.barrier()
    if stop('c2'):
        return fin()

    with ExitStack() as st:
        WuB = sb(st, "WuB", [128, 8, 1024], BF16)
        with ExitStack() as s2:
            wstg = sb(s2, "wstgA", [128, 8, 1024], F32)
            load_weight_bf16((wstg, WuB), wu, 0, 1024, g1s, 'WuB')
            P.barrier()
            if stop('c30'):
                return fin()
        xt = [sb(st, "xtA%d" % i, [128, 1024], F32) for i in range(2)]
        junk = [sb(st, "junkA%d" % i, [128, 1024], F32) for i in range(2)]
        ssA = [sb(st, "ssA%d" % i, [128, 4], F32) for i in range(2)]
        hb = [sb(st, "hbA%d" % i, [128, 1024], BF16) for i in range(2)]
        hT = [sb(st, "hTA%d" % i, [128, 8, 512], BF16) for i in range(2)]
        uT = [sb(st, "uTA%d" % i, [128, 8, 512], BF16) for i in range(2)]
        S0b = [sb(st, "S0_%d" % i, [128, 32, 2, 64], F32) for i in range(2)]
        Z = sb(st, "Z", [128, 32, 2, 64], F32)
        ta = sb(st, "ta", [128, 32, 64], F32)
        tb = sb(st, "tb", [128, 32, 64], F32)
        Hin = sb(st, "Hin", [128, 2, 32], F32)
        cr_ = sb(st, "cr_", [128, 2, 32], F32)
        cq1 = sb(st, "cq1", [128, 32], F32); cq2 = sb(st, "cq2", [128, 32], F32)
        Hb16 = [sb(st, "Hb16_%d" % i, [128, 32, 2, 16], BF16) for i in range(2)]
        P.op('dve', lambda: V.memset(Hin[:], 0.0), w=['Hin'])
        bank_rot = [0]

        def nb():
            b = bank_rot[0]
            bank_rot[0] = (b + 1) % 8
            return b

        def stageA(t):
            hTt = hT[t % 2]; uTt = uT[t % 2]
            hn = 'hT%d' % (t % 2); un = 'uT%d' % (t % 2)
            S0 = S0b[t % 2]; s0n = 'S0_%d' % (t % 2)
            for blk in range(4):
                i2 = blk % 2
                b = nb()
                norm_transpose((xt[i2], junk[i2], ssA[i2], hb[i2]), xp[(t * 4 + blk) * 128:(t * 4 + blk + 1) * 128, :], hTt, blk * 128, 'A%d' % i2, b, act_scale=True)
                P.op('act', lambda b=b, blk=blk: A.copy(out=hTt[:, :, blk * 128:(blk + 1) * 128], in_=pb16(b).rearrange("p (kc n) -> p kc n", kc=8)),
                     r=[pbn[b]], w=[hn])
            for oc in range(8):
                b = nb()
                for kc in range(8):
                    P.op('pe', lambda b=b, oc=oc, kc=kc: T.matmul(pb[b][:, :], lhsT=WuB[:, kc, oc * 128:(oc + 1) * 128], rhs=hTt[:, kc, :],
                                                                   start=(kc == 0), stop=(kc == 7), skip_group_check=True),
                         r=['WuB', hn], w=[pbn[b]])
                P.op('act', lambda b=b, oc=oc: A.copy(out=uTt[:, oc, :], in_=pb[b][:, :]), r=[pbn[b]], w=[un])
            P.dma(HT_d[t].rearrange("p (kc n) -> p kc n", kc=8), hTt[:, :, 384:512], r=[hn], w=['HT_d'])
            P.dma(U_d[t].rearrange("p (kc n) -> p kc n", kc=8), uTt[:, :, 384:512], r=[un], w=['U_d'])
            for kc in range(8):
                for comp in range(2):
                    c0 = comp * 64
                    for s_ in range(8):
                        for pp in range(4):
                            b = 4 * (kc % 2) + pp
                            P.op('pe', lambda b=b, kc=kc, pp=pp, comp=comp, s_=s_, c0=c0: T.matmul(
                                pb[b][:, c0:c0 + 64], lhsT=W1[32 * pp:32 * pp + 32, kc, comp, s_, :], rhs=uTt[32 * pp:32 * pp + 32, kc, s_::8],
                                start=(s_ == 0), stop=(s_ == 7), skip_group_check=True, tile_position=(32 * pp, 0)),
                                r=['W1', un], w=[pbn[b]])
                for pp in range(4):
                    b = 4 * (kc % 2) + pp
                    S0v = S0[:, 4 * kc + pp, :, :].rearrange("p c m -> p (c m)")
                    P.op('act', lambda b=b, S0v=S0v: A.copy(out=S0v, in_=pb[b][:, 0:128]), r=[pbn[b]], w=[s0n])

        def stageB(t):
            S0 = S0b[t % 2]; s0n = 'S0_%d' % (t % 2)
            Hh = S0
            Sr, Si = S0[:, :, 0, :], S0[:, :, 1, :]
            Zr, Zi = Z[:, :, 0, :], Z[:, :, 1, :]
            Rr = [s0n, 'Z', 'ta', 'tb']
            P.op('dve', lambda: V.tensor_tensor(out=ta[:], in0=cosT[:], in1=Sr, op=ALU.mult), r=Rr, w=['ta'])
            P.op('pool', lambda: G.tensor_tensor(out=tb[:], in0=sinT[:], in1=Si, op=ALU.mult), r=Rr, w=['tb'])
            P.op('dve', lambda: V.tensor_tensor(out=Zr, in0=ta[:], in1=tb[:], op=ALU.add), r=Rr, w=['Z'])
            P.op('dve', lambda: V.tensor_tensor(out=ta[:], in0=cosT[:], in1=Si, op=ALU.mult), r=Rr, w=['ta'])
            P.op('pool', lambda: G.tensor_tensor(out=tb[:], in0=sinT[:], in1=Sr, op=ALU.mult), r=Rr, w=['tb'])
            P.op('dve', lambda: V.tensor_tensor(out=Zi, in0=ta[:], in1=tb[:], op=ALU.subtract), r=Rr, w=['Z'])
            Rc = ['Hin', 'cr_', 'cq']
            P.op('dve', lambda: V.tensor_tensor(out=cq1[:], in0=A8r[:], in1=Hin[:, 0, :], op=ALU.mult), r=Rc, w=Rc)
            P.op('dve', lambda: V.tensor_tensor(out=cq2[:], in0=A8i[:], in1=Hin[:, 1, :], op=ALU.mult), r=Rc, w=Rc)
            P.op('dve', lambda: V.tensor_tensor(out=cr_[:, 0, :], in0=cq1[:], in1=cq2[:], op=ALU.subtract), r=Rc, w=Rc)
            P.op('dve', lambda: V.tensor_tensor(out=cq1[:], in0=A8r[:], in1=Hin[:, 1, :], op=ALU.mult), r=Rc, w=Rc)
            P.op('dve', lambda: V.tensor_tensor(out=cq2[:], in0=A8i[:], in1=Hin[:, 0, :], op=ALU.mult), r=Rc, w=Rc)
            P.op('dve', lambda: V.tensor_tensor(out=cr_[:, 1, :], in0=cq1[:], in1=cq2[:], op=ALU.add), r=Rc, w=Rc)
            P.op('dve', lambda: V.tensor_tensor(out=Z[:, :, 0, 0], in0=Z[:, :, 0, 0], in1=cr_[:, 0, :], op=ALU.add), r=Rc + ['Z'], w=['Z'])
            P.op('dve', lambda: V.tensor_tensor(out=Z[:, :, 1, 0], in0=Z[:, :, 1, 0], in1=cr_[:, 1, :], op=ALU.add), r=Rc + ['Z'], w=['Z'])
            fl = lambda ap: ap.rearrange("p a m -> p (a m)")
            P.op('dve', lambda: V.tensor_copy(out=ta[:], in_=Zr), r=['Z'], w=['ta'])
            P.op('pool', lambda: G.tensor_copy(out=tb[:], in_=Zi), r=['Z'], w=['tb'])
            P.op('dve', lambda: V.tensor_tensor_scan(out=fl(ta[:]), data0=fl(R8z[:]), data1=fl(ta[:]), initial=0.0,
                                                    op0=ALU.mult, op1=ALU.add), r=['ta'], w=['ta'])
            P.op('dve', lambda: V.tensor_tensor_scan(out=fl(tb[:]), data0=fl(R8z[:]), data1=fl(tb[:]), initial=0.0,
                                                    op0=ALU.mult, op1=ALU.add), r=['tb'], w=['tb'])
            Rh = ['ta', 'tb', 'Z', s0n]
            P.op('dve', lambda: V.tensor_tensor(out=Zr, in0=cosT[:], in1=ta[:], op=ALU.mult), r=Rh, w=['Z'])
            P.op('pool', lambda: G.tensor_tensor(out=Zi, in0=sinT[:], in1=tb[:], op=ALU.mult), r=Rh, w=['Z'])
            P.op('dve', lambda: V.tensor_tensor(out=Hh[:, :, 0, :], in0=Zr, in1=Zi, op=ALU.subtract), r=Rh, w=[s0n])
            P.op('dve', lambda: V.tensor_tensor(out=Zr, in0=cosT[:], in1=tb[:], op=ALU.mult), r=Rh, w=['Z'])
            P.op('pool', lambda: G.tensor_tensor(out=Zi, in0=sinT[:], in1=ta[:], op=ALU.mult), r=Rh, w=['Z'])
            P.op('dve', lambda: V.tensor_tensor(out=Hh[:, :, 1, :], in0=Zr, in1=Zi, op=ALU.add), r=Rh, w=[s0n])
            P.op('dve', lambda: V.tensor_copy(out=Hin[:, 0, :], in_=Hh[:, :, 0, 63]), r=[s0n], w=['Hin'])
            P.op('dve', lambda: V.tensor_copy(out=Hin[:, 1, :], in_=Hh[:, :, 1, 63]), r=[s0n], w=['Hin'])
            hbt = Hb16[t % 2]
            P.op('dve', lambda hbt=hbt: V.tensor_copy(out=hbt[:], in_=Hh[:, :, :, 47:63]), r=[s0n], w=['Hb16_%d' % (t % 2)])
            P.dma(HS_d[t].rearrange("p (a c m) -> p a c m", a=32, c=2), hbt[:], r=['Hb16_%d' % (t % 2)], w=['HS_d'])

        stageA(0)
        for t in range(NT):
            if t + 1 < NT:
                stageA(t + 1)
            stageB(t)
    P.barrier()
    if stop('c4'):
        return fin()
    s5.close()

    with ExitStack() as st:
        CC = sb(st, "CC", [128, 32, 2, 256], BF16)
        KN = sb(st, "KN", [128, 8, 2, 8, 128], BF16)
        dfm = sb(st, "dfm", [128, 8], F32)
        P.dma(dfm[:], DFM[:, :], w=['dfm'])
        with ExitStack() as s2:
            ctr = sb(s2, "ctr", [128, 32, 16], F32); cti = sb(s2, "cti", [128, 32, 16], F32)
            ber = sb(s2, "ber", [128, 32, 32], F32); bei = sb(s2, "bei", [128, 32, 32], F32)
            P.dma(ctr[:].rearrange("p a c -> p (a c)"), CTR[:, :], w=['ctr'])
            P.dma(cti[:].rearrange("p a c -> p (a c)"), CTI[:, :], w=['cti'])
            P.dma(ber[:].rearrange("p a c -> p (a c)"), BER[:, :], w=['ber'])
            P.dma(bei[:].rearrange("p a c -> p (a c)"), BEI[:, :], w=['bei'])
            ctrb = sb(s2, "ctrb", [128, 32, 16], BF16); nctib = sb(s2, "nctib", [128, 32, 16], BF16)
            P.op('dve', lambda: V.tensor_copy(out=ctrb[:], in_=ctr[:]), r=['ctr'], w=['ctrb'])
            P.op('dve', lambda: V.tensor_scalar(out=nctib[:], in0=cti[:], scalar1=-1.0, scalar2=None, op0=ALU.mult), r=['cti'], w=['nctib'])
            k1 = sb(s2, "k1", [128, 32, 32], F32); k2 = sb(s2, "k2", [128, 32, 32], F32)
            xre = sb(s2, "xre", [128, 32, 32], BF16); xim = sb(s2, "xim", [128, 32, 32], BF16)
            R = ['cc']
            b16 = lambda ap: ap.unsqueeze(2).to_broadcast([128, 32, 16])
            b32 = lambda ap: ap.unsqueeze(2).to_broadcast([128, 32, 32])
            P.op('pool', lambda: G.memset(CC[:].rearrange("p a b c -> p (a b c)"), 0.0), w=['CC'])
            for s_ in range(8):
                qr, qi = QP[:, s_ + 1, 0, :], QP[:, s_ + 1, 1, :]
                P.op('dve', lambda: V.tensor_tensor(out=k1[:, :, 0:16], in0=ctr[:], in1=b16(qr), op=ALU.mult), r=R + ['ctr'], w=R)
                P.op('dve', lambda: V.tensor_tensor(out=k2[:, :, 0:16], in0=cti[:], in1=b16(qi), op=ALU.mult), r=R + ['cti'], w=R)
                for gg in range(2):
                    rs = slice(64 * gg, 64 * gg + 64)
                    P.op('dve', lambda s_=s_, gg=gg, rs=rs: V.tensor_tensor(out=CC[rs, :, 0, gg * 128 + s_ * 16:gg * 128 + (s_ + 1) * 16],
                                                                         in0=k1[rs, :, 0:16], in1=k2[rs, :, 0:16], op=ALU.subtract), r=R, w=R + ['CC'])
                P.op('dve', lambda: V.tensor_tensor(out=k1[:, :, 0:16], in0=ctr[:], in1=b16(qi), op=ALU.mult), r=R, w=R)
                P.op('dve', lambda: V.tensor_tensor(out=k2[:, :, 0:16], in0=cti[:], in1=b16(qr), op=ALU.mult), r=R, w=R)
                P.op('dve', lambda: V.tensor_tensor(out=k1[:, :, 0:16], in0=k1[:, :, 0:16], in1=k2[:, :, 0:16], op=ALU.add), r=R, w=R)
                for gg in range(2):
                    rs = slice(64 * gg, 64 * gg + 64)
                    P.op('dve', lambda s_=s_, gg=gg, rs=rs: V.tensor_scalar(out=CC[rs, :, 1, gg * 128 + s_ * 16:gg * 128 + (s_ + 1) * 16],
                                                                         in0=k1[rs, :, 0:16], scalar1=-1.0, scalar2=None, op0=ALU.mult), r=R, w=R + ['CC'])
            for tau in range(8):
                fr_, fi_ = FP[:, tau, 0, :], FP[:, tau, 1, :]
                P.op('dve', lambda: V.tensor_tensor(out=k1[:], in0=ber[:], in1=b32(fr_), op=ALU.mult), r=R + ['ber', 'xre', 'xim'], w=R)
                P.op('dve', lambda: V.tensor_tensor(out=k2[:], in0=bei[:], in1=b32(fi_), op=ALU.mult), r=R + ['bei'], w=R)
                P.op('dve', lambda: V.tensor_tensor(out=xre[:], in0=k1[:], in1=k2[:], op=ALU.subtract), r=R, w=R + ['xre'])
                P.op('dve', lambda: V.tensor_tensor(out=k1[:], in0=bei[:], in1=b32(fr_), op=ALU.mult), r=R, w=R)
                P.op('dve', lambda: V.tensor_tensor(out=k2[:], in0=ber[:], in1=b32(fi_), op=ALU.mult), r=R, w=R)
                P.op('dve', lambda: V.tensor_tensor(out=xim[:], in0=k1[:], in1=k2[:], op=ALU.add), r=R, w=R + ['xim'])
                for gp in range(32):
                    kc, pp = gp // 4, gp % 4
                    for gg in range(2):
                        bnk = 2 * gg + tau // 4
                        c0 = (tau % 4) * 128 + kc * 16
                        rs = slice(64 * gg, 64 * gg + 64)
                        P.op('pe', lambda bnk=bnk, gp=gp, gg=gg, pp=pp, c0=c0, rs=rs: T.matmul(
                            pb[bnk][32 * pp:32 * pp + 32, c0:c0 + 16], lhsT=xre[rs, gp, :], rhs=ctrb[rs, gp, :], start=True, stop=False,
                            skip_group_check=True, tile_position=(64 * gg, 32 * pp)), r=['xre', 'ctrb'], w=[pbn[bnk]])
                        P.op('pe', lambda bnk=bnk, gp=gp, gg=gg, pp=pp, c0=c0, rs=rs: T.matmul(
                            pb[bnk][32 * pp:32 * pp + 32, c0:c0 + 16], lhsT=xim[rs, gp, :], rhs=nctib[rs, gp, :], start=False, stop=True,
                            skip_group_check=True, tile_position=(64 * gg, 32 * pp)), r=['xim', 'nctib'], w=[pbn[bnk]])
            P.op('pool', lambda: G.memset(KN[:].rearrange("p a b c d -> p (a b c d)"), 0.0), w=['KN'])
            for tau in range(8):
                for gg in range(2):
                    bnk = 2 * gg + tau // 4
                    src = pb[bnk][:, (tau % 4) * 128:(tau % 4) * 128 + 128].rearrange("p (a c) -> p a c", a=8)
                    for sp_ in range(8 - tau):
                        s_ = sp_ + tau
                        P.op('dve', lambda src=src, sp_=sp_, s_=s_, gg=gg: V.tensor_copy(out=KN[:, :, gg, sp_, s_ * 16:(s_ + 1) * 16], in_=src),
                             r=[pbn[bnk]], w=['KN'])
        P.barrier()
        if stop('c5'):
            return fin()
        Hb_all = sb(st, "Hb_all", [128, 32, 2, 256], BF16)
        uo_all = sb(st, "uo_all", [128, 8, 2048], BF16)
        hstg = [sb(st, "hstg%d" % i, [128, 32, 2, 16], BF16) for i in range(2)]
        for t in range(NT):
            hs = hstg[t % 2]; hsn = 'hstg%d' % (t % 2)
            P.dma(hs[:], HS_d[t].rearrange("p (a c m) -> p a c m", a=32, c=2), r=['HS_d'], w=[hsn])
            if t % 2 == 0:
                P.op('act', lambda hs=hs, t=t: A.copy(out=Hb_all[:, :, :, t * 16:(t + 1) * 16], in_=hs[:]), r=[hsn], w=['Hb_all'])
            else:
                P.op('dve', lambda hs=hs, t=t: V.tensor_copy(out=Hb_all[:, :, :, t * 16:(t + 1) * 16], in_=hs[:]), r=[hsn], w=['Hb_all'])
            P.dma(uo_all[:, :, t * 128:(t + 1) * 128], U_d[t].rearrange("p (kc n) -> p kc n", kc=8), r=['U_d'], w=['uo_all'])
        nsb = [sb(st, "nsb%d" % i, [128, 256], F32) for i in range(2)]
        Yg = [sb(st, "Yg%d" % i, [128, 256], BF16) for i in range(2)]
        ytm = [sb(st, "ytm%d" % i, [128, 2, 8, 8, 16], BF16) for i in range(2)]
        ypre = [sb(st, "ypre0", [128, 2048], F32)] * 2
        g1_ = sb(st, "g1_", [128, 2048], F32); g2_ = g1_
        yg = [sb(st, "yg%d" % i, [128, 2048], BF16) for i in range(2)]
        Yd_v = Y_d.rearrange("t p (kc n) -> p t kc n", kc=8)
        for kc in range(8):
            yt = ytm[kc % 2]; ytn = 'ytm%d' % (kc % 2)
            for gl in range(8):
                gp, gg, pp = 4 * kc + gl // 2, gl % 2, gl // 2
                bF = gl % 2
                for comp in range(2):
                    P.op('pe', lambda bF=bF, gp=gp, comp=comp, gg=gg: T.matmul(
                        pb[bF][:, 0:256], lhsT=CC[:, gp, comp, gg * 128:(gg + 1) * 128], rhs=Hb_all[:, gp, comp, :], start=(comp == 0), stop=(comp == 1),
                        skip_group_check=True), r=['Hb_all', 'CC'], w=[pbn[bF]])
                bN = 2 + pp
                for sp_ in range(8):
                    P.op('pe', lambda bN=bN, pp=pp, kc=kc, gg=gg, sp_=sp_: T.matmul(
                        pb[bN][:, 0:256], lhsT=KN[32 * pp:32 * pp + 32, kc, gg, sp_, :], rhs=uo_all[32 * pp:32 * pp + 32, kc, sp_::8],
                        start=(sp_ == 0), stop=(sp_ == 7), skip_group_check=True, tile_position=(32 * pp, 0)), r=['uo_all', 'KN'], w=[pbn[bN]])
                ns = nsb[gl % 2]; nsn = 'nsb%d' % (gl % 2)
                ygt = Yg[gl % 2]; ygn = 'Yg%d' % (gl % 2)
                P.op('act', lambda bN=bN, ns=ns: A.copy(out=ns[:], in_=pb[bN][:, 0:256]), r=[pbn[bN]], w=[nsn])
                P.op('dve', lambda bF=bF, ns=ns, ygt=ygt: V.tensor_tensor(out=ygt[:], in0=pb[bF][:, 0:256], in1=ns[:], op=ALU.add), r=[pbn[bF], nsn], w=[ygn])
                for mh in range(2):
                    P.op('pe', lambda ygt=ygt, mh=mh: T.transpose(out=pb16(6)[:, mh * 128:(mh + 1) * 128], in_=ygt[:, mh * 128:(mh + 1) * 128], identity=identb[:]),
                         r=[ygn, 'identb'], w=[pbn[6]])
                P.op('act', lambda yt=yt, gl=gl: A.copy(out=yt[:, :, :, gl, :], in_=pb16(6)[:, 0:256].rearrange("p (h s c) -> p h s c", h=2, s=8)),
                     r=[pbn[6]], w=[ytn])
            yp = ypre[0]; ypn = 'ypre0'
            for mh in range(2):
                for s_ in range(8):
                    P.op('pe', lambda yt=yt, mh=mh, s_=s_: T.transpose(out=pb16(7)[:, s_ * 128:(s_ + 1) * 128], in_=yt[:, mh, s_, :, :].rearrange("p g c -> p (g c)"),
                                                                       identity=identb[:]), r=[ytn, 'identb'], w=[pbn[7]])
                ov = yp[:, mh * 1024:(mh + 1) * 1024].rearrange("p (m s) -> p s m", s=8)
                uv = uo_all[:, kc, mh * 1024:(mh + 1) * 1024].rearrange("p (m s) -> p s m", s=8)
                P.op('dve', lambda kc=kc, ov=ov, uv=uv: V.scalar_tensor_tensor(
                    out=ov, in0=uv, scalar=dfm[:, kc:kc + 1], in1=pb16(7)[:, 0:1024].rearrange("p (s m) -> p s m", s=8), op0=ALU.mult, op1=ALU.add),
                    r=['uo_all', 'dfm', pbn[7]], w=[ypn])
            yf = yp[:]
            ygo = yg[kc % 2]; ygon = 'yg%d' % (kc % 2)
            P.op('dve', lambda yf=yf: V.tensor_tensor(out=g1_[:], in0=yf, in1=yf, op=ALU.mult), r=[ypn], w=['g1_'])
            P.op('dve', lambda: V.tensor_scalar(out=g1_[:], in0=g1_[:], scalar1=0.044715, scalar2=1.0, op0=ALU.mult, op1=ALU.add), r=['g1_'], w=['g1_'])
            P.op('dve', lambda yf=yf: V.tensor_tensor(out=g1_[:], in0=g1_[:], in1=yf, op=ALU.mult), r=['g1_', ypn], w=['g1_'])
            P.op('act', lambda: A.activation(out=g1_[:], in_=g1_[:], func=AF.Sigmoid, scale=2.0 * 0.7978845608028654), r=['g1_'], w=['g1_'])
            P.op('dve', lambda ygo=ygo, yf=yf: V.tensor_tensor(out=ygo[:], in0=g1_[:], in1=yf, op=ALU.mult), r=['g1_', ypn], w=[ygon])
            P.dma(Yd_v[:, :, kc, :], ygo[:].rearrange("p (t n) -> p t n", t=NT), r=[ygon], w=['Y_d'])
    P.barrier()

    sq.close()
    if dbg and dbg[0] == 'Y':
        with ExitStack() as st:
            tmpb = sb(st, "tmpb", [128, 1024], BF16); tmpf = sb(st, "tmpf", [128, 1024], F32)
            for t in range(NT):
                P.dma(tmpb[:], Y_d[t], r=['Y_d'], w=['tmpb'])
                P.op('dve', lambda: V.tensor_copy(out=tmpf[:], in_=tmpb[:]), r=['tmpb'], w=['tmpf'])
                P.dma(dbg_d[t], tmpf[:], r=['tmpf'], w=['dbg'])
        P.barrier()
        for e in ['sp']:
            pass
        es.close()
        return nc

    if stop('Y2'):
        return fin()
    ISQ = 1.0 / math.sqrt(128.0)

    with ExitStack() as st:
        kT_all = sb(st, "kT_all", [128, 2, L], BF16)
        V_all = sb(st, "V_all", [128, 64, 256], BF16)
        kiT_all = sb(st, "kiT_all", [128, L], BF16)
        with ExitStack() as s2:
            WkvB = sb(s2, "WkvB", [128, 8, 640], BF16)
            with ExitStack() as s3:
                wstg = sb(s3, "wstgK", [128, 8, 640], F32)
                load_weight_bf16((wstg, WkvB), wkv, 0, 640, g1s, 'WkvB')
                P.barrier()
            xt = [sb(s2, "xtK%d" % i, [128, 1024], F32) for i in range(4)]
            junk = [sb(s2, "junkK0", [128, 1024], F32)] * 4
            ssA = [sb(s2, "ssK%d" % i, [128, 4], F32) for i in range(4)]
            hb = [sb(s2, "hbK%d" % i, [128, 1024], BF16) for i in range(4)]
            hT = [sb(s2, "hTK%d" % i, [128, 8, 512], BF16) for i in range(2)]
            ctab = [sb(s2, "ctabK%d" % i, [128, 512], F32) for i in range(2)]; stab = [sb(s2, "stabK%d" % i, [128, 512], F32) for i in range(2)]
            ctabI = [sb(s2, "ctabI%d" % i, [128, 512], F32) for i in range(2)]; stabI = [sb(s2, "stabI%d" % i, [128, 512], F32) for i in range(2)]
            rt1 = sb(s2, "rt1", [128, 512], F32); rt2 = sb(s2, "rt2", [128, 512], F32)
            brot = [0]

            def nb2():
                b = brot[0]
                brot[0] = (b + 1) % 8
                return b

            def a2_stage1(t):
                hTt = hT[t % 2]; hn = 'hTK%d' % (t % 2)
                for blk in range(4):
                    i2 = blk % 4
                    b = nb2()
                    norm_transpose((xt[i2], junk[i2], ssA[i2], hb[i2]), xp[(t * 4 + blk) * 128:(t * 4 + blk + 1) * 128, :], hTt, blk * 128, 'K%d' % i2, b)
                    if blk % 2 == 0:
                        P.op('act', lambda b=b, blk=blk: A.copy(out=hTt[:, :, blk * 128:(blk + 1) * 128], in_=pb16(b).rearrange("p (kc n) -> p kc n", kc=8)),
                             r=[pbn[b]], w=[hn])
                    else:
                        P.op('dve', lambda b=b, blk=blk: V.tensor_copy(out=hTt[:, :, blk * 128:(blk + 1) * 128], in_=pb16(b).rearrange("p (kc n) -> p kc n", kc=8)),
                             r=[pbn[b]], w=[hn])

            def a2_stage2(t):
                hTt = hT[t % 2]; hn = 'hTK%d' % (t % 2)
                t2 = t % 2
                cs = slice(t * 512, (t + 1) * 512)
                P.dma(ctab[t2][:], cAk[:, cs], w=['ctabK%d' % t2]); P.dma(stab[t2][:], sAk[:, cs], w=['stabK%d' % t2])
                P.dma(ctabI[t2][:], cIk[:, cs], w=['ctabI%d' % t2]); P.dma(stabI[t2][:], sIk[:, cs], w=['stabI%d' % t2])
                dsts = []
                for oc in range(3):
                    b = nb2()
                    for kc in range(8):
                        P.op('pe', lambda b=b, oc=oc, kc=kc: T.matmul(pb[b][:, :], lhsT=WkvB[:, kc, oc * 128:(oc + 1) * 128], rhs=hTt[:, kc, :],
                                                                       start=(kc == 0), stop=(kc == 7), skip_group_check=True),
                             r=['WkvB', hn], w=[pbn[b]])
                    if oc < 2:
                        dst = kT_all[:, oc, cs]
                        P.op('act', lambda b=b, dst=dst: A.copy(out=dst, in_=pb[b][:, :]), r=[pbn[b]], w=['kTt%d' % t2])
                    else:
                        dst = kiT_all[:, cs]
                        P.op('act', lambda b=b, dst=dst: A.copy(out=dst, in_=pb[b][:, :]), r=[pbn[b]], w=['kiTt%d' % t2])
                    dsts.append(dst)
                for blk in range(4):
                    b = nb2()
                    for kc in range(8):
                        P.op('pe', lambda b=b, blk=blk, kc=kc: T.matmul(pb[b][:, 0:256], lhsT=hTt[:, kc, blk * 128:(blk + 1) * 128], rhs=WkvB[:, kc, 384:640],
                                                                         start=(kc == 0), stop=(kc == 7), skip_group_check=True),
                             r=['WkvB', hn], w=[pbn[b]])
                    P.op('act' if blk % 2 == 0 else 'dve',
                         (lambda b=b, blk=blk, t=t: A.copy(out=V_all[:, 4 * t + blk, :], in_=pb[b][:, 0:256])) if blk % 2 == 0 else
                         (lambda b=b, blk=blk, t=t: V.tensor_copy(out=V_all[:, 4 * t + blk, :], in_=pb[b][:, 0:256])), r=[pbn[b]], w=['V_all'])
                for oc in range(3):
                    if oc < 2:
                        rope(dsts[oc], 128, permA, ctab[t2][:], stab[t2][:], rt1, rt2, nb2(), ['ctabK%d' % t2, 'stabK%d' % t2], 'kTt%d' % t2, 512)
                    else:
                        rope(dsts[oc], 128, permI, ctabI[t2][:], stabI[t2][:], rt1, rt2, nb2(), ['ctabI%d' % t2, 'stabI%d' % t2], 'kiTt%d' % t2, 512)

            a2_stage1(0)
            for t in range(NT):
                if t + 1 < NT:
                    a2_stage1(t + 1)
                a2_stage2(t)
        P.barrier()
        if stop('KV'):
            return fin()
        hTo = sb(st, "hTo", [128, 8, 512], BF16)
        qT = sb(st, "qT", [128, 4, 8, 128], BF16)
        qiT = sb(st, "qiT", [128, 8, 512], BF16)
        wis = sb(st, "wis", [128, 4, 16], F32)
        wst = [sb(st, "wstQ0", [128, 8, 128], F32)] * 2
        wbf = [sb(st, "wbfQ%d" % i, [128, 8, 128], BF16) for i in range(2)]
        qtmp = sb(st, "qtmp", [128, 512], BF16)
        ctq = sb(st, "ctq", [128, 512], F32); stq = sb(st, "stq", [128, 512], F32)
        rt1 = sb(st, "rt1q", [128, 512], F32); rt2 = sb(st, "rt2q", [128, 512], F32)
        Dg = sb(st, "Dg", [128, 16, 128], BF16)
        Rb = [sb(st, "Rb%d" % i, [128, 1024], BF16) for i in range(3)]
        score = sb(st, "score", [128, L], F32)
        cjunk = sb(st, "cjunk", [128, L], mybir.dt.uint8)
        mtile = [sb(st, "mtile%d" % i, [128, 512], F32) for i in range(3)]
        P.dma(mtile[0][:], mfirst[:, :], w=['mt0']); P.dma(mtile[1][:], mlast[:, :], w=['mt1']); P.dma(mtile[2][:], mboth[:, :], w=['mt2'])
        bs = sb(st, "bs", [128, 8], F32)
        mq = [sb(st, "mq0", [128, 512], BF16)] * 2
        maskT = sb(st, "maskT", [128, 64, 128], BF16)
        Eb = [sb(st, "Eb%d" % i, [128, 1024], BF16) for i in range(2)]
        PT = [sb(st, "PT%d" % i, [128, 1024], BF16) for i in range(2)]
        rden = rt1
        ybt = [sb(st, "ybt0", [128, 1024], BF16)] * 2
        for half in range(4):
            for i in range(4):
                P.dma(hTo[:, :, i * 128:(i + 1) * 128], HT_d[4 * half + i].rearrange("p (kc n) -> p kc n", kc=8), r=['HT_d'], w=['hTo'])
            wcnt = 0
            for hh in range(16):
                wi2 = wcnt % 2; wcnt += 1
                load_weight_bf16((wst[wi2], wbf[wi2]), wq, hh * 128, 128, g1s, 'wbfQ%d' % wi2, stag='wstQ')
                for j in range(1):
                    b = j
                    for kc in range(8):
                        P.op('pe', lambda b=b, kc=kc, wi2=wi2, j=j: T.matmul(pb[b][:, :], lhsT=wbf[wi2][:, kc, :], rhs=hTo[:, kc, j * 512:(j + 1) * 512],
                                                                             start=(kc == 0), stop=(kc == 7), skip_group_check=True),
                             r=['wbfQ%d' % wi2, 'hTo'], w=[pbn[b]])
                    tcs = slice((half * 4) * 128, (half * 4) * 128 + 512)
                    if hh < 8:
                        P.dma(ctq[:], cAq[:, tcs], w=['ctq']); P.dma(stq[:], sAq[:, tcs], w=['stq'])
                        P.op('act', lambda b=b: A.copy(out=qtmp[:], in_=pb[b][:, :]), r=[pbn[b]], w=['qtmp'])
                        rope(qtmp[:], 128, permA, ctq[:], stq[:], rt1, rt2, 2 + j, ['ctq', 'stq'], 'qtmp', 512)
                        P.op('act', lambda hh=hh, j=j: A.copy(out=qT[:, 4 * j:4 * j + 4, hh, :], in_=qtmp[:].rearrange("p (a n) -> p a n", a=4)),
                             r=['qtmp'], w=['qT'])
                    else:
                        P.dma(ctq[:], cIq[:, tcs], w=['ctq']); P.dma(stq[:], sIq[:, tcs], w=['stq'])
                        dst = qiT[:, hh - 8, j * 512:(j + 1) * 512]
                        P.op('act', lambda b=b, dst=dst: A.copy(out=dst, in_=pb[b][:, :]), r=[pbn[b]], w=['qiT'])
                        rope(dst, 128, permI, ctq[:], stq[:], rt1, rt2, 2 + j, ['ctq', 'stq'], 'qiT', 512)
            load_weight_bf16((wst[0], wbf[0]), wwi, 0, 16, g1s, 'wbfQ0', stag='wstQ')
            for blk in range(4):
                b = 4 + blk % 2
                for kc in range(8):
                    P.op('pe', lambda b=b, kc=kc, blk=blk: T.matmul(pb[b][:, 0:16], lhsT=hTo[:, kc, blk * 128:(blk + 1) * 128], rhs=wbf[0][:, kc, 0:16],
                                                                     start=(kc == 0), stop=(kc == 7), skip_group_check=True),
                         r=['wbfQ0', 'hTo'], w=[pbn[b]])
                P.op('dve', lambda b=b, blk=blk: V.tensor_copy(out=wis[:, blk, :], in_=pb[b][:, 0:16]), r=[pbn[b]], w=['wis'])
            pending = None
            for blk in range(4):
                gi = 4 * half + blk
                nkt = gi + 1
                n = nkt * 512
                for h in range(16):
                    if h % 2 == 0:
                        P.op('act', lambda h=h, blk=blk: A.activation(out=Dg[:, h, :], in_=ident32[:], func=AF.Copy, scale=wis[:, blk, h:h + 1]),
                             r=['wis', 'ident32'], w=['Dg'])
                    else:
                        P.op('dve', lambda h=h, blk=blk: V.tensor_scalar(out=Dg[:, h, :], in0=ident32[:], scalar1=wis[:, blk, h:h + 1], scalar2=None, op0=ALU.mult),
                             r=['wis', 'ident32'], w=['Dg'])
                for kt in range(nkt):
                    bsc = 6 + kt % 2

                    def rel_pair(p, kt=kt, blk=blk):
                        X = 2 * (p % 3)
                        for hh in range(2):
                            rs = slice(64 * hh, 64 * hh + 64)
                            P.op('pe', lambda hh=hh, rs=rs: T.matmul(
                                pb[X + hh][:, :], lhsT=qiT[rs, p, blk * 128:(blk + 1) * 128], rhs=kiT_all[rs, kt * 512:(kt + 1) * 512],
                                start=True, stop=True, skip_group_check=True, tile_position=(64 * hh, 0)), r=['qiT', 'kiT_all'], w=[pbn[X + hh]])
                        rbuf = Rb[p % 3]; rn = 'Rb%d' % (p % 3)
                        src2 = pball[:, X * 512:(X + 2) * 512]
                        if p % 2 == 0:
                            P.op('act', lambda: A.activation(out=rbuf[:], in_=src2, func=AF.Relu), r=[pbn[X], pbn[X + 1]], w=[rn])
                        else:
                            P.op('dve', lambda: V.tensor_scalar(out=rbuf[:], in0=src2, scalar1=0.0, scalar2=None, op0=ALU.max), r=[pbn[X], pbn[X + 1]], w=[rn])

                    def score_pair(p, bsc=bsc):
                        rbuf = Rb[p % 3]; rn = 'Rb%d' % (p % 3)
                        for hh in range(2):
                            h = 2 * p + hh
                            P.op('pe', lambda h=h, hh=hh: T.matmul(pb[bsc][:, :], lhsT=Dg[:, h, :], rhs=rbuf[:, hh * 512:(hh + 1) * 512], start=(h == 0), stop=(h == 15),
                                                                   skip_group_check=True), r=['Dg', rn], w=[pbn[bsc]])
                    for step in range(8 + 2):
                        if step < 8:
                            rel_pair(step)
                        if step >= 2:
                            score_pair(step - 2)
                    mt = None
                    if kt == 0 and kt == nkt - 1:
                        mt = 2
                    elif kt == 0:
                        mt = 0
                    elif kt == nkt - 1:
                        mt = 1
                    dsts = score[:, kt * 512:(kt + 1) * 512]
                    if mt is None:
                        P.op('act', lambda bsc=bsc, dsts=dsts: A.copy(out=dsts, in_=pb[bsc][:, :]), r=[pbn[bsc]], w=['score'])
                    else:
                        P.op('dve', lambda bsc=bsc, dsts=dsts, mt=mt: V.tensor_tensor(out=dsts, in0=pb[bsc][:, :], in1=mtile[mt][:], op=ALU.add),
                             r=[pbn[bsc], 'mt%d' % mt], w=['score'])
                n_act = 512 * ((gi + 1) // 2)
                thr_c = 255.5 - 0.5 * n_act
                stps = [BIS_B * 2.0 / (2.0 ** (it + 1)) for it in range(NITER)]
                P.op('dve', lambda: V.memset(bs[:, 1:2], -BIS_B + stps[0]), r=['bs1'], w=['bs1'])
                def bis_iter(it, n=n, n_act=n_act, thr_c=thr_c, stps=stps):
                    if n_act > 0:
                        P.op('act', lambda n_act=n_act: A.activation(out=cjunk[:, 0:n_act], in_=score[:, 0:n_act], func=AF.Sign, bias=bs[:, 1:2], scale=-1.0,
                                                                    accum_out=bs[:, 5:6]), r=['bs1', 'score'], w=['bs5', 'cjunkA'])
                    P.op('dve', lambda n=n, n_act=n_act: V.tensor_scalar(out=cjunk[:, n_act:n], in0=score[:, n_act:n], scalar1=bs[:, 1:2], scalar2=0.0, op0=ALU.is_ge, op1=ALU.add,
                                                                        accum_out=bs[:, 2:3]), r=['bs1', 'score'], w=['bs2', 'cjunk'])
                    if n_act > 0:
                        P.op('dve', lambda: V.scalar_tensor_tensor(out=bs[:, 6:7], in0=bs[:, 5:6], scalar=-0.5, in1=bs[:, 2:3], op0=ALU.mult, op1=ALU.add), r=['bs2', 'bs5'], w=['bs6'])
                        tcol = 6
                    else:
                        tcol = 2
                    P.op('dve', lambda it=it, tcol=tcol, thr_c=thr_c: V.tensor_scalar(out=bs[:, 3:4], in0=bs[:, tcol:tcol + 1], scalar1=thr_c, scalar2=stps[it], op0=ALU.is_ge, op1=ALU.mult),
                         r=['bs%d' % tcol], w=['bs3'])
                    if it < NITER - 1:
                        P.op('dve', lambda it=it: V.scalar_tensor_tensor(out=bs[:, 1:2], in0=bs[:, 3:4], scalar=-stps[it + 1], in1=bs[:, 1:2], op0=ALU.add, op1=ALU.add), r=['bs3', 'bs1'], w=['bs1'])
                    else:
                        P.op('dve', lambda it=it: V.scalar_tensor_tensor(out=bs[:, 0:1], in0=bs[:, 3:4], scalar=-stps[it], in1=bs[:, 1:2], op0=ALU.add, op1=ALU.add), r=['bs3', 'bs1'], w=['bs'])
                if pending is not None:
                    na = len(pending)
                    for it in range(NITER):
                        bis_iter(it)
                        for f in pending[it * na // NITER:(it + 1) * na // NITER]:
                            f()
                    pending = None
                else:
                    for it in range(NITER):
                        bis_iter(it)
                for kt in range(nkt):
                    mqt = mq[0]; mn = 'mq0'
                    P.op('dve', lambda mqt=mqt, kt=kt: V.tensor_scalar(out=mqt[:], in0=score[:, kt * 512:(kt + 1) * 512], scalar1=bs[:, 0:1], scalar2=None, op0=ALU.is_ge),
                         r=['bs', 'score'], w=[mn])
                    bt_ = 4 + kt % 2
                    for a in range(4):
                        P.op('pe', lambda mqt=mqt, a=a, bt_=bt_: T.transpose(out=pb16(bt_)[:, a * 128:(a + 1) * 128], in_=mqt[:, a * 128:(a + 1) * 128], identity=identb[:]),
                             r=[mn, 'identb'], w=[pbn[bt_]])
                    P.op('act', lambda kt=kt, bt_=bt_: A.copy(out=maskT[:, 4 * kt:4 * kt + 4, :], in_=pb16(bt_)[:, 0:512].rearrange("p (a n) -> p a n", a=4)),
                         r=[pbn[bt_]], w=['maskT'])
                def make_att(blk=blk, gi=gi, nkt=nkt):
                    steps = []
                    ybtt = ybt[0]; ybn = 'ybt0'
                    nkb = 4 * nkt
                    npair = nkb // 2
                    for g in range(2):
                        rhsq = qT[:, blk, 4 * g:4 * g + 4, :].rearrange("p a n -> p (a n)")

                        def qk_pair(pq, g=g, rhsq=rhsq):
                            X = 2 * (pq % 2)
                            for j in range(2):
                                kb = 2 * pq + j
                                P.op('pe', lambda kb=kb, j=j: T.matmul(pb[X + j][:, :], lhsT=kT_all[:, g, kb * 128:(kb + 1) * 128], rhs=rhsq,
                                                                       start=True, stop=True, skip_group_check=True), r=['kT_all', 'qT'], w=[pbn[X + j]])
                            e = Eb[pq % 2]; en = 'Eb%d' % (pq % 2)
                            P.op('act', lambda: A.activation(out=e[:], in_=pball[:, X * 512:(X + 2) * 512], func=AF.Exp, scale=ISQ), r=[pbn[X], pbn[X + 1]], w=[en])
                            pt = PT[pq % 2]; pn = 'PT%d' % (pq % 2)
                            P.op('dve', lambda: V.tensor_tensor(out=pt[:].rearrange("p (k a n) -> p k a n", k=2, a=4), in0=e[:].rearrange("p (k a n) -> p k a n", k=2, a=4),
                                                                in1=maskT[:, 2 * pq:2 * pq + 2, :].unsqueeze(2).to_broadcast([128, 2, 4, 128]), op=ALU.mult),
                                 r=[en, 'maskT'], w=[pn])

                        def pv_pair(pq, g=g, nkb=nkb):
                            pt = PT[pq % 2]; pn = 'PT%d' % (pq % 2)
                            for j in range(2):
                                kb = 2 * pq + j
                                P.op('pe', lambda kb=kb, j=j: T.matmul(pb[6][:, :], lhsT=V_all[:, kb, g * 128:(g + 1) * 128], rhs=pt[:, j * 512:(j + 1) * 512],
                                                                       start=(kb == 0), stop=(kb == nkb - 1), skip_group_check=True), r=['V_all', pn], w=[pbn[6]])
                                P.op('pe', lambda kb=kb, j=j: T.matmul(pb[7][:, :], lhsT=onesb[:], rhs=pt[:, j * 512:(j + 1) * 512],
                                                                       start=(kb == 0), stop=(kb == nkb - 1), skip_group_check=True), r=['onesb', pn], w=[pbn[7]])

                        def one_step(step, qk_pair=qk_pair, pv_pair=pv_pair, npair=npair):
                            if step < npair:
                                qk_pair(step)
                            if step >= 1:
                                pv_pair(step - 1)
                        for step in range(npair + 1):
                            steps.append(lambda step=step, one_step=one_step: one_step(step))

                        def fin_g(g=g):
                            P.op('dve', lambda: V.reciprocal(out=rden[:], in_=pb[7][:, :]), r=[pbn[7]], w=['ropet1'])
                            P.op('dve', lambda: V.tensor_tensor(out=ybtt[:, g * 512:(g + 1) * 512], in0=pb[6][:, :], in1=rden[:], op=ALU.mult),
                                 r=[pbn[6], 'ropet1'], w=[ybn])
                        steps.append(fin_g)
                    steps.append(lambda: P.dma(YB_d[gi], ybtt[:], r=[ybn], w=['YB_d']))
                    return steps
                att = make_att()
                if blk == 3:
                    for f in att:
                        f()
                    pending = None
                else:
                    pending = att
    P.barrier()
    if stop('ATT'):
        return fin()

    with ExitStack() as st:
        h2T = sb(st, "h2T", [128, 8, 2048], BF16)
        sA = ExitStack()
        mT = sb(sA, "mT", [128, 8, 2048], BF16)
        with ExitStack() as s2:
            yaT = sb(s2, "yaT", [128, 8, 2048], BF16)
            wst = [sb(s2, "wstD%d" % i, [128, 8, 128], F32) for i in range(4)]
            wbf = [sb(s2, "wbfD%d" % i, [128, 8, 128], BF16) for i in range(4)]
            sg = [sb(s2, "sgD%d" % i, [128, 512], F32) for i in range(4)]
            with ExitStack() as s3:
                yT = sb(s3, "yT", [128, 8, 2048], BF16)
                for t in range(NT):
                    P.dma(yT[:, :, t * 128:(t + 1) * 128], Y_d[t].rearrange("p (kc n) -> p kc n", kc=8), r=['Y_d'], w=['yT'])
                for oc in range(8):
                    w2 = oc % 2
                    load_weight_bf16((wst[w2], wbf[w2]), wglu, oc * 128, 128, None, 'wbfD%d' % w2)
                    for j in range(4):
                        b = (oc * 4 + j) % 8
                        for kc in range(8):
                            P.op('pe', lambda b=b, kc=kc, w2=w2, j=j: T.matmul(pb[b][:, :], lhsT=wbf[w2][:, kc, :], rhs=yT[:, kc, j * 512:(j + 1) * 512],
                                                                               start=(kc == 0), stop=(kc == 7), skip_group_check=True), r=['wbfD%d' % w2, 'yT'], w=[pbn[b]])
                        sgt = sg[j % 2]; sn = 'sgD%d' % (j % 2)
                        P.op('act', lambda b=b, sgt=sgt: A.activation(out=sgt[:], in_=pb[b][:, :], func=AF.Sigmoid), r=[pbn[b]], w=[sn])
                        P.op('dve', lambda oc=oc, j=j, sgt=sgt: V.tensor_tensor(out=yaT[:, oc, j * 512:(j + 1) * 512], in0=sgt[:], in1=yT[:, oc, j * 512:(j + 1) * 512], op=ALU.mult),
                             r=[sn, 'yT'], w=['yaT'])
                P.barrier()
            ybT = sb(s2, "ybT", [128, 8, 2048], BF16)
            hTa = sb(s2, "hTa", [128, 8, 2048], BF16)
            for t in range(NT):
                P.dma(ybT[:, :, t * 128:(t + 1) * 128], YB_d[t].rearrange("p (h n) -> p h n", h=8), r=['YB_d'], w=['ybT'])
                P.dma(hTa[:, :, t * 128:(t + 1) * 128], HT_d[t].rearrange("p (kc n) -> p kc n", kc=8), r=['HT_d'], w=['hTa'])
            m1 = sb(s2, "mm1", [128, 512], F32); m2 = sb(s2, "mm2", [128, 512], F32)
            for oc in range(8):
                load_weight_bf16((wst[0], wbf[0]), wba, oc * 128, 128, None, 'wbfD0')
                load_weight_bf16((wst[1], wbf[1]), wbb, oc * 128, 128, None, 'wbfD1')
                load_weight_bf16((wst[2], wbf[2]), wg, oc * 128, 128, g1s, 'wbfD2')
                load_weight_bf16((wst[3], wbf[3]), wg, 1024 + oc * 128, 128, g1s, 'wbfD3')
                for j in range(4):
                    js = slice(j * 512, (j + 1) * 512)
                    acts = [yaT, ybT, hTa, hTa]; anm = ['yaT', 'ybT', 'hTa', 'hTa']
                    for q4 in range(4):
                        b = 4 * (j % 2) + q4
                        for kc in range(8):
                            P.op('pe', lambda b=b, kc=kc, q4=q4, js=js, acts=acts: T.matmul(pb[b][:, :], lhsT=wbf[q4][:, kc, :], rhs=acts[q4][:, kc, js],
                                                                                          start=(kc == 0), stop=(kc == 7), skip_group_check=True),
                                 r=['wbfD%d' % q4, anm[q4]], w=[pbn[b]])
                    b0 = 4 * (j % 2)
                    P.op('act', lambda b0=b0: A.activation(out=sg[0][:], in_=pb[b0 + 2][:, :], func=AF.Sigmoid), r=[pbn[b0 + 2]], w=['sgD0'])
                    P.op('act', lambda b0=b0: A.activation(out=sg[1][:], in_=pb[b0 + 3][:, :], func=AF.Sigmoid), r=[pbn[b0 + 3]], w=['sgD1'])
                    P.op('dve', lambda b0=b0: V.tensor_tensor(out=m1[:], in0=pb[b0][:, :], in1=sg[0][:], op=ALU.mult), r=[pbn[b0], 'sgD0'], w=['m1'])
                    P.op('dve', lambda b0=b0: V.tensor_tensor(out=m2[:], in0=pb[b0 + 1][:, :], in1=sg[1][:], op=ALU.mult), r=[pbn[b0 + 1], 'sgD1'], w=['m2'])
                    P.op('dve', lambda oc=oc, js=js: V.tensor_tensor(out=mT[:, oc, js], in0=m1[:], in1=m2[:], op=ALU.add), r=['m1', 'm2'], w=['mT'])
            P.barrier()
        if stop('MRG'):
            return fin()
        with ExitStack() as s2:
            WoB = sb(s2, "WoB", [128, 8, 1024], BF16)
            with ExitStack() as s3:
                wstg = sb(s3, "wstgO", [128, 8, 1024], F32)
                load_weight_bf16((wstg, WoB), wout, 0, 1024, None, 'WoB')
                P.barrier()
            xt = [sb(s2, "xtF%d" % i, [128, 1024], F32) for i in range(2)]
            x1t = [sb(s2, "x1F%d" % i, [128, 1024], F32) for i in range(2)]
            junk = sb(s2, "junkF", [128, 1024], F32)
            ssF = [sb(s2, "ssF%d" % i, [128, 4], F32) for i in range(2)]
            hbF = [sb(s2, "hbF%d" % i, [128, 1024], BF16) for i in range(2)]
            for gi in range(NT):
                i2 = gi % 2
                rows = slice((4 * gi + 3) * 128, (4 * gi + 4) * 128)
                P.dma(xt[i2][:], xp[rows, :], w=['xtF%d' % i2])
                for hf in range(2):
                    b = 2 * i2 + hf
                    for kc in range(8):
                        P.op('pe', lambda b=b, kc=kc, gi=gi, hf=hf: T.matmul(pb[b][:, :], lhsT=mT[:, kc, gi * 128:(gi + 1) * 128], rhs=WoB[:, kc, hf * 512:(hf + 1) * 512],
                                                                             start=(kc == 0), stop=(kc == 7), skip_group_check=True), r=['mT', 'WoB'], w=[pbn[b]])
                    P.op('dve', lambda b=b, hf=hf, i2=i2: V.tensor_tensor(out=x1t[i2][:, hf * 512:(hf + 1) * 512], in0=pb[b][:, :], in1=xt[i2][:, hf * 512:(hf + 1) * 512], op=ALU.add),
                         r=[pbn[b], 'xtF%d' % i2], w=['x1F%d' % i2])
                P.dma(X1_d[gi], x1t[i2][:], r=['x1F%d' % i2], w=['X1_d'])
                tg = 'F%d' % i2
                P.op('act', lambda i2=i2: A.activation(out=junk[:], in_=x1t[i2][:], func=AF.Square, accum_out=ssF[i2][:, 0:1]), r=['x1F%d' % i2], w=['junkF', tg + 'ss'])
                P.op('dve', lambda i2=i2: V.tensor_scalar(out=ssF[i2][:, 1:2], in0=ssF[i2][:, 0:1], scalar1=1.0 / D, scalar2=EPS, op0=ALU.mult, op1=ALU.add), r=[tg + 'ss'], w=[tg + 'ss'])
                P.op('act', lambda i2=i2: A.activation(out=ssF[i2][:, 2:3], in_=ssF[i2][:, 1:2], func=AF.Sqrt), r=[tg + 'ss'], w=[tg + 'ss'])
                P.op('dve', lambda i2=i2: V.reciprocal(out=ssF[i2][:, 3:4], in_=ssF[i2][:, 2:3]), r=[tg + 'ss'], w=[tg + 'ss'])
                P.op('dve', lambda i2=i2: V.tensor_scalar(out=hbF[i2][:], in0=x1t[i2][:], scalar1=ssF[i2][:, 3:4], scalar2=None, op0=ALU.mult), r=['x1F%d' % i2, tg + 'ss'], w=[tg + 'hb'])
                bt_ = 4 + i2
                for kc in range(8):
                    P.op('pe', lambda kc=kc, i2=i2, bt_=bt_: T.transpose(out=pb16(bt_)[:, kc * 128:(kc + 1) * 128], in_=hbF[i2][:, kc * 128:(kc + 1) * 128], identity=identb[:]),
                         r=[tg + 'hb', 'identb'], w=[pbn[bt_]])
                P.op('act', lambda gi=gi, bt_=bt_: A.copy(out=h2T[:, :, gi * 128:(gi + 1) * 128], in_=pb16(bt_).rearrange("p (kc n) -> p kc n", kc=8)), r=[pbn[bt_]], w=['h2T'])
            P.barrier()
        sA.close()
        if stop('F'):
            return fin()
        actT = sb(st, "actT", [128, 22, 2048], BF16)
        with ExitStack() as s2:
            wst = [sb(s2, "wstG%d" % i, [128, 8, 128], F32) for i in range(2)]
            wbf = [sb(s2, "wbfG%d" % i, [128, 8, 128], BF16) for i in range(2)]
            sgl = [sb(s2, "sgG%d" % i, [128, 512], F32) for i in range(2)]
            for jh in range(22):
                load_weight_bf16((wst[0], wbf[0]), wfi, jh * 128, 128, g2s, 'wbfG0')
                load_weight_bf16((wst[1], wbf[1]), wfi, FH + jh * 128, 128, g2s, 'wbfG1')
                for tg_ in range(4):
                    ts_ = slice(tg_ * 512, (tg_ + 1) * 512)
                    b0 = 2 * (tg_ % 4)
                    for q2 in range(2):
                        for kc in range(8):
                            P.op('pe', lambda b0=b0, q2=q2, kc=kc, ts_=ts_: T.matmul(pb[b0 + q2][:, :], lhsT=wbf[q2][:, kc, :], rhs=h2T[:, kc, ts_],
                                                                                   start=(kc == 0), stop=(kc == 7), skip_group_check=True), r=['wbfG%d' % q2, 'h2T'], w=[pbn[b0 + q2]])
                    sgt = sgl[tg_ % 2]; sn = 'sgG%d' % (tg_ % 2)
                    P.op('act', lambda b0=b0, sgt=sgt: A.activation(out=sgt[:], in_=pb[b0][:, :], func=AF.Silu), r=[pbn[b0]], w=[sn])
                    P.op('dve', lambda b0=b0, sgt=sgt, jh=jh, ts_=ts_: V.tensor_tensor(out=actT[:, jh, ts_], in0=pb[b0 + 1][:, :], in1=sgt[:], op=ALU.mult),
                         r=[pbn[b0 + 1], sn], w=['actT'])
            P.barrier()
        with ExitStack() as s2:
            WfoB = sb(s2, "WfoB", [128, 22, 1024], BF16)
            wstg2 = [sb(s2, "wstgFo%d" % i, [128, 1024], F32) for i in range(2)]
            for jh in range(22):
                i2 = jh % 2
                P.dma(wstg2[i2][:], wfo[jh * 128:(jh + 1) * 128, :], w=['wstgFo%d' % i2])
                if i2 == 0:
                    P.op('act', lambda jh=jh, i2=i2: A.copy(out=WfoB[:, jh, :], in_=wstg2[i2][:]), r=['wstgFo%d' % i2], w=['WfoB'])
                else:
                    P.op('dve', lambda jh=jh, i2=i2: V.tensor_copy(out=WfoB[:, jh, :], in_=wstg2[i2][:]), r=['wstgFo%d' % i2], w=['WfoB'])
            gft = sb(s2, "gft", [128, 1024], F32)
            P.dma(gft[:], gfb[:, :], w=['gft'])
            x1r = [sb(s2, "x1r%d" % i, [128, 1024], F32) for i in range(2)]
            x2 = [sb(s2, "x2_%d" % i, [128, 1024], F32) for i in range(2)]
            junkG = sb(s2, "junkG", [128, 1024], F32)
            ssG = [sb(s2, "ssG%d" % i, [128, 4], F32) for i in range(2)]
            ot = [sb(s2, "ot%d" % i, [128, 1024], F32) for i in range(2)]
            for gi in range(NT):
                i2 = gi % 2
                P.dma(x1r[i2][:], X1_d[gi], r=['X1_d'], w=['x1r%d' % i2])
                for hf in range(2):
                    b = 2 * i2 + hf
                    for jh in range(22):
                        P.op('pe', lambda b=b, jh=jh, gi=gi, hf=hf: T.matmul(pb[b][:, :], lhsT=actT[:, jh, gi * 128:(gi + 1) * 128], rhs=WfoB[:, jh, hf * 512:(hf + 1) * 512],
                                                                             start=(jh == 0), stop=(jh == 21), skip_group_check=True), r=['actT', 'WfoB'], w=[pbn[b]])
                    P.op('dve', lambda b=b, hf=hf, i2=i2: V.tensor_tensor(out=x2[i2][:, hf * 512:(hf + 1) * 512], in0=pb[b][:, :], in1=x1r[i2][:, hf * 512:(hf + 1) * 512], op=ALU.add),
                         r=[pbn[b], 'x1r%d' % i2], w=['x2_%d' % i2])
                tg = 'G%d' % i2
                P.op('act', lambda i2=i2: A.activation(out=junkG[:], in_=x2[i2][:], func=AF.Square, accum_out=ssG[i2][:, 0:1]), r=['x2_%d' % i2], w=['junkG', tg + 'ss'])
                P.op('dve', lambda i2=i2: V.tensor_scalar(out=ssG[i2][:, 1:2], in0=ssG[i2][:, 0:1], scalar1=1.0 / D, scalar2=EPS, op0=ALU.mult, op1=ALU.add), r=[tg + 'ss'], w=[tg + 'ss'])
                P.op('act', lambda i2=i2: A.activation(out=ssG[i2][:, 2:3], in_=ssG[i2][:, 1:2], func=AF.Sqrt), r=[tg + 'ss'], w=[tg + 'ss'])
                P.op('dve', lambda i2=i2: V.reciprocal(out=ssG[i2][:, 3:4], in_=ssG[i2][:, 2:3]), r=[tg + 'ss'], w=[tg + 'ss'])
                P.op('dve', lambda i2=i2: V.tensor_scalar(out=ot[i2][:], in0=x2[i2][:], scalar1=ssG[i2][:, 3:4], scalar2=None, op0=ALU.mult), r=['x2_%d' % i2, tg + 'ss'], w=['ot%d' % i2])
                P.op('dve', lambda i2=i2: V.tensor_tensor(out=ot[i2][:], in0=ot[i2][:], in1=gft[:], op=ALU.mult), r=['ot%d' % i2, 'gft'], w=['ot%d' % i2])
                P.dma(out_d[gi * 128:(gi + 1) * 128, :], ot[i2][:], r=['ot%d' % i2], w=['out_d'])
            P.barrier()
    P.barrier()
    return nc


def _host_prep(inputs):
    x = np.asarray(inputs['x'], np.float32)
    w_in = np.asarray(inputs['w_in'], np.float32)[0]
    pts = np.cumsum([1024, 1024, 256, 256, 1024, 64, 16, 1024, 1024])
    wu, wq_, wk, wv, wqi, wki, wwi, wga, wgb = np.split(w_in, pts[:-1], axis=1)
    a_re = np.asarray(inputs['a_re'], np.float32)[0]; a_im = np.asarray(inputs['a_im'], np.float32)[0]
    log_dt = np.asarray(inputs['log_dt'], np.float32)[0]
    b_re = np.asarray(inputs['b_re'], np.float32)[0]; b_im = np.asarray(inputs['b_im'], np.float32)[0]
    c_re = np.asarray(inputs['c_re'], np.float32)[0]; c_im = np.asarray(inputs['c_im'], np.float32)[0]
    d_skip = np.asarray(inputs['d_skip'], np.float32)[0]

    def gT(g):
        return np.ascontiguousarray(np.asarray(g, np.float32).reshape(8, 128).T)

    r = np.arange(128); kc = np.arange(8)
    pp = r // 32; ggr = (r // 16) % 2; cr = r % 16
    AR1 = np.zeros((128, 8, 2, 64), np.float32); AI1 = np.zeros_like(AR1); DT1 = np.zeros_like(AR1)
    BR1 = np.zeros_like(AR1); BI1 = np.zeros_like(AR1)
    for k in range(8):
        for g2 in range(2):
            g = 2 * (4 * k + pp) + g2
            AR1[:, k, g2, :] = a_re[g, :]
            AI1[:, k, g2, :] = a_im[g, :]
            DT1[:, k, g2, :] = log_dt[g][:, None]
            sel = (ggr == g2)
            BR1[sel, k, g2, :] = b_re[g[sel], :, cr[sel]]
            BI1[sel, k, g2, :] = b_im[g[sel], :, cr[sel]]
    gg = np.arange(128) // 64; p_ = np.arange(128) % 64
    gidx = 2 * np.arange(32)[None, :] + gg[:, None]
    ARE = a_re[gidx, p_[:, None]]; AIE = a_im[gidx, p_[:, None]]; DTE = log_dt[gidx]
    CTR = c_re[gidx, :, p_[:, None]]
    CTI = c_im[gidx, :, p_[:, None]]
    BER = np.zeros((128, 32, 2, 16), np.float32); BEI = np.zeros_like(BER)
    for g2 in range(2):
        sel = gg == g2
        BER[sel, :, g2, :] = b_re[gidx[sel], p_[sel][:, None], :]
        BEI[sel, :, g2, :] = b_im[gidx[sel], p_[sel][:, None], :]
    DFM = np.ascontiguousarray(d_skip.reshape(8, 128).T)
    common = dict(
        wu=np.ascontiguousarray(wu),
        wkv=np.ascontiguousarray(np.concatenate([wk, wki, wki, wv], axis=1)),
        wq=np.ascontiguousarray(np.concatenate([wq_, wqi], axis=1)),
        wwi=np.ascontiguousarray(wwi),
        wg=np.ascontiguousarray(np.concatenate([wga, wgb], axis=1)),
        wglu=np.asarray(inputs['w_glu'], np.float32)[0], wba=np.asarray(inputs['w_branch_a'], np.float32)[0],
        wbb=np.asarray(inputs['w_branch_b'], np.float32)[0], wout=np.asarray(inputs['w_out'], np.float32)[0],
        wfi=np.asarray(inputs['w_ffn_in'], np.float32)[0], wfo=np.asarray(inputs['w_ffn_out'], np.float32)[0],
        g1T=gT(inputs['norm1_g'][0]), g2T=gT(inputs['norm2_g'][0]),
        gfb=np.ascontiguousarray(np.broadcast_to(np.asarray(inputs['norm_f_g'], np.float32)[None, :], (128, D))),
        AR1=AR1.reshape(128, 1024), AI1=AI1.reshape(128, 1024), DT1=DT1.reshape(128, 1024),
        BR1=BR1.reshape(128, 1024), BI1=BI1.reshape(128, 1024),
        ARE=np.ascontiguousarray(ARE), AIE=np.ascontiguousarray(AIE), DTE=np.ascontiguousarray(DTE),
        CTR=np.ascontiguousarray(CTR).reshape(128, 512), CTI=np.ascontiguousarray(CTI).reshape(128, 512),
        BER=BER.reshape(128, 1024), BEI=BEI.reshape(128, 1024), DFM=DFM,
        identf=np.eye(128, dtype=np.float32),
    )
    permA = np.zeros((128, 128), np.float32)
    for m in range(32):
        permA[m + 16 if m < 16 else m - 16, m] = 1
    permI = np.zeros((128, 128), np.float32)
    for hb in (0, 64):
        for m in range(16):
            permI[hb + (m + 8 if m < 8 else m - 8), hb + m] = 1
    common['permA'] = permA; common['permI'] = permI

    def tables(pos, kind):
        pos = pos.astype(np.float32)
        cos = np.ones((128, pos.shape[0]), np.float32); sin = np.zeros_like(cos)
        if kind == 'A':
            half = 16
            inv = (np.float32(500000.0) ** (-np.arange(half, dtype=np.float32) / half)).astype(np.float32)
            ang = pos[None, :] * inv[:, None]
            cos[0:16] = np.cos(ang); cos[16:32] = np.cos(ang)
            sin[0:16] = -np.sin(ang); sin[16:32] = np.sin(ang)
        else:
            half = 8
            inv = (np.float32(500000.0) ** (-np.arange(half, dtype=np.float32) / half)).astype(np.float32)
            ang = pos[None, :] * inv[:, None]
            for hb in (0, 64):
                cos[hb:hb + 8] = np.cos(ang); cos[hb + 8:hb + 16] = np.cos(ang)
                sin[hb:hb + 8] = -np.sin(ang); sin[hb + 8:hb + 16] = np.sin(ang)
        return cos, sin

    in_maps = []
    for c in range(8):
        b, r_ = c // 4, c % 4
        pad = (3 - r_) * 128
        xpad = np.zeros((L, D), np.float32)
        xpad[pad:] = x[b, :L - pad]
        pos_all = np.maximum(np.arange(L) - pad, 0)
        own = np.concatenate([np.arange(128) + (4 * i + 3) * 128 for i in range(NT)])
        pos_own = own - pad
        m = dict(common)
        m['xp'] = xpad
        m['cAk'], m['sAk'] = tables(pos_all, 'A')
        m['cIk'], m['sIk'] = tables(pos_all, 'I')
        m['cAq'], m['sAq'] = tables(pos_own, 'A')
        m['cIq'], m['sIq'] = tables(pos_own, 'I')
        q = np.arange(128)[:, None]; kk = np.arange(512)[None, :]
        caus = np.where(((kk < 384) | ((kk - 384) // 64 <= q // 64)), 0.0, NEG).astype(np.float32)
        padm = np.where(kk >= pad, 0.0, NEG).astype(np.float32) * np.ones((128, 1), np.float32)
        m['mfirst'] = np.ascontiguousarray(padm)
        m['mlast'] = np.ascontiguousarray(caus)
        m['mboth'] = np.minimum(padm, caus).astype(np.float32)
        in_maps.append(m)
    return in_maps


def kernel(**inputs):
    in_maps = _host_prep(inputs)
    nc = build_program()
    res = run_bass_kernel_spmd(nc, in_maps, core_ids=list(range(8)))
    out = np.zeros((2, L, D), np.float32)
    for c in range(8):
        b, r_ = c // 4, c % 4
        o = res.results[c]["out"].reshape(NT, 128, D)
        for i in range(NT):
            j = 4 * i + r_
            out[b, j * 128:(j + 1) * 128] = o[i]
    return out
```

```python
import math
from contextlib import ExitStack
import numpy as np
import concourse.bass as bass
import concourse.mybir as mybir
from concourse.bass_utils import run_bass_kernel_spmd

F32 = mybir.dt.float32
BF16 = mybir.dt.bfloat16
AF = mybir.ActivationFunctionType
ALU = mybir.AluOpType

D = 1024
L = 8192
NT = 16
FH = 2816
EPS = 1e-6
NEG = -1e30
NITER = 18
BIS_B = 64.0
NDS = 24


class Prog:
    def __init__(self, nc, es):
        self.nc = nc
        self.E = {'pe': nc.tensor, 'act': nc.scalar, 'dve': nc.vector, 'pool': nc.gpsimd, 'sp': nc.sync}
        self.sem = {k: es.enter_context(nc.semaphore('s_' + k)) for k in ['pe', 'act', 'dve', 'pool']}
        self.cnt = {k: 0 for k in self.sem}
        self.dsem = [es.enter_context(nc.semaphore('d%d' % i)) for i in range(NDS)]
        self.dcnt = [0] * NDS
        self.dnext = 0
        self.seen = {e: {} for e in self.E}
        self.lw = {}
        self.rd = {}

    def _semobj(self, key):
        return self.dsem[key[1]] if isinstance(key, tuple) else self.sem[key]

    def _wait(self, eng, ev):
        key, val = ev
        if self.seen[eng].get(key, 0) >= val:
            return
        self.seen[eng][key] = val
        self.E[eng].wait_ge(self._semobj(key), val)

    def _deps(self, eng, r, w):
        evs = {}
        for x in r:
            if x in self.lw:
                k, v = self.lw[x]
                evs[k] = max(evs.get(k, 0), v)
        for x in w:
            if x in self.lw:
                k, v = self.lw[x]
                evs[k] = max(evs.get(k, 0), v)
            for k, v in self.rd.get(x, {}).items():
                evs[k] = max(evs.get(k, 0), v)
        for k, v in evs.items():
            if eng == 'pe' and k == 'pe':
                continue
            self._wait(eng, (k, v))

    def _record(self, me, r, w):
        k, v = me
        for x in r:
            d = self.rd.setdefault(x, {})
            d[k] = max(d.get(k, 0), v)
        for x in w:
            self.lw[x] = me
            self.rd[x] = {}

    def op(self, eng, fn, r=(), w=()):
        self._deps(eng, r, w)
        ins = fn()
        self.cnt[eng] += 1
        ins.then_inc(self.sem[eng], 1)
        self._record((eng, self.cnt[eng]), r, w)

    def dma(self, out, in_, r=(), w=(), q='sp'):
        self._deps(q, r, w)
        i = self.dnext
        self.dnext = (i + 1) % NDS
        if self.dcnt[i] > 0:
            self._wait(q, (('d', i), self.dcnt[i]))
        self.E[q].dma_start(out=out, in_=in_).then_inc(self.dsem[i], 16)
        self.dcnt[i] += 16
        self._record((('d', i), self.dcnt[i]), r, w)

    def barrier(self):
        for e in ['pe', 'act', 'dve', 'pool', 'sp']:
            for k in self.sem:
                if self.cnt[k] > 0:
                    self._wait(e, (k, self.cnt[k]))
            for i in range(NDS):
                if self.dcnt[i] > 0:
                    self._wait(e, (('d', i), self.dcnt[i]))
        self.lw = {}
        self.rd = {}


def build_program(dbg=None):
    nc = bass.Bass("TRN2", target_bir_lowering=False)

    def din(name, shape, dt=F32):
        return nc.dram_tensor(name, list(shape), dt, kind="ExternalInput").ap()

    def dscr(name, shape, dt):
        return nc.dram_tensor(name, list(shape), dt, kind="Internal").ap()

    xp = din("xp", [L, D])
    wu = din("wu", [D, 1024])
    wkv = din("wkv", [D, 640])
    wq = din("wq", [D, 2048])
    wwi = din("wwi", [D, 16])
    wg = din("wg", [D, 2048])
    wglu = din("wglu", [D, D])
    wba = din("wba", [D, D])
    wbb = din("wbb", [D, D])
    wout = din("wout", [D, D])
    wfi = din("wfi", [D, 2 * FH])
    wfo = din("wfo", [FH, D])
    g1T = din("g1T", [128, 8])
    g2T = din("g2T", [128, 8])
    gfb = din("gfb", [128, D])
    AR1 = din("AR1", [128, 1024]); AI1 = din("AI1", [128, 1024]); DT1 = din("DT1", [128, 1024])
    BR1 = din("BR1", [128, 1024]); BI1 = din("BI1", [128, 1024])
    ARE = din("ARE", [128, 32]); AIE = din("AIE", [128, 32]); DTE = din("DTE", [128, 32])
    CTR = din("CTR", [128, 512]); CTI = din("CTI", [128, 512])
    BER = din("BER", [128, 1024]); BEI = din("BEI", [128, 1024])
    DFM = din("DFM", [128, 8])
    identf_d = din("identf", [128, 128])
    permA_d = din("permA", [128, 128]); permI_d = din("permI", [128, 128])
    cAk = din("cAk", [128, L]); sAk = din("sAk", [128, L])
    cIk = din("cIk", [128, L]); sIk = din("sIk", [128, L])
    cAq = din("cAq", [128, 2048]); sAq = din("sAq", [128, 2048])
    cIq = din("cIq", [128, 2048]); sIq = din("sIq", [128, 2048])
    mfirst = din("mfirst", [128, 512]); mlast = din("mlast", [128, 512]); mboth = din("mboth", [128, 512])
    out_d = nc.dram_tensor("out", [2048, D], F32, kind="ExternalOutput").ap()
    dbg_d = None
    if dbg:
        dbg_d = nc.dram_tensor("dbg", list(dbg[1]), F32, kind="ExternalOutput").ap()

    HT_d = dscr("HT_d", [NT, 128, 1024], BF16)
    U_d = dscr("U_d", [NT, 128, 1024], BF16)
    HS_d = dscr("HS_d", [NT, 128, 1024], BF16)
    Y_d = dscr("Y_d", [NT, 128, 1024], BF16)
    YB_d = dscr("YB_d", [NT, 128, 1024], BF16)
    X1_d = dscr("X1_d", [NT, 128, 1024], F32)

    es = ExitStack()
    P = Prog(nc, es)
    V, A, G, T = nc.vector, nc.scalar, nc.gpsimd, nc.tensor

    def sb(stack, name, shape, dt):
        return stack.enter_context(nc.sbuf_tensor(name, list(shape), dt))

    def fin():
        P.barrier()
        return nc

    def stop(name):
        return bool(dbg) and dbg[0] == name

    identf = sb(es, "identf_s", [128, 128], F32)
    identb = sb(es, "identb", [128, 128], BF16)
    permA = sb(es, "permA_s", [128, 128], BF16)
    permI = sb(es, "permI_s", [128, 128], BF16)
    onesb = sb(es, "onesb", [128, 128], BF16)
    g1s = sb(es, "g1s", [128, 8], F32)
    g2s = sb(es, "g2s", [128, 8], F32)
    ptmp = sb(es, "ptmp", [128, 128], F32)
    P.dma(identf[:], identf_d[:, :], w=['identf'])
    P.op('dve', lambda: V.tensor_copy(out=identb[:], in_=identf[:]), r=['identf'], w=['identb'])
    P.dma(ptmp[:], permA_d[:, :], w=['ptmp'])
    P.op('dve', lambda: V.tensor_copy(out=permA[:], in_=ptmp[:]), r=['ptmp'], w=['permA'])
    P.dma(ptmp[:], permI_d[:, :], w=['ptmp'])
    P.op('dve', lambda: V.tensor_copy(out=permI[:], in_=ptmp[:]), r=['ptmp'], w=['permI'])
    P.op('pool', lambda: G.memset(onesb[:], 1.0), w=['onesb'])
    ident32 = sb(es, "ident32", [128, 128], F32)
    P.op('dve', lambda: V.tensor_scalar(out=ident32[:], in0=identf[:], scalar1=1.0 / 32.0, scalar2=None, op0=ALU.mult), r=['identf'], w=['ident32'])
    P.dma(g1s[:], g1T[:, :], w=['g1s'])
    P.dma(g2s[:], g2T[:, :], w=['g2s'])

    pball = es.enter_context(nc.psum_tensor("pball", [128, 4096], F32))
    pb = [pball[:, i * 512:(i + 1) * 512] for i in range(8)]
    pbn = ['pb%d' % i for i in range(8)]

    def pb16(i):
        return pb[i][:, :].bitcast(BF16)

    def load_weight_bf16(stack_tiles, wdram, c0, ncols, gscale, tag, stag=None):
        stg, dst = stack_tiles
        stag = stag or tag
        for kc in range(8):
            P.dma(stg[:, kc, 0:ncols], wdram[kc * 128:(kc + 1) * 128, c0:c0 + ncols], w=[stag + 's%d' % kc])
            if gscale is None:
                if kc % 2 == 0:
                    P.op('act', lambda kc=kc: A.copy(out=dst[:, kc, 0:ncols], in_=stg[:, kc, 0:ncols]), r=[stag + 's%d' % kc], w=[tag])
                else:
                    P.op('dve', lambda kc=kc: V.tensor_copy(out=dst[:, kc, 0:ncols], in_=stg[:, kc, 0:ncols]), r=[stag + 's%d' % kc], w=[tag])
            else:
                P.op('dve', lambda kc=kc: V.tensor_scalar(out=dst[:, kc, 0:ncols], in0=stg[:, kc, 0:ncols], scalar1=gscale[:, kc:kc + 1], scalar2=None, op0=ALU.mult),
                     r=[stag + 's%d' % kc, 'g1s', 'g2s'], w=[tag])

    def norm_transpose(stack_t, xsrc_rows, hT, col0, tagx, gate_bank, act_scale=False):
        xt, junk, ss, hb = stack_t
        P.dma(xt[:], xsrc_rows, w=[tagx + 'xt'])
        P.op('act', lambda: A.activation(out=junk[:], in_=xt[:], func=AF.Square, accum_out=ss[:, 0:1]),
             r=[tagx + 'xt'], w=[tagx + 'junk', tagx + 'ss'])
        P.op('dve', lambda: V.tensor_scalar(out=ss[:, 1:2], in0=ss[:, 0:1], scalar1=1.0 / D, scalar2=EPS, op0=ALU.mult, op1=ALU.add),
             r=[tagx + 'ss'], w=[tagx + 'ss'])
        P.op('act', lambda: A.activation(out=ss[:, 2:3], in_=ss[:, 1:2], func=AF.Sqrt), r=[tagx + 'ss'], w=[tagx + 'ss'])
        P.op('dve', lambda: V.reciprocal(out=ss[:, 3:4], in_=ss[:, 2:3]), r=[tagx + 'ss'], w=[tagx + 'ss'])
        if act_scale:
            P.op('act', lambda: A.activation(out=hb[:], in_=xt[:], func=AF.Copy, scale=ss[:, 3:4]),
                 r=[tagx + 'xt', tagx + 'ss'], w=[tagx + 'hb'])
        else:
            P.op('dve', lambda: V.tensor_scalar(out=hb[:], in0=xt[:], scalar1=ss[:, 3:4], scalar2=None, op0=ALU.mult),
                 r=[tagx + 'xt', tagx + 'ss'], w=[tagx + 'hb'])
        b = gate_bank
        for kc in range(8):
            P.op('pe', lambda kc=kc: T.transpose(out=pb16(b)[:, kc * 128:(kc + 1) * 128], in_=hb[:, kc * 128:(kc + 1) * 128], identity=identb[:]),
                 r=[tagx + 'hb', 'identb'], w=[pbn[b]])
        return b

    def rope(dst, nrows, perm, ctab, stab, tmp1, tmp2, bank, tags_r, tag_w, ncols):
        P.op('pe', lambda: T.matmul(pb[bank][0:nrows, 0:ncols], lhsT=perm[0:nrows, 0:nrows], rhs=dst, start=True, stop=True, skip_group_check=True),
             r=[tag_w, 'permA', 'permI'], w=[pbn[bank]])
        P.op('dve', lambda: V.tensor_tensor(out=tmp1[0:nrows, 0:ncols], in0=dst, in1=ctab, op=ALU.mult), r=[tag_w] + tags_r, w=['ropet1'])
        P.op('dve', lambda: V.tensor_tensor(out=tmp2[0:nrows, 0:ncols], in0=pb[bank][0:nrows, 0:ncols], in1=stab, op=ALU.mult),
             r=[pbn[bank]] + tags_r, w=['ropet2'])
        P.op('dve', lambda: V.tensor_tensor(out=dst, in0=tmp1[0:nrows, 0:ncols], in1=tmp2[0:nrows, 0:ncols], op=ALU.add),
             r=['ropet1', 'ropet2'], w=[tag_w])

    sq = ExitStack()
    QP = sb(sq, "QP", [128, 9, 2, 32], F32)
    FP = sb(sq, "FP", [128, 8, 2, 32], F32)
    s5 = ExitStack()
    W1 = sb(s5, "W1", [128, 8, 2, 8, 128], BF16)
    cosT = sb(s5, "cosT", [128, 32, 64], F32)
    sinT = sb(s5, "sinT", [128, 32, 64], F32)
    R8z = sb(s5, "R8z", [128, 32, 64], F32)
    A8r = sb(s5, "A8r", [128, 32], F32)
    A8i = sb(s5, "A8i", [128, 32], F32)

    def sincos_lb(stack, n, ar, ai, dtl, pref):
        t = {}
        for nm in ['dt', 'ang', 'ex', 'mag', 'nn', 'red', 'sn', 'cs', 'lbr', 'lbi', 'fr', 'fi', 'u1', 'u2', 'u3']:
            t[nm] = sb(stack, pref + nm, [128, n], F32)
        R = [pref]
        def vv(fn):
            P.op('dve', fn, r=R, w=R)
        def aa(fn):
            P.op('act', fn, r=R, w=R)
        aa(lambda: A.activation(out=t['dt'][:], in_=dtl, func=AF.Exp))
        vv(lambda: V.tensor_tensor(out=t['ang'][:], in0=ai, in1=t['dt'][:], op=ALU.mult))
        vv(lambda: V.tensor_tensor(out=t['ex'][:], in0=ar, in1=t['dt'][:], op=ALU.mult))
        aa(lambda: A.activation(out=t['mag'][:], in_=t['ex'][:], func=AF.Exp))
        C1 = float(np.float32(2 * math.pi))
        C2 = float(2 * math.pi - np.float64(np.float32(2 * math.pi)))
        for which, off in (('sn', 0.0), ('cs', math.pi / 2)):
            vv(lambda off=off: V.tensor_scalar(out=t['u1'][:], in0=t['ang'][:], scalar1=off, scalar2=None, op0=ALU.add))
            vv(lambda: V.tensor_scalar(out=t['nn'][:], in0=t['u1'][:], scalar1=math.pi, scalar2=None, op0=ALU.is_gt))
            for kk in (3, 5, 7):
                vv(lambda kk=kk: V.tensor_scalar(out=t['u2'][:], in0=t['u1'][:], scalar1=kk * math.pi, scalar2=None, op0=ALU.is_gt))
                vv(lambda: V.tensor_tensor(out=t['nn'][:], in0=t['nn'][:], in1=t['u2'][:], op=ALU.add))
            vv(lambda: V.scalar_tensor_tensor(out=t['red'][:], in0=t['nn'][:], scalar=-C1, in1=t['u1'][:], op0=ALU.mult, op1=ALU.add))
            vv(lambda: V.scalar_tensor_tensor(out=t['red'][:], in0=t['nn'][:], scalar=-C2, in1=t['red'][:], op0=ALU.mult, op1=ALU.add))
            vv(lambda: V.tensor_scalar(out=t['red'][:], in0=t['red'][:], scalar1=math.pi, scalar2=-math.pi, op0=ALU.min, op1=ALU.max))
            aa(lambda which=which: A.activation(out=t[which][:], in_=t['red'][:], func=AF.Sin))
        vv(lambda: V.tensor_tensor(out=t['lbr'][:], in0=t['mag'][:], in1=t['cs'][:], op=ALU.mult))
        vv(lambda: V.tensor_tensor(out=t['lbi'][:], in0=t['mag'][:], in1=t['sn'][:], op=ALU.mult))
        vv(lambda: V.tensor_tensor(out=t['u1'][:], in0=ar, in1=ar, op=ALU.mult))
        vv(lambda: V.tensor_tensor(out=t['u2'][:], in0=ai, in1=ai, op=ALU.mult))
        vv(lambda: V.tensor_tensor(out=t['u1'][:], in0=t['u1'][:], in1=t['u2'][:], op=ALU.add))
        vv(lambda: V.reciprocal(out=t['u3'][:], in_=t['u1'][:]))
        vv(lambda: V.tensor_scalar(out=t['u1'][:], in0=t['lbr'][:], scalar1=-1.0, scalar2=None, op0=ALU.add))
        vv(lambda: V.tensor_tensor(out=t['fr'][:], in0=t['u1'][:], in1=ar, op=ALU.mult))
        vv(lambda: V.tensor_tensor(out=t['u2'][:], in0=t['lbi'][:], in1=ai, op=ALU.mult))
        vv(lambda: V.tensor_tensor(out=t['fr'][:], in0=t['fr'][:], in1=t['u2'][:], op=ALU.add))
        vv(lambda: V.tensor_tensor(out=t['fr'][:], in0=t['fr'][:], in1=t['u3'][:], op=ALU.mult))
        vv(lambda: V.tensor_tensor(out=t['fi'][:], in0=t['lbi'][:], in1=ar, op=ALU.mult))
        vv(lambda: V.tensor_tensor(out=t['u2'][:], in0=t['u1'][:], in1=ai, op=ALU.mult))
        vv(lambda: V.tensor_tensor(out=t['fi'][:], in0=t['fi'][:], in1=t['u2'][:], op=ALU.subtract))
        vv(lambda: V.tensor_tensor(out=t['fi'][:], in0=t['fi'][:], in1=t['u3'][:], op=ALU.mult))
        return t

    def cmul(outr, outi, ar_, ai_, br_, bi_, t1, t2, R):
        P.op('dve', lambda: V.tensor_tensor(out=t1, in0=ar_, in1=br_, op=ALU.mult), r=R, w=R)
        P.op('dve', lambda: V.tensor_tensor(out=t2, in0=ai_, in1=bi_, op=ALU.mult), r=R, w=R)
        P.op('dve', lambda: V.tensor_tensor(out=outr, in0=t1, in1=t2, op=ALU.subtract), r=R, w=R)
        P.op('dve', lambda: V.tensor_tensor(out=t1, in0=ar_, in1=bi_, op=ALU.mult), r=R, w=R)
        P.op('dve', lambda: V.tensor_tensor(out=t2, in0=ai_, in1=br_, op=ALU.mult), r=R, w=R)
        P.op('dve', lambda: V.tensor_tensor(out=outi, in0=t1, in1=t2, op=ALU.add), r=R, w=R)

    with ExitStack() as st:
        arl = sb(st, "arl", [128, 1024], F32); ail = sb(st, "ail", [128, 1024], F32); dtl = sb(st, "dtl", [128, 1024], F32)
        brl = sb(st, "brl", [128, 1024], F32); bil = sb(st, "bil", [128, 1024], F32)
        for tl, src in ((arl, AR1), (ail, AI1), (dtl, DT1), (brl, BR1), (bil, BI1)):
            P.dma(tl[:], src[:, :], w=['L1'])
        tt = sincos_lb(st, 1024, arl[:], ail[:], dtl[:], 'L1')
        pr = [sb(st, "pr%d" % i, [128, 1024], F32) for i in range(2)]
        pi = [sb(st, "pi%d" % i, [128, 1024], F32) for i in range(2)]
        c1 = sb(st, "c1", [128, 1024], F32); c2 = sb(st, "c2", [128, 1024], F32)
        R = ['L1']
        cur_r, cur_i = tt['fr'], tt['fi']
        for k in range(8):
            s_ = 7 - k
            v3 = lambda ap: ap.rearrange("p (kc n) -> p kc n", kc=8)
            P.op('dve', lambda: V.tensor_tensor(out=c1[:], in0=cur_r[:], in1=brl[:], op=ALU.mult), r=R, w=R)
            P.op('dve', lambda: V.tensor_tensor(out=c2[:], in0=cur_i[:], in1=bil[:], op=ALU.mult), r=R, w=R)
            P.op('dve', lambda s_=s_: V.tensor_tensor(out=W1[:, :, 0, s_, :], in0=v3(c1[:]), in1=v3(c2[:]), op=ALU.subtract), r=R, w=R + ['W1'])
            P.op('dve', lambda: V.tensor_tensor(out=c1[:], in0=cur_r[:], in1=bil[:], op=ALU.mult), r=R, w=R)
            P.op('dve', lambda: V.tensor_tensor(out=c2[:], in0=cur_i[:], in1=brl[:], op=ALU.mult), r=R, w=R)
            P.op('dve', lambda s_=s_: V.tensor_tensor(out=W1[:, :, 1, s_, :], in0=v3(c1[:]), in1=v3(c2[:]), op=ALU.add), r=R, w=R + ['W1'])
            if k < 7:
                nr, ni = pr[k % 2], pi[k % 2]
                cmul(nr[:], ni[:], cur_r[:], cur_i[:], tt['lbr'][:], tt['lbi'][:], c1[:], c2[:], R)
                cur_r, cur_i = nr, ni
    P.barrier()
    if stop('c1'):
        return fin()
    with ExitStack() as st:
        are = sb(st, "are", [128, 32], F32); aie = sb(st, "aie", [128, 32], F32); dte = sb(st, "dte", [128, 32], F32)
        for tl, src in ((are, ARE), (aie, AIE), (dte, DTE)):
            P.dma(tl[:], src[:, :], w=['E'])
        te = sincos_lb(st, 32, are[:], aie[:], dte[:], 'E')
        e1 = sb(st, "e1", [128, 32], F32); e2 = sb(st, "e2", [128, 32], F32)
        R = ['E']
        P.op('dve', lambda: V.memset(QP[:, 0, 0, :], 1.0), r=R, w=R)
        P.op('dve', lambda: V.memset(QP[:, 0, 1, :], 0.0), r=R, w=R)
        for k in range(1, 9):
            cmul(QP[:, k, 0, :], QP[:, k, 1, :], QP[:, k - 1, 0, :], QP[:, k - 1, 1, :], te['lbr'][:], te['lbi'][:], e1[:], e2[:], R)
        P.op('dve', lambda: V.tensor_copy(out=FP[:, 0, 0, :], in_=te['fr'][:]), r=R, w=R)
        P.op('dve', lambda: V.tensor_copy(out=FP[:, 0, 1, :], in_=te['fi'][:]), r=R, w=R)
        for k in range(1, 8):
            cmul(FP[:, k, 0, :], FP[:, k, 1, :], FP[:, k - 1, 0, :], FP[:, k - 1, 1, :], te['lbr'][:], te['lbi'][:], e1[:], e2[:], R)
        P.op('dve', lambda: V.tensor_copy(out=A8r[:], in_=QP[:, 8, 0, :]), r=R, w=R)
        P.op('dve', lambda: V.tensor_copy(out=A8i[:], in_=QP[:, 8, 1, :]), r=R, w=R)
        m2 = sb(st, "m2", [128, 32], F32); m4 = sb(st, "m4", [128, 32], F32); m8 = sb(st, "m8", [128, 32], F32)
        P.op('dve', lambda: V.tensor_tensor(out=m2[:], in0=te['mag'][:], in1=te['mag'][:], op=ALU.mult), r=R, w=R)
        P.op('dve', lambda: V.tensor_tensor(out=m4[:], in0=m2[:], in1=m2[:], op=ALU.mult), r=R, w=R)
        P.op('dve', lambda: V.tensor_tensor(out=m8[:], in0=m4[:], in1=m4[:], op=ALU.mult), r=R, w=R)
        wr = [sb(st, "wr%d" % i, [128, 32], F32) for i in range(4)]
        wi_ = [sb(st, "wi%d" % i, [128, 32], F32) for i in range(4)]
        P.op('dve', lambda: V.tensor_copy(out=wr[0][:], in_=te['cs'][:]), r=R, w=R)
        P.op('dve', lambda: V.tensor_copy(out=wi_[0][:], in_=te['sn'][:]), r=R, w=R)
        for k in range(1, 4):
            cmul(wr[k][:], wi_[k][:], wr[k - 1][:], wi_[k - 1][:], wr[k - 1][:], wi_[k - 1][:], e1[:], e2[:], R)
        P.op('dve', lambda: V.memset(cosT[:, :, 0:1], 1.0), r=R, w=R)
        P.op('dve', lambda: V.memset(sinT[:, :, 0:1], 0.0), r=R, w=R)
        sr = [sb(st, "sr%d" % i, [128, 32], F32) for i in range(2)]
        si = [sb(st, "si%d" % i, [128, 32], F32) for i in range(2)]
        big1 = sb(st, "big1", [128, 32, 32], F32); big2 = sb(st, "big2", [128, 32, 32], F32)
        stepr, stepi = wr[3], wi_[3]
        for lv in range(6):
            n = 1 << lv
            bc = lambda ap, n=n: ap.unsqueeze(2).to_broadcast([128, 32, n])
            P.op('dve', lambda: V.tensor_tensor(out=big1[:, :, 0:n], in0=cosT[:, :, 0:n], in1=bc(stepr[:]), op=ALU.mult), r=R, w=R)
            P.op('dve', lambda: V.tensor_tensor(out=big2[:, :, 0:n], in0=sinT[:, :, 0:n], in1=bc(stepi[:]), op=ALU.mult), r=R, w=R)
            P.op('dve', lambda: V.tensor_tensor(out=cosT[:, :, n:2 * n], in0=big1[:, :, 0:n], in1=big2[:, :, 0:n], op=ALU.subtract), r=R, w=R)
            P.op('dve', lambda: V.tensor_tensor(out=big1[:, :, 0:n], in0=cosT[:, :, 0:n], in1=bc(stepi[:]), op=ALU.mult), r=R, w=R)
            P.op('dve', lambda: V.tensor_tensor(out=big2[:, :, 0:n], in0=sinT[:, :, 0:n], in1=bc(stepr[:]), op=ALU.mult), r=R, w=R)
            P.op('dve', lambda: V.tensor_tensor(out=sinT[:, :, n:2 * n], in0=big1[:, :, 0:n], in1=big2[:, :, 0:n], op=ALU.add), r=R, w=R)
            if lv < 5:
                nr, ni = sr[lv % 2], si[lv % 2]
                cmul(nr[:], ni[:], stepr[:], stepi[:], stepr[:], stepi[:], e1[:], e2[:], R)
                stepr, stepi = nr, ni
        P.op('dve', lambda: V.tensor_copy(out=R8z[:], in_=m8[:].unsqueeze(2).to_broadcast([128, 32, 64])), r=R, w=R)
        P.op('dve', lambda: V.memset(R8z[:, :, 0:1], 0.0), r=R, w=R)
    P.barrier()
    if stop('c2'):
        return fin()

    with ExitStack() as st:
        WuB = sb(st, "WuB", [128, 8, 1024], BF16)
        with ExitStack() as s2:
            wstg = sb(s2, "wstgA", [128, 8, 1024], F32)
            load_weight_bf16((wstg, WuB), wu, 0, 1024, g1s, 'WuB')
            P.barrier()
            if stop('c30'):
                return fin()
        xt = [sb(st, "xtA%d" % i, [128, 1024], F32) for i in range(2)]
        junk = [sb(st, "junkA%d" % i, [128, 1024], F32) for i in range(2)]
        ssA = [sb(st, "ssA%d" % i, [128, 4], F32) for i in range(2)]
        hb = [sb(st, "hbA%d" % i, [128, 1024], BF16) for i in range(2)]
        hT = [sb(st, "hTA%d" % i, [128, 8, 512], BF16) for i in range(2)]
        uT = [sb(st, "uTA%d" % i, [128, 8, 512], BF16) for i in range(2)]
        S0b = [sb(st, "S0_%d" % i, [128, 32, 2, 64], F32) for i in range(2)]
        Z = sb(st, "Z", [128, 32, 2, 64], F32)
        ta = sb(st, "ta", [128, 32, 64], F32)
        tb = sb(st, "tb", [128, 32, 64], F32)
        Hin = sb(st, "Hin", [128, 2, 32], F32)
        cr_ = sb(st, "cr_", [128, 2, 32], F32)
        cq1 = sb(st, "cq1", [128, 32], F32); cq2 = sb(st, "cq2", [128, 32], F32)
        Hb16 = [sb(st, "Hb16_%d" % i, [128, 32, 2, 16], BF16) for i in range(2)]
        P.op('dve', lambda: V.memset(Hin[:], 0.0), w=['Hin'])
        bank_rot = [0]

        def nb():
            b = bank_rot[0]
            bank_rot[0] = (b + 1) % 8
            return b

        def stageA(t):
            hTt = hT[t % 2]; uTt = uT[t % 2]
            hn = 'hT%d' % (t % 2); un = 'uT%d' % (t % 2)
            S0 = S0b[t % 2]; s0n = 'S0_%d' % (t % 2)
            for blk in range(4):
                i2 = blk % 2
                b = nb()
                norm_transpose((xt[i2], junk[i2], ssA[i2], hb[i2]), xp[(t * 4 + blk) * 128:(t * 4 + blk + 1) * 128, :], hTt, blk * 128, 'A%d' % i2, b, act_scale=True)
                P.op('act', lambda b=b, blk=blk: A.copy(out=hTt[:, :, blk * 128:(blk + 1) * 128], in_=pb16(b).rearrange("p (kc n) -> p kc n", kc=8)),
                     r=[pbn[b]], w=[hn])
            for oc in range(8):
                b = nb()
                for kc in range(8):
                    P.op('pe', lambda b=b, oc=oc, kc=kc: T.matmul(pb[b][:, :], lhsT=WuB[:, kc, oc * 128:(oc + 1) * 128], rhs=hTt[:, kc, :],
                                                                   start=(kc == 0), stop=(kc == 7), skip_group_check=True),
                         r=['WuB', hn], w=[pbn[b]])
                P.op('act', lambda b=b, oc=oc: A.copy(out=uTt[:, oc, :], in_=pb[b][:, :]), r=[pbn[b]], w=[un])
            P.dma(HT_d[t].rearrange("p (kc n) -> p kc n", kc=8), hTt[:, :, 384:512], r=[hn], w=['HT_d'])
            P.dma(U_d[t].rearrange("p (kc n) -> p kc n", kc=8), uTt[:, :, 384:512], r=[un], w=['U_d'])
            for kc in range(8):
                for comp in range(2):
                    c0 = comp * 64
                    for s_ in range(8):
                        for pp in range(4):
                            b = 4 * (kc % 2) + pp
                            P.op('pe', lambda b=b, kc=kc, pp=pp, comp=comp, s_=s_, c0=c0: T.matmul(
                                pb[b][:, c0:c0 + 64], lhsT=W1[32 * pp:32 * pp + 32, kc, comp, s_, :], rhs=uTt[32 * pp:32 * pp + 32, kc, s_::8],
                                start=(s_ == 0), stop=(s_ == 7), skip_group_check=True, tile_position=(32 * pp, 0)),
                                r=['W1', un], w=[pbn[b]])
                for pp in range(4):
                    b = 4 * (kc % 2) + pp
                    S0v = S0[:, 4 * kc + pp, :, :].rearrange("p c m -> p (c m)")
                    P.op('act', lambda b=b, S0v=S0v: A.copy(out=S0v, in_=pb[b][:, 0:128]), r=[pbn[b]], w=[s0n])

        def stageB(t):
            S0 = S0b[t % 2]; s0n = 'S0_%d' % (t % 2)
            Hh = S0
            Sr, Si = S0[:, :, 0, :], S0[:, :, 1, :]
            Zr, Zi = Z[:, :, 0, :], Z[:, :, 1, :]
            Rr = [s0n, 'Z', 'ta', 'tb']
            P.op('dve', lambda: V.tensor_tensor(out=ta[:], in0=cosT[:], in1=Sr, op=ALU.mult), r=Rr, w=['ta'])
            P.op('pool', lambda: G.tensor_tensor(out=tb[:], in0=sinT[:], in1=Si, op=ALU.mult), r=Rr, w=['tb'])
            P.op('dve', lambda: V.tensor_tensor(out=Zr, in0=ta[:], in1=tb[:], op=ALU.add), r=Rr, w=['Z'])
            P.op('dve', lambda: V.tensor_tensor(out=ta[:], in0=cosT[:], in1=Si, op=ALU.mult), r=Rr, w=['ta'])
            P.op('pool', lambda: G.tensor_tensor(out=tb[:], in0=sinT[:], in1=Sr, op=ALU.mult), r=Rr, w=['tb'])
            P.op('dve', lambda: V.tensor_tensor(out=Zi, in0=ta[:], in1=tb[:], op=ALU.subtract), r=Rr, w=['Z'])
            Rc = ['Hin', 'cr_', 'cq']
            P.op('dve', lambda: V.tensor_tensor(out=cq1[:], in0=A8r[:], in1=Hin[:, 0, :], op=ALU.mult), r=Rc, w=Rc)
            P.op('dve', lambda: V.tensor_tensor(out=cq2[:], in0=A8i[:], in1=Hin[:, 1, :], op=ALU.mult), r=Rc, w=Rc)
            P.op('dve', lambda: V.tensor_tensor(out=cr_[:, 0, :], in0=cq1[:], in1=cq2[:], op=ALU.subtract), r=Rc, w=Rc)
            P.op('dve', lambda: V.tensor_tensor(out=cq1[:], in0=A8r[:], in1=Hin[:, 1, :], op=ALU.mult), r=Rc, w=Rc)
            P.op('dve', lambda: V.tensor_tensor(out=cq2[:], in0=A8i[:], in1=Hin[:, 0, :], op=ALU.mult), r=Rc, w=Rc)
            P.op('dve', lambda: V.tensor_tensor(out=cr_[:, 1, :], in0=cq1[:], in1=cq2[:], op=ALU.add), r=Rc, w=Rc)
            P.op('dve', lambda: V.tensor_tensor(out=Z[:, :, 0, 0], in0=Z[:, :, 0, 0], in1=cr_[:, 0, :], op=ALU.add), r=Rc + ['Z'], w=['Z'])
            P.op('dve', lambda: V.tensor_tensor(out=Z[:, :, 1, 0], in0=Z[:, :, 1, 0], in1=cr_[:, 1, :], op=ALU.add), r=Rc + ['Z'], w=['Z'])
            fl = lambda ap: ap.rearrange("p a m -> p (a m)")
            P.op('dve', lambda: V.tensor_copy(out=ta[:], in_=Zr), r=['Z'], w=['ta'])
            P.op('pool', lambda: G.tensor_copy(out=tb[:], in_=Zi), r=['Z'], w=['tb'])
            P.op('dve', lambda: V.tensor_tensor_scan(out=fl(ta[:]), data0=fl(R8z[:]), data1=fl(ta[:]), initial=0.0,
                                                    op0=ALU.mult, op1=ALU.add), r=['ta'], w=['ta'])
            P.op('dve', lambda: V.tensor_tensor_scan(out=fl(tb[:]), data0=fl(R8z[:]), data1=fl(tb[:]), initial=0.0,
                                                    op0=ALU.mult, op1=ALU.add), r=['tb'], w=['tb'])
            Rh = ['ta', 'tb', 'Z', s0n]
            P.op('dve', lambda: V.tensor_tensor(out=Zr, in0=cosT[:], in1=ta[:], op=ALU.mult), r=Rh, w=['Z'])
            P.op('pool', lambda: G.tensor_tensor(out=Zi, in0=sinT[:], in1=tb[:], op=ALU.mult), r=Rh, w=['Z'])
            P.op('dve', lambda: V.tensor_tensor(out=Hh[:, :, 0, :], in0=Zr, in1=Zi, op=ALU.subtract), r=Rh, w=[s0n])
            P.op('dve', lambda: V.tensor_tensor(out=Zr, in0=cosT[:], in1=tb[:], op=ALU.mult), r=Rh, w=['Z'])
            P.op('pool', lambda: G.tensor_tensor(out=Zi, in0=sinT[:], in1=ta[:], op=ALU.mult), r=Rh, w=['Z'])
            P.op('dve', lambda: V.tensor_tensor(out=Hh[:, :, 1, :], in0=Zr, in1=Zi, op=ALU.add), r=Rh, w=[s0n])
            P.op('dve', lambda: V.tensor_copy(out=Hin[:, 0, :], in_=Hh[:, :, 0, 63]), r=[s0n], w=['Hin'])
            P.op('dve', lambda: V.tensor_copy(out=Hin[:, 1, :], in_=Hh[:, :, 1, 63]), r=[s0n], w=['Hin'])
            hbt = Hb16[t % 2]
            P.op('dve', lambda hbt=hbt: V.tensor_copy(out=hbt[:], in_=Hh[:, :, :, 47:63]), r=[s0n], w=['Hb16_%d' % (t % 2)])
            P.dma(HS_d[t].rearrange("p (a c m) -> p a c m", a=32, c=2), hbt[:], r=['Hb16_%d' % (t % 2)], w=['HS_d'])

        stageA(0)
        for t in range(NT):
            if t + 1 < NT:
                stageA(t + 1)
            stageB(t)
    P.barrier()
    if stop('c4'):
        return fin()
    s5.close()

    with ExitStack() as st:
        CC = sb(st, "CC", [128, 32, 2, 256], BF16)
        KN = sb(st, "KN", [128, 8, 2, 8, 128], BF16)
        dfm = sb(st, "dfm", [128, 8], F32)
        P.dma(dfm[:], DFM[:, :], w=['dfm'])
        with ExitStack() as s2:
            ctr = sb(s2, "ctr", [128, 32, 16], F32); cti = sb(s2, "cti", [128, 32, 16], F32)
            ber = sb(s2, "ber", [128, 32, 32], F32); bei = sb(s2, "bei", [128, 32, 32], F32)
            P.dma(ctr[:].rearrange("p a c -> p (a c)"), CTR[:, :], w=['ctr'])
            P.dma(cti[:].rearrange("p a c -> p (a c)"), CTI[:, :], w=['cti'])
            P.dma(ber[:].rearrange("p a c -> p (a c)"), BER[:, :], w=['ber'])
            P.dma(bei[:].rearrange("p a c -> p (a c)"), BEI[:, :], w=['bei'])
            ctrb = sb(s2, "ctrb", [128, 32, 16], BF16); nctib = sb(s2, "nctib", [128, 32, 16], BF16)
            P.op('dve', lambda: V.tensor_copy(out=ctrb[:], in_=ctr[:]), r=['ctr'], w=['ctrb'])
            P.op('dve', lambda: V.tensor_scalar(out=nctib[:], in0=cti[:], scalar1=-1.0, scalar2=None, op0=ALU.mult), r=['cti'], w=['nctib'])
            k1 = sb(s2, "k1", [128, 32, 32], F32); k2 = sb(s2, "k2", [128, 32, 32], F32)
            xre = sb(s2, "xre", [128, 32, 32], BF16); xim = sb(s2, "xim", [128, 32, 32], BF16)
            R = ['cc']
            b16 = lambda ap: ap.unsqueeze(2).to_broadcast([128, 32, 16])
            b32 = lambda ap: ap.unsqueeze(2).to_broadcast([128, 32, 32])
            P.op('pool', lambda: G.memset(CC[:].rearrange("p a b c -> p (a b c)"), 0.0), w=['CC'])
            for s_ in range(8):
                qr, qi = QP[:, s_ + 1, 0, :], QP[:, s_ + 1, 1, :]
                P.op('dve', lambda: V.tensor_tensor(out=k1[:, :, 0:16], in0=ctr[:], in1=b16(qr), op=ALU.mult), r=R + ['ctr'], w=R)
                P.op('dve', lambda: V.tensor_tensor(out=k2[:, :, 0:16], in0=cti[:], in1=b16(qi), op=ALU.mult), r=R + ['cti'], w=R)
                for gg in range(2):
                    rs = slice(64 * gg, 64 * gg + 64)
                    P.op('dve', lambda s_=s_, gg=gg, rs=rs: V.tensor_tensor(out=CC[rs, :, 0, gg * 128 + s_ * 16:gg * 128 + (s_ + 1) * 16],
                                                                         in0=k1[rs, :, 0:16], in1=k2[rs, :, 0:16], op=ALU.subtract), r=R, w=R + ['CC'])
                P.op('dve', lambda: V.tensor_tensor(out=k1[:, :, 0:16], in0=ctr[:], in1=b16(qi), op=ALU.mult), r=R, w=R)
                P.op('dve', lambda: V.tensor_tensor(out=k2[:, :, 0:16], in0=cti[:], in1=b16(qr), op=ALU.mult), r=R, w=R)
                P.op('dve', lambda: V.tensor_tensor(out=k1[:, :, 0:16], in0=k1[:, :, 0:16], in1=k2[:, :, 0:16], op=ALU.add), r=R, w=R)
                for gg in range(2):
                    rs = slice(64 * gg, 64 * gg + 64)
                    P.op('dve', lambda s_=s_, gg=gg, rs=rs: V.tensor_scalar(out=CC[rs, :, 1, gg * 128 + s_ * 16:gg * 128 + (s_ + 1) * 16],
                                                                         in0=k1[rs, :, 0:16], scalar1=-1.0, scalar2=None, op0=ALU.mult), r=R, w=R + ['CC'])
            for tau in range(8):
                fr_, fi_ = FP[:, tau, 0, :], FP[:, tau, 1, :]
                P.op('dve', lambda: V.tensor_tensor(out=k1[:], in0=ber[:], in1=b32(fr_), op=ALU.mult), r=R + ['ber', 'xre', 'xim'], w=R)
                P.op('dve', lambda: V.tensor_tensor(out=k2[:], in0=bei[:], in1=b32(fi_), op=ALU.mult), r=R + ['bei'], w=R)
                P.op('dve', lambda: V.tensor_tensor(out=xre[:], in0=k1[:], in1=k2[:], op=ALU.subtract), r=R, w=R + ['xre'])
                P.op('dve', lambda: V.tensor_tensor(out=k1[:], in0=bei[:], in1=b32(fr_), op=ALU.mult), r=R, w=R)
                P.op('dve', lambda: V.tensor_tensor(out=k2[:], in0=ber[:], in1=b32(fi_), op=ALU.mult), r=R, w=R)
                P.op('dve', lambda: V.tensor_tensor(out=xim[:], in0=k1[:], in1=k2[:], op=ALU.add), r=R, w=R + ['xim'])
                for gp in range(32):
                    kc, pp = gp // 4, gp % 4
                    for gg in range(2):
                        bnk = 2 * gg + tau // 4
                        c0 = (tau % 4) * 128 + kc * 16
                        rs = slice(64 * gg, 64 * gg + 64)
                        P.op('pe', lambda bnk=bnk, gp=gp, gg=gg, pp=pp, c0=c0, rs=rs: T.matmul(
                            pb[bnk][32 * pp:32 * pp + 32, c0:c0 + 16], lhsT=xre[rs, gp, :], rhs=ctrb[rs, gp, :], start=True, stop=False,
                            skip_group_check=True, tile_position=(64 * gg, 32 * pp)), r=['xre', 'ctrb'], w=[pbn[bnk]])
                        P.op('pe', lambda bnk=bnk, gp=gp, gg=gg, pp=pp, c0=c0, rs=rs: T.matmul(
                            pb[bnk][32 * pp:32 * pp + 32, c0:c0 + 16], lhsT=xim[rs, gp, :], rhs=nctib[rs, gp, :], start=False, stop=True,
                            skip_group_check=True, tile_position=(64 * gg, 32 * pp)), r=['xim', 'nctib'], w=[pbn[bnk]])
            P.op('pool', lambda: G.memset(KN[:].rearrange("p a b c d -> p (a b c d)"), 0.0), w=['KN'])
            for tau in range(8):
                for gg in range(2):
                    bnk = 2 * gg + tau // 4
                    src = pb[bnk][:, (tau % 4) * 128:(tau % 4) * 128 + 128].rearrange("p (a c) -> p a c", a=8)
                    for sp_ in range(8 - tau):
                        s_ = sp_ + tau
                        P.op('dve', lambda src=src, sp_=sp_, s_=s_, gg=gg: V.tensor_copy(out=KN[:, :, gg, sp_, s_ * 16:(s_ + 1) * 16], in_=src),
                             r=[pbn[bnk]], w=['KN'])
        P.barrier()
        if stop('c5'):
            return fin()
        Hb_all = sb(st, "Hb_all", [128, 32, 2, 256], BF16)
        uo_all = sb(st, "uo_all", [128, 8, 2048], BF16)
        hstg = [sb(st, "hstg%d" % i, [128, 32, 2, 16], BF16) for i in range(2)]
        for t in range(NT):
            hs = hstg[t % 2]; hsn = 'hstg%d' % (t % 2)
            P.dma(hs[:], HS_d[t].rearrange("p (a c m) -> p a c m", a=32, c=2), r=['HS_d'], w=[hsn])
            if t % 2 == 0:
                P.op('act', lambda hs=hs, t=t: A.copy(out=Hb_all[:, :, :, t * 16:(t + 1) * 16], in_=hs[:]), r=[hsn], w=['Hb_all'])
            else:
                P.op('dve', lambda hs=hs, t=t: V.tensor_copy(out=Hb_all[:, :, :, t * 16:(t + 1) * 16], in_=hs[:]), r=[hsn], w=['Hb_all'])
            P.dma(uo_all[:, :, t * 128:(t + 1) * 128], U_d[t].rearrange("p (kc n) -> p kc n", kc=8), r=['U_d'], w=['uo_all'])
        nsb = [sb(st, "nsb%d" % i, [128, 256], F32) for i in range(2)]
        Yg = [sb(st, "Yg%d" % i, [128, 256], BF16) for i in range(2)]
        ytm = [sb(st, "ytm%d" % i, [128, 2, 8, 8, 16], BF16) for i in range(2)]
        ypre = [sb(st, "ypre0", [128, 2048], F32)] * 2
        g1_ = sb(st, "g1_", [128, 2048], F32); g2_ = g1_
        yg = [sb(st, "yg%d" % i, [128, 2048], BF16) for i in range(2)]
        Yd_v = Y_d.rearrange("t p (kc n) -> p t kc n", kc=8)
        def stX(kc, gl):
            gp, gg, pp = 4 * kc + gl // 2, gl % 2, gl // 2
            bF = gl % 2
            for comp in range(2):
                P.op('pe', lambda comp=comp: T.matmul(
                    pb[bF][:, 0:256], lhsT=CC[:, gp, comp, gg * 128:(gg + 1) * 128], rhs=Hb_all[:, gp, comp, :], start=(comp == 0), stop=(comp == 1),
                    skip_group_check=True), r=['Hb_all', 'CC'], w=[pbn[bF]])
            bN = 2 + gg + 2 * (pp % 2)
            for sp_ in range(8):
                P.op('pe', lambda sp_=sp_: T.matmul(
                    pb[bN][:, 0:256], lhsT=KN[32 * pp:32 * pp + 32, kc, gg, sp_, :], rhs=uo_all[32 * pp:32 * pp + 32, kc, sp_::8],
                    start=(sp_ == 0), stop=(sp_ == 7), skip_group_check=True, tile_position=(32 * pp, 0)), r=['uo_all', 'KN'], w=[pbn[bN]])
            ns = nsb[gl % 2]; nsn = 'nsb%d' % (gl % 2)
            ygt = Yg[gl % 2]; ygn = 'Yg%d' % (gl % 2)
            P.op('act', lambda: A.copy(out=ns[:], in_=pb[bN][:, 0:256]), r=[pbn[bN]], w=[nsn])
            P.op('dve', lambda: V.tensor_tensor(out=ygt[:], in0=pb[bF][:, 0:256], in1=ns[:], op=ALU.add), r=[pbn[bF], nsn], w=[ygn])

        def stY(kc, gl):
            yt = ytm[kc % 2]; ytn = 'ytm%d' % (kc % 2)
            ygt = Yg[gl % 2]; ygn = 'Yg%d' % (gl % 2)
            for mh in range(2):
                P.op('pe', lambda mh=mh: T.transpose(out=pb16(6)[:, mh * 128:(mh + 1) * 128], in_=ygt[:, mh * 128:(mh + 1) * 128], identity=identb[:]),
                     r=[ygn, 'identb'], w=[pbn[6]])
            P.op('act', lambda: A.copy(out=yt[:, :, :, gl, :], in_=pb16(6)[:, 0:256].rearrange("p (h s c) -> p h s c", h=2, s=8)),
                 r=[pbn[6]], w=[ytn])

        groups = [(kc, gl) for kc in range(8) for gl in range(8)]
        stX(*groups[0])
        for gi_, (kc, gl) in enumerate(groups):
            if gi_ + 1 < len(groups):
                stX(*groups[gi_ + 1])
            stY(kc, gl)
            if gl != 7:
                continue
            yt = ytm[kc % 2]; ytn = 'ytm%d' % (kc % 2)
            yp = ypre[0]; ypn = 'ypre0'
            for mh in range(2):
                for s_ in range(8):
                    P.op('pe', lambda yt=yt, mh=mh, s_=s_: T.transpose(out=pb16(7)[:, s_ * 128:(s_ + 1) * 128], in_=yt[:, mh, s_, :, :].rearrange("p g c -> p (g c)"),
                                                                       identity=identb[:]), r=[ytn, 'identb'], w=[pbn[7]])
                ov = yp[:, mh * 1024:(mh + 1) * 1024].rearrange("p (m s) -> p s m", s=8)
                uv = uo_all[:, kc, mh * 1024:(mh + 1) * 1024].rearrange("p (m s) -> p s m", s=8)
                P.op('dve', lambda kc=kc, ov=ov, uv=uv: V.scalar_tensor_tensor(
                    out=ov, in0=uv, scalar=dfm[:, kc:kc + 1], in1=pb16(7)[:, 0:1024].rearrange("p (s m) -> p s m", s=8), op0=ALU.mult, op1=ALU.add),
                    r=['uo_all', 'dfm', pbn[7]], w=[ypn])
            yf = yp[:]
            ygo = yg[kc % 2]; ygon = 'yg%d' % (kc % 2)
            P.op('dve', lambda yf=yf: V.tensor_tensor(out=g1_[:], in0=yf, in1=yf, op=ALU.mult), r=[ypn], w=['g1_'])
            P.op('dve', lambda: V.tensor_scalar(out=g1_[:], in0=g1_[:], scalar1=0.044715, scalar2=1.0, op0=ALU.mult, op1=ALU.add), r=['g1_'], w=['g1_'])
            P.op('dve', lambda yf=yf: V.tensor_tensor(out=g1_[:], in0=g1_[:], in1=yf, op=ALU.mult), r=['g1_', ypn], w=['g1_'])
            P.op('act', lambda: A.activation(out=g1_[:], in_=g1_[:], func=AF.Sigmoid, scale=2.0 * 0.7978845608028654), r=['g1_'], w=['g1_'])
            P.op('dve', lambda ygo=ygo, yf=yf: V.tensor_tensor(out=ygo[:], in0=g1_[:], in1=yf, op=ALU.mult), r=['g1_', ypn], w=[ygon])
            P.dma(Yd_v[:, :, kc, :], ygo[:].rearrange("p (t n) -> p t n", t=NT), r=[ygon], w=['Y_d'])
    P.barrier()

    sq.close()
    if dbg and dbg[0] == 'Y':
        with ExitStack() as st:
            tmpb = sb(st, "tmpb", [128, 1024], BF16); tmpf = sb(st, "tmpf", [128, 1024], F32)
            for t in range(NT):
                P.dma(tmpb[:], Y_d[t], r=['Y_d'], w=['tmpb'])
                P.op('dve', lambda: V.tensor_copy(out=tmpf[:], in_=tmpb[:]), r=['tmpb'], w=['tmpf'])
                P.dma(dbg_d[t], tmpf[:], r=['tmpf'], w=['dbg'])
        P.barrier()
        for e in ['sp']:
            pass
        es.close()
        return nc

    if stop('Y2'):
        return fin()
    ISQ = 1.0 / math.sqrt(128.0)

    with ExitStack() as st:
        kT_all = sb(st, "kT_all", [128, 2, L], BF16)
        V_all = sb(st, "V_all", [128, 64, 256], BF16)
        kiT_all = sb(st, "kiT_all", [128, L], BF16)
        with ExitStack() as s2:
            WkvB = sb(s2, "WkvB", [128, 8, 640], BF16)
            with ExitStack() as s3:
                wstg = sb(s3, "wstgK", [128, 8, 640], F32)
                load_weight_bf16((wstg, WkvB), wkv, 0, 640, g1s, 'WkvB')
                P.barrier()
            xt = [sb(s2, "xtK%d" % i, [128, 1024], F32) for i in range(4)]
            junk = [sb(s2, "junkK0", [128, 1024], F32)] * 4
            ssA = [sb(s2, "ssK%d" % i, [128, 4], F32) for i in range(4)]
            hb = [sb(s2, "hbK%d" % i, [128, 1024], BF16) for i in range(4)]
            hT = [sb(s2, "hTK%d" % i, [128, 8, 512], BF16) for i in range(2)]
            ctab = [sb(s2, "ctabK%d" % i, [128, 512], F32) for i in range(2)]; stab = [sb(s2, "stabK%d" % i, [128, 512], F32) for i in range(2)]
            ctabI = [sb(s2, "ctabI%d" % i, [128, 512], F32) for i in range(2)]; stabI = [sb(s2, "stabI%d" % i, [128, 512], F32) for i in range(2)]
            rt1 = sb(s2, "rt1", [128, 512], F32); rt2 = sb(s2, "rt2", [128, 512], F32)
            brot = [0]

            def nb2():
                b = brot[0]
                brot[0] = (b + 1) % 8
                return b

            def a2_stage1(t):
                hTt = hT[t % 2]; hn = 'hTK%d' % (t % 2)
                for blk in range(4):
                    i2 = blk % 4
                    b = nb2()
                    norm_transpose((xt[i2], junk[i2], ssA[i2], hb[i2]), xp[(t * 4 + blk) * 128:(t * 4 + blk + 1) * 128, :], hTt, blk * 128, 'K%d' % i2, b)
                    if blk % 2 == 0:
                        P.op('act', lambda b=b, blk=blk: A.copy(out=hTt[:, :, blk * 128:(blk + 1) * 128], in_=pb16(b).rearrange("p (kc n) -> p kc n", kc=8)),
                             r=[pbn[b]], w=[hn])
                    else:
                        P.op('dve', lambda b=b, blk=blk: V.tensor_copy(out=hTt[:, :, blk * 128:(blk + 1) * 128], in_=pb16(b).rearrange("p (kc n) -> p kc n", kc=8)),
                             r=[pbn[b]], w=[hn])

            def a2_stage2(t):
                hTt = hT[t % 2]; hn = 'hTK%d' % (t % 2)
                t2 = t % 2
                cs = slice(t * 512, (t + 1) * 512)
                P.dma(ctab[t2][:], cAk[:, cs], w=['ctabK%d' % t2]); P.dma(stab[t2][:], sAk[:, cs], w=['stabK%d' % t2])
                P.dma(ctabI[t2][:], cIk[:, cs], w=['ctabI%d' % t2]); P.dma(stabI[t2][:], sIk[:, cs], w=['stabI%d' % t2])
                dsts = []
                for oc in range(3):
                    b = nb2()
                    for kc in range(8):
                        P.op('pe', lambda b=b, oc=oc, kc=kc: T.matmul(pb[b][:, :], lhsT=WkvB[:, kc, oc * 128:(oc + 1) * 128], rhs=hTt[:, kc, :],
                                                                       start=(kc == 0), stop=(kc == 7), skip_group_check=True),
                             r=['WkvB', hn], w=[pbn[b]])
                    if oc < 2:
                        dst = kT_all[:, oc, cs]
                        P.op('act', lambda b=b, dst=dst: A.copy(out=dst, in_=pb[b][:, :]), r=[pbn[b]], w=['kTt%d' % t2])
                    else:
                        dst = kiT_all[:, cs]
                        P.op('act', lambda b=b, dst=dst: A.copy(out=dst, in_=pb[b][:, :]), r=[pbn[b]], w=['kiTt%d' % t2])
                    dsts.append(dst)
                for blk in range(4):
                    b = nb2()
                    for kc in range(8):
                        P.op('pe', lambda b=b, blk=blk, kc=kc: T.matmul(pb[b][:, 0:256], lhsT=hTt[:, kc, blk * 128:(blk + 1) * 128], rhs=WkvB[:, kc, 384:640],
                                                                         start=(kc == 0), stop=(kc == 7), skip_group_check=True),
                             r=['WkvB', hn], w=[pbn[b]])
                    P.op('act' if blk % 2 == 0 else 'dve',
                         (lambda b=b, blk=blk, t=t: A.copy(out=V_all[:, 4 * t + blk, :], in_=pb[b][:, 0:256])) if blk % 2 == 0 else
                         (lambda b=b, blk=blk, t=t: V.tensor_copy(out=V_all[:, 4 * t + blk, :], in_=pb[b][:, 0:256])), r=[pbn[b]], w=['V_all'])
                for oc in range(3):
                    if oc < 2:
                        rope(dsts[oc], 128, permA, ctab[t2][:], stab[t2][:], rt1, rt2, nb2(), ['ctabK%d' % t2, 'stabK%d' % t2], 'kTt%d' % t2, 512)
                    else:
                        rope(dsts[oc], 128, permI, ctabI[t2][:], stabI[t2][:], rt1, rt2, nb2(), ['ctabI%d' % t2, 'stabI%d' % t2], 'kiTt%d' % t2, 512)

            a2_stage1(0)
            for t in range(NT):
                if t + 1 < NT:
                    a2_stage1(t + 1)
                a2_stage2(t)
        P.barrier()
        if stop('KV'):
            return fin()
        hTo = sb(st, "hTo", [128, 8, 512], BF16)
        qT = sb(st, "qT", [128, 4, 8, 128], BF16)
        qiT = sb(st, "qiT", [128, 8, 512], BF16)
        wis = sb(st, "wis", [128, 4, 16], F32)
        wst = [sb(st, "wstQ0", [128, 8, 128], F32)] * 2
        wbf = [sb(st, "wbfQ%d" % i, [128, 8, 128], BF16) for i in range(2)]
        qtmp = sb(st, "qtmp", [128, 512], BF16)
        ctq = sb(st, "ctq", [128, 512], F32); stq = sb(st, "stq", [128, 512], F32)
        rt1 = sb(st, "rt1q", [128, 512], F32); rt2 = sb(st, "rt2q", [128, 512], F32)
        Dg = sb(st, "Dg", [128, 16, 128], BF16)
        Rb = [sb(st, "Rb%d" % i, [128, 1024], BF16) for i in range(3)]
        score = sb(st, "score", [128, L], F32)
        cjunk = sb(st, "cjunk", [128, L], mybir.dt.uint8)
        mtile = [sb(st, "mtile%d" % i, [128, 512], F32) for i in range(3)]
        P.dma(mtile[0][:], mfirst[:, :], w=['mt0']); P.dma(mtile[1][:], mlast[:, :], w=['mt1']); P.dma(mtile[2][:], mboth[:, :], w=['mt2'])
        bs = sb(st, "bs", [128, 8], F32)
        mq = [sb(st, "mq0", [128, 512], BF16)] * 2
        maskT = sb(st, "maskT", [128, 64, 128], BF16)
        Eb = [sb(st, "Eb%d" % i, [128, 1024], BF16) for i in range(2)]
        PT = [sb(st, "PT%d" % i, [128, 1024], BF16) for i in range(2)]
        rden = rt1
        ybt = [sb(st, "ybt0", [128, 1024], BF16)] * 2
        for half in range(4):
            for i in range(4):
                P.dma(hTo[:, :, i * 128:(i + 1) * 128], HT_d[4 * half + i].rearrange("p (kc n) -> p kc n", kc=8), r=['HT_d'], w=['hTo'])
            wcnt = 0
            for hh in range(16):
                wi2 = wcnt % 2; wcnt += 1
                load_weight_bf16((wst[wi2], wbf[wi2]), wq, hh * 128, 128, g1s, 'wbfQ%d' % wi2, stag='wstQ')
                for j in range(1):
                    b = j
                    for kc in range(8):
                        P.op('pe', lambda b=b, kc=kc, wi2=wi2, j=j: T.matmul(pb[b][:, :], lhsT=wbf[wi2][:, kc, :], rhs=hTo[:, kc, j * 512:(j + 1) * 512],
                                                                             start=(kc == 0), stop=(kc == 7), skip_group_check=True),
                             r=['wbfQ%d' % wi2, 'hTo'], w=[pbn[b]])
                    tcs = slice((half * 4) * 128, (half * 4) * 128 + 512)
                    if hh < 8:
                        P.dma(ctq[:], cAq[:, tcs], w=['ctq']); P.dma(stq[:], sAq[:, tcs], w=['stq'])
                        P.op('act', lambda b=b: A.copy(out=qtmp[:], in_=pb[b][:, :]), r=[pbn[b]], w=['qtmp'])
                        rope(qtmp[:], 128, permA, ctq[:], stq[:], rt1, rt2, 2 + j, ['ctq', 'stq'], 'qtmp', 512)
                        P.op('act', lambda hh=hh, j=j: A.copy(out=qT[:, 4 * j:4 * j + 4, hh, :], in_=qtmp[:].rearrange("p (a n) -> p a n", a=4)),
                             r=['qtmp'], w=['qT'])
                    else:
                        P.dma(ctq[:], cIq[:, tcs], w=['ctq']); P.dma(stq[:], sIq[:, tcs], w=['stq'])
                        dst = qiT[:, hh - 8, j * 512:(j + 1) * 512]
                        P.op('act', lambda b=b, dst=dst: A.copy(out=dst, in_=pb[b][:, :]), r=[pbn[b]], w=['qiT'])
                        rope(dst, 128, permI, ctq[:], stq[:], rt1, rt2, 2 + j, ['ctq', 'stq'], 'qiT', 512)
            load_weight_bf16((wst[0], wbf[0]), wwi, 0, 16, g1s, 'wbfQ0', stag='wstQ')
            for blk in range(4):
                b = 4 + blk % 2
                for kc in range(8):
                    P.op('pe', lambda b=b, kc=kc, blk=blk: T.matmul(pb[b][:, 0:16], lhsT=hTo[:, kc, blk * 128:(blk + 1) * 128], rhs=wbf[0][:, kc, 0:16],
                                                                     start=(kc == 0), stop=(kc == 7), skip_group_check=True),
                         r=['wbfQ0', 'hTo'], w=[pbn[b]])
                P.op('dve', lambda b=b, blk=blk: V.tensor_copy(out=wis[:, blk, :], in_=pb[b][:, 0:16]), r=[pbn[b]], w=['wis'])
            pending = None
            for blk in range(4):
                gi = 4 * half + blk
                nkt = gi + 1
                n = nkt * 512
                for h in range(16):
                    if h % 2 == 0:
                        P.op('act', lambda h=h, blk=blk: A.activation(out=Dg[:, h, :], in_=ident32[:], func=AF.Copy, scale=wis[:, blk, h:h + 1]),
                             r=['wis', 'ident32'], w=['Dg'])
                    else:
                        P.op('dve', lambda h=h, blk=blk: V.tensor_scalar(out=Dg[:, h, :], in0=ident32[:], scalar1=wis[:, blk, h:h + 1], scalar2=None, op0=ALU.mult),
                             r=['wis', 'ident32'], w=['Dg'])
                for kt in range(nkt):
                    bsc = 6 + kt % 2

                    def rel_pair(p, kt=kt, blk=blk):
                        X = 2 * (p % 3)
                        for hh in range(2):
                            rs = slice(64 * hh, 64 * hh + 64)
                            P.op('pe', lambda hh=hh, rs=rs: T.matmul(
                                pb[X + hh][:, :], lhsT=qiT[rs, p, blk * 128:(blk + 1) * 128], rhs=kiT_all[rs, kt * 512:(kt + 1) * 512],
                                start=True, stop=True, skip_group_check=True, tile_position=(64 * hh, 0)), r=['qiT', 'kiT_all'], w=[pbn[X + hh]])
                        rbuf = Rb[p % 3]; rn = 'Rb%d' % (p % 3)
                        src2 = pball[:, X * 512:(X + 2) * 512]
                        if p % 2 == 0:
                            P.op('act', lambda: A.activation(out=rbuf[:], in_=src2, func=AF.Relu), r=[pbn[X], pbn[X + 1]], w=[rn])
                        else:
                            P.op('dve', lambda: V.tensor_scalar(out=rbuf[:], in0=src2, scalar1=0.0, scalar2=None, op0=ALU.max), r=[pbn[X], pbn[X + 1]], w=[rn])

                    def score_pair(p, bsc=bsc):
                        rbuf = Rb[p % 3]; rn = 'Rb%d' % (p % 3)
                        for hh in range(2):
                            h = 2 * p + hh
                            P.op('pe', lambda h=h, hh=hh: T.matmul(pb[bsc][:, :], lhsT=Dg[:, h, :], rhs=rbuf[:, hh * 512:(hh + 1) * 512], start=(h == 0), stop=(h == 15),
                                                                   skip_group_check=True), r=['Dg', rn], w=[pbn[bsc]])
                    for step in range(8 + 2):
                        if step < 8:
                            rel_pair(step)
                        if step >= 2:
                            score_pair(step - 2)
                    mt = None
                    if kt == 0 and kt == nkt - 1:
                        mt = 2
                    elif kt == 0:
                        mt = 0
                    elif kt == nkt - 1:
                        mt = 1
                    dsts = score[:, kt * 512:(kt + 1) * 512]
                    if mt is None:
                        P.op('act', lambda bsc=bsc, dsts=dsts: A.copy(out=dsts, in_=pb[bsc][:, :]), r=[pbn[bsc]], w=['score'])
                    else:
                        P.op('dve', lambda bsc=bsc, dsts=dsts, mt=mt: V.tensor_tensor(out=dsts, in0=pb[bsc][:, :], in1=mtile[mt][:], op=ALU.add),
                             r=[pbn[bsc], 'mt%d' % mt], w=['score'])
                n_act = 512 * ((gi + 1) // 2)
                thr_c = 255.5 - 0.5 * n_act
                stps = [BIS_B * 2.0 / (2.0 ** (it + 1)) for it in range(NITER)]
                P.op('dve', lambda: V.memset(bs[:, 1:2], -BIS_B + stps[0]), r=['bs1'], w=['bs1'])
                def bis_iter(it, n=n, n_act=n_act, thr_c=thr_c, stps=stps):
                    if n_act > 0:
                        P.op('act', lambda n_act=n_act: A.activation(out=cjunk[:, 0:n_act], in_=score[:, 0:n_act], func=AF.Sign, bias=bs[:, 1:2], scale=-1.0,
                                                                    accum_out=bs[:, 5:6]), r=['bs1', 'score'], w=['bs5', 'cjunkA'])
                    P.op('dve', lambda n=n, n_act=n_act: V.tensor_scalar(out=cjunk[:, n_act:n], in0=score[:, n_act:n], scalar1=bs[:, 1:2], scalar2=0.0, op0=ALU.is_ge, op1=ALU.add,
                                                                        accum_out=bs[:, 2:3]), r=['bs1', 'score'], w=['bs2', 'cjunk'])
                    if n_act > 0:
                        P.op('dve', lambda: V.scalar_tensor_tensor(out=bs[:, 6:7], in0=bs[:, 5:6], scalar=-0.5, in1=bs[:, 2:3], op0=ALU.mult, op1=ALU.add), r=['bs2', 'bs5'], w=['bs6'])
                        tcol = 6
                    else:
                        tcol = 2
                    P.op('dve', lambda it=it, tcol=tcol, thr_c=thr_c: V.tensor_scalar(out=bs[:, 3:4], in0=bs[:, tcol:tcol + 1], scalar1=thr_c, scalar2=stps[it], op0=ALU.is_ge, op1=ALU.mult),
                         r=['bs%d' % tcol], w=['bs3'])
                    if it < NITER - 1:
                        P.op('dve', lambda it=it: V.scalar_tensor_tensor(out=bs[:, 1:2], in0=bs[:, 3:4], scalar=-stps[it + 1], in1=bs[:, 1:2], op0=ALU.add, op1=ALU.add), r=['bs3', 'bs1'], w=['bs1'])
                    else:
                        P.op('dve', lambda it=it: V.scalar_tensor_tensor(out=bs[:, 0:1], in0=bs[:, 3:4], scalar=-stps[it], in1=bs[:, 1:2], op0=ALU.add, op1=ALU.add), r=['bs3', 'bs1'], w=['bs'])
                if pending is not None:
                    na = len(pending)
                    for it in range(NITER):
                        bis_iter(it)
                        for f in pending[it * na // NITER:(it + 1) * na // NITER]:
                            f()
                    pending = None
                else:
                    for it in range(NITER):
                        bis_iter(it)
                for kt in range(nkt):
                    mqt = mq[0]; mn = 'mq0'
                    P.op('dve', lambda mqt=mqt, kt=kt: V.tensor_scalar(out=mqt[:], in0=score[:, kt * 512:(kt + 1) * 512], scalar1=bs[:, 0:1], scalar2=None, op0=ALU.is_ge),
                         r=['bs', 'score'], w=[mn])
                    bt_ = 4 + kt % 2
                    for a in range(4):
                        P.op('pe', lambda mqt=mqt, a=a, bt_=bt_: T.transpose(out=pb16(bt_)[:, a * 128:(a + 1) * 128], in_=mqt[:, a * 128:(a + 1) * 128], identity=identb[:]),
                             r=[mn, 'identb'], w=[pbn[bt_]])
                    P.op('act', lambda kt=kt, bt_=bt_: A.copy(out=maskT[:, 4 * kt:4 * kt + 4, :], in_=pb16(bt_)[:, 0:512].rearrange("p (a n) -> p a n", a=4)),
                         r=[pbn[bt_]], w=['maskT'])
                def make_att(blk=blk, gi=gi, nkt=nkt):
                    steps = []
                    ybtt = ybt[0]; ybn = 'ybt0'
                    nkb = 4 * nkt
                    npair = nkb // 2
                    for g in range(2):
                        rhsq = qT[:, blk, 4 * g:4 * g + 4, :].rearrange("p a n -> p (a n)")

                        def qk_pair(pq, g=g, rhsq=rhsq):
                            X = 2 * (pq % 2)
                            for j in range(2):
                                kb = 2 * pq + j
                                P.op('pe', lambda kb=kb, j=j: T.matmul(pb[X + j][:, :], lhsT=kT_all[:, g, kb * 128:(kb + 1) * 128], rhs=rhsq,
                                                                       start=True, stop=True, skip_group_check=True), r=['kT_all', 'qT'], w=[pbn[X + j]])
                            e = Eb[pq % 2]; en = 'Eb%d' % (pq % 2)
                            P.op('act', lambda: A.activation(out=e[:], in_=pball[:, X * 512:(X + 2) * 512], func=AF.Exp, scale=ISQ), r=[pbn[X], pbn[X + 1]], w=[en])
                            pt = PT[pq % 2]; pn = 'PT%d' % (pq % 2)
                            P.op('dve', lambda: V.tensor_tensor(out=pt[:].rearrange("p (k a n) -> p k a n", k=2, a=4), in0=e[:].rearrange("p (k a n) -> p k a n", k=2, a=4),
                                                                in1=maskT[:, 2 * pq:2 * pq + 2, :].unsqueeze(2).to_broadcast([128, 2, 4, 128]), op=ALU.mult),
                                 r=[en, 'maskT'], w=[pn])

                        def pv_pair(pq, g=g, nkb=nkb):
                            pt = PT[pq % 2]; pn = 'PT%d' % (pq % 2)
                            for j in range(2):
                                kb = 2 * pq + j
                                P.op('pe', lambda kb=kb, j=j: T.matmul(pb[6][:, :], lhsT=V_all[:, kb, g * 128:(g + 1) * 128], rhs=pt[:, j * 512:(j + 1) * 512],
                                                                       start=(kb == 0), stop=(kb == nkb - 1), skip_group_check=True), r=['V_all', pn], w=[pbn[6]])
                                P.op('pe', lambda kb=kb, j=j: T.matmul(pb[7][:, :], lhsT=onesb[:], rhs=pt[:, j * 512:(j + 1) * 512],
                                                                       start=(kb == 0), stop=(kb == nkb - 1), skip_group_check=True), r=['onesb', pn], w=[pbn[7]])

                        def one_step(step, qk_pair=qk_pair, pv_pair=pv_pair, npair=npair):
                            if step < npair:
                                qk_pair(step)
                            if step >= 1:
                                pv_pair(step - 1)
                        for step in range(npair + 1):
                            steps.append(lambda step=step, one_step=one_step: one_step(step))

                        def fin_g(g=g):
                            P.op('dve', lambda: V.reciprocal(out=rden[:], in_=pb[7][:, :]), r=[pbn[7]], w=['ropet1'])
                            P.op('dve', lambda: V.tensor_tensor(out=ybtt[:, g * 512:(g + 1) * 512], in0=pb[6][:, :], in1=rden[:], op=ALU.mult),
                                 r=[pbn[6], 'ropet1'], w=[ybn])
                        steps.append(fin_g)
                    steps.append(lambda: P.dma(YB_d[gi], ybtt[:], r=[ybn], w=['YB_d']))
                    return steps
                att = make_att()
                if blk == 3:
                    for f in att:
                        f()
                    pending = None
                else:
                    pending = att
    P.barrier()
    if stop('ATT'):
        return fin()

    with ExitStack() as st:
        h2T = sb(st, "h2T", [128, 8, 2048], BF16)
        sA = ExitStack()
        mT = sb(sA, "mT", [128, 8, 2048], BF16)
        with ExitStack() as s2:
            yaT = sb(s2, "yaT", [128, 8, 2048], BF16)
            wst = [sb(s2, "wstD%d" % i, [128, 8, 128], F32) for i in range(4)]
            wbf = [sb(s2, "wbfD%d" % i, [128, 8, 128], BF16) for i in range(4)]
            sg = [sb(s2, "sgD%d" % i, [128, 512], F32) for i in range(4)]
            with ExitStack() as s3:
                yT = sb(s3, "yT", [128, 8, 2048], BF16)
                for t in range(NT):
                    P.dma(yT[:, :, t * 128:(t + 1) * 128], Y_d[t].rearrange("p (kc n) -> p kc n", kc=8), r=['Y_d'], w=['yT'])
                for oc in range(8):
                    w2 = oc % 2
                    load_weight_bf16((wst[w2], wbf[w2]), wglu, oc * 128, 128, None, 'wbfD%d' % w2)
                    for j in range(4):
                        b = (oc * 4 + j) % 8
                        for kc in range(8):
                            P.op('pe', lambda b=b, kc=kc, w2=w2, j=j: T.matmul(pb[b][:, :], lhsT=wbf[w2][:, kc, :], rhs=yT[:, kc, j * 512:(j + 1) * 512],
                                                                               start=(kc == 0), stop=(kc == 7), skip_group_check=True), r=['wbfD%d' % w2, 'yT'], w=[pbn[b]])
                        sgt = sg[j % 2]; sn = 'sgD%d' % (j % 2)
                        P.op('act', lambda b=b, sgt=sgt: A.activation(out=sgt[:], in_=pb[b][:, :], func=AF.Sigmoid), r=[pbn[b]], w=[sn])
                        P.op('dve', lambda oc=oc, j=j, sgt=sgt: V.tensor_tensor(out=yaT[:, oc, j * 512:(j + 1) * 512], in0=sgt[:], in1=yT[:, oc, j * 512:(j + 1) * 512], op=ALU.mult),
                             r=[sn, 'yT'], w=['yaT'])
                P.barrier()
            ybT = sb(s2, "ybT", [128, 8, 2048], BF16)
            hTa = sb(s2, "hTa", [128, 8, 2048], BF16)
            for t in range(NT):
                P.dma(ybT[:, :, t * 128:(t + 1) * 128], YB_d[t].rearrange("p (h n) -> p h n", h=8), r=['YB_d'], w=['ybT'])
                P.dma(hTa[:, :, t * 128:(t + 1) * 128], HT_d[t].rearrange("p (kc n) -> p kc n", kc=8), r=['HT_d'], w=['hTa'])
            m1 = sb(s2, "mm1", [128, 512], F32); m2 = sb(s2, "mm2", [128, 512], F32)
            for oc in range(8):
                load_weight_bf16((wst[0], wbf[0]), wba, oc * 128, 128, None, 'wbfD0')
                load_weight_bf16((wst[1], wbf[1]), wbb, oc * 128, 128, None, 'wbfD1')
                load_weight_bf16((wst[2], wbf[2]), wg, oc * 128, 128, g1s, 'wbfD2')
                load_weight_bf16((wst[3], wbf[3]), wg, 1024 + oc * 128, 128, g1s, 'wbfD3')
                for j in range(4):
                    js = slice(j * 512, (j + 1) * 512)
                    acts = [yaT, ybT, hTa, hTa]; anm = ['yaT', 'ybT', 'hTa', 'hTa']
                    for q4 in range(4):
                        b = 4 * (j % 2) + q4
                        for kc in range(8):
                            P.op('pe', lambda b=b, kc=kc, q4=q4, js=js, acts=acts: T.matmul(pb[b][:, :], lhsT=wbf[q4][:, kc, :], rhs=acts[q4][:, kc, js],
                                                                                          start=(kc == 0), stop=(kc == 7), skip_group_check=True),
                                 r=['wbfD%d' % q4, anm[q4]], w=[pbn[b]])
                    b0 = 4 * (j % 2)
                    P.op('act', lambda b0=b0: A.activation(out=sg[0][:], in_=pb[b0 + 2][:, :], func=AF.Sigmoid), r=[pbn[b0 + 2]], w=['sgD0'])
                    P.op('act', lambda b0=b0: A.activation(out=sg[1][:], in_=pb[b0 + 3][:, :], func=AF.Sigmoid), r=[pbn[b0 + 3]], w=['sgD1'])
                    P.op('dve', lambda b0=b0: V.tensor_tensor(out=m1[:], in0=pb[b0][:, :], in1=sg[0][:], op=ALU.mult), r=[pbn[b0], 'sgD0'], w=['m1'])
                    P.op('dve', lambda b0=b0: V.tensor_tensor(out=m2[:], in0=pb[b0 + 1][:, :], in1=sg[1][:], op=ALU.mult), r=[pbn[b0 + 1], 'sgD1'], w=['m2'])
                    P.op('dve', lambda oc=oc, js=js: V.tensor_tensor(out=mT[:, oc, js], in0=m1[:], in1=m2[:], op=ALU.add), r=['m1', 'm2'], w=['mT'])
            P.barrier()
        if stop('MRG'):
            return fin()
        with ExitStack() as s2:
            WoB = sb(s2, "WoB", [128, 8, 1024], BF16)
            with ExitStack() as s3:
                wstg = sb(s3, "wstgO", [128, 8, 1024], F32)
                load_weight_bf16((wstg, WoB), wout, 0, 1024, None, 'WoB')
                P.barrier()
            xt = [sb(s2, "xtF%d" % i, [128, 1024], F32) for i in range(2)]
            x1t = [sb(s2, "x1F%d" % i, [128, 1024], F32) for i in range(2)]
            junk = sb(s2, "junkF", [128, 1024], F32)
            ssF = [sb(s2, "ssF%d" % i, [128, 4], F32) for i in range(2)]
            hbF = [sb(s2, "hbF%d" % i, [128, 1024], BF16) for i in range(2)]
            for gi in range(NT):
                i2 = gi % 2
                rows = slice((4 * gi + 3) * 128, (4 * gi + 4) * 128)
                P.dma(xt[i2][:], xp[rows, :], w=['xtF%d' % i2])
                for hf in range(2):
                    b = 2 * i2 + hf
                    for kc in range(8):
                        P.op('pe', lambda b=b, kc=kc, gi=gi, hf=hf: T.matmul(pb[b][:, :], lhsT=mT[:, kc, gi * 128:(gi + 1) * 128], rhs=WoB[:, kc, hf * 512:(hf + 1) * 512],
                                                                             start=(kc == 0), stop=(kc == 7), skip_group_check=True), r=['mT', 'WoB'], w=[pbn[b]])
                    P.op('dve', lambda b=b, hf=hf, i2=i2: V.tensor_tensor(out=x1t[i2][:, hf * 512:(hf + 1) * 512], in0=pb[b][:, :], in1=xt[i2][:, hf * 512:(hf + 1) * 512], op=ALU.add),
                         r=[pbn[b], 'xtF%d' % i2], w=['x1F%d' % i2])
                P.dma(X1_d[gi], x1t[i2][:], r=['x1F%d' % i2], w=['X1_d'])
                tg = 'F%d' % i2
                P.op('act', lambda i2=i2: A.activation(out=junk[:], in_=x1t[i2][:], func=AF.Square, accum_out=ssF[i2][:, 0:1]), r=['x1F%d' % i2], w=['junkF', tg + 'ss'])
                P.op('dve', lambda i2=i2: V.tensor_scalar(out=ssF[i2][:, 1:2], in0=ssF[i2][:, 0:1], scalar1=1.0 / D, scalar2=EPS, op0=ALU.mult, op1=ALU.add), r=[tg + 'ss'], w=[tg + 'ss'])
                P.op('act', lambda i2=i2: A.activation(out=ssF[i2][:, 2:3], in_=ssF[i2][:, 1:2], func=AF.Sqrt), r=[tg + 'ss'], w=[tg + 'ss'])
                P.op('dve', lambda i2=i2: V.reciprocal(out=ssF[i2][:, 3:4], in_=ssF[i2][:, 2:3]), r=[tg + 'ss'], w=[tg + 'ss'])
                P.op('dve', lambda i2=i2: V.tensor_scalar(out=hbF[i2][:], in0=x1t[i2][:], scalar1=ssF[i2][:, 3:4], scalar2=None, op0=ALU.mult), r=['x1F%d' % i2, tg + 'ss'], w=[tg + 'hb'])
                bt_ = 4 + i2
                for kc in range(8):
                    P.op('pe', lambda kc=kc, i2=i2, bt_=bt_: T.transpose(out=pb16(bt_)[:, kc * 128:(kc + 1) * 128], in_=hbF[i2][:, kc * 128:(kc + 1) * 128], identity=identb[:]),
                         r=[tg + 'hb', 'identb'], w=[pbn[bt_]])
                P.op('act', lambda gi=gi, bt_=bt_: A.copy(out=h2T[:, :, gi * 128:(gi + 1) * 128], in_=pb16(bt_).rearrange("p (kc n) -> p kc n", kc=8)), r=[pbn[bt_]], w=['h2T'])
            P.barrier()
        sA.close()
        if stop('F'):
            return fin()
        actT = sb(st, "actT", [128, 22, 2048], BF16)
        with ExitStack() as s2:
            wst = [sb(s2, "wstG%d" % i, [128, 8, 128], F32) for i in range(2)]
            wbf = [sb(s2, "wbfG%d" % i, [128, 8, 128], BF16) for i in range(2)]
            sgl = [sb(s2, "sgG%d" % i, [128, 512], F32) for i in range(2)]
            for jh in range(22):
                load_weight_bf16((wst[0], wbf[0]), wfi, jh * 128, 128, g2s, 'wbfG0')
                load_weight_bf16((wst[1], wbf[1]), wfi, FH + jh * 128, 128, g2s, 'wbfG1')
                for tg_ in range(4):
                    ts_ = slice(tg_ * 512, (tg_ + 1) * 512)
                    b0 = 2 * (tg_ % 4)
                    for q2 in range(2):
                        for kc in range(8):
                            P.op('pe', lambda b0=b0, q2=q2, kc=kc, ts_=ts_: T.matmul(pb[b0 + q2][:, :], lhsT=wbf[q2][:, kc, :], rhs=h2T[:, kc, ts_],
                                                                                   start=(kc == 0), stop=(kc == 7), skip_group_check=True), r=['wbfG%d' % q2, 'h2T'], w=[pbn[b0 + q2]])
                    sgt = sgl[tg_ % 2]; sn = 'sgG%d' % (tg_ % 2)
                    P.op('act', lambda b0=b0, sgt=sgt: A.activation(out=sgt[:], in_=pb[b0][:, :], func=AF.Silu), r=[pbn[b0]], w=[sn])
                    P.op('dve', lambda b0=b0, sgt=sgt, jh=jh, ts_=ts_: V.tensor_tensor(out=actT[:, jh, ts_], in0=pb[b0 + 1][:, :], in1=sgt[:], op=ALU.mult),
                         r=[pbn[b0 + 1], sn], w=['actT'])
            P.barrier()
        with ExitStack() as s2:
            WfoB = sb(s2, "WfoB", [128, 22, 1024], BF16)
            wstg2 = [sb(s2, "wstgFo%d" % i, [128, 1024], F32) for i in range(2)]
            for jh in range(22):
                i2 = jh % 2
                P.dma(wstg2[i2][:], wfo[jh * 128:(jh + 1) * 128, :], w=['wstgFo%d' % i2])
                if i2 == 0:
                    P.op('act', lambda jh=jh, i2=i2: A.copy(out=WfoB[:, jh, :], in_=wstg2[i2][:]), r=['wstgFo%d' % i2], w=['WfoB'])
                else:
                    P.op('dve', lambda jh=jh, i2=i2: V.tensor_copy(out=WfoB[:, jh, :], in_=wstg2[i2][:]), r=['wstgFo%d' % i2], w=['WfoB'])
            gft = sb(s2, "gft", [128, 1024], F32)
            P.dma(gft[:], gfb[:, :], w=['gft'])
            x1r = [sb(s2, "x1r%d" % i, [128, 1024], F32) for i in range(2)]
            x2 = [sb(s2, "x2_%d" % i, [128, 1024], F32) for i in range(2)]
            junkG = sb(s2, "junkG", [128, 1024], F32)
            ssG = [sb(s2, "ssG%d" % i, [128, 4], F32) for i in range(2)]
            ot = [sb(s2, "ot%d" % i, [128, 1024], F32) for i in range(2)]
            for gi in range(NT):
                i2 = gi % 2
                P.dma(x1r[i2][:], X1_d[gi], r=['X1_d'], w=['x1r%d' % i2])
                for hf in range(2):
                    b = 2 * i2 + hf
                    for jh in range(22):
                        P.op('pe', lambda b=b, jh=jh, gi=gi, hf=hf: T.matmul(pb[b][:, :], lhsT=actT[:, jh, gi * 128:(gi + 1) * 128], rhs=WfoB[:, jh, hf * 512:(hf + 1) * 512],
                                                                             start=(jh == 0), stop=(jh == 21), skip_group_check=True), r=['actT', 'WfoB'], w=[pbn[b]])
                    P.op('dve', lambda b=b, hf=hf, i2=i2: V.tensor_tensor(out=x2[i2][:, hf * 512:(hf + 1) * 512], in0=pb[b][:, :], in1=x1r[i2][:, hf * 512:(hf + 1) * 512], op=ALU.add),
                         r=[pbn[b], 'x1r%d' % i2], w=['x2_%d' % i2])
                tg = 'G%d' % i2
                P.op('act', lambda i2=i2: A.activation(out=junkG[:], in_=x2[i2][:], func=AF.Square, accum_out=ssG[i2][:, 0:1]), r=['x2_%d' % i2], w=['junkG', tg + 'ss'])
                P.op('dve', lambda i2=i2: V.tensor_scalar(out=ssG[i2][:, 1:2], in0=ssG[i2][:, 0:1], scalar1=1.0 / D, scalar2=EPS, op0=ALU.mult, op1=ALU.add), r=[tg + 'ss'], w=[tg + 'ss'])
                P.op('act', lambda i2=i2: A.activation(out=ssG[i2][:, 2:3], in_=ssG[i2][:, 1:2], func=AF.Sqrt), r=[tg + 'ss'], w=[tg + 'ss'])
                P.op('dve', lambda i2=i2: V.reciprocal(out=ssG[i2][:, 3:4], in_=ssG[i2][:, 2:3]), r=[tg + 'ss'], w=[tg + 'ss'])
                P.op('dve', lambda i2=i2: V.tensor_scalar(out=ot[i2][:], in0=x2[i2][:], scalar1=ssG[i2][:, 3:4], scalar2=None, op0=ALU.mult), r=['x2_%d' % i2, tg + 'ss'], w=['ot%d' % i2])
                P.op('dve', lambda i2=i2: V.tensor_tensor(out=ot[i2][:], in0=ot[i2][:], in1=gft[:], op=ALU.mult), r=['ot%d' % i2, 'gft'], w=['ot%d' % i2])
                P.dma(out_d[gi * 128:(gi + 1) * 128, :], ot[i2][:], r=['ot%d' % i2], w=['out_d'])
            P.barrier()
    P.barrier()
    return nc


def _host_prep(inputs):
    x = np.asarray(inputs['x'], np.float32)
    w_in = np.asarray(inputs['w_in'], np.float32)[0]
    pts = np.cumsum([1024, 1024, 256, 256, 1024, 64, 16, 1024, 1024])
    wu, wq_, wk, wv, wqi, wki, wwi, wga, wgb = np.split(w_in, pts[:-1], axis=1)
    a_re = np.asarray(inputs['a_re'], np.float32)[0]; a_im = np.asarray(inputs['a_im'], np.float32)[0]
    log_dt = np.asarray(inputs['log_dt'], np.float32)[0]
    b_re = np.asarray(inputs['b_re'], np.float32)[0]; b_im = np.asarray(inputs['b_im'], np.float32)[0]
    c_re = np.asarray(inputs['c_re'], np.float32)[0]; c_im = np.asarray(inputs['c_im'], np.float32)[0]
    d_skip = np.asarray(inputs['d_skip'], np.float32)[0]

    def gT(g):
        return np.ascontiguousarray(np.asarray(g, np.float32).reshape(8, 128).T)

    r = np.arange(128); kc = np.arange(8)
    pp = r // 32; ggr = (r // 16) % 2; cr = r % 16
    AR1 = np.zeros((128, 8, 2, 64), np.float32); AI1 = np.zeros_like(AR1); DT1 = np.zeros_like(AR1)
    BR1 = np.zeros_like(AR1); BI1 = np.zeros_like(AR1)
    for k in range(8):
        for g2 in range(2):
            g = 2 * (4 * k + pp) + g2
            AR1[:, k, g2, :] = a_re[g, :]
            AI1[:, k, g2, :] = a_im[g, :]
            DT1[:, k, g2, :] = log_dt[g][:, None]
            sel = (ggr == g2)
            BR1[sel, k, g2, :] = b_re[g[sel], :, cr[sel]]
            BI1[sel, k, g2, :] = b_im[g[sel], :, cr[sel]]
    gg = np.arange(128) // 64; p_ = np.arange(128) % 64
    gidx = 2 * np.arange(32)[None, :] + gg[:, None]
    ARE = a_re[gidx, p_[:, None]]; AIE = a_im[gidx, p_[:, None]]; DTE = log_dt[gidx]
    CTR = c_re[gidx, :, p_[:, None]]
    CTI = c_im[gidx, :, p_[:, None]]
    BER = np.zeros((128, 32, 2, 16), np.float32); BEI = np.zeros_like(BER)
    for g2 in range(2):
        sel = gg == g2
        BER[sel, :, g2, :] = b_re[gidx[sel], p_[sel][:, None], :]
        BEI[sel, :, g2, :] = b_im[gidx[sel], p_[sel][:, None], :]
    DFM = np.ascontiguousarray(d_skip.reshape(8, 128).T)
    common = dict(
        wu=np.ascontiguousarray(wu),
        wkv=np.ascontiguousarray(np.concatenate([wk, wki, wki, wv], axis=1)),
        wq=np.ascontiguousarray(np.concatenate([wq_, wqi], axis=1)),
        wwi=np.ascontiguousarray(wwi),
        wg=np.ascontiguousarray(np.concatenate([wga, wgb], axis=1)),
        wglu=np.asarray(inputs['w_glu'], np.float32)[0], wba=np.asarray(inputs['w_branch_a'], np.float32)[0],
        wbb=np.asarray(inputs['w_branch_b'], np.float32)[0], wout=np.asarray(inputs['w_out'], np.float32)[0],
        wfi=np.asarray(inputs['w_ffn_in'], np.float32)[0], wfo=np.asarray(inputs['w_ffn_out'], np.float32)[0],
        g1T=gT(inputs['norm1_g'][0]), g2T=gT(inputs['norm2_g'][0]),
        gfb=np.ascontiguousarray(np.broadcast_to(np.asarray(inputs['norm_f_g'], np.float32)[None, :], (128, D))),
        AR1=AR1.reshape(128, 1024), AI1=AI1.reshape(128, 1024), DT1=DT1.reshape(128, 1024),
        BR1=BR1.reshape(128, 1024), BI1=BI1.reshape(128, 1024),
        ARE=np.ascontiguousarray(ARE), AIE=np.ascontiguousarray(AIE), DTE=np.ascontiguousarray(DTE),
        CTR=np.ascontiguousarray(CTR).reshape(128, 512), CTI=np.ascontiguousarray(CTI).reshape(128, 512),
        BER=BER.reshape(128, 1024), BEI=BEI.reshape(128, 1024), DFM=DFM,
        identf=np.eye(128, dtype=np.float32),
    )
    permA = np.zeros((128, 128), np.float32)
    for m in range(32):
        permA[m + 16 if m < 16 else m - 16, m] = 1
    permI = np.zeros((128, 128), np.float32)
    for hb in (0, 64):
        for m in range(16):
            permI[hb + (m + 8 if m < 8 else m - 8), hb + m] = 1
    common['permA'] = permA; common['permI'] = permI

    def tables(pos, kind):
        pos = pos.astype(np.float32)
        cos = np.ones((128, pos.shape[0]), np.float32); sin = np.zeros_like(cos)
        if kind == 'A':
            half = 16
            inv = (np.float32(500000.0) ** (-np.arange(half, dtype=np.float32) / half)).astype(np.float32)
            ang = pos[None, :] * inv[:, None]
            cos[0:16] = np.cos(ang); cos[16:32] = np.cos(ang)
            sin[0:16] = -np.sin(ang); sin[16:32] = np.sin(ang)
        else:
            half = 8
            inv = (np.float32(500000.0) ** (-np.arange(half, dtype=np.float32) / half)).astype(np.float32)
            ang = pos[None, :] * inv[:, None]
            for hb in (0, 64):
                cos[hb:hb + 8] = np.cos(ang); cos[hb + 8:hb + 16] = np.cos(ang)
                sin[hb:hb + 8] = -np.sin(ang); sin[hb + 8:hb + 16] = np.sin(ang)
        return cos, sin

    in_maps = []
    for c in range(8):
        b, r_ = c // 4, c % 4
        pad = (3 - r_) * 128
        xpad = np.zeros((L, D), np.float32)
        xpad[pad:] = x[b, :L - pad]
        pos_all = np.maximum(np.arange(L) - pad, 0)
        own = np.concatenate([np.arange(128) + (4 * i + 3) * 128 for i in range(NT)])
        pos_own = own - pad
        m = dict(common)
        m['xp'] = xpad
        m['cAk'], m['sAk'] = tables(pos_all, 'A')
        m['cIk'], m['sIk'] = tables(pos_all, 'I')
        m['cAq'], m['sAq'] = tables(pos_own, 'A')
        m['cIq'], m['sIq'] = tables(pos_own, 'I')
        q = np.arange(128)[:, None]; kk = np.arange(512)[None, :]
        caus = np.where(((kk < 384) | ((kk - 384) // 64 <= q // 64)), 0.0, NEG).astype(np.float32)
        padm = np.where(kk >= pad, 0.0, NEG).astype(np.float32) * np.ones((128, 1), np.float32)
        m['mfirst'] = np.ascontiguousarray(padm)
        m['mlast'] = np.ascontiguousarray(caus)
        m['mboth'] = np.minimum(padm, caus).astype(np.float32)
        in_maps.append(m)
    return in_maps


def kernel(**inputs):
    in_maps = _host_prep(inputs)
    nc = build_program()
    res = run_bass_kernel_spmd(nc, in_maps, core_ids=list(range(8)))
    out = np.zeros((2, L, D), np.float32)
    for c in range(8):
        b, r_ = c // 4, c % 4
        o = res.results[c]["out"].reshape(NT, 128, D)
        for i in range(NT):
            j = 4 * i + r_
            out[b, j * 128:(j + 1) * 128] = o[i]
    return out
```

```python
import math
from contextlib import ExitStack
import numpy as np
import concourse.bass as bass
import concourse.mybir as mybir
from concourse.bass_utils import run_bass_kernel_spmd

F32 = mybir.dt.float32
BF16 = mybir.dt.bfloat16
AF = mybir.ActivationFunctionType
ALU = mybir.AluOpType

D = 1024
L = 8192
NT = 16
FH = 2816
EPS = 1e-6
NEG = -1e30
NITER = 18
BIS_B = 64.0
NDS = 24


class Prog:
    def __init__(self, nc, es):
        self.nc = nc
        self.E = {'pe': nc.tensor, 'act': nc.scalar, 'dve': nc.vector, 'pool': nc.gpsimd, 'sp': nc.sync}
        self.sem = {k: es.enter_context(nc.semaphore('s_' + k)) for k in ['pe', 'act', 'dve', 'pool']}
        self.cnt = {k: 0 for k in self.sem}
        self.dsem = [es.enter_context(nc.semaphore('d%d' % i)) for i in range(NDS)]
        self.dcnt = [0] * NDS
        self.dnext = 0
        self.seen = {e: {} for e in self.E}
        self.lw = {}
        self.rd = {}

    def _semobj(self, key):
        return self.dsem[key[1]] if isinstance(key, tuple) else self.sem[key]

    def _wait(self, eng, ev):
        key, val = ev
        if self.seen[eng].get(key, 0) >= val:
            return
        self.seen[eng][key] = val
        self.E[eng].wait_ge(self._semobj(key), val)

    def _deps(self, eng, r, w):
        evs = {}
        for x in r:
            if x in self.lw:
                k, v = self.lw[x]
                evs[k] = max(evs.get(k, 0), v)
        for x in w:
            if x in self.lw:
                k, v = self.lw[x]
                evs[k] = max(evs.get(k, 0), v)
            for k, v in self.rd.get(x, {}).items():
                evs[k] = max(evs.get(k, 0), v)
        for k, v in evs.items():
            if eng == 'pe' and k == 'pe':
                continue
            self._wait(eng, (k, v))

    def _record(self, me, r, w):
        k, v = me
        for x in r:
            d = self.rd.setdefault(x, {})
            d[k] = max(d.get(k, 0), v)
        for x in w:
            self.lw[x] = me
            self.rd[x] = {}

    def op(self, eng, fn, r=(), w=()):
        self._deps(eng, r, w)
        ins = fn()
        self.cnt[eng] += 1
        ins.then_inc(self.sem[eng], 1)
        self._record((eng, self.cnt[eng]), r, w)

    def dma(self, out, in_, r=(), w=(), q='sp'):
        self._deps(q, r, w)
        i = self.dnext
        self.dnext = (i + 1) % NDS
        if self.dcnt[i] > 0:
            self._wait(q, (('d', i), self.dcnt[i]))
        self.E[q].dma_start(out=out, in_=in_).then_inc(self.dsem[i], 16)
        self.dcnt[i] += 16
        self._record((('d', i), self.dcnt[i]), r, w)

    def barrier(self):
        for e in ['pe', 'act', 'dve', 'pool', 'sp']:
            for k in self.sem:
                if self.cnt[k] > 0:
                    self._wait(e, (k, self.cnt[k]))
            for i in range(NDS):
                if self.dcnt[i] > 0:
                    self._wait(e, (('d', i), self.dcnt[i]))
        self.lw = {}
        self.rd = {}


def build_program(dbg=None):
    nc = bass.Bass("TRN2", target_bir_lowering=False)

    def din(name, shape, dt=F32):
        return nc.dram_tensor(name, list(shape), dt, kind="ExternalInput").ap()

    def dscr(name, shape, dt):
        return nc.dram_tensor(name, list(shape), dt, kind="Internal").ap()

    xp = din("xp", [L, D])
    wu = din("wu", [D, 1024])
    wkv = din("wkv", [D, 640])
    wq = din("wq", [D, 2048])
    wwi = din("wwi", [D, 16])
    wg = din("wg", [D, 2048])
    wglu = din("wglu", [D, D])
    wba = din("wba", [D, D])
    wbb = din("wbb", [D, D])
    wout = din("wout", [D, D])
    wfi = din("wfi", [D, 2 * FH])
    wfo = din("wfo", [FH, D])
    g1T = din("g1T", [128, 8])
    g2T = din("g2T", [128, 8])
    gfb = din("gfb", [128, D])
    AR1 = din("AR1", [128, 1024]); AI1 = din("AI1", [128, 1024]); DT1 = din("DT1", [128, 1024])
    BR1 = din("BR1", [128, 1024]); BI1 = din("BI1", [128, 1024])
    ARE = din("ARE", [128, 32]); AIE = din("AIE", [128, 32]); DTE = din("DTE", [128, 32])
    CTR = din("CTR", [128, 512]); CTI = din("CTI", [128, 512])
    BER = din("BER", [128, 1024]); BEI = din("BEI", [128, 1024])
    DFM = din("DFM", [128, 8])
    identf_d = din("identf", [128, 128])
    permA_d = din("permA", [128, 128]); permI_d = din("permI", [128, 128])
    cAk = din("cAk", [128, L]); sAk = din("sAk", [128, L])
    cIk = din("cIk", [128, L]); sIk = din("sIk", [128, L])
    cAq = din("cAq", [128, 2048]); sAq = din("sAq", [128, 2048])
    cIq = din("cIq", [128, 2048]); sIq = din("sIq", [128, 2048])
    mfirst = din("mfirst", [128, 512]); mlast = din("mlast", [128, 512]); mboth = din("mboth", [128, 512])
    out_d = nc.dram_tensor("out", [2048, D], F32, kind="ExternalOutput").ap()
    dbg_d = None
    if dbg:
        dbg_d = nc.dram_tensor("dbg", list(dbg[1]), F32, kind="ExternalOutput").ap()

    HT_d = dscr("HT_d", [NT, 128, 1024], BF16)
    U_d = dscr("U_d", [NT, 128, 1024], BF16)
    HS_d = dscr("HS_d", [NT, 128, 1024], BF16)
    Y_d = dscr("Y_d", [NT, 128, 1024], BF16)
    YB_d = dscr("YB_d", [NT, 128, 1024], BF16)
    X1_d = dscr("X1_d", [NT, 128, 1024], F32)
    HTALL_d = dscr("HTALL_d", [NT, 128, 4096], BF16)

    es = ExitStack()
    P = Prog(nc, es)
    V, A, G, T = nc.vector, nc.scalar, nc.gpsimd, nc.tensor

    def sb(stack, name, shape, dt):
        return stack.enter_context(nc.sbuf_tensor(name, list(shape), dt))

    def fin():
        P.barrier()
        return nc

    def stop(name):
        return bool(dbg) and dbg[0] == name

    identf = sb(es, "identf_s", [128, 128], F32)
    identb = sb(es, "identb", [128, 128], BF16)
    permA = sb(es, "permA_s", [128, 128], BF16)
    permI = sb(es, "permI_s", [128, 128], BF16)
    onesb = sb(es, "onesb", [128, 128], BF16)
    g1s = sb(es, "g1s", [128, 8], F32)
    g2s = sb(es, "g2s", [128, 8], F32)
    ptmp = sb(es, "ptmp", [128, 128], F32)
    P.dma(identf[:], identf_d[:, :], w=['identf'])
    P.op('dve', lambda: V.tensor_copy(out=identb[:], in_=identf[:]), r=['identf'], w=['identb'])
    P.dma(ptmp[:], permA_d[:, :], w=['ptmp'])
    P.op('dve', lambda: V.tensor_copy(out=permA[:], in_=ptmp[:]), r=['ptmp'], w=['permA'])
    P.dma(ptmp[:], permI_d[:, :], w=['ptmp'])
    P.op('dve', lambda: V.tensor_copy(out=permI[:], in_=ptmp[:]), r=['ptmp'], w=['permI'])
    P.op('pool', lambda: G.memset(onesb[:], 1.0), w=['onesb'])
    ident32 = sb(es, "ident32", [128, 128], F32)
    P.op('dve', lambda: V.tensor_scalar(out=ident32[:], in0=identf[:], scalar1=1.0 / 32.0, scalar2=None, op0=ALU.mult), r=['identf'], w=['ident32'])
    P.dma(g1s[:], g1T[:, :], w=['g1s'])
    P.dma(g2s[:], g2T[:, :], w=['g2s'])

    pball = es.enter_context(nc.psum_tensor("pball", [128, 4096], F32))
    pb = [pball[:, i * 512:(i + 1) * 512] for i in range(8)]
    pbn = ['pb%d' % i for i in range(8)]

    def pb16(i):
        return pb[i][:, :].bitcast(BF16)

    def load_weight_bf16(stack_tiles, wdram, c0, ncols, gscale, tag, stag=None):
        stg, dst = stack_tiles
        stag = stag or tag
        for kc in range(8):
            P.dma(stg[:, kc, 0:ncols], wdram[kc * 128:(kc + 1) * 128, c0:c0 + ncols], w=[stag + 's%d' % kc])
            if gscale is None:
                if kc % 2 == 0:
                    P.op('act', lambda kc=kc: A.copy(out=dst[:, kc, 0:ncols], in_=stg[:, kc, 0:ncols]), r=[stag + 's%d' % kc], w=[tag])
                else:
                    P.op('dve', lambda kc=kc: V.tensor_copy(out=dst[:, kc, 0:ncols], in_=stg[:, kc, 0:ncols]), r=[stag + 's%d' % kc], w=[tag])
            else:
                P.op('dve', lambda kc=kc: V.tensor_scalar(out=dst[:, kc, 0:ncols], in0=stg[:, kc, 0:ncols], scalar1=gscale[:, kc:kc + 1], scalar2=None, op0=ALU.mult),
                     r=[stag + 's%d' % kc, 'g1s', 'g2s'], w=[tag])

    def norm_transpose(stack_t, xsrc_rows, hT, col0, tagx, gate_bank, act_scale=False):
        xt, junk, ss, hb = stack_t
        P.dma(xt[:], xsrc_rows, w=[tagx + 'xt'])
        P.op('act', lambda: A.activation(out=junk[:], in_=xt[:], func=AF.Square, accum_out=ss[:, 0:1]),
             r=[tagx + 'xt'], w=[tagx + 'junk', tagx + 'ss'])
        P.op('dve', lambda: V.tensor_scalar(out=ss[:, 1:2], in0=ss[:, 0:1], scalar1=1.0 / D, scalar2=EPS, op0=ALU.mult, op1=ALU.add),
             r=[tagx + 'ss'], w=[tagx + 'ss'])
        P.op('act', lambda: A.activation(out=ss[:, 2:3], in_=ss[:, 1:2], func=AF.Sqrt), r=[tagx + 'ss'], w=[tagx + 'ss'])
        P.op('dve', lambda: V.reciprocal(out=ss[:, 3:4], in_=ss[:, 2:3]), r=[tagx + 'ss'], w=[tagx + 'ss'])
        if act_scale:
            P.op('act', lambda: A.activation(out=hb[:], in_=xt[:], func=AF.Copy, scale=ss[:, 3:4]),
                 r=[tagx + 'xt', tagx + 'ss'], w=[tagx + 'hb'])
        else:
            P.op('dve', lambda: V.tensor_scalar(out=hb[:], in0=xt[:], scalar1=ss[:, 3:4], scalar2=None, op0=ALU.mult),
                 r=[tagx + 'xt', tagx + 'ss'], w=[tagx + 'hb'])
        b = gate_bank
        for kc in range(8):
            P.op('pe', lambda kc=kc: T.transpose(out=pb16(b)[:, kc * 128:(kc + 1) * 128], in_=hb[:, kc * 128:(kc + 1) * 128], identity=identb[:]),
                 r=[tagx + 'hb', 'identb'], w=[pbn[b]])
        return b

    def rope(dst, nrows, perm, ctab, stab, tmp1, tmp2, bank, tags_r, tag_w, ncols):
        P.op('pe', lambda: T.matmul(pb[bank][0:nrows, 0:ncols], lhsT=perm[0:nrows, 0:nrows], rhs=dst, start=True, stop=True, skip_group_check=True),
             r=[tag_w, 'permA', 'permI'], w=[pbn[bank]])
        P.op('dve', lambda: V.tensor_tensor(out=tmp1[0:nrows, 0:ncols], in0=dst, in1=ctab, op=ALU.mult), r=[tag_w] + tags_r, w=['ropet1'])
        P.op('dve', lambda: V.tensor_tensor(out=tmp2[0:nrows, 0:ncols], in0=pb[bank][0:nrows, 0:ncols], in1=stab, op=ALU.mult),
             r=[pbn[bank]] + tags_r, w=['ropet2'])
        P.op('dve', lambda: V.tensor_tensor(out=dst, in0=tmp1[0:nrows, 0:ncols], in1=tmp2[0:nrows, 0:ncols], op=ALU.add),
             r=['ropet1', 'ropet2'], w=[tag_w])

    sq = ExitStack()
    QP = sb(sq, "QP", [128, 9, 2, 32], F32)
    FP = sb(sq, "FP", [128, 8, 2, 32], F32)
    s5 = ExitStack()
    W1 = sb(s5, "W1", [128, 8, 2, 8, 128], BF16)
    cosT = sb(s5, "cosT", [128, 32, 64], F32)
    sinT = sb(s5, "sinT", [128, 32, 64], F32)
    R8z = sb(s5, "R8z", [128, 32, 64], F32)
    A8r = sb(s5, "A8r", [128, 32], F32)
    A8i = sb(s5, "A8i", [128, 32], F32)

    def sincos_lb(stack, n, ar, ai, dtl, pref):
        t = {}
        for nm in ['dt', 'ang', 'ex', 'mag', 'nn', 'red', 'sn', 'cs', 'lbr', 'lbi', 'fr', 'fi', 'u1', 'u2', 'u3']:
            t[nm] = sb(stack, pref + nm, [128, n], F32)
        R = [pref]
        def vv(fn):
            P.op('dve', fn, r=R, w=R)
        def aa(fn):
            P.op('act', fn, r=R, w=R)
        aa(lambda: A.activation(out=t['dt'][:], in_=dtl, func=AF.Exp))
        vv(lambda: V.tensor_tensor(out=t['ang'][:], in0=ai, in1=t['dt'][:], op=ALU.mult))
        vv(lambda: V.tensor_tensor(out=t['ex'][:], in0=ar, in1=t['dt'][:], op=ALU.mult))
        aa(lambda: A.activation(out=t['mag'][:], in_=t['ex'][:], func=AF.Exp))
        C1 = float(np.float32(2 * math.pi))
        C2 = float(2 * math.pi - np.float64(np.float32(2 * math.pi)))
        for which, off in (('sn', 0.0), ('cs', math.pi / 2)):
            vv(lambda off=off: V.tensor_scalar(out=t['u1'][:], in0=t['ang'][:], scalar1=off, scalar2=None, op0=ALU.add))
            vv(lambda: V.tensor_scalar(out=t['nn'][:], in0=t['u1'][:], scalar1=math.pi, scalar2=None, op0=ALU.is_gt))
            for kk in (3, 5, 7):
                vv(lambda kk=kk: V.tensor_scalar(out=t['u2'][:], in0=t['u1'][:], scalar1=kk * math.pi, scalar2=None, op0=ALU.is_gt))
                vv(lambda: V.tensor_tensor(out=t['nn'][:], in0=t['nn'][:], in1=t['u2'][:], op=ALU.add))
            vv(lambda: V.scalar_tensor_tensor(out=t['red'][:], in0=t['nn'][:], scalar=-C1, in1=t['u1'][:], op0=ALU.mult, op1=ALU.add))
            vv(lambda: V.scalar_tensor_tensor(out=t['red'][:], in0=t['nn'][:], scalar=-C2, in1=t['red'][:], op0=ALU.mult, op1=ALU.add))
            vv(lambda: V.tensor_scalar(out=t['red'][:], in0=t['red'][:], scalar1=math.pi, scalar2=-math.pi, op0=ALU.min, op1=ALU.max))
            aa(lambda which=which: A.activation(out=t[which][:], in_=t['red'][:], func=AF.Sin))
        vv(lambda: V.tensor_tensor(out=t['lbr'][:], in0=t['mag'][:], in1=t['cs'][:], op=ALU.mult))
        vv(lambda: V.tensor_tensor(out=t['lbi'][:], in0=t['mag'][:], in1=t['sn'][:], op=ALU.mult))
        vv(lambda: V.tensor_tensor(out=t['u1'][:], in0=ar, in1=ar, op=ALU.mult))
        vv(lambda: V.tensor_tensor(out=t['u2'][:], in0=ai, in1=ai, op=ALU.mult))
        vv(lambda: V.tensor_tensor(out=t['u1'][:], in0=t['u1'][:], in1=t['u2'][:], op=ALU.add))
        vv(lambda: V.reciprocal(out=t['u3'][:], in_=t['u1'][:]))
        vv(lambda: V.tensor_scalar(out=t['u1'][:], in0=t['lbr'][:], scalar1=-1.0, scalar2=None, op0=ALU.add))
        vv(lambda: V.tensor_tensor(out=t['fr'][:], in0=t['u1'][:], in1=ar, op=ALU.mult))
        vv(lambda: V.tensor_tensor(out=t['u2'][:], in0=t['lbi'][:], in1=ai, op=ALU.mult))
        vv(lambda: V.tensor_tensor(out=t['fr'][:], in0=t['fr'][:], in1=t['u2'][:], op=ALU.add))
        vv(lambda: V.tensor_tensor(out=t['fr'][:], in0=t['fr'][:], in1=t['u3'][:], op=ALU.mult))
        vv(lambda: V.tensor_tensor(out=t['fi'][:], in0=t['lbi'][:], in1=ar, op=ALU.mult))
        vv(lambda: V.tensor_tensor(out=t['u2'][:], in0=t['u1'][:], in1=ai, op=ALU.mult))
        vv(lambda: V.tensor_tensor(out=t['fi'][:], in0=t['fi'][:], in1=t['u2'][:], op=ALU.subtract))
        vv(lambda: V.tensor_tensor(out=t['fi'][:], in0=t['fi'][:], in1=t['u3'][:], op=ALU.mult))
        return t

    def cmul(outr, outi, ar_, ai_, br_, bi_, t1, t2, R):
        P.op('dve', lambda: V.tensor_tensor(out=t1, in0=ar_, in1=br_, op=ALU.mult), r=R, w=R)
        P.op('dve', lambda: V.tensor_tensor(out=t2, in0=ai_, in1=bi_, op=ALU.mult), r=R, w=R)
        P.op('dve', lambda: V.tensor_tensor(out=outr, in0=t1, in1=t2, op=ALU.subtract), r=R, w=R)
        P.op('dve', lambda: V.tensor_tensor(out=t1, in0=ar_, in1=bi_, op=ALU.mult), r=R, w=R)
        P.op('dve', lambda: V.tensor_tensor(out=t2, in0=ai_, in1=br_, op=ALU.mult), r=R, w=R)
        P.op('dve', lambda: V.tensor_tensor(out=outi, in0=t1, in1=t2, op=ALU.add), r=R, w=R)

    with ExitStack() as st:
        arl = sb(st, "arl", [128, 1024], F32); ail = sb(st, "ail", [128, 1024], F32); dtl = sb(st, "dtl", [128, 1024], F32)
        brl = sb(st, "brl", [128, 1024], F32); bil = sb(st, "bil", [128, 1024], F32)
        for tl, src in ((arl, AR1), (ail, AI1), (dtl, DT1), (brl, BR1), (bil, BI1)):
            P.dma(tl[:], src[:, :], w=['L1'])
        tt = sincos_lb(st, 1024, arl[:], ail[:], dtl[:], 'L1')
        pr = [sb(st, "pr%d" % i, [128, 1024], F32) for i in range(2)]
        pi = [sb(st, "pi%d" % i, [128, 1024], F32) for i in range(2)]
        c1 = sb(st, "c1", [128, 1024], F32); c2 = sb(st, "c2", [128, 1024], F32)
        R = ['L1']
        cur_r, cur_i = tt['fr'], tt['fi']
        for k in range(8):
            s_ = 7 - k
            v3 = lambda ap: ap.rearrange("p (kc n) -> p kc n", kc=8)
            P.op('dve', lambda: V.tensor_tensor(out=c1[:], in0=cur_r[:], in1=brl[:], op=ALU.mult), r=R, w=R)
            P.op('dve', lambda: V.tensor_tensor(out=c2[:], in0=cur_i[:], in1=bil[:], op=ALU.mult), r=R, w=R)
            P.op('dve', lambda s_=s_: V.tensor_tensor(out=W1[:, :, 0, s_, :], in0=v3(c1[:]), in1=v3(c2[:]), op=ALU.subtract), r=R, w=R + ['W1'])
            P.op('dve', lambda: V.tensor_tensor(out=c1[:], in0=cur_r[:], in1=bil[:], op=ALU.mult), r=R, w=R)
            P.op('dve', lambda: V.tensor_tensor(out=c2[:], in0=cur_i[:], in1=brl[:], op=ALU.mult), r=R, w=R)
            P.op('dve', lambda s_=s_: V.tensor_tensor(out=W1[:, :, 1, s_, :], in0=v3(c1[:]), in1=v3(c2[:]), op=ALU.add), r=R, w=R + ['W1'])
            if k < 7:
                nr, ni = pr[k % 2], pi[k % 2]
                cmul(nr[:], ni[:], cur_r[:], cur_i[:], tt['lbr'][:], tt['lbi'][:], c1[:], c2[:], R)
                cur_r, cur_i = nr, ni
    P.barrier()
    if stop('c1'):
        return fin()
    with ExitStack() as st:
        are = sb(st, "are", [128, 32], F32); aie = sb(st, "aie", [128, 32], F32); dte = sb(st, "dte", [128, 32], F32)
        for tl, src in ((are, ARE), (aie, AIE), (dte, DTE)):
            P.dma(tl[:], src[:, :], w=['E'])
        te = sincos_lb(st, 32, are[:], aie[:], dte[:], 'E')
        e1 = sb(st, "e1", [128, 32], F32); e2 = sb(st, "e2", [128, 32], F32)
        R = ['E']
        P.op('dve', lambda: V.memset(QP[:, 0, 0, :], 1.0), r=R, w=R)
        P.op('dve', lambda: V.memset(QP[:, 0, 1, :], 0.0), r=R, w=R)
        for k in range(1, 9):
            cmul(QP[:, k, 0, :], QP[:, k, 1, :], QP[:, k - 1, 0, :], QP[:, k - 1, 1, :], te['lbr'][:], te['lbi'][:], e1[:], e2[:], R)
        P.op('dve', lambda: V.tensor_copy(out=FP[:, 0, 0, :], in_=te['fr'][:]), r=R, w=R)
        P.op('dve', lambda: V.tensor_copy(out=FP[:, 0, 1, :], in_=te['fi'][:]), r=R, w=R)
        for k in range(1, 8):
            cmul(FP[:, k, 0, :], FP[:, k, 1, :], FP[:, k - 1, 0, :], FP[:, k - 1, 1, :], te['lbr'][:], te['lbi'][:], e1[:], e2[:], R)
        P.op('dve', lambda: V.tensor_copy(out=A8r[:], in_=QP[:, 8, 0, :]), r=R, w=R)
        P.op('dve', lambda: V.tensor_copy(out=A8i[:], in_=QP[:, 8, 1, :]), r=R, w=R)
        m2 = sb(st, "m2", [128, 32], F32); m4 = sb(st, "m4", [128, 32], F32); m8 = sb(st, "m8", [128, 32], F32)
        P.op('dve', lambda: V.tensor_tensor(out=m2[:], in0=te['mag'][:], in1=te['mag'][:], op=ALU.mult), r=R, w=R)
        P.op('dve', lambda: V.tensor_tensor(out=m4[:], in0=m2[:], in1=m2[:], op=ALU.mult), r=R, w=R)
        P.op('dve', lambda: V.tensor_tensor(out=m8[:], in0=m4[:], in1=m4[:], op=ALU.mult), r=R, w=R)
        wr = [sb(st, "wr%d" % i, [128, 32], F32) for i in range(4)]
        wi_ = [sb(st, "wi%d" % i, [128, 32], F32) for i in range(4)]
        P.op('dve', lambda: V.tensor_copy(out=wr[0][:], in_=te['cs'][:]), r=R, w=R)
        P.op('dve', lambda: V.tensor_copy(out=wi_[0][:], in_=te['sn'][:]), r=R, w=R)
        for k in range(1, 4):
            cmul(wr[k][:], wi_[k][:], wr[k - 1][:], wi_[k - 1][:], wr[k - 1][:], wi_[k - 1][:], e1[:], e2[:], R)
        P.op('dve', lambda: V.memset(cosT[:, :, 0:1], 1.0), r=R, w=R)
        P.op('dve', lambda: V.memset(sinT[:, :, 0:1], 0.0), r=R, w=R)
        sr = [sb(st, "sr%d" % i, [128, 32], F32) for i in range(2)]
        si = [sb(st, "si%d" % i, [128, 32], F32) for i in range(2)]
        big1 = sb(st, "big1", [128, 32, 32], F32); big2 = sb(st, "big2", [128, 32, 32], F32)
        stepr, stepi = wr[3], wi_[3]
        for lv in range(6):
            n = 1 << lv
            bc = lambda ap, n=n: ap.unsqueeze(2).to_broadcast([128, 32, n])
            P.op('dve', lambda: V.tensor_tensor(out=big1[:, :, 0:n], in0=cosT[:, :, 0:n], in1=bc(stepr[:]), op=ALU.mult), r=R, w=R)
            P.op('dve', lambda: V.tensor_tensor(out=big2[:, :, 0:n], in0=sinT[:, :, 0:n], in1=bc(stepi[:]), op=ALU.mult), r=R, w=R)
            P.op('dve', lambda: V.tensor_tensor(out=cosT[:, :, n:2 * n], in0=big1[:, :, 0:n], in1=big2[:, :, 0:n], op=ALU.subtract), r=R, w=R)
            P.op('dve', lambda: V.tensor_tensor(out=big1[:, :, 0:n], in0=cosT[:, :, 0:n], in1=bc(stepi[:]), op=ALU.mult), r=R, w=R)
            P.op('dve', lambda: V.tensor_tensor(out=big2[:, :, 0:n], in0=sinT[:, :, 0:n], in1=bc(stepr[:]), op=ALU.mult), r=R, w=R)
            P.op('dve', lambda: V.tensor_tensor(out=sinT[:, :, n:2 * n], in0=big1[:, :, 0:n], in1=big2[:, :, 0:n], op=ALU.add), r=R, w=R)
            if lv < 5:
                nr, ni = sr[lv % 2], si[lv % 2]
                cmul(nr[:], ni[:], stepr[:], stepi[:], stepr[:], stepi[:], e1[:], e2[:], R)
                stepr, stepi = nr, ni
        P.op('dve', lambda: V.tensor_copy(out=R8z[:], in_=m8[:].unsqueeze(2).to_broadcast([128, 32, 64])), r=R, w=R)
        P.op('dve', lambda: V.memset(R8z[:, :, 0:1], 0.0), r=R, w=R)
    P.barrier()
    if stop('c2'):
        return fin()

    with ExitStack() as st:
        WuB = sb(st, "WuB", [128, 8, 1024], BF16)
        with ExitStack() as s2:
            wstg = sb(s2, "wstgA", [128, 8, 1024], F32)
            load_weight_bf16((wstg, WuB), wu, 0, 1024, g1s, 'WuB')
            P.barrier()
            if stop('c30'):
                return fin()
        xt = [sb(st, "xtA%d" % i, [128, 1024], F32) for i in range(2)]
        junk = [sb(st, "junkA%d" % i, [128, 1024], F32) for i in range(2)]
        ssA = [sb(st, "ssA%d" % i, [128, 4], F32) for i in range(2)]
        hb = [sb(st, "hbA%d" % i, [128, 1024], BF16) for i in range(2)]
        hT = [sb(st, "hTA%d" % i, [128, 8, 512], BF16) for i in range(2)]
        uT = [sb(st, "uTA%d" % i, [128, 8, 512], BF16) for i in range(2)]
        S0b = [sb(st, "S0_%d" % i, [128, 32, 2, 64], F32) for i in range(2)]
        Z = sb(st, "Z", [128, 32, 2, 64], F32)
        ta = sb(st, "ta", [128, 32, 64], F32)
        tb = sb(st, "tb", [128, 32, 64], F32)
        Hin = sb(st, "Hin", [128, 2, 32], F32)
        cr_ = sb(st, "cr_", [128, 2, 32], F32)
        cq1 = sb(st, "cq1", [128, 32], F32); cq2 = sb(st, "cq2", [128, 32], F32)
        Hb16 = [sb(st, "Hb16_%d" % i, [128, 32, 2, 16], BF16) for i in range(2)]
        P.op('dve', lambda: V.memset(Hin[:], 0.0), w=['Hin'])
        bank_rot = [0]

        def nb():
            b = bank_rot[0]
            bank_rot[0] = (b + 1) % 8
            return b

        def stageA(t):
            hTt = hT[t % 2]; uTt = uT[t % 2]
            hn = 'hT%d' % (t % 2); un = 'uT%d' % (t % 2)
            S0 = S0b[t % 2]; s0n = 'S0_%d' % (t % 2)
            for blk in range(4):
                i2 = blk % 2
                b = nb()
                norm_transpose((xt[i2], junk[i2], ssA[i2], hb[i2]), xp[(t * 4 + blk) * 128:(t * 4 + blk + 1) * 128, :], hTt, blk * 128, 'A%d' % i2, b, act_scale=True)
                P.op('act', lambda b=b, blk=blk: A.copy(out=hTt[:, :, blk * 128:(blk + 1) * 128], in_=pb16(b).rearrange("p (kc n) -> p kc n", kc=8)),
                     r=[pbn[b]], w=[hn])
            for oc in range(8):
                b = nb()
                for kc in range(8):
                    P.op('pe', lambda b=b, oc=oc, kc=kc: T.matmul(pb[b][:, :], lhsT=WuB[:, kc, oc * 128:(oc + 1) * 128], rhs=hTt[:, kc, :],
                                                                   start=(kc == 0), stop=(kc == 7), skip_group_check=True),
                         r=['WuB', hn], w=[pbn[b]])
                P.op('act', lambda b=b, oc=oc: A.copy(out=uTt[:, oc, :], in_=pb[b][:, :]), r=[pbn[b]], w=[un])
            P.dma(HT_d[t].rearrange("p (kc n) -> p kc n", kc=8), hTt[:, :, 384:512], r=[hn], w=['HT_d'])
            P.dma(HTALL_d[t], hTt[:].rearrange("p kc n -> p (kc n)"), r=[hn], w=['HTALL_d'])
            P.dma(U_d[t].rearrange("p (kc n) -> p kc n", kc=8), uTt[:, :, 384:512], r=[un], w=['U_d'])
            for kc in range(8):
                for comp in range(2):
                    c0 = comp * 64
                    for s_ in range(8):
                        for pp in range(4):
                            b = 4 * (kc % 2) + pp
                            P.op('pe', lambda b=b, kc=kc, pp=pp, comp=comp, s_=s_, c0=c0: T.matmul(
                                pb[b][:, c0:c0 + 64], lhsT=W1[32 * pp:32 * pp + 32, kc, comp, s_, :], rhs=uTt[32 * pp:32 * pp + 32, kc, s_::8],
                                start=(s_ == 0), stop=(s_ == 7), skip_group_check=True, tile_position=(32 * pp, 0)),
                                r=['W1', un], w=[pbn[b]])
                for pp in range(4):
                    b = 4 * (kc % 2) + pp
                    S0v = S0[:, 4 * kc + pp, :, :].rearrange("p c m -> p (c m)")
                    P.op('act', lambda b=b, S0v=S0v: A.copy(out=S0v, in_=pb[b][:, 0:128]), r=[pbn[b]], w=[s0n])

        def stageB(t):
            S0 = S0b[t % 2]; s0n = 'S0_%d' % (t % 2)
            Hh = S0
            Sr, Si = S0[:, :, 0, :], S0[:, :, 1, :]
            Zr, Zi = Z[:, :, 0, :], Z[:, :, 1, :]
            Rr = [s0n, 'Z', 'ta', 'tb']
            P.op('dve', lambda: V.tensor_tensor(out=ta[:], in0=cosT[:], in1=Sr, op=ALU.mult), r=Rr, w=['ta'])
            P.op('pool', lambda: G.tensor_tensor(out=tb[:], in0=sinT[:], in1=Si, op=ALU.mult), r=Rr, w=['tb'])
            P.op('dve', lambda: V.tensor_tensor(out=Zr, in0=ta[:], in1=tb[:], op=ALU.add), r=Rr, w=['Z'])
            P.op('dve', lambda: V.tensor_tensor(out=ta[:], in0=cosT[:], in1=Si, op=ALU.mult), r=Rr, w=['ta'])
            P.op('pool', lambda: G.tensor_tensor(out=tb[:], in0=sinT[:], in1=Sr, op=ALU.mult), r=Rr, w=['tb'])
            P.op('dve', lambda: V.tensor_tensor(out=Zi, in0=ta[:], in1=tb[:], op=ALU.subtract), r=Rr, w=['Z'])
            Rc = ['Hin', 'cr_', 'cq']
            P.op('dve', lambda: V.tensor_tensor(out=cq1[:], in0=A8r[:], in1=Hin[:, 0, :], op=ALU.mult), r=Rc, w=Rc)
            P.op('dve', lambda: V.tensor_tensor(out=cq2[:], in0=A8i[:], in1=Hin[:, 1, :], op=ALU.mult), r=Rc, w=Rc)
            P.op('dve', lambda: V.tensor_tensor(out=cr_[:, 0, :], in0=cq1[:], in1=cq2[:], op=ALU.subtract), r=Rc, w=Rc)
            P.op('dve', lambda: V.tensor_tensor(out=cq1[:], in0=A8r[:], in1=Hin[:, 1, :], op=ALU.mult), r=Rc, w=Rc)
            P.op('dve', lambda: V.tensor_tensor(out=cq2[:], in0=A8i[:], in1=Hin[:, 0, :], op=ALU.mult), r=Rc, w=Rc)
            P.op('dve', lambda: V.tensor_tensor(out=cr_[:, 1, :], in0=cq1[:], in1=cq2[:], op=ALU.add), r=Rc, w=Rc)
            P.op('dve', lambda: V.tensor_tensor(out=Z[:, :, 0, 0], in0=Z[:, :, 0, 0], in1=cr_[:, 0, :], op=ALU.add), r=Rc + ['Z'], w=['Z'])
            P.op('dve', lambda: V.tensor_tensor(out=Z[:, :, 1, 0], in0=Z[:, :, 1, 0], in1=cr_[:, 1, :], op=ALU.add), r=Rc + ['Z'], w=['Z'])
            fl = lambda ap: ap.rearrange("p a m -> p (a m)")
            P.op('dve', lambda: V.tensor_copy(out=ta[:], in_=Zr), r=['Z'], w=['ta'])
            P.op('pool', lambda: G.tensor_copy(out=tb[:], in_=Zi), r=['Z'], w=['tb'])
            P.op('dve', lambda: V.tensor_tensor_scan(out=fl(ta[:]), data0=fl(R8z[:]), data1=fl(ta[:]), initial=0.0,
                                                    op0=ALU.mult, op1=ALU.add), r=['ta'], w=['ta'])
            P.op('dve', lambda: V.tensor_tensor_scan(out=fl(tb[:]), data0=fl(R8z[:]), data1=fl(tb[:]), initial=0.0,
                                                    op0=ALU.mult, op1=ALU.add), r=['tb'], w=['tb'])
            Rh = ['ta', 'tb', 'Z', s0n]
            P.op('dve', lambda: V.tensor_tensor(out=Zr, in0=cosT[:], in1=ta[:], op=ALU.mult), r=Rh, w=['Z'])
            P.op('pool', lambda: G.tensor_tensor(out=Zi, in0=sinT[:], in1=tb[:], op=ALU.mult), r=Rh, w=['Z'])
            P.op('dve', lambda: V.tensor_tensor(out=Hh[:, :, 0, :], in0=Zr, in1=Zi, op=ALU.subtract), r=Rh, w=[s0n])
            P.op('dve', lambda: V.tensor_tensor(out=Zr, in0=cosT[:], in1=tb[:], op=ALU.mult), r=Rh, w=['Z'])
            P.op('pool', lambda: G.tensor_tensor(out=Zi, in0=sinT[:], in1=ta[:], op=ALU.mult), r=Rh, w=['Z'])
            P.op('dve', lambda: V.tensor_tensor(out=Hh[:, :, 1, :], in0=Zr, in1=Zi, op=ALU.add), r=Rh, w=[s0n])
            P.op('dve', lambda: V.tensor_copy(out=Hin[:, 0, :], in_=Hh[:, :, 0, 63]), r=[s0n], w=['Hin'])
            P.op('dve', lambda: V.tensor_copy(out=Hin[:, 1, :], in_=Hh[:, :, 1, 63]), r=[s0n], w=['Hin'])
            hbt = Hb16[t % 2]
            P.op('dve', lambda hbt=hbt: V.tensor_copy(out=hbt[:], in_=Hh[:, :, :, 47:63]), r=[s0n], w=['Hb16_%d' % (t % 2)])
            P.dma(HS_d[t].rearrange("p (a c m) -> p a c m", a=32, c=2), hbt[:], r=['Hb16_%d' % (t % 2)], w=['HS_d'])

        stageA(0)
        for t in range(NT):
            if t + 1 < NT:
                stageA(t + 1)
            stageB(t)
    P.barrier()
    if stop('c4'):
        return fin()
    s5.close()

    with ExitStack() as st:
        CC = sb(st, "CC", [128, 32, 2, 256], BF16)
        KN = sb(st, "KN", [128, 8, 2, 8, 128], BF16)
        dfm = sb(st, "dfm", [128, 8], F32)
        P.dma(dfm[:], DFM[:, :], w=['dfm'])
        with ExitStack() as s2:
            ctr = sb(s2, "ctr", [128, 32, 16], F32); cti = sb(s2, "cti", [128, 32, 16], F32)
            ber = sb(s2, "ber", [128, 32, 32], F32); bei = sb(s2, "bei", [128, 32, 32], F32)
            P.dma(ctr[:].rearrange("p a c -> p (a c)"), CTR[:, :], w=['ctr'])
            P.dma(cti[:].rearrange("p a c -> p (a c)"), CTI[:, :], w=['cti'])
            P.dma(ber[:].rearrange("p a c -> p (a c)"), BER[:, :], w=['ber'])
            P.dma(bei[:].rearrange("p a c -> p (a c)"), BEI[:, :], w=['bei'])
            ctrb = sb(s2, "ctrb", [128, 32, 16], BF16); nctib = sb(s2, "nctib", [128, 32, 16], BF16)
            P.op('dve', lambda: V.tensor_copy(out=ctrb[:], in_=ctr[:]), r=['ctr'], w=['ctrb'])
            P.op('dve', lambda: V.tensor_scalar(out=nctib[:], in0=cti[:], scalar1=-1.0, scalar2=None, op0=ALU.mult), r=['cti'], w=['nctib'])
            k1 = sb(s2, "k1", [128, 32, 32], F32); k2 = sb(s2, "k2", [128, 32, 32], F32)
            xre = sb(s2, "xre", [128, 32, 32], BF16); xim = sb(s2, "xim", [128, 32, 32], BF16)
            R = ['cc']
            b16 = lambda ap: ap.unsqueeze(2).to_broadcast([128, 32, 16])
            b32 = lambda ap: ap.unsqueeze(2).to_broadcast([128, 32, 32])
            P.op('pool', lambda: G.memset(CC[:].rearrange("p a b c -> p (a b c)"), 0.0), w=['CC'])
            for s_ in range(8):
                qr, qi = QP[:, s_ + 1, 0, :], QP[:, s_ + 1, 1, :]
                P.op('dve', lambda: V.tensor_tensor(out=k1[:, :, 0:16], in0=ctr[:], in1=b16(qr), op=ALU.mult), r=R + ['ctr'], w=R)
                P.op('dve', lambda: V.tensor_tensor(out=k2[:, :, 0:16], in0=cti[:], in1=b16(qi), op=ALU.mult), r=R + ['cti'], w=R)
                for gg in range(2):
                    rs = slice(64 * gg, 64 * gg + 64)
                    P.op('dve', lambda s_=s_, gg=gg, rs=rs: V.tensor_tensor(out=CC[rs, :, 0, gg * 128 + s_ * 16:gg * 128 + (s_ + 1) * 16],
                                                                         in0=k1[rs, :, 0:16], in1=k2[rs, :, 0:16], op=ALU.subtract), r=R, w=R + ['CC'])
                P.op('dve', lambda: V.tensor_tensor(out=k1[:, :, 0:16], in0=ctr[:], in1=b16(qi), op=ALU.mult), r=R, w=R)
                P.op('dve', lambda: V.tensor_tensor(out=k2[:, :, 0:16], in0=cti[:], in1=b16(qr), op=ALU.mult), r=R, w=R)
                P.op('dve', lambda: V.tensor_tensor(out=k1[:, :, 0:16], in0=k1[:, :, 0:16], in1=k2[:, :, 0:16], op=ALU.add), r=R, w=R)
                for gg in range(2):
                    rs = slice(64 * gg, 64 * gg + 64)
                    P.op('dve', lambda s_=s_, gg=gg, rs=rs: V.tensor_scalar(out=CC[rs, :, 1, gg * 128 + s_ * 16:gg * 128 + (s_ + 1) * 16],
                                                                         in0=k1[rs, :, 0:16], scalar1=-1.0, scalar2=None, op0=ALU.mult), r=R, w=R + ['CC'])
            for tau in range(8):
                fr_, fi_ = FP[:, tau, 0, :], FP[:, tau, 1, :]
                P.op('dve', lambda: V.tensor_tensor(out=k1[:], in0=ber[:], in1=b32(fr_), op=ALU.mult), r=R + ['ber', 'xre', 'xim'], w=R)
                P.op('dve', lambda: V.tensor_tensor(out=k2[:], in0=bei[:], in1=b32(fi_), op=ALU.mult), r=R + ['bei'], w=R)
                P.op('dve', lambda: V.tensor_tensor(out=xre[:], in0=k1[:], in1=k2[:], op=ALU.subtract), r=R, w=R + ['xre'])
                P.op('dve', lambda: V.tensor_tensor(out=k1[:], in0=bei[:], in1=b32(fr_), op=ALU.mult), r=R, w=R)
                P.op('dve', lambda: V.tensor_tensor(out=k2[:], in0=ber[:], in1=b32(fi_), op=ALU.mult), r=R, w=R)
                P.op('dve', lambda: V.tensor_tensor(out=xim[:], in0=k1[:], in1=k2[:], op=ALU.add), r=R, w=R + ['xim'])
                for gp in range(32):
                    kc, pp = gp // 4, gp % 4
                    for gg in range(2):
                        bnk = 2 * gg + tau // 4
                        c0 = (tau % 4) * 128 + kc * 16
                        rs = slice(64 * gg, 64 * gg + 64)
                        P.op('pe', lambda bnk=bnk, gp=gp, gg=gg, pp=pp, c0=c0, rs=rs: T.matmul(
                            pb[bnk][32 * pp:32 * pp + 32, c0:c0 + 16], lhsT=xre[rs, gp, :], rhs=ctrb[rs, gp, :], start=True, stop=False,
                            skip_group_check=True, tile_position=(64 * gg, 32 * pp)), r=['xre', 'ctrb'], w=[pbn[bnk]])
                        P.op('pe', lambda bnk=bnk, gp=gp, gg=gg, pp=pp, c0=c0, rs=rs: T.matmul(
                            pb[bnk][32 * pp:32 * pp + 32, c0:c0 + 16], lhsT=xim[rs, gp, :], rhs=nctib[rs, gp, :], start=False, stop=True,
                            skip_group_check=True, tile_position=(64 * gg, 32 * pp)), r=['xim', 'nctib'], w=[pbn[bnk]])
            P.op('pool', lambda: G.memset(KN[:].rearrange("p a b c d -> p (a b c d)"), 0.0), w=['KN'])
            for tau in range(8):
                for gg in range(2):
                    bnk = 2 * gg + tau // 4
                    src = pb[bnk][:, (tau % 4) * 128:(tau % 4) * 128 + 128].rearrange("p (a c) -> p a c", a=8)
                    for sp_ in range(8 - tau):
                        s_ = sp_ + tau
                        P.op('dve', lambda src=src, sp_=sp_, s_=s_, gg=gg: V.tensor_copy(out=KN[:, :, gg, sp_, s_ * 16:(s_ + 1) * 16], in_=src),
                             r=[pbn[bnk]], w=['KN'])
        P.barrier()
        if stop('c5'):
            return fin()
        Hb_all = sb(st, "Hb_all", [128, 32, 2, 256], BF16)
        uo_all = sb(st, "uo_all", [128, 8, 2048], BF16)
        hstg = [sb(st, "hstg%d" % i, [128, 32, 2, 16], BF16) for i in range(2)]
        for t in range(NT):
            hs = hstg[t % 2]; hsn = 'hstg%d' % (t % 2)
            P.dma(hs[:], HS_d[t].rearrange("p (a c m) -> p a c m", a=32, c=2), r=['HS_d'], w=[hsn])
            if t % 2 == 0:
                P.op('act', lambda hs=hs, t=t: A.copy(out=Hb_all[:, :, :, t * 16:(t + 1) * 16], in_=hs[:]), r=[hsn], w=['Hb_all'])
            else:
                P.op('dve', lambda hs=hs, t=t: V.tensor_copy(out=Hb_all[:, :, :, t * 16:(t + 1) * 16], in_=hs[:]), r=[hsn], w=['Hb_all'])
            P.dma(uo_all[:, :, t * 128:(t + 1) * 128], U_d[t].rearrange("p (kc n) -> p kc n", kc=8), r=['U_d'], w=['uo_all'])
        nsb = [sb(st, "nsb%d" % i, [128, 256], F32) for i in range(2)]
        Yg = [sb(st, "Yg%d" % i, [128, 256], BF16) for i in range(2)]
        ytm = [sb(st, "ytm%d" % i, [128, 2, 8, 8, 16], BF16) for i in range(2)]
        ypre = [sb(st, "ypre0", [128, 2048], F32)] * 2
        g1_ = sb(st, "g1_", [128, 2048], F32); g2_ = g1_
        yg = [sb(st, "yg%d" % i, [128, 2048], BF16) for i in range(2)]
        Yd_v = Y_d.rearrange("t p (kc n) -> p t kc n", kc=8)
        def stX(kc, gl):
            gp, gg, pp = 4 * kc + gl // 2, gl % 2, gl // 2
            bF = gl % 2
            for comp in range(2):
                P.op('pe', lambda comp=comp: T.matmul(
                    pb[bF][:, 0:256], lhsT=CC[:, gp, comp, gg * 128:(gg + 1) * 128], rhs=Hb_all[:, gp, comp, :], start=(comp == 0), stop=(comp == 1),
                    skip_group_check=True), r=['Hb_all', 'CC'], w=[pbn[bF]])
            bN = 2 + gg + 2 * (pp % 2)
            for sp_ in range(8):
                P.op('pe', lambda sp_=sp_: T.matmul(
                    pb[bN][:, 0:256], lhsT=KN[32 * pp:32 * pp + 32, kc, gg, sp_, :], rhs=uo_all[32 * pp:32 * pp + 32, kc, sp_::8],
                    start=(sp_ == 0), stop=(sp_ == 7), skip_group_check=True, tile_position=(32 * pp, 0)), r=['uo_all', 'KN'], w=[pbn[bN]])
            ns = nsb[gl % 2]; nsn = 'nsb%d' % (gl % 2)
            ygt = Yg[gl % 2]; ygn = 'Yg%d' % (gl % 2)
            P.op('act', lambda: A.copy(out=ns[:], in_=pb[bN][:, 0:256]), r=[pbn[bN]], w=[nsn])
            P.op('dve', lambda: V.tensor_tensor(out=ygt[:], in0=pb[bF][:, 0:256], in1=ns[:], op=ALU.add), r=[pbn[bF], nsn], w=[ygn])

        def stY(kc, gl):
            yt = ytm[kc % 2]; ytn = 'ytm%d' % (kc % 2)
            ygt = Yg[gl % 2]; ygn = 'Yg%d' % (gl % 2)
            for mh in range(2):
                P.op('pe', lambda mh=mh: T.transpose(out=pb16(6)[:, mh * 128:(mh + 1) * 128], in_=ygt[:, mh * 128:(mh + 1) * 128], identity=identb[:]),
                     r=[ygn, 'identb'], w=[pbn[6]])
            P.op('act', lambda: A.copy(out=yt[:, :, :, gl, :], in_=pb16(6)[:, 0:256].rearrange("p (h s c) -> p h s c", h=2, s=8)),
                 r=[pbn[6]], w=[ytn])

        groups = [(kc, gl) for kc in range(8) for gl in range(8)]
        stX(*groups[0])
        for gi_, (kc, gl) in enumerate(groups):
            if gi_ + 1 < len(groups):
                stX(*groups[gi_ + 1])
            stY(kc, gl)
            if gl != 7:
                continue
            yt = ytm[kc % 2]; ytn = 'ytm%d' % (kc % 2)
            yp = ypre[0]; ypn = 'ypre0'
            for mh in range(2):
                for s_ in range(8):
                    P.op('pe', lambda yt=yt, mh=mh, s_=s_: T.transpose(out=pb16(7)[:, s_ * 128:(s_ + 1) * 128], in_=yt[:, mh, s_, :, :].rearrange("p g c -> p (g c)"),
                                                                       identity=identb[:]), r=[ytn, 'identb'], w=[pbn[7]])
                ov = yp[:, mh * 1024:(mh + 1) * 1024].rearrange("p (m s) -> p s m", s=8)
                uv = uo_all[:, kc, mh * 1024:(mh + 1) * 1024].rearrange("p (m s) -> p s m", s=8)
                P.op('dve', lambda kc=kc, ov=ov, uv=uv: V.scalar_tensor_tensor(
                    out=ov, in0=uv, scalar=dfm[:, kc:kc + 1], in1=pb16(7)[:, 0:1024].rearrange("p (s m) -> p s m", s=8), op0=ALU.mult, op1=ALU.add),
                    r=['uo_all', 'dfm', pbn[7]], w=[ypn])
            yf = yp[:]
            ygo = yg[kc % 2]; ygon = 'yg%d' % (kc % 2)
            P.op('dve', lambda yf=yf: V.tensor_tensor(out=g1_[:], in0=yf, in1=yf, op=ALU.mult), r=[ypn], w=['g1_'])
            P.op('dve', lambda: V.tensor_scalar(out=g1_[:], in0=g1_[:], scalar1=0.044715, scalar2=1.0, op0=ALU.mult, op1=ALU.add), r=['g1_'], w=['g1_'])
            P.op('dve', lambda yf=yf: V.tensor_tensor(out=g1_[:], in0=g1_[:], in1=yf, op=ALU.mult), r=['g1_', ypn], w=['g1_'])
            P.op('act', lambda: A.activation(out=g1_[:], in_=g1_[:], func=AF.Sigmoid, scale=2.0 * 0.7978845608028654), r=['g1_'], w=['g1_'])
            P.op('dve', lambda ygo=ygo, yf=yf: V.tensor_tensor(out=ygo[:], in0=g1_[:], in1=yf, op=ALU.mult), r=['g1_', ypn], w=[ygon])
            P.dma(Yd_v[:, :, kc, :], ygo[:].rearrange("p (t n) -> p t n", t=NT), r=[ygon], w=['Y_d'])
    P.barrier()

    sq.close()
    if dbg and dbg[0] == 'Y':
        with ExitStack() as st:
            tmpb = sb(st, "tmpb", [128, 1024], BF16); tmpf = sb(st, "tmpf", [128, 1024], F32)
            for t in range(NT):
                P.dma(tmpb[:], Y_d[t], r=['Y_d'], w=['tmpb'])
                P.op('dve', lambda: V.tensor_copy(out=tmpf[:], in_=tmpb[:]), r=['tmpb'], w=['tmpf'])
                P.dma(dbg_d[t], tmpf[:], r=['tmpf'], w=['dbg'])
        P.barrier()
        for e in ['sp']:
            pass
        es.close()
        return nc

    if stop('Y2'):
        return fin()
    ISQ = 1.0 / math.sqrt(128.0)

    with ExitStack() as st:
        kT_all = sb(st, "kT_all", [128, 2, L], BF16)
        V_all = sb(st, "V_all", [128, 64, 256], BF16)
        kiT_all = sb(st, "kiT_all", [128, L], BF16)
        with ExitStack() as s2:
            WkvB = sb(s2, "WkvB", [128, 8, 640], BF16)
            with ExitStack() as s3:
                wstg = sb(s3, "wstgK", [128, 8, 640], F32)
                load_weight_bf16((wstg, WkvB), wkv, 0, 640, g1s, 'WkvB')
                P.barrier()
            xt = [sb(s2, "xtK%d" % i, [128, 1024], F32) for i in range(4)]
            junk = [sb(s2, "junkK0", [128, 1024], F32)] * 4
            ssA = [sb(s2, "ssK%d" % i, [128, 4], F32) for i in range(4)]
            hb = [sb(s2, "hbK%d" % i, [128, 1024], BF16) for i in range(4)]
            hT = [sb(s2, "hTK%d" % i, [128, 8, 512], BF16) for i in range(2)]
            ctab = [sb(s2, "ctabK%d" % i, [128, 512], F32) for i in range(2)]; stab = [sb(s2, "stabK%d" % i, [128, 512], F32) for i in range(2)]
            ctabI = [sb(s2, "ctabI%d" % i, [128, 512], F32) for i in range(2)]; stabI = [sb(s2, "stabI%d" % i, [128, 512], F32) for i in range(2)]
            rt1 = sb(s2, "rt1", [128, 512], F32); rt2 = sb(s2, "rt2", [128, 512], F32)
            brot = [0]

            def nb2():
                b = brot[0]
                brot[0] = (b + 1) % 8
                return b

            def a2_stage1(t):
                hTt = hT[t % 2]; hn = 'hTK%d' % (t % 2)
                P.dma(hTt[:].rearrange("p kc n -> p (kc n)"), HTALL_d[t], r=['HTALL_d'], w=[hn])

            def a2_stage2(t):
                hTt = hT[t % 2]; hn = 'hTK%d' % (t % 2)
                t2 = t % 2
                cs = slice(t * 512, (t + 1) * 512)
                P.dma(ctab[t2][:], cAk[:, cs], w=['ctabK%d' % t2]); P.dma(stab[t2][:], sAk[:, cs], w=['stabK%d' % t2])
                P.dma(ctabI[t2][:], cIk[:, cs], w=['ctabI%d' % t2]); P.dma(stabI[t2][:], sIk[:, cs], w=['stabI%d' % t2])
                dsts = []
                for oc in range(3):
                    b = nb2()
                    for kc in range(8):
                        P.op('pe', lambda b=b, oc=oc, kc=kc: T.matmul(pb[b][:, :], lhsT=WkvB[:, kc, oc * 128:(oc + 1) * 128], rhs=hTt[:, kc, :],
                                                                       start=(kc == 0), stop=(kc == 7), skip_group_check=True),
                             r=['WkvB', hn], w=[pbn[b]])
                    if oc < 2:
                        dst = kT_all[:, oc, cs]
                        P.op('act', lambda b=b, dst=dst: A.copy(out=dst, in_=pb[b][:, :]), r=[pbn[b]], w=['kTt%d' % t2])
                    else:
                        dst = kiT_all[:, cs]
                        P.op('act', lambda b=b, dst=dst: A.copy(out=dst, in_=pb[b][:, :]), r=[pbn[b]], w=['kiTt%d' % t2])
                    dsts.append(dst)
                for blk in range(4):
                    b = nb2()
                    for kc in range(8):
                        P.op('pe', lambda b=b, blk=blk, kc=kc: T.matmul(pb[b][:, 0:256], lhsT=hTt[:, kc, blk * 128:(blk + 1) * 128], rhs=WkvB[:, kc, 384:640],
                                                                         start=(kc == 0), stop=(kc == 7), skip_group_check=True),
                             r=['WkvB', hn], w=[pbn[b]])
                    P.op('act' if blk % 2 == 0 else 'dve',
                         (lambda b=b, blk=blk, t=t: A.copy(out=V_all[:, 4 * t + blk, :], in_=pb[b][:, 0:256])) if blk % 2 == 0 else
                         (lambda b=b, blk=blk, t=t: V.tensor_copy(out=V_all[:, 4 * t + blk, :], in_=pb[b][:, 0:256])), r=[pbn[b]], w=['V_all'])
                for oc in range(3):
                    if oc < 2:
                        rope(dsts[oc], 128, permA, ctab[t2][:], stab[t2][:], rt1, rt2, nb2(), ['ctabK%d' % t2, 'stabK%d' % t2], 'kTt%d' % t2, 512)
                    else:
                        rope(dsts[oc], 128, permI, ctabI[t2][:], stabI[t2][:], rt1, rt2, nb2(), ['ctabI%d' % t2, 'stabI%d' % t2], 'kiTt%d' % t2, 512)

            a2_stage1(0)
            for t in range(NT):
                if t + 1 < NT:
                    a2_stage1(t + 1)
                a2_stage2(t)
        P.barrier()
        if stop('KV'):
            return fin()
        hTo = sb(st, "hTo", [128, 8, 512], BF16)
        qT = sb(st, "qT", [128, 4, 8, 128], BF16)
        qiT = sb(st, "qiT", [128, 8, 512], BF16)
        wis = sb(st, "wis", [128, 4, 16], F32)
        wst = [sb(st, "wstQ0", [128, 8, 128], F32)] * 2
        wbf = [sb(st, "wbfQ%d" % i, [128, 8, 128], BF16) for i in range(2)]
        qtmp = sb(st, "qtmp", [128, 512], BF16)
        ctq = sb(st, "ctq", [128, 512], F32); stq = sb(st, "stq", [128, 512], F32)
        rt1 = sb(st, "rt1q", [128, 512], F32); rt2 = sb(st, "rt2q", [128, 512], F32)
        Dg = sb(st, "Dg", [128, 16, 128], BF16)
        Rb = [sb(st, "Rb%d" % i, [128, 1024], BF16) for i in range(3)]
        score = sb(st, "score", [128, L], F32)
        cjunk = sb(st, "cjunk", [128, L], mybir.dt.uint8)
        mtile = [sb(st, "mtile%d" % i, [128, 512], F32) for i in range(3)]
        P.dma(mtile[0][:], mfirst[:, :], w=['mt0']); P.dma(mtile[1][:], mlast[:, :], w=['mt1']); P.dma(mtile[2][:], mboth[:, :], w=['mt2'])
        bs = sb(st, "bs", [128, 8], F32)
        mq = [sb(st, "mq0", [128, 512], BF16)] * 2
        maskT = sb(st, "maskT", [128, 64, 128], BF16)
        Eb = [sb(st, "Eb%d" % i, [128, 1024], BF16) for i in range(2)]
        PT = [sb(st, "PT%d" % i, [128, 1024], BF16) for i in range(2)]
        rden = rt1
        ybt = [sb(st, "ybt0", [128, 1024], BF16)] * 2
        for half in range(4):
            for i in range(4):
                P.dma(hTo[:, :, i * 128:(i + 1) * 128], HT_d[4 * half + i].rearrange("p (kc n) -> p kc n", kc=8), r=['HT_d'], w=['hTo'])
            wcnt = 0
            for hh in range(16):
                wi2 = wcnt % 2; wcnt += 1
                load_weight_bf16((wst[wi2], wbf[wi2]), wq, hh * 128, 128, g1s, 'wbfQ%d' % wi2, stag='wstQ')
                for j in range(1):
                    b = j
                    for kc in range(8):
                        P.op('pe', lambda b=b, kc=kc, wi2=wi2, j=j: T.matmul(pb[b][:, :], lhsT=wbf[wi2][:, kc, :], rhs=hTo[:, kc, j * 512:(j + 1) * 512],
                                                                             start=(kc == 0), stop=(kc == 7), skip_group_check=True),
                             r=['wbfQ%d' % wi2, 'hTo'], w=[pbn[b]])
                    tcs = slice((half * 4) * 128, (half * 4) * 128 + 512)
                    if hh < 8:
                        P.dma(ctq[:], cAq[:, tcs], w=['ctq']); P.dma(stq[:], sAq[:, tcs], w=['stq'])
                        P.op('act', lambda b=b: A.copy(out=qtmp[:], in_=pb[b][:, :]), r=[pbn[b]], w=['qtmp'])
                        rope(qtmp[:], 128, permA, ctq[:], stq[:], rt1, rt2, 2 + j, ['ctq', 'stq'], 'qtmp', 512)
                        P.op('act', lambda hh=hh, j=j: A.copy(out=qT[:, 4 * j:4 * j + 4, hh, :], in_=qtmp[:].rearrange("p (a n) -> p a n", a=4)),
                             r=['qtmp'], w=['qT'])
                    else:
                        P.dma(ctq[:], cIq[:, tcs], w=['ctq']); P.dma(stq[:], sIq[:, tcs], w=['stq'])
                        dst = qiT[:, hh - 8, j * 512:(j + 1) * 512]
                        P.op('act', lambda b=b, dst=dst: A.copy(out=dst, in_=pb[b][:, :]), r=[pbn[b]], w=['qiT'])
                        rope(dst, 128, permI, ctq[:], stq[:], rt1, rt2, 2 + j, ['ctq', 'stq'], 'qiT', 512)
            load_weight_bf16((wst[0], wbf[0]), wwi, 0, 16, g1s, 'wbfQ0', stag='wstQ')
            for blk in range(4):
                b = 4 + blk % 2
                for kc in range(8):
                    P.op('pe', lambda b=b, kc=kc, blk=blk: T.matmul(pb[b][:, 0:16], lhsT=hTo[:, kc, blk * 128:(blk + 1) * 128], rhs=wbf[0][:, kc, 0:16],
                                                                     start=(kc == 0), stop=(kc == 7), skip_group_check=True),
                         r=['wbfQ0', 'hTo'], w=[pbn[b]])
                P.op('dve', lambda b=b, blk=blk: V.tensor_copy(out=wis[:, blk, :], in_=pb[b][:, 0:16]), r=[pbn[b]], w=['wis'])
            pending = None
            for blk in range(4):
                gi = 4 * half + blk
                nkt = gi + 1
                n = nkt * 512
                for h in range(16):
                    if h % 2 == 0:
                        P.op('act', lambda h=h, blk=blk: A.activation(out=Dg[:, h, :], in_=ident32[:], func=AF.Copy, scale=wis[:, blk, h:h + 1]),
                             r=['wis', 'ident32'], w=['Dg'])
                    else:
                        P.op('dve', lambda h=h, blk=blk: V.tensor_scalar(out=Dg[:, h, :], in0=ident32[:], scalar1=wis[:, blk, h:h + 1], scalar2=None, op0=ALU.mult),
                             r=['wis', 'ident32'], w=['Dg'])
                for kt in range(nkt):
                    bsc = 6 + kt % 2

                    def rel_pair(p, kt=kt, blk=blk):
                        X = 2 * (p % 3)
                        for hh in range(2):
                            rs = slice(64 * hh, 64 * hh + 64)
                            P.op('pe', lambda hh=hh, rs=rs: T.matmul(
                                pb[X + hh][:, :], lhsT=qiT[rs, p, blk * 128:(blk + 1) * 128], rhs=kiT_all[rs, kt * 512:(kt + 1) * 512],
                                start=True, stop=True, skip_group_check=True, tile_position=(64 * hh, 0)), r=['qiT', 'kiT_all'], w=[pbn[X + hh]])
                        rbuf = Rb[p % 3]; rn = 'Rb%d' % (p % 3)
                        src2 = pball[:, X * 512:(X + 2) * 512]
                        if p % 2 == 0:
                            P.op('act', lambda: A.activation(out=rbuf[:], in_=src2, func=AF.Relu), r=[pbn[X], pbn[X + 1]], w=[rn])
                        else:
                            P.op('dve', lambda: V.tensor_scalar(out=rbuf[:], in0=src2, scalar1=0.0, scalar2=None, op0=ALU.max), r=[pbn[X], pbn[X + 1]], w=[rn])

                    def score_pair(p, bsc=bsc):
                        rbuf = Rb[p % 3]; rn = 'Rb%d' % (p % 3)
                        for hh in range(2):
                            h = 2 * p + hh
                            P.op('pe', lambda h=h, hh=hh: T.matmul(pb[bsc][:, :], lhsT=Dg[:, h, :], rhs=rbuf[:, hh * 512:(hh + 1) * 512], start=(h == 0), stop=(h == 15),
                                                                   skip_group_check=True), r=['Dg', rn], w=[pbn[bsc]])
                    for step in range(8 + 2):
                        if step < 8:
                            rel_pair(step)
                        if step >= 2:
                            score_pair(step - 2)
                    mt = None
                    if kt == 0 and kt == nkt - 1:
                        mt = 2
                    elif kt == 0:
                        mt = 0
                    elif kt == nkt - 1:
                        mt = 1
                    dsts = score[:, kt * 512:(kt + 1) * 512]
                    if mt is None:
                        P.op('act', lambda bsc=bsc, dsts=dsts: A.copy(out=dsts, in_=pb[bsc][:, :]), r=[pbn[bsc]], w=['score'])
                    else:
                        P.op('dve', lambda bsc=bsc, dsts=dsts, mt=mt: V.tensor_tensor(out=dsts, in0=pb[bsc][:, :], in1=mtile[mt][:], op=ALU.add),
                             r=[pbn[bsc], 'mt%d' % mt], w=['score'])
                n_act = 512 * ((gi + 1) // 2)
                thr_c = 255.5 - 0.5 * n_act
                stps = [BIS_B * 2.0 / (2.0 ** (it + 1)) for it in range(NITER)]
                P.op('dve', lambda: V.memset(bs[:, 1:2], -BIS_B + stps[0]), r=['bs1'], w=['bs1'])
                def bis_iter(it, n=n, n_act=n_act, thr_c=thr_c, stps=stps):
                    if n_act > 0:
                        P.op('act', lambda n_act=n_act: A.activation(out=cjunk[:, 0:n_act], in_=score[:, 0:n_act], func=AF.Sign, bias=bs[:, 1:2], scale=-1.0,
                                                                    accum_out=bs[:, 5:6]), r=['bs1', 'score'], w=['bs5', 'cjunkA'])
                    P.op('dve', lambda n=n, n_act=n_act: V.tensor_scalar(out=cjunk[:, n_act:n], in0=score[:, n_act:n], scalar1=bs[:, 1:2], scalar2=0.0, op0=ALU.is_ge, op1=ALU.add,
                                                                        accum_out=bs[:, 2:3]), r=['bs1', 'score'], w=['bs2', 'cjunk'])
                    if n_act > 0:
                        P.op('dve', lambda: V.scalar_tensor_tensor(out=bs[:, 6:7], in0=bs[:, 5:6], scalar=-0.5, in1=bs[:, 2:3], op0=ALU.mult, op1=ALU.add), r=['bs2', 'bs5'], w=['bs6'])
                        tcol = 6
                    else:
                        tcol = 2
                    P.op('dve', lambda it=it, tcol=tcol, thr_c=thr_c: V.tensor_scalar(out=bs[:, 3:4], in0=bs[:, tcol:tcol + 1], scalar1=thr_c, scalar2=stps[it], op0=ALU.is_ge, op1=ALU.mult),
                         r=['bs%d' % tcol], w=['bs3'])
                    if it < NITER - 1:
                        P.op('dve', lambda it=it: V.scalar_tensor_tensor(out=bs[:, 1:2], in0=bs[:, 3:4], scalar=-stps[it + 1], in1=bs[:, 1:2], op0=ALU.add, op1=ALU.add), r=['bs3', 'bs1'], w=['bs1'])
                    else:
                        P.op('dve', lambda it=it: V.scalar_tensor_tensor(out=bs[:, 0:1], in0=bs[:, 3:4], scalar=-stps[it], in1=bs[:, 1:2], op0=ALU.add, op1=ALU.add), r=['bs3', 'bs1'], w=['bs'])
                if pending is not None:
                    na = len(pending)
                    for it in range(NITER):
                        bis_iter(it)
                        for f in pending[it * na // NITER:(it + 1) * na // NITER]:
                            f()
                    pending = None
                else:
                    for it in range(NITER):
                        bis_iter(it)
                for kt in range(nkt):
                    mqt = mq[0]; mn = 'mq0'
                    P.op('dve', lambda mqt=mqt, kt=kt: V.tensor_scalar(out=mqt[:], in0=score[:, kt * 512:(kt + 1) * 512], scalar1=bs[:, 0:1], scalar2=None, op0=ALU.is_ge),
                         r=['bs', 'score'], w=[mn])
                    bt_ = 4 + kt % 2
                    for a in range(4):
                        P.op('pe', lambda mqt=mqt, a=a, bt_=bt_: T.transpose(out=pb16(bt_)[:, a * 128:(a + 1) * 128], in_=mqt[:, a * 128:(a + 1) * 128], identity=identb[:]),
                             r=[mn, 'identb'], w=[pbn[bt_]])
                    P.op('act', lambda kt=kt, bt_=bt_: A.copy(out=maskT[:, 4 * kt:4 * kt + 4, :], in_=pb16(bt_)[:, 0:512].rearrange("p (a n) -> p a n", a=4)),
                         r=[pbn[bt_]], w=['maskT'])
                def make_att(blk=blk, gi=gi, nkt=nkt):
                    steps = []
                    ybtt = ybt[0]; ybn = 'ybt0'
                    nkb = 4 * nkt
                    npair = nkb // 2
                    for g in range(2):
                        rhsq = qT[:, blk, 4 * g:4 * g + 4, :].rearrange("p a n -> p (a n)")

                        def qk_pair(pq, g=g, rhsq=rhsq):
                            X = 2 * (pq % 2)
                            for j in range(2):
                                kb = 2 * pq + j
                                P.op('pe', lambda kb=kb, j=j: T.matmul(pb[X + j][:, :], lhsT=kT_all[:, g, kb * 128:(kb + 1) * 128], rhs=rhsq,
                                                                       start=True, stop=True, skip_group_check=True), r=['kT_all', 'qT'], w=[pbn[X + j]])
                            e = Eb[pq % 2]; en = 'Eb%d' % (pq % 2)
                            P.op('act', lambda: A.activation(out=e[:], in_=pball[:, X * 512:(X + 2) * 512], func=AF.Exp, scale=ISQ), r=[pbn[X], pbn[X + 1]], w=[en])
                            pt = PT[pq % 2]; pn = 'PT%d' % (pq % 2)
                            P.op('dve', lambda: V.tensor_tensor(out=pt[:].rearrange("p (k a n) -> p k a n", k=2, a=4), in0=e[:].rearrange("p (k a n) -> p k a n", k=2, a=4),
                                                                in1=maskT[:, 2 * pq:2 * pq + 2, :].unsqueeze(2).to_broadcast([128, 2, 4, 128]), op=ALU.mult),
                                 r=[en, 'maskT'], w=[pn])

                        def pv_pair(pq, g=g, nkb=nkb):
                            pt = PT[pq % 2]; pn = 'PT%d' % (pq % 2)
                            for j in range(2):
                                kb = 2 * pq + j
                                P.op('pe', lambda kb=kb, j=j: T.matmul(pb[6][:, :], lhsT=V_all[:, kb, g * 128:(g + 1) * 128], rhs=pt[:, j * 512:(j + 1) * 512],
                                                                       start=(kb == 0), stop=(kb == nkb - 1), skip_group_check=True), r=['V_all', pn], w=[pbn[6]])
                                P.op('pe', lambda kb=kb, j=j: T.matmul(pb[7][:, :], lhsT=onesb[:], rhs=pt[:, j * 512:(j + 1) * 512],
                                                                       start=(kb == 0), stop=(kb == nkb - 1), skip_group_check=True), r=['onesb', pn], w=[pbn[7]])

                        def one_step(step, qk_pair=qk_pair, pv_pair=pv_pair, npair=npair):
                            if step < npair:
                                qk_pair(step)
                            if step >= 1:
                                pv_pair(step - 1)
                        for step in range(npair + 1):
                            steps.append(lambda step=step, one_step=one_step: one_step(step))

                        def fin_g(g=g):
                            P.op('dve', lambda: V.reciprocal(out=rden[:], in_=pb[7][:, :]), r=[pbn[7]], w=['ropet1'])
                            P.op('dve', lambda: V.tensor_tensor(out=ybtt[:, g * 512:(g + 1) * 512], in0=pb[6][:, :], in1=rden[:], op=ALU.mult),
                                 r=[pbn[6], 'ropet1'], w=[ybn])
                        steps.append(fin_g)
                    steps.append(lambda: P.dma(YB_d[gi], ybtt[:], r=[ybn], w=['YB_d']))
                    return steps
                att = make_att()
                if blk == 3:
                    for f in att:
                        f()
                    pending = None
                else:
                    pending = att
    P.barrier()
    if stop('ATT'):
        return fin()

    with ExitStack() as st:
        h2T = sb(st, "h2T", [128, 8, 2048], BF16)
        sA = ExitStack()
        mT = sb(sA, "mT", [128, 8, 2048], BF16)
        with ExitStack() as s2:
            yaT = sb(s2, "yaT", [128, 8, 2048], BF16)
            wst = [sb(s2, "wstD%d" % i, [128, 8, 128], F32) for i in range(4)]
            wbf = [sb(s2, "wbfD%d" % i, [128, 8, 128], BF16) for i in range(4)]
            sg = [sb(s2, "sgD%d" % i, [128, 512], F32) for i in range(4)]
            with ExitStack() as s3:
                yT = sb(s3, "yT", [128, 8, 2048], BF16)
                for t in range(NT):
                    P.dma(yT[:, :, t * 128:(t + 1) * 128], Y_d[t].rearrange("p (kc n) -> p kc n", kc=8), r=['Y_d'], w=['yT'])
                for oc in range(8):
                    w2 = oc % 2
                    load_weight_bf16((wst[w2], wbf[w2]), wglu, oc * 128, 128, None, 'wbfD%d' % w2)
                    for j in range(4):
                        b = (oc * 4 + j) % 8
                        for kc in range(8):
                            P.op('pe', lambda b=b, kc=kc, w2=w2, j=j: T.matmul(pb[b][:, :], lhsT=wbf[w2][:, kc, :], rhs=yT[:, kc, j * 512:(j + 1) * 512],
                                                                               start=(kc == 0), stop=(kc == 7), skip_group_check=True), r=['wbfD%d' % w2, 'yT'], w=[pbn[b]])
                        sgt = sg[j % 2]; sn = 'sgD%d' % (j % 2)
                        P.op('act', lambda b=b, sgt=sgt: A.activation(out=sgt[:], in_=pb[b][:, :], func=AF.Sigmoid), r=[pbn[b]], w=[sn])
                        P.op('dve', lambda oc=oc, j=j, sgt=sgt: V.tensor_tensor(out=yaT[:, oc, j * 512:(j + 1) * 512], in0=sgt[:], in1=yT[:, oc, j * 512:(j + 1) * 512], op=ALU.mult),
                             r=[sn, 'yT'], w=['yaT'])
                P.barrier()
            ybT = sb(s2, "ybT", [128, 8, 2048], BF16)
            hTa = sb(s2, "hTa", [128, 8, 2048], BF16)
            for t in range(NT):
                P.dma(ybT[:, :, t * 128:(t + 1) * 128], YB_d[t].rearrange("p (h n) -> p h n", h=8), r=['YB_d'], w=['ybT'])
                P.dma(hTa[:, :, t * 128:(t + 1) * 128], HT_d[t].rearrange("p (kc n) -> p kc n", kc=8), r=['HT_d'], w=['hTa'])
            m1 = sb(s2, "mm1", [128, 512], F32); m2 = sb(s2, "mm2", [128, 512], F32)
            for oc in range(8):
                load_weight_bf16((wst[0], wbf[0]), wba, oc * 128, 128, None, 'wbfD0')
                load_weight_bf16((wst[1], wbf[1]), wbb, oc * 128, 128, None, 'wbfD1')
                load_weight_bf16((wst[2], wbf[2]), wg, oc * 128, 128, g1s, 'wbfD2')
                load_weight_bf16((wst[3], wbf[3]), wg, 1024 + oc * 128, 128, g1s, 'wbfD3')
                for j in range(4):
                    js = slice(j * 512, (j + 1) * 512)
                    acts = [yaT, ybT, hTa, hTa]; anm = ['yaT', 'ybT', 'hTa', 'hTa']
                    for q4 in range(4):
                        b = 4 * (j % 2) + q4
                        for kc in range(8):
                            P.op('pe', lambda b=b, kc=kc, q4=q4, js=js, acts=acts: T.matmul(pb[b][:, :], lhsT=wbf[q4][:, kc, :], rhs=acts[q4][:, kc, js],
                                                                                          start=(kc == 0), stop=(kc == 7), skip_group_check=True),
                                 r=['wbfD%d' % q4, anm[q4]], w=[pbn[b]])
                    b0 = 4 * (j % 2)
                    P.op('act', lambda b0=b0: A.activation(out=sg[0][:], in_=pb[b0 + 2][:, :], func=AF.Sigmoid), r=[pbn[b0 + 2]], w=['sgD0'])
                    P.op('act', lambda b0=b0: A.activation(out=sg[1][:], in_=pb[b0 + 3][:, :], func=AF.Sigmoid), r=[pbn[b0 + 3]], w=['sgD1'])
                    P.op('dve', lambda b0=b0: V.tensor_tensor(out=m1[:], in0=pb[b0][:, :], in1=sg[0][:], op=ALU.mult), r=[pbn[b0], 'sgD0'], w=['m1'])
                    P.op('dve', lambda b0=b0: V.tensor_tensor(out=m2[:], in0=pb[b0 + 1][:, :], in1=sg[1][:], op=ALU.mult), r=[pbn[b0 + 1], 'sgD1'], w=['m2'])
                    P.op('dve', lambda oc=oc, js=js: V.tensor_tensor(out=mT[:, oc, js], in0=m1[:], in1=m2[:], op=ALU.add), r=['m1', 'm2'], w=['mT'])
            P.barrier()
        if stop('MRG'):
            return fin()
        with ExitStack() as s2:
            WoB = sb(s2, "WoB", [128, 8, 1024], BF16)
            with ExitStack() as s3:
                wstg = sb(s3, "wstgO", [128, 8, 1024], F32)
                load_weight_bf16((wstg, WoB), wout, 0, 1024, None, 'WoB')
                P.barrier()
            xt = [sb(s2, "xtF%d" % i, [128, 1024], F32) for i in range(2)]
            x1t = [sb(s2, "x1F%d" % i, [128, 1024], F32) for i in range(2)]
            junk = sb(s2, "junkF", [128, 1024], F32)
            ssF = [sb(s2, "ssF%d" % i, [128, 4], F32) for i in range(2)]
            hbF = [sb(s2, "hbF%d" % i, [128, 1024], BF16) for i in range(2)]
            for gi in range(NT):
                i2 = gi % 2
                rows = slice((4 * gi + 3) * 128, (4 * gi + 4) * 128)
                P.dma(xt[i2][:], xp[rows, :], w=['xtF%d' % i2])
                for hf in range(2):
                    b = 2 * i2 + hf
                    for kc in range(8):
                        P.op('pe', lambda b=b, kc=kc, gi=gi, hf=hf: T.matmul(pb[b][:, :], lhsT=mT[:, kc, gi * 128:(gi + 1) * 128], rhs=WoB[:, kc, hf * 512:(hf + 1) * 512],
                                                                             start=(kc == 0), stop=(kc == 7), skip_group_check=True), r=['mT', 'WoB'], w=[pbn[b]])
                    P.op('dve', lambda b=b, hf=hf, i2=i2: V.tensor_tensor(out=x1t[i2][:, hf * 512:(hf + 1) * 512], in0=pb[b][:, :], in1=xt[i2][:, hf * 512:(hf + 1) * 512], op=ALU.add),
                         r=[pbn[b], 'xtF%d' % i2], w=['x1F%d' % i2])
                P.dma(X1_d[gi], x1t[i2][:], r=['x1F%d' % i2], w=['X1_d'])
                tg = 'F%d' % i2
                P.op('act', lambda i2=i2: A.activation(out=junk[:], in_=x1t[i2][:], func=AF.Square, accum_out=ssF[i2][:, 0:1]), r=['x1F%d' % i2], w=['junkF', tg + 'ss'])
                P.op('dve', lambda i2=i2: V.tensor_scalar(out=ssF[i2][:, 1:2], in0=ssF[i2][:, 0:1], scalar1=1.0 / D, scalar2=EPS, op0=ALU.mult, op1=ALU.add), r=[tg + 'ss'], w=[tg + 'ss'])
                P.op('act', lambda i2=i2: A.activation(out=ssF[i2][:, 2:3], in_=ssF[i2][:, 1:2], func=AF.Sqrt), r=[tg + 'ss'], w=[tg + 'ss'])
                P.op('dve', lambda i2=i2: V.reciprocal(out=ssF[i2][:, 3:4], in_=ssF[i2][:, 2:3]), r=[tg + 'ss'], w=[tg + 'ss'])
                P.op('dve', lambda i2=i2: V.tensor_scalar(out=hbF[i2][:], in0=x1t[i2][:], scalar1=ssF[i2][:, 3:4], scalar2=None, op0=ALU.mult), r=['x1F%d' % i2, tg + 'ss'], w=[tg + 'hb'])
                bt_ = 4 + i2
                for kc in range(8):
                    P.op('pe', lambda kc=kc, i2=i2, bt_=bt_: T.transpose(out=pb16(bt_)[:, kc * 128:(kc + 1) * 128], in_=hbF[i2][:, kc * 128:(kc + 1) * 128], identity=identb[:]),
                         r=[tg + 'hb', 'identb'], w=[pbn[bt_]])
                P.op('act', lambda gi=gi, bt_=bt_: A.copy(out=h2T[:, :, gi * 128:(gi + 1) * 128], in_=pb16(bt_).rearrange("p (kc n) -> p kc n", kc=8)), r=[pbn[bt_]], w=['h2T'])
            P.barrier()
        sA.close()
        if stop('F'):
            return fin()
        actT = sb(st, "actT", [128, 22, 2048], BF16)
        with ExitStack() as s2:
            wst = [sb(s2, "wstG%d" % i, [128, 8, 128], F32) for i in range(4)]
            wbf = [sb(s2, "wbfG%d" % i, [128, 8, 128], BF16) for i in range(4)]
            sgl = [sb(s2, "sgG%d" % i, [128, 512], F32) for i in range(2)]
            for jh in range(22):
                wo = 2 * (jh % 2)
                load_weight_bf16((wst[wo], wbf[wo]), wfi, jh * 128, 128, g2s, 'wbfG%d' % wo)
                load_weight_bf16((wst[wo + 1], wbf[wo + 1]), wfi, FH + jh * 128, 128, g2s, 'wbfG%d' % (wo + 1))
                for tg_ in range(4):
                    ts_ = slice(tg_ * 512, (tg_ + 1) * 512)
                    b0 = 2 * (tg_ % 4)
                    for q2 in range(2):
                        for kc in range(8):
                            P.op('pe', lambda b0=b0, q2=q2, kc=kc, ts_=ts_, wo=wo: T.matmul(pb[b0 + q2][:, :], lhsT=wbf[wo + q2][:, kc, :], rhs=h2T[:, kc, ts_],
                                                                                   start=(kc == 0), stop=(kc == 7), skip_group_check=True), r=['wbfG%d' % (wo + q2), 'h2T'], w=[pbn[b0 + q2]])
                    sgt = sgl[tg_ % 2]; sn = 'sgG%d' % (tg_ % 2)
                    P.op('act', lambda b0=b0, sgt=sgt: A.activation(out=sgt[:], in_=pb[b0][:, :], func=AF.Silu), r=[pbn[b0]], w=[sn])
                    P.op('dve', lambda b0=b0, sgt=sgt, jh=jh, ts_=ts_: V.tensor_tensor(out=actT[:, jh, ts_], in0=pb[b0 + 1][:, :], in1=sgt[:], op=ALU.mult),
                         r=[pbn[b0 + 1], sn], w=['actT'])
            P.barrier()
        with ExitStack() as s2:
            WfoB = sb(s2, "WfoB", [128, 22, 1024], BF16)
            wstg2 = [sb(s2, "wstgFo%d" % i, [128, 1024], F32) for i in range(2)]
            for jh in range(22):
                i2 = jh % 2
                P.dma(wstg2[i2][:], wfo[jh * 128:(jh + 1) * 128, :], w=['wstgFo%d' % i2])
                if i2 == 0:
                    P.op('act', lambda jh=jh, i2=i2: A.copy(out=WfoB[:, jh, :], in_=wstg2[i2][:]), r=['wstgFo%d' % i2], w=['WfoB'])
                else:
                    P.op('dve', lambda jh=jh, i2=i2: V.tensor_copy(out=WfoB[:, jh, :], in_=wstg2[i2][:]), r=['wstgFo%d' % i2], w=['WfoB'])
            gft = sb(s2, "gft", [128, 1024], F32)
            P.dma(gft[:], gfb[:, :], w=['gft'])
            x1r = [sb(s2, "x1r%d" % i, [128, 1024], F32) for i in range(2)]
            x2 = [sb(s2, "x2_%d" % i, [128, 1024], F32) for i in range(2)]
            junkG = sb(s2, "junkG", [128, 1024], F32)
            ssG = [sb(s2, "ssG%d" % i, [128, 4], F32) for i in range(2)]
            ot = [sb(s2, "ot%d" % i, [128, 1024], F32) for i in range(2)]
            for gi in range(NT):
                i2 = gi % 2
                P.dma(x1r[i2][:], X1_d[gi], r=['X1_d'], w=['x1r%d' % i2])
                for hf in range(2):
                    b = 2 * i2 + hf
                    for jh in range(22):
                        P.op('pe', lambda b=b, jh=jh, gi=gi, hf=hf: T.matmul(pb[b][:, :], lhsT=actT[:, jh, gi * 128:(gi + 1) * 128], rhs=WfoB[:, jh, hf * 512:(hf + 1) * 512],
                                                                             start=(jh == 0), stop=(jh == 21), skip_group_check=True), r=['actT', 'WfoB'], w=[pbn[b]])
                    P.op('dve', lambda b=b, hf=hf, i2=i2: V.tensor_tensor(out=x2[i2][:, hf * 512:(hf + 1) * 512], in0=pb[b][:, :], in1=x1r[i2][:, hf * 512:(hf + 1) * 512], op=ALU.add),
                         r=[pbn[b], 'x1r%d' % i2], w=['x2_%d' % i2])
                tg = 'G%d' % i2
                P.op('act', lambda i2=i2: A.activation(out=junkG[:], in_=x2[i2][:], func=AF.Square, accum_out=ssG[i2][:, 0:1]), r=['x2_%d' % i2], w=['junkG', tg + 'ss'])
                P.op('dve', lambda i2=i2: V.tensor_scalar(out=ssG[i2][:, 1:2], in0=ssG[i2][:, 0:1], scalar1=1.0 / D, scalar2=EPS, op0=ALU.mult, op1=ALU.add), r=[tg + 'ss'], w=[tg + 'ss'])
                P.op('act', lambda i2=i2: A.activation(out=ssG[i2][:, 2:3], in_=ssG[i2][:, 1:2], func=AF.Sqrt), r=[tg + 'ss'], w=[tg + 'ss'])
                P.op('dve', lambda i2=i2: V.reciprocal(out=ssG[i2][:, 3:4], in_=ssG[i2][:, 2:3]), r=[tg + 'ss'], w=[tg + 'ss'])
                P.op('dve', lambda i2=i2: V.tensor_scalar(out=ot[i2][:], in0=x2[i2][:], scalar1=ssG[i2][:, 3:4], scalar2=None, op0=ALU.mult), r=['x2_%d' % i2, tg + 'ss'], w=['ot%d' % i2])
                P.op('dve', lambda i2=i2: V.tensor_tensor(out=ot[i2][:], in0=ot[i2][:], in1=gft[:], op=ALU.mult), r=['ot%d' % i2, 'gft'], w=['ot%d' % i2])
                P.dma(out_d[gi * 128:(gi + 1) * 128, :], ot[i2][:], r=['ot%d' % i2], w=['out_d'])
            P.barrier()
    P.barrier()
    return nc


def _host_prep(inputs):
    x = np.asarray(inputs['x'], np.float32)
    w_in = np.asarray(inputs['w_in'], np.float32)[0]
    pts = np.cumsum([1024, 1024, 256, 256, 1024, 64, 16, 1024, 1024])
    wu, wq_, wk, wv, wqi, wki, wwi, wga, wgb = np.split(w_in, pts[:-1], axis=1)
    a_re = np.asarray(inputs['a_re'], np.float32)[0]; a_im = np.asarray(inputs['a_im'], np.float32)[0]
    log_dt = np.asarray(inputs['log_dt'], np.float32)[0]
    b_re = np.asarray(inputs['b_re'], np.float32)[0]; b_im = np.asarray(inputs['b_im'], np.float32)[0]
    c_re = np.asarray(inputs['c_re'], np.float32)[0]; c_im = np.asarray(inputs['c_im'], np.float32)[0]
    d_skip = np.asarray(inputs['d_skip'], np.float32)[0]

    def gT(g):
        return np.ascontiguousarray(np.asarray(g, np.float32).reshape(8, 128).T)

    r = np.arange(128); kc = np.arange(8)
    pp = r // 32; ggr = (r // 16) % 2; cr = r % 16
    AR1 = np.zeros((128, 8, 2, 64), np.float32); AI1 = np.zeros_like(AR1); DT1 = np.zeros_like(AR1)
    BR1 = np.zeros_like(AR1); BI1 = np.zeros_like(AR1)
    for k in range(8):
        for g2 in range(2):
            g = 2 * (4 * k + pp) + g2
            AR1[:, k, g2, :] = a_re[g, :]
            AI1[:, k, g2, :] = a_im[g, :]
            DT1[:, k, g2, :] = log_dt[g][:, None]
            sel = (ggr == g2)
            BR1[sel, k, g2, :] = b_re[g[sel], :, cr[sel]]
            BI1[sel, k, g2, :] = b_im[g[sel], :, cr[sel]]
    gg = np.arange(128) // 64; p_ = np.arange(128) % 64
    gidx = 2 * np.arange(32)[None, :] + gg[:, None]
    ARE = a_re[gidx, p_[:, None]]; AIE = a_im[gidx, p_[:, None]]; DTE = log_dt[gidx]
    CTR = c_re[gidx, :, p_[:, None]]
    CTI = c_im[gidx, :, p_[:, None]]
    BER = np.zeros((128, 32, 2, 16), np.float32); BEI = np.zeros_like(BER)
    for g2 in range(2):
        sel = gg == g2
        BER[sel, :, g2, :] = b_re[gidx[sel], p_[sel][:, None], :]
        BEI[sel, :, g2, :] = b_im[gidx[sel], p_[sel][:, None], :]
    DFM = np.ascontiguousarray(d_skip.reshape(8, 128).T)
    common = dict(
        wu=np.ascontiguousarray(wu),
        wkv=np.ascontiguousarray(np.concatenate([wk, wki, wki, wv], axis=1)),
        wq=np.ascontiguousarray(np.concatenate([wq_, wqi], axis=1)),
        wwi=np.ascontiguousarray(wwi),
        wg=np.ascontiguousarray(np.concatenate([wga, wgb], axis=1)),
        wglu=np.asarray(inputs['w_glu'], np.float32)[0], wba=np.asarray(inputs['w_branch_a'], np.float32)[0],
        wbb=np.asarray(inputs['w_branch_b'], np.float32)[0], wout=np.asarray(inputs['w_out'], np.float32)[0],
        wfi=np.asarray(inputs['w_ffn_in'], np.float32)[0], wfo=np.asarray(inputs['w_ffn_out'], np.float32)[0],
        g1T=gT(inputs['norm1_g'][0]), g2T=gT(inputs['norm2_g'][0]),
        gfb=np.ascontiguousarray(np.broadcast_to(np.asarray(inputs['norm_f_g'], np.float32)[None, :], (128, D))),
        AR1=AR1.reshape(128, 1024), AI1=AI1.reshape(128, 1024), DT1=DT1.reshape(128, 1024),
        BR1=BR1.reshape(128, 1024), BI1=BI1.reshape(128, 1024),
        ARE=np.ascontiguousarray(ARE), AIE=np.ascontiguousarray(AIE), DTE=np.ascontiguousarray(DTE),
        CTR=np.ascontiguousarray(CTR).reshape(128, 512), CTI=np.ascontiguousarray(CTI).reshape(128, 512),
        BER=BER.reshape(128, 1024), BEI=BEI.reshape(128, 1024), DFM=DFM,
        identf=np.eye(128, dtype=np.float32),
    )
    permA = np.zeros((128, 128), np.float32)
    for m in range(32):
        permA[m + 16 if m < 16 else m - 16, m] = 1
    permI = np.zeros((128, 128), np.float32)
    for hb in (0, 64):
        for m in range(16):
            permI[hb + (m + 8 if m < 8 else m - 8), hb + m] = 1
    common['permA'] = permA; common['permI'] = permI

    def tables(pos, kind):
        pos = pos.astype(np.float32)
        cos = np.ones((128, pos.shape[0]), np.float32); sin = np.zeros_like(cos)
        if kind == 'A':
            half = 16
            inv = (np.float32(500000.0) ** (-np.arange(half, dtype=np.float32) / half)).astype(np.float32)
            ang = pos[None, :] * inv[:, None]
            cos[0:16] = np.cos(ang); cos[16:32] = np.cos(ang)
            sin[0:16] = -np.sin(ang); sin[16:32] = np.sin(ang)
        else:
            half = 8
            inv = (np.float32(500000.0) ** (-np.arange(half, dtype=np.float32) / half)).astype(np.float32)
            ang = pos[None, :] * inv[:, None]
            for hb in (0, 64):
                cos[hb:hb + 8] = np.cos(ang); cos[hb + 8:hb + 16] = np.cos(ang)
                sin[hb:hb + 8] = -np.sin(ang); sin[hb + 8:hb + 16] = np.sin(ang)
        return cos, sin

    in_maps = []
    for c in range(8):
        b, r_ = c // 4, c % 4
        pad = (3 - r_) * 128
        xpad = np.zeros((L, D), np.float32)
        xpad[pad:] = x[b, :L - pad]
        pos_all = np.maximum(np.arange(L) - pad, 0)
        own = np.concatenate([np.arange(128) + (4 * i + 3) * 128 for i in range(NT)])
        pos_own = own - pad
        m = dict(common)
        m['xp'] = xpad
        m['cAk'], m['sAk'] = tables(pos_all, 'A')
        m['cIk'], m['sIk'] = tables(pos_all, 'I')
        m['cAq'], m['sAq'] = tables(pos_own, 'A')
        m['cIq'], m['sIq'] = tables(pos_own, 'I')
        q = np.arange(128)[:, None]; kk = np.arange(512)[None, :]
        caus = np.where(((kk < 384) | ((kk - 384) // 64 <= q // 64)), 0.0, NEG).astype(np.float32)
        padm = np.where(kk >= pad, 0.0, NEG).astype(np.float32) * np.ones((128, 1), np.float32)
        m['mfirst'] = np.ascontiguousarray(padm)
        m['mlast'] = np.ascontiguousarray(caus)
        m['mboth'] = np.minimum(padm, caus).astype(np.float32)
        in_maps.append(m)
    return in_maps


def kernel(**inputs):
    in_maps = _host_prep(inputs)
    nc = build_program()
    res = run_bass_kernel_spmd(nc, in_maps, core_ids=list(range(8)))
    out = np.zeros((2, L, D), np.float32)
    for c in range(8):
        b, r_ = c // 4, c % 4
        o = res.results[c]["out"].reshape(NT, 128, D)
        for i in range(NT):
            j = 4 * i + r_
            out[b, j * 128:(j + 1) * 128] = o[i]
    return out
```
